# Optimizing a Trainium2 kernel written in Bass

```python
import math
import jax, jax.numpy as jnp
from jax import lax
import numpy as np

D_MODEL = 1024
BATCH = 8
SEQ = 2048
DEPTH = 4

N_MIXERS = 3
N_A = (DEPTH + 2) // 3
N_B = (DEPTH + 1) // 3
N_C = DEPTH // 3
DEEPNORM_ALPHA = (2.0 * DEPTH) ** 0.25
DEEPNORM_BETA = (8.0 * DEPTH) ** -0.25
LN_EPS = 1e-5

LRU_WIDTH = D_MODEL
LRU_HEADS = 8
LRU_BLOCK = LRU_WIDTH // LRU_HEADS
CONV_WIDTH = 4
LRU_C = 8.0

RET_HEADS = 4
RET_QK = D_MODEL // RET_HEADS
RET_V = 2 * RET_QK
RET_QK_WIDTH = RET_HEADS * RET_QK
RET_V_WIDTH = RET_HEADS * RET_V
RET_CHUNK = 128
RET_EPS = 1e-6

ATTN_PATTERNS = ((128, 1), (512, 4), (2048, 16))
ATTN_GROUPS = len(ATTN_PATTERNS)
ATTN_HEADS = 16
ATTN_HEAD_DIM = D_MODEL // ATTN_HEADS
ATTN_WIDTH = ATTN_HEADS * ATTN_HEAD_DIM

N_EXPERTS = 32
TOP_K = 4
D_FF = D_MODEL
SWIGLU_LIMIT = 7.0
SWIGLU_ALPHA = 1.702
MOE_BLOCK = 256

kernel_name = "hybrid_rglru_retention_dilated_moe_deepnorm"


def layer_norm(x, gain, bias):
    xf = x.astype(jnp.float32)
    mu = jnp.mean(xf, axis=-1, keepdims=True)
    var = jnp.mean(jnp.square(xf - mu), axis=-1, keepdims=True)
    return ((xf - mu) * lax.rsqrt(var + LN_EPS) * gain + bias).astype(x.dtype)


def _linear_combine(left, right):
    a_l, b_l = left
    a_r, b_r = right
    return a_l * a_r, a_r * b_l + b_r


def rglru_mixer(x, w_in, conv_w, conv_b, w_rgate, b_rgate, w_igate, b_igate, lam, w_out):
    bsz, seq, _ = x.shape
    u = x @ w_in
    gate_branch, rec = u[..., :LRU_WIDTH], u[..., LRU_WIDTH:]
    rec = lax.conv_general_dilated(
        rec, conv_w[:, None, :], window_strides=(1,), padding=((CONV_WIDTH - 1, 0),),
        dimension_numbers=('NWC', 'WIO', 'NWC'), feature_group_count=LRU_WIDTH) + conv_b
    xb = rec.reshape(bsz, seq, LRU_HEADS, LRU_BLOCK)
    r = jax.nn.sigmoid(jnp.einsum('bshi,hij->bshj', xb, w_rgate).reshape(bsz, seq, LRU_WIDTH) + b_rgate)
    i = jax.nn.sigmoid(jnp.einsum('bshi,hij->bshj', xb, w_igate).reshape(bsz, seq, LRU_WIDTH) + b_igate)
    log_a = -LRU_C * r.astype(jnp.float32) * jax.nn.softplus(-lam.astype(jnp.float32))
    a = jnp.exp(log_a)
    b = jnp.sqrt(-jnp.expm1(2.0 * log_a)) * (i * rec).astype(jnp.float32)
    _, h = lax.associative_scan(_linear_combine, (a, b), axis=1)
    y = jax.nn.gelu(gate_branch) * h.astype(x.dtype)
    return y @ w_out


def retention_mixer(x, w_in, w_out):
    bsz, seq, _ = x.shape
    f32 = jnp.float32
    u = x @ w_in
    q, k, v, g = jnp.split(u, [RET_QK_WIDTH, 2 * RET_QK_WIDTH, 2 * RET_QK_WIDTH + RET_V_WIDTH], axis=-1)
    nc = seq // RET_CHUNK

    def chunks(t, d):
        return t.reshape(bsz, nc, RET_CHUNK, RET_HEADS, d).transpose(1, 0, 3, 2, 4).astype(f32)

    qc = chunks(q, RET_QK)
    kc = chunks(k, RET_QK) * RET_QK ** -0.5
    vc = chunks(v, RET_V)
    log_gamma = jnp.log1p(-jnp.exp2(-5.0 - jnp.arange(RET_HEADS, dtype=f32)))
    pos = jnp.arange(RET_CHUNK, dtype=f32)
    rel = pos[:, None] - pos[None, :]
    intra_decay = jnp.where(rel >= 0, jnp.exp(log_gamma[:, None, None] * jnp.maximum(rel, 0.0)), 0.0)
    query_decay = jnp.exp(log_gamma[:, None] * (pos + 1.0))
    key_decay = jnp.exp(log_gamma[:, None] * (RET_CHUNK - 1.0 - pos))
    chunk_decay = jnp.exp(log_gamma * RET_CHUNK)

    def step(state, inp):
        qi, ki, vi = inp
        intra = jnp.einsum('bhnd,bhmd->bhnm', qi, ki) * intra_decay
        out = (jnp.einsum('bhnm,bhmv->bhnv', intra, vi)
               + jnp.einsum('bhnd,bhdv->bhnv', qi, state) * query_decay[..., None])
        state = (chunk_decay[:, None, None] * state
                 + jnp.einsum('bhmd,bhmv->bhdv', ki * key_decay[..., None], vi))
        return state, out

    state0 = jnp.zeros((bsz, RET_HEADS, RET_QK, RET_V), f32)
    _, o = lax.scan(step, state0, (qc, kc, vc))
    o = o.transpose(1, 0, 3, 2, 4).reshape(bsz, seq, RET_HEADS, RET_V)
    o = o * lax.rsqrt(jnp.mean(o * o, axis=-1, keepdims=True) + RET_EPS)
    y = (jax.nn.silu(g.astype(f32)) * o.reshape(bsz, seq, RET_V_WIDTH)).astype(x.dtype)
    return y @ w_out


def _dilated_group(q, k, v, slopes, window, dilation):
    bsz, seq, h, dh = q.shape
    f32 = jnp.float32
    nw = window // dilation
    sub = seq // dilation
    nb = -(-sub // nw)
    pad = nb * nw - sub

    def to_sub(t):
        t = t.reshape(bsz, sub, dilation, h, dh).transpose(0, 2, 1, 3, 4).reshape(bsz * dilation, sub, h, dh)
        t = jnp.pad(t, ((0, 0), (0, pad), (0, 0), (0, 0)))
        return t.reshape(bsz * dilation, nb, nw, h, dh)

    def with_prev(t):
        prev = jnp.concatenate([jnp.zeros_like(t[:, :1]), t[:, :-1]], axis=1)
        return jnp.concatenate([prev, t], axis=2)

    qb = to_sub(q)
    kk = with_prev(to_sub(k))
    vv = with_prev(to_sub(v))
    scores = jnp.einsum('bnqhd,bnkhd->bnhqk', qb, kk, preferred_element_type=f32) * (dh ** -0.5)
    qi = jnp.arange(nw)[:, None]
    ki = jnp.arange(2 * nw)[None, :]
    steps = qi + nw - ki
    key_pos = (jnp.arange(nb)[:, None, None] - 1) * nw + ki
    valid = (steps >= 0) & (steps <= nw) & (key_pos >= 0)
    alibi = -slopes[:, None, None] * (steps * dilation).astype(f32)
    scores = jnp.where(valid[None, :, None], scores + alibi[None, None], -jnp.inf)
    m = jnp.max(scores, axis=-1)
    p = jnp.exp(scores - m[..., None])
    s = jnp.sum(p, axis=-1)
    o = jnp.einsum('bnhqk,bnkhd->bnqhd', p, vv.astype(f32))

    def from_sub(t):
        rest = t.shape[3:]
        t = t.reshape(bsz, dilation, nb * nw, *rest)[:, :, :sub]
        return jnp.swapaxes(t, 1, 2).reshape(bsz, seq, *rest)

    return from_sub(jnp.swapaxes(m, 2, 3)), from_sub(jnp.swapaxes(s, 2, 3)), from_sub(o)


def dilated_attention_mixer(x, w_in, w_out):
    bsz, seq, _ = x.shape
    u = (x @ w_in).reshape(bsz, seq, 3, ATTN_GROUPS, ATTN_HEADS, ATTN_HEAD_DIM)
    slopes = jnp.exp2(-8.0 * jnp.arange(1, ATTN_HEADS + 1, dtype=jnp.float32) / ATTN_HEADS)
    maxes, dens, nums = [], [], []
    for g, (window, dilation) in enumerate(ATTN_PATTERNS):
        m, s, o = _dilated_group(u[:, :, 0, g], u[:, :, 1, g], u[:, :, 2, g], slopes, window, dilation)
        maxes.append(m)
        dens.append(s)
        nums.append(o)
    m_all = jnp.stack(maxes)
    wgt = jnp.exp(m_all - jnp.max(m_all, axis=0))
    den = jnp.sum(wgt * jnp.stack(dens), axis=0)
    num = jnp.sum(wgt[..., None] * jnp.stack(nums), axis=0)
    y = (num / den[..., None]).reshape(bsz, seq, ATTN_WIDTH).astype(x.dtype)
    return y @ w_out


def moe_ffn(x, w_router, b_router, w_gu, b_gu, w_down, b_down):
    bsz, seq, d = x.shape
    t = bsz * seq
    xt = x.reshape(t, d)
    logits = (xt @ w_router + b_router).astype(jnp.float32)
    top_logit, top_exp = lax.top_k(logits, TOP_K)
    gates = jax.nn.softmax(top_logit, axis=-1)
    n_assign = t * TOP_K
    flat_exp = top_exp.reshape(n_assign)
    order = jnp.argsort(flat_exp)
    sorted_exp = flat_exp[order]
    counts = jnp.bincount(flat_exp, length=N_EXPERTS)
    padded = ((counts + MOE_BLOCK - 1) // MOE_BLOCK) * MOE_BLOCK
    pad_end = jnp.cumsum(padded)
    pad_start = pad_end - padded
    start = jnp.cumsum(counts) - counts
    dest_sorted = pad_start[sorted_exp] + jnp.arange(n_assign) - start[sorted_exp]
    n_blocks = -(-n_assign // MOE_BLOCK) + N_EXPERTS
    n_rows = n_blocks * MOE_BLOCK
    row_token = jnp.full((n_rows,), t, jnp.int32).at[dest_sorted].set((order // TOP_K).astype(jnp.int32))
    x_rows = jnp.concatenate([xt, jnp.zeros((1, d), xt.dtype)], axis=0)[row_token]
    x_rows = x_rows.reshape(n_blocks, MOE_BLOCK, d)
    block_exp = jnp.minimum(jnp.searchsorted(pad_end, jnp.arange(n_blocks) * MOE_BLOCK, side='right'),
                            N_EXPERTS - 1)

    def expert_block(args):
        xb, e = args
        gu = xb @ w_gu[e] + b_gu[e]
        gate, up = gu[:, :D_FF], gu[:, D_FF:]
        gate = jnp.minimum(gate, SWIGLU_LIMIT)
        up = jnp.clip(up, -SWIGLU_LIMIT, SWIGLU_LIMIT)
        hidden = (up + 1.0) * gate * jax.nn.sigmoid(SWIGLU_ALPHA * gate)
        return hidden @ w_down[e] + b_down[e]

    y_rows = lax.map(expert_block, (x_rows, block_exp)).reshape(n_rows, d)
    dest = jnp.zeros((n_assign,), dest_sorted.dtype).at[order].set(dest_sorted)
    y_sel = y_rows[dest].reshape(t, TOP_K, d)
    out = jnp.einsum('tk,tkd->td', gates.astype(x.dtype), y_sel)
    return out.reshape(bsz, seq, d)


def setup_inputs(seed: int = 0) -> dict:
    key = jax.random.key(seed)
    ks = jax.random.split(key, 24)
    f32 = jnp.float32

    def nrm(k, shape, scale):
        return jax.random.normal(k, shape, f32) * scale

    a0 = jax.random.uniform(ks[9], (N_A, LRU_WIDTH), f32, minval=0.9, maxval=0.999)
    root = a0 ** (1.0 / LRU_C)
    return {
        'x': nrm(ks[0], (BATCH, SEQ, D_MODEL), 1.0),
        'a_w_in': nrm(ks[1], (N_A, D_MODEL, 2 * LRU_WIDTH), D_MODEL ** -0.5),
        'a_conv_w': nrm(ks[2], (N_A, CONV_WIDTH, LRU_WIDTH), CONV_WIDTH ** -0.5),
        'a_conv_b': nrm(ks[3], (N_A, LRU_WIDTH), 0.02),
        'a_w_rgate': nrm(ks[4], (N_A, LRU_HEADS, LRU_BLOCK, LRU_BLOCK), LRU_BLOCK ** -0.5),
        'a_b_rgate': nrm(ks[5], (N_A, LRU_WIDTH), 0.02),
        'a_w_igate': nrm(ks[6], (N_A, LRU_HEADS, LRU_BLOCK, LRU_BLOCK), LRU_BLOCK ** -0.5),
        'a_b_igate': nrm(ks[7], (N_A, LRU_WIDTH), 0.02),
        'a_lambda': jnp.log(root) - jnp.log1p(-root),
        'a_w_out': nrm(ks[8], (N_A, LRU_WIDTH, D_MODEL), DEEPNORM_BETA * LRU_WIDTH ** -0.5),
        'b_w_in': nrm(ks[10], (N_B, D_MODEL, 2 * RET_QK_WIDTH + 2 * RET_V_WIDTH), D_MODEL ** -0.5),
        'b_w_out': nrm(ks[11], (N_B, RET_V_WIDTH, D_MODEL), DEEPNORM_BETA * RET_V_WIDTH ** -0.5),
        'c_w_in': nrm(ks[12], (N_C, D_MODEL, 3 * ATTN_GROUPS * ATTN_WIDTH), D_MODEL ** -0.5),
        'c_w_out': nrm(ks[13], (N_C, ATTN_WIDTH, D_MODEL), DEEPNORM_BETA * ATTN_WIDTH ** -0.5),
        'ln_gain': 1.0 + nrm(ks[14], (DEPTH, 2, D_MODEL), 0.02),
        'ln_bias': nrm(ks[15], (DEPTH, 2, D_MODEL), 0.02),
        'moe_w_router': nrm(ks[16], (DEPTH, D_MODEL, N_EXPERTS), D_MODEL ** -0.5),
        'moe_b_router': nrm(ks[17], (DEPTH, N_EXPERTS), 0.01),
        'moe_w_gu': nrm(ks[18], (DEPTH, N_EXPERTS, D_MODEL, 2 * D_FF), D_MODEL ** -0.5),
        'moe_b_gu': nrm(ks[19], (DEPTH, N_EXPERTS, 2 * D_FF), 0.02),
        'moe_w_down': nrm(ks[20], (DEPTH, N_EXPERTS, D_FF, D_MODEL), DEEPNORM_BETA * D_FF ** -0.5),
        'moe_b_down': nrm(ks[21], (DEPTH, N_EXPERTS, D_MODEL), 0.02),
    }


def reference(x, a_w_in, a_conv_w, a_conv_b, a_w_rgate, a_b_rgate, a_w_igate, a_b_igate, a_lambda,
              a_w_out, b_w_in, b_w_out, c_w_in, c_w_out, ln_gain, ln_bias, moe_w_router,
              moe_b_router, moe_w_gu, moe_b_gu, moe_w_down, moe_b_down):
    for layer in range(DEPTH):
        kind, idx = layer % N_MIXERS, layer // N_MIXERS
        if kind == 0:
            y = rglru_mixer(x, a_w_in[idx], a_conv_w[idx], a_conv_b[idx], a_w_rgate[idx], a_b_rgate[idx],
                            a_w_igate[idx], a_b_igate[idx], a_lambda[idx], a_w_out[idx])
        elif kind == 1:
            y = retention_mixer(x, b_w_in[idx], b_w_out[idx])
        else:
            y = dilated_attention_mixer(x, c_w_in[idx], c_w_out[idx])
        x = layer_norm(DEEPNORM_ALPHA * x + y, ln_gain[layer, 0], ln_bias[layer, 0])
        f = moe_ffn(x, moe_w_router[layer], moe_b_router[layer], moe_w_gu[layer], moe_b_gu[layer],
                    moe_w_down[layer], moe_b_down[layer])
        x = layer_norm(DEEPNORM_ALPHA * x + f, ln_gain[layer, 1], ln_bias[layer, 1])
    return x
```

```python
import contextlib
import numpy as np
import concourse.bass as bass
import concourse.mybir as mybir
from concourse.bass_utils import run_bass_kernel_spmd

F32 = mybir.dt.float32
BF16 = mybir.dt.bfloat16
I32 = mybir.dt.int32
U8 = mybir.dt.uint8
AF = mybir.ActivationFunctionType
ALU = mybir.AluOpType
AX = mybir.AxisListType

D = 1024
S = 2048
NT = 16
NE = 32
CAP = 384
NST = CAP // 128
DEPTH = 4
ALPHA = (2.0 * DEPTH) ** 0.25
LN_EPS = 1e-5
ENGS = ("pe", "act", "dve", "pool", "sp")


class Op:
    __slots__ = ("eng", "fn", "deps", "is_dma", "sig", "cnt", "sem", "target")

    def __init__(s, eng, fn, is_dma):
        s.eng = eng; s.fn = fn; s.deps = []; s.is_dma = is_dma
        s.sig = False; s.cnt = 0; s.sem = None; s.target = 0


class Prog:
    def __init__(s, nc):
        s.nc = nc
        s.ops = {e: [] for e in ENGS}
        s.last_w = {}
        s.readers = {}
        s.n_dma_sems = {"sp": 16, "act": 8, "pool": 16}
        s.since_barrier = []

    def _add(s, eng, fn, reads, writes, is_dma):
        op = Op(eng, fn, is_dma)
        deps = set()
        for k in reads:
            w = s.last_w.get(k)
            if w is not None:
                deps.add(w)
        for k in writes:
            w = s.last_w.get(k)
            if w is not None and not (w.eng == eng and not w.is_dma and not is_dma):
                deps.add(w)
        for k in writes:
            for r in s.readers.get(k, ()):
                if r.eng == eng and not r.is_dma and not is_dma:
                    continue
                deps.add(r)
        op.deps = list(deps)
        for k in reads:
            s.readers.setdefault(k, []).append(op)
        for k in writes:
            s.last_w[k] = op
            s.readers[k] = []
        s.ops[eng].append(op)
        s.since_barrier.append(op)
        return op

    def op(s, eng, fn, reads=(), writes=()):
        return s._add(eng, fn, reads, writes, False)

    def dma(s, eng, fn, reads=(), writes=()):
        return s._add(eng, fn, reads, writes, True)

    def barrier(s):
        prev = s.since_barrier
        s.since_barrier = []
        lastc = {}
        dmas = []
        for op in prev:
            if op.fn is None:
                continue
            if op.is_dma:
                dmas.append(op)
            else:
                lastc[op.eng] = op
        deps = list(lastc.values()) + dmas
        for e in ENGS:
            v = Op(e, None, False)
            v.deps = list(deps)
            s.ops[e].append(v)
        s.last_w = {}
        s.readers = {}

    def emit(s, final_wait_ops=()):
        nc = s.nc
        for e in ENGS:
            for op in s.ops[e]:
                for d in op.deps:
                    if not d.is_dma:
                        d.sig = True
        for e in ENGS:
            c = 0
            for op in s.ops[e]:
                if not op.is_dma and op.sig and op.fn is not None:
                    c += 1
                    op.cnt = c
        stack = contextlib.ExitStack()
        prog_sem = {}
        for e in ("pe", "act", "dve", "pool"):
            prog_sem[e] = stack.enter_context(nc.semaphore("prog_" + e))
        for q in ("sp", "act", "pool"):
            sems = [stack.enter_context(nc.semaphore(f"dq_{q}_{i}")) for i in range(s.n_dma_sems[q])]
            cnts = [0] * len(sems)
            prev = [None] * len(sems)
            i = 0
            for op in s.ops[q]:
                if op.is_dma:
                    j = i % len(sems)
                    i += 1
                    cnts[j] += 16
                    op.sem = sems[j]
                    op.target = cnts[j]
                    if prev[j] is not None:
                        op.deps.append(prev[j])
                    prev[j] = op
        engobj = {"pe": nc.tensor, "act": nc.scalar, "dve": nc.vector, "pool": nc.gpsimd, "sp": nc.sync}
        finals = list(final_wait_ops)

        def run_engine(e):
            eng = engobj[e]
            waited = {}
            for op in s.ops[e]:
                need = {}
                for d in op.deps:
                    if d.is_dma:
                        key = ("d", id(d.sem)); val = d.target; sem = d.sem
                    else:
                        key = ("c", d.eng); val = d.cnt; sem = prog_sem[d.eng]
                    if val > need.get(key, (0, None))[0]:
                        need[key] = (val, sem)
                for key, (val, sem) in need.items():
                    if waited.get(key, 0) >= val:
                        continue
                    waited[key] = val
                    eng.wait_ge(sem, val)
                if op.fn is None:
                    continue
                ins = op.fn(eng)
                if op.is_dma:
                    ins.then_inc(op.sem, 16)
                elif op.sig:
                    ins.then_inc(prog_sem[e], 1)
            if e == "sp":
                for d in finals:
                    eng.wait_ge(d.sem, d.target)

        with stack:
            with nc.Block() as block:
                @block.tensor
                def _(t):
                    run_engine("pe")

                @block.scalar
                def _(t):
                    run_engine("act")

                @block.vector
                def _(t):
                    run_engine("dve")

                @block.gpsimd
                def _(t):
                    run_engine("pool")

                @block.sync
                def _(t):
                    run_engine("sp")


class Ctx:
    pass


class Arena:
    def __init__(s, nc, base, limit):
        s.nc = nc; s.base = base; s.limit = limit; s.off = base; s.n = 0

    def reset(s, off=None):
        s.off = s.base if off is None else off

    def alloc(s, name, shape, dtype):
        sz = int(np.prod(shape[1:])) * mybir.dt.size(dtype)
        sz = (sz + 63) // 64 * 64
        assert s.off + sz <= s.limit, (name, s.off, sz, s.limit)
        s.n += 1
        t = s.nc.alloc_sbuf_tensor_at(f"{name}_{s.n}", list(shape), dtype, offset=s.off)
        s.off += sz
        return t


def build(plan, weights_shapes):
    nc = bass.Bass("TRN2", target_bir_lowering=False)
    c = Ctx()
    c.nc = nc
    P = Prog(nc)
    c.P = P
    dr = {}
    dr["x"] = nc.dram_tensor("x", [S, D], F32, kind="ExternalInput").ap()
    for name, shp in weights_shapes.items():
        dr[name] = nc.dram_tensor(name, list(shp), F32, kind="ExternalInput").ap()
    dr["c_ident"] = nc.dram_tensor("c_ident", [128, 128], F32, kind="ExternalInput").ap()
    dr["c_tri"] = nc.dram_tensor("c_tri", [128, 128], F32, kind="ExternalInput").ap()
    dr["c_ec"] = nc.dram_tensor("c_ec", [128, NE], F32, kind="ExternalInput").ap()
    rc, _ = ret_consts()
    for k_, v_ in rc.items():
        dr[k_] = nc.dram_tensor(k_, list(v_.shape), F32, kind="ExternalInput").ap()
    for k_, v_ in att_consts().items():
        dr[k_] = nc.dram_tensor(k_, list(v_.shape), F32, kind="ExternalInput").ap()
    dr["att_o"] = nc.dram_tensor("att_o_scr", [3, S, D], F32, kind="Internal").ap()
    dr["att_ms"] = nc.dram_tensor("att_ms_scr", [3, S, 16, 2], F32, kind="Internal").ap()
    dr["out"] = nc.dram_tensor("out", [S, D], F32, kind="ExternalOutput").ap()
    dr["xg"] = nc.dram_tensor("xg_scr", [NE * CAP, D], BF16, kind="Internal").ap()
    dr["yy"] = nc.dram_tensor("yy_scr", [NE * CAP, D], F32, kind="Internal").ap()
    c.dr = dr

    slab = nc.alloc_sbuf_tensor("arena_slab", [128, 206 * 1024], U8)
    base = nc.lookup_mloc(slab).addr
    A = Arena(nc, base, base + 206 * 1024)
    c.A = A
    c.X = A.alloc("X", [128, NT, D], F32)
    c.XT = A.alloc("XT", [128, 8, S], BF16)
    c.xt_off = A.off - 8 * S * 2
    c.ident_f = A.alloc("ident_f", [128, 128], F32)
    c.ident_b = A.alloc("ident_b", [128, 128], BF16)
    c.tri_b = A.alloc("tri_b", [128, 128], BF16)
    c.ones_b = A.alloc("ones_b", [128, 128], BF16)
    c.ec = A.alloc("ec", [128, NE], F32)
    c.tri_f = A.alloc("tri_f", [128, 128], F32)
    c.eps_t = A.alloc("eps", [128, 1], F32)
    c.one_t = A.alloc("one", [128, 1], F32)
    c.ADRI = A.alloc("ADRI", [128, NT, 4], I32)
    c.G = A.alloc("G", [128, NT, 4], F32)
    c.phase_base = A.off
    c.ps = [nc.alloc_psum_tensor(f"ps{i}", [128, 512], F32) for i in range(8)]
    c.bank = 0

    X = c.X
    P.dma("sp", lambda e: e.dma_start(out=c.ident_f[:], in_=dr["c_ident"]), writes=[("ident_f",)])
    P.dma("sp", lambda e: e.dma_start(out=c.tri_f[:], in_=dr["c_tri"]), writes=[("tri_f",)])
    P.dma("sp", lambda e: e.dma_start(out=c.ec[:], in_=dr["c_ec"]), writes=[("ec",)])
    P.op("dve", lambda e: e.tensor_copy(out=c.ident_b[:], in_=c.ident_f[:]), reads=[("ident_f",)], writes=[("ident_b",)])
    P.op("dve", lambda e: e.tensor_copy(out=c.tri_b[:], in_=c.tri_f[:]), reads=[("tri_f",)], writes=[("tri_b",)])
    P.op("dve", lambda e: e.memset(c.ones_b[:], 1.0), writes=[("ones_b",)])
    P.op("dve", lambda e: e.memset(c.eps_t[:], LN_EPS), writes=[("eps",)])
    P.op("dve", lambda e: e.memset(c.one_t[:], 1.0), writes=[("one",)])
    for i in range(NT):
        P.dma("sp", lambda e, i=i: e.dma_start(out=X[:, i, :], in_=dr["x"][i * 128:(i + 1) * 128, :]),
              writes=[("X", i)])

    for ph in plan:
        A.reset(c.phase_base)
        if ph[0] == "prep":
            phase_prep(c)
        elif ph[0] == "moe":
            phase_moe(c, ph[1], do_route_prep=ph[2])
        elif ph[0] == "rglru":
            phase_rglru(c, ph[1], ph[2])
        elif ph[0] == "ret":
            phase_ret(c, ph[1], ph[2])
        elif ph[0] == "attn":
            phase_attn(c, ph[1], ph[2])
        else:
            raise ValueError(ph)
        P.barrier()

    outs = []
    for i in range(NT):
        outs.append(P.dma("sp", lambda e, i=i: e.dma_start(out=dr["out"][i * 128:(i + 1) * 128, :], in_=X[:, i, :]),
                          reads=[("X", i)]))
    P.emit(final_wait_ops=outs)
    return nc


def next_bank(c):
    b = c.bank
    c.bank = (c.bank + 1) % 8
    return b


def load_ln_params(c, layer, which, tag):
    A, P, dr = c.A, c.P, c.dr
    g = A.alloc("lng" + tag, [128, D], F32)
    b = A.alloc("lnb" + tag, [128, D], F32)
    P.dma("sp", lambda e: e.dma_start(out=g[:], in_=dr["ln_gain"][layer, which, :].partition_broadcast(128)),
          writes=[("lng", tag)])
    P.dma("sp", lambda e: e.dma_start(out=b[:], in_=dr["ln_bias"][layer, which, :].partition_broadcast(128)),
          writes=[("lnb", tag)])
    return g, b


def alloc_ln_tmp(c):
    A = c.A
    t = Ctx()
    t.st = [A.alloc("lnst", [128, 2, 6], F32) for _ in range(2)]
    t.mv = [A.alloc("lnmv", [128, 2], F32) for _ in range(2)]
    t.sd = [A.alloc("lnsd", [128, 1], F32) for _ in range(2)]
    t.rs = [A.alloc("lnrs", [128, 1], F32) for _ in range(2)]
    t.xn = [A.alloc("lnxn", [128, D], F32) for _ in range(2)]
    return t


def emit_ln(c, t, Z, zkey, i, g, b, tag):
    P = c.P
    X = c.X
    p = i % 2
    st, mv, sd, rs, xn = t.st[p], t.mv[p], t.sd[p], t.rs[p], t.xn[p]
    for h in range(2):
        P.op("dve", lambda e, h=h: e.bn_stats(out=st[:, h, :], in_=Z[:, h * 512:(h + 1) * 512]),
             reads=[zkey], writes=[("lnst", p, h)])
    P.op("dve", lambda e: e.bn_aggr(out=mv[:], in_=st[:].rearrange("p a b -> p (a b)")),
         reads=[("lnst", p, 0), ("lnst", p, 1)], writes=[("lnmv", p)])
    P.op("act", lambda e: e.activation(out=sd[:], in_=mv[:, 1:2], func=AF.Sqrt, bias=c.eps_t[:], scale=1.0),
         reads=[("lnmv", p), ("eps",)], writes=[("lnsd", p)])
    P.op("dve", lambda e: e.reciprocal(out=rs[:], in_=sd[:]), reads=[("lnsd", p)], writes=[("lnrs", p)])
    P.op("dve", lambda e: e.tensor_scalar(out=xn[:], in0=Z, scalar1=mv[:, 0:1], scalar2=rs[:, 0:1],
                                          op0=ALU.subtract, op1=ALU.mult),
         reads=[zkey, ("lnmv", p), ("lnrs", p)], writes=[("lnxn", p)])
    P.op("pool", lambda e: e.tensor_tensor(out=xn[:], in0=xn[:], in1=g[:], op=ALU.mult),
         reads=[("lnxn", p), ("lng", tag)], writes=[("lnxn", p)])
    P.op("pool", lambda e: e.tensor_tensor(out=X[:, i, :], in0=xn[:], in1=b[:], op=ALU.add),
         reads=[("lnxn", p), ("lnb", tag)], writes=[("X", i)])


def alloc_prep(c):
    A = c.A
    t = Ctx()
    t.xtf = [A.alloc("xtf", [128, 8, 128], F32) for _ in range(2)]
    return t


def emit_transpose_tile(c, t, i, write_xt=True):
    P = c.P
    X, XT = c.X, c.XT
    p = i % 2
    xtf = t.xtf[p]
    for half in range(2):
        bk = next_bank(c)
        ps = c.ps[bk]
        for q in range(4):
            ch = half * 4 + q
            P.op("pe", lambda e, ch=ch, q=q, ps=ps: e.transpose(out=ps[:, q * 128:(q + 1) * 128],
                                                               in_=X[:, i, ch * 128:(ch + 1) * 128],
                                                               identity=c.ident_f[:]),
                 reads=[("X", i), ("ident_f",)], writes=[("ps", bk)])
        P.op("act", lambda e, half=half, ps=ps: e.activation(
            out=xtf[:, half * 4:(half + 1) * 4, :], in_=ps[:].rearrange("p (a b) -> p a b", a=4), func=AF.Copy),
            reads=[("ps", bk)], writes=[("xtf", p, half)])
        if write_xt:
            P.op("pool", lambda e, half=half: e.tensor_copy(
                out=XT[:, half * 4:(half + 1) * 4, i * 128:(i + 1) * 128], in_=xtf[:, half * 4:(half + 1) * 4, :]),
                reads=[("xtf", p, half)], writes=[("XT", i)])


def phase_prep(c):
    t = alloc_prep(c)
    zt = c.A.alloc("zt", [128, NST, D], BF16)
    c.P.op("pool", lambda e: e.memset(zt[:], 0.0), writes=[("zt",)])
    for e_ in range(NE):
        c.P.dma("sp", lambda e, e_=e_: e.dma_start(out=c.dr["xg"][e_ * CAP:(e_ + 1) * CAP, :].rearrange("(t p) d -> p t d", p=128), in_=zt[:]),
                reads=[("zt",)], writes=[("xg_zero", e_)])
    for i in range(NT):
        emit_transpose_tile(c, t, i)


def alloc_route(c, layer):
    A, P, dr = c.A, c.P, c.dr
    r = Ctx()
    r.wr = A.alloc("wr", [128, 8, NE], F32)
    r.br = A.alloc("br", [128, NE], F32)
    r.L = A.alloc("L", [128, NT, NE], F32)
    r.T8 = A.alloc("T8", [128, NT, 8], F32)
    r.M = A.alloc("M", [128, NT, NE], BF16)
    r.CUM = A.alloc("CUM", [128, NT, NE], BF16)
    r.At = [A.alloc("At", [128, NE], F32) for _ in range(2)]
    r.junk = A.alloc("junk", [128, 4, NE], F32)
    r.ADRF = A.alloc("ADRF", [128, NT, 4], F32)
    r.ADRI = c.ADRI
    r.G = c.G
    r.E4 = A.alloc("E4", [128, NT, 4], F32)
    r.nmx = A.alloc("nmx", [128, NT], F32)
    r.sm = A.alloc("sm", [128, NT], F32)
    r.rsm = A.alloc("rsm", [128, NT], F32)
    r.XB = [A.alloc("XB", [128, D], BF16) for _ in range(2)]
    P.dma("sp", lambda e: e.dma_start(out=r.wr[:], in_=dr["moe_w_router"][layer].rearrange("(k p) n -> p k n", p=128)),
          writes=[("wr",)])
    P.dma("sp", lambda e: e.dma_start(out=r.br[:], in_=dr["moe_b_router"][layer, :].partition_broadcast(128)),
          writes=[("br",)])
    return r


def emit_route_tile(c, r, t, i):
    P, dr = c.P, c.dr
    X = c.X
    p = i % 2
    xtf = t.xtf[p]
    bk = next_bank(c)
    ps = c.ps[bk]
    for k in range(8):
        P.op("pe", lambda e, k=k: e.matmul(ps[:, 0:NE], lhsT=xtf[:, k, :], rhs=r.wr[:, k, :], start=(k == 0), stop=(k == 7)),
             reads=[("xtf", p, k // 4), ("wr",)], writes=[("ps", bk)])
    P.op("dve", lambda e: e.tensor_tensor(out=r.L[:, i, :], in0=ps[:, 0:NE], in1=r.br[:], op=ALU.add),
         reads=[("ps", bk), ("br",)], writes=[("L", i)])
    P.op("dve", lambda e: e.max(out=r.T8[:, i, :], in_=r.L[:, i, :]), reads=[("L", i)], writes=[("T8", i)])
    P.op("dve", lambda e: e.tensor_scalar(out=r.M[:, i, :], in0=r.L[:, i, :], scalar1=r.T8[:, i, 3:4], scalar2=None,
                                          op0=ALU.is_ge),
         reads=[("L", i), ("T8", i)], writes=[("M", i)])
    bk2 = next_bank(c)
    ps2 = c.ps[bk2]
    P.op("pe", lambda e: e.matmul(ps2[:, 0:NE], lhsT=c.tri_b[:], rhs=r.M[:, i, :], start=True, stop=(i == 0)),
         reads=[("tri_b",), ("M", i)], writes=[("ps", bk2)])
    if i > 0:
        P.op("pe", lambda e: e.matmul(ps2[:, 0:NE], lhsT=c.ones_b[:], rhs=r.CUM[:, i - 1, :], start=False, stop=True),
             reads=[("ones_b",), ("CUM", i - 1)], writes=[("ps", bk2)])
        P.op("pool", lambda e: e.tensor_tensor(out=r.CUM[:, i, :], in0=r.CUM[:, i - 1, :], in1=r.M[:, i, :], op=ALU.add),
             reads=[("CUM", i - 1), ("M", i)], writes=[("CUM", i)])
    else:
        P.op("pool", lambda e: e.tensor_copy(out=r.CUM[:, 0, :], in_=r.M[:, 0, :]), reads=[("M", 0)], writes=[("CUM", 0)])
    At = r.At[p]
    P.op("dve", lambda e: e.tensor_tensor(out=At[:], in0=ps2[:, 0:NE], in1=c.ec[:], op=ALU.add),
         reads=[("ps", bk2), ("ec",)], writes=[("At", p)])
    for k in range(4):
        P.op("dve", lambda e, k=k: e.scalar_tensor_tensor(out=r.junk[:, k, :], in0=r.L[:, i, :], scalar=r.T8[:, i, k:k + 1],
                                                          in1=At[:], op0=ALU.is_equal, op1=ALU.mult),
             reads=[("L", i), ("T8", i), ("At", p)], writes=[("junk", k)])
    P.op("dve", lambda e: e.reduce_sum(out=r.ADRF[:, i, :], in_=r.junk[:], axis=AX.X),
         reads=[("junk", k) for k in range(4)], writes=[("ADRF", i)])
    P.op("dve", lambda e: e.tensor_copy(out=r.ADRI[:, i, :], in_=r.ADRF[:, i, :]),
         reads=[("ADRF", i)], writes=[("ADRI", i)])
    P.op("dve", lambda e: e.tensor_scalar(out=r.nmx[:, i:i + 1], in0=r.T8[:, i, 0:1], scalar1=-1.0, scalar2=None, op0=ALU.mult),
         reads=[("T8", i)], writes=[("nmx", i)])
    P.op("act", lambda e: e.activation(out=r.E4[:, i, :], in_=r.T8[:, i, 0:4], func=AF.Exp, bias=r.nmx[:, i:i + 1], scale=1.0),
         reads=[("T8", i), ("nmx", i)], writes=[("E4", i)])
    P.op("dve", lambda e: e.reduce_sum(out=r.sm[:, i:i + 1], in_=r.E4[:, i, :], axis=AX.X), reads=[("E4", i)], writes=[("sm", i)])
    P.op("dve", lambda e: e.reciprocal(out=r.rsm[:, i:i + 1], in_=r.sm[:, i:i + 1]), reads=[("sm", i)], writes=[("rsm", i)])
    P.op("dve", lambda e: e.tensor_scalar(out=r.G[:, i, :], in0=r.E4[:, i, :], scalar1=r.rsm[:, i:i + 1], scalar2=None, op0=ALU.mult),
         reads=[("E4", i), ("rsm", i)], writes=[("G", i)])
    XB = r.XB[p]
    P.op("pool", lambda e: e.tensor_copy(out=XB[:], in_=X[:, i, :]), reads=[("X", i)], writes=[("XB", p)])
    for k in range(4):
        P.dma("pool", lambda e, k=k: e.indirect_dma_start(
            out=dr["xg"], out_offset=bass.IndirectOffsetOnAxis(ap=r.ADRI[:, i, k:k + 1], axis=0),
            in_=XB[:], in_offset=None),
            reads=[("XB", p), ("ADRI", i)], writes=[("xg_dram", i, k)])


def phase_moe(c, layer, do_route_prep):
    A, P, dr = c.A, c.P, c.dr
    X, XT = c.X, c.XT
    nc = c.nc
    r = alloc_route(c, layer) if do_route_prep else None
    t = alloc_prep(c)
    if do_route_prep:
        for i in range(NT):
            emit_transpose_tile(c, t, i, write_xt=False)
            emit_route_tile(c, r, t, i)
    mark = A.off
    RING = 6 if do_route_prep else 7
    wring = [A.alloc("wring", [128, 8, 512], BF16) for _ in range(RING)]
    bgu = A.alloc("bgu", [128, 16, NE], F32)
    braw = A.alloc("braw", [NE, 2 * D], F32)
    P.dma("sp", lambda e: e.dma_start(out=braw[:], in_=dr["moe_b_gu"][layer]), writes=[("braw",)])
    bkb = next_bank(c)
    for cc in range(16):
        P.op("pe", lambda e, cc=cc: e.transpose(out=c.ps[bkb][:, cc * NE:(cc + 1) * NE], in_=braw[:, cc * 128:(cc + 1) * 128],
                                                identity=c.ident_f[0:NE, 0:NE]),
             reads=[("braw",), ("ident_f",)], writes=[("ps", bkb)])
    P.op("dve", lambda e: e.tensor_copy(out=bgu[:].rearrange("p a b -> p (a b)"), in_=c.ps[bkb][:, :]),
         reads=[("ps", bkb)], writes=[("bgu",)])
    P.op("dve", lambda e: e.tensor_scalar(out=bgu[:, 8:16, :], in0=bgu[:, 8:16, :], scalar1=1.0, scalar2=None, op0=ALU.add),
         reads=[("bgu",)], writes=[("bgu",)])
    bd = [A.alloc("bd", [128, D], F32) for _ in range(2)]
    ytile = [A.alloc("ytile", [128, D], F32) for _ in range(2)]
    gt = [A.alloc("gt", [128, CAP], F32) for _ in range(2)]
    ut = [A.alloc("ut", [128, CAP], F32) for _ in range(2)]
    sg = [A.alloc("sg", [128, CAP], F32) for _ in range(2)]
    A2 = Arena(nc, c.xt_off, c.xt_off + 8 * S * 2)
    A2.n = 1000
    xgtok = A2.alloc("xgtok", [128, NST, D], BF16)
    xgT = [A2.alloc("xgT", [128, 8, CAP], BF16) for _ in range(2)]
    hT = A2.alloc("hT", [128, 8, CAP], BF16)

    pieces = []
    for e_ in range(NE):
        for pc in (0, 2, 1, 3):
            pieces.append((e_, "gu", pc))
        for pc in (0, 1):
            pieces.append((e_, "dn", pc))
    piece_slot = {}

    def issue_piece(n):
        if n >= len(pieces):
            return
        e_, kind, pc = pieces[n]
        slot = n % RING
        piece_slot[(e_, kind, pc)] = slot
        if kind == "gu":
            src = dr["moe_w_gu"][layer, e_, :, pc * 512:(pc + 1) * 512]
        else:
            src = dr["moe_w_down"][layer, e_, :, pc * 512:(pc + 1) * 512]
        P.dma("pool", lambda e, src=src, slot=slot: e.dma_start(out=wring[slot][:], in_=src.rearrange("(k p) n -> p k n", p=128)),
              writes=[("wring", slot)])

    PRE = RING - 1
    for n in range(PRE):
        issue_piece(n)
    nissued = PRE
    pidx = 0
    for e_ in range(NE):
        pe2 = e_ % 2
        P.dma("sp", lambda e, e_=e_: e.dma_start(out=xgtok[:], in_=dr["xg"][e_ * CAP:(e_ + 1) * CAP, :].rearrange("(t p) d -> p t d", p=128)),
              reads=[("xg_dram", i_, k_) for i_ in range(NT) for k_ in range(4)], writes=[("xgtok",)])
        P.dma("sp", lambda e, e_=e_, pe2=pe2: e.dma_start(out=bd[pe2][:], in_=dr["moe_b_down"][layer, e_, :].partition_broadcast(128)),
              writes=[("bd", pe2)])
        for st in range(NST):
            for half in range(2):
                bk = next_bank(c)
                psb = c.ps[bk][:].bitcast(BF16)
                for q in range(4):
                    ch = half * 4 + q
                    P.op("pe", lambda e, st=st, ch=ch, q=q, psb=psb: e.transpose(
                        out=psb[:, q * 128:(q + 1) * 128], in_=xgtok[:, st, ch * 128:(ch + 1) * 128], identity=c.ident_b[:]),
                        reads=[("xgtok",), ("ident_b",)], writes=[("ps", bk)])
                P.op("act", lambda e, st=st, half=half, psb=psb, pe2=pe2: e.activation(
                    out=xgT[pe2][:, half * 4:(half + 1) * 4, st * 128:(st + 1) * 128],
                    in_=psb[:, 0:512].rearrange("p (a b) -> p a b", a=4), func=AF.Copy),
                    reads=[("ps", bk)], writes=[("xgT", pe2)])
        for fc in range(8):
            pcg = fc // 4
            pcu = 2 + fc // 4
            lc = (fc % 4) * 128
            if fc % 4 == 0:
                pass
            sg_ = piece_slot[(e_, "gu", pcg)]
            su_ = piece_slot[(e_, "gu", pcu)]
            bkg = next_bank(c); bku = next_bank(c)
            psg, psu = c.ps[bkg], c.ps[bku]
            for k in range(8):
                P.op("pe", lambda e, k=k, sg_=sg_, lc=lc, psg=psg, pe2=pe2: e.matmul(
                    psg[:, 0:CAP], lhsT=wring[sg_][:, k, lc:lc + 128], rhs=xgT[pe2][:, k, :], start=(k == 0), stop=(k == 7)),
                    reads=[("wring", sg_), ("xgT", pe2)], writes=[("ps", bkg)])
            for k in range(8):
                P.op("pe", lambda e, k=k, su_=su_, lc=lc, psu=psu, pe2=pe2: e.matmul(
                    psu[:, 0:CAP], lhsT=wring[su_][:, k, lc:lc + 128], rhs=xgT[pe2][:, k, :], start=(k == 0), stop=(k == 7)),
                    reads=[("wring", su_), ("xgT", pe2)], writes=[("ps", bku)])
            pp = fc % 2
            P.op("dve", lambda e, psg=psg, fc=fc, pp=pp, e_=e_: e.tensor_scalar(
                out=gt[pp][:], in0=psg[:, 0:CAP], scalar1=bgu[:, fc, e_:e_ + 1], scalar2=7.0, op0=ALU.add, op1=ALU.min),
                reads=[("ps", bkg), ("bgu",)], writes=[("gt", pp)])
            P.op("dve", lambda e, psu=psu, fc=fc, pp=pp, e_=e_: e.tensor_scalar(
                out=ut[pp][:], in0=psu[:, 0:CAP], scalar1=bgu[:, 8 + fc, e_:e_ + 1], scalar2=8.0, op0=ALU.add, op1=ALU.min),
                reads=[("ps", bku), ("bgu",)], writes=[("ut", pp)])
            P.op("act", lambda e, pp=pp: e.activation(out=sg[pp][:], in_=gt[pp][:], func=AF.Silu, scale=1.702),
                 reads=[("gt", pp)], writes=[("sg", pp)])
            P.op("dve", lambda e, pp=pp, fc=fc: e.scalar_tensor_tensor(out=hT[:, fc, :], in0=ut[pp][:], scalar=-6.0, in1=sg[pp][:],
                                                                    op0=ALU.max, op1=ALU.mult),
                 reads=[("ut", pp), ("sg", pp)], writes=[("hT", fc)])
            if fc % 4 == 3:
                issue_piece(nissued); issue_piece(nissued + 1)
                nissued += 2
        for st in range(NST):
            yp = st % 2
            for nh in range(2):
                sd_ = piece_slot[(e_, "dn", nh)]
                bk = next_bank(c)
                psd = c.ps[bk]
                for fc in range(8):
                    P.op("pe", lambda e, fc=fc, st=st, sd_=sd_, psd=psd: e.matmul(
                        psd[:, :], lhsT=hT[:, fc, st * 128:(st + 1) * 128], rhs=wring[sd_][:, fc, :], start=(fc == 0), stop=(fc == 7)),
                        reads=[("hT", fc), ("wring", sd_)], writes=[("ps", bk)])
                P.op("dve", lambda e, nh=nh, yp=yp, psd=psd, pe2=pe2: e.scalar_tensor_tensor(
                    out=ytile[yp][:, nh * 512:(nh + 1) * 512], in0=psd[:, :], scalar=1.0 / 1.702, in1=bd[pe2][:, nh * 512:(nh + 1) * 512],
                    op0=ALU.mult, op1=ALU.add),
                    reads=[("ps", bk), ("bd", pe2)], writes=[("ytile", yp, nh)])
            P.dma("sp", lambda e, e_=e_, st=st, yp=yp: e.dma_start(
                out=dr["yy"][e_ * CAP + st * 128:e_ * CAP + (st + 1) * 128, :], in_=ytile[yp][:]),
                reads=[("ytile", yp, 0), ("ytile", yp, 1)], writes=[("yy_dram", e_, st)])
        issue_piece(nissued); issue_piece(nissued + 1)
        nissued += 2

    P.barrier()
    A.reset(mark)
    g2, b2 = load_ln_params(c, layer, 1, "b")
    lt = alloc_ln_tmp(c)
    YK = [[A.alloc("YK", [128, D], F32) for _ in range(4)] for _ in range(2)]
    Z = [A.alloc("Z", [128, D], F32) for _ in range(2)]
    t2 = t
    for i in range(NT):
        p = i % 2
        for k in range(4):
            P.dma("pool", lambda e, k=k, p=p, i=i: e.indirect_dma_start(
                out=YK[p][k][:], out_offset=None, in_=dr["yy"],
                in_offset=bass.IndirectOffsetOnAxis(ap=c.ADRI[:, i, k:k + 1], axis=0)),
                reads=[("ADRI", i)], writes=[("YK", p, k)])
        P.op("dve", lambda e, p=p, i=i: e.tensor_scalar(out=Z[p][:], in0=YK[p][0][:], scalar1=c.G[:, i, 0:1], scalar2=None, op0=ALU.mult),
             reads=[("YK", p, 0), ("G", i)], writes=[("Z", p)])
        for k in range(1, 4):
            P.op("dve", lambda e, p=p, i=i, k=k: e.scalar_tensor_tensor(
                out=Z[p][:], in0=YK[p][k][:], scalar=c.G[:, i, k:k + 1], in1=Z[p][:], op0=ALU.mult, op1=ALU.add),
                reads=[("YK", p, k), ("G", i), ("Z", p)], writes=[("Z", p)])
        P.op("dve", lambda e, p=p, i=i: e.scalar_tensor_tensor(
            out=Z[p][:], in0=X[:, i, :], scalar=ALPHA, in1=Z[p][:], op0=ALU.mult, op1=ALU.add),
            reads=[("X", i), ("Z", p)], writes=[("Z", p)])
        emit_ln(c, lt, Z[p][:], ("Z", p), i, g2, b2, "b")
        emit_transpose_tile(c, t2, i, write_xt=True)


def emit_mixer_epilogue(c, layer, YT, kc, wout_dram):
    A, P, dr = c.A, c.P, c.dr
    X = c.X
    wout = A.alloc("wout", [128, kc, D], BF16)
    for k2 in range(0, kc, 4):
        P.dma("pool", lambda e, k2=k2: e.dma_start(
            out=wout[:, k2:k2 + 4, :], in_=wout_dram[k2 * 128:(k2 + 4) * 128, :].rearrange("(k p) n -> p k n", p=128)),
            writes=[("wout", k2)])
    r = alloc_route(c, layer)
    g1, b1 = load_ln_params(c, layer, 0, "a")
    lt = alloc_ln_tmp(c)
    t = alloc_prep(c)
    Z = [A.alloc("Z", [128, D], F32) for _ in range(2)]
    for i in range(NT):
        p = i % 2
        for nh in range(2):
            bk = next_bank(c)
            ps = c.ps[bk]
            for k in range(kc):
                P.op("pe", lambda e, k=k, nh=nh, ps=ps, i=i: e.matmul(
                    ps[:, :], lhsT=YT[:, k, i * 128:(i + 1) * 128], rhs=wout[:, k, nh * 512:(nh + 1) * 512],
                    start=(k == 0), stop=(k == kc - 1)),
                    reads=[("YT", k), ("wout", (k // 4) * 4)], writes=[("ps", bk)])
            P.op("dve", lambda e, nh=nh, ps=ps, p=p, i=i: e.scalar_tensor_tensor(
                out=Z[p][:, nh * 512:(nh + 1) * 512], in0=X[:, i, nh * 512:(nh + 1) * 512], scalar=ALPHA, in1=ps[:, :],
                op0=ALU.mult, op1=ALU.add),
                reads=[("X", i), ("ps", bk)], writes=[("Z", p, nh)])
        P.op("dve", lambda e: e.engine_nop(), reads=[("Z", p, 0), ("Z", p, 1)], writes=[("Z", p)])
        emit_ln(c, lt, Z[p][:], ("Z", p), i, g1, b1, "a")
        emit_transpose_tile(c, t, i, write_xt=False)
        emit_route_tile(c, r, t, i)


def load_small_vecs(c, rows, nvec):
    A, P = c.A, c.P
    braw = A.alloc("svraw", [nvec, D], F32)
    pv = A.alloc("pv", [128, 8, nvec], F32)
    for j, ap in enumerate(rows):
        P.dma("sp", lambda e, j=j, ap=ap: e.dma_start(out=braw[j:j + 1, :], in_=ap.unsqueeze(0)), writes=[("svraw", j)])
    bk = next_bank(c)
    for cc in range(8):
        P.op("pe", lambda e, cc=cc: e.transpose(out=c.ps[bk][:, cc * nvec:(cc + 1) * nvec], in_=braw[:, cc * 128:(cc + 1) * 128],
                                                identity=c.ident_f[0:nvec, 0:nvec]),
             reads=[("svraw", j) for j in range(nvec)] + [("ident_f",)], writes=[("ps", bk)])
    P.op("dve", lambda e: e.tensor_copy(out=pv[:].rearrange("p a b -> p (a b)"), in_=c.ps[bk][:, 0:8 * nvec]),
         reads=[("ps", bk)], writes=[("pv",)])
    return pv


def phase_rglru(c, layer, idx):
    A, P, dr = c.A, c.P, c.dr
    X, XT = c.X, c.XT
    TB = 512
    NTB = S // TB
    YT = A.alloc("YT", [128, 8, S], BF16)
    mark_after_yt = A.off
    pv = load_small_vecs(c, [dr["a_conv_w"][idx, 0], dr["a_conv_w"][idx, 1], dr["a_conv_w"][idx, 2], dr["a_conv_w"][idx, 3],
                             dr["a_conv_b"][idx], dr["a_b_rgate"][idx], dr["a_b_igate"][idx], dr["a_lambda"][idx]], 8)
    cv1 = A.alloc("cv1", [128, 8], F32)
    cv2 = A.alloc("cv2", [128, 8], F32)
    sp_e = A.alloc("sp_e", [128, 8], F32)
    sp_l = A.alloc("sp_l", [128, 8], F32)
    P.op("act", lambda e: e.activation(out=sp_e[:], in_=pv[:, :, 7], func=AF.Exp, scale=-1.0), reads=[("pv",)], writes=[("sp_e",)])
    P.op("act", lambda e: e.activation(out=sp_l[:], in_=sp_e[:], func=AF.Ln, bias=c.one_t[:], scale=1.0),
         reads=[("sp_e",), ("one",)], writes=[("sp_l",)])
    P.op("dve", lambda e: e.tensor_scalar(out=cv1[:], in0=sp_l[:], scalar1=-8.0, scalar2=None, op0=ALU.mult), reads=[("sp_l",)], writes=[("cv1",)])
    P.op("dve", lambda e: e.tensor_scalar(out=cv2[:], in0=sp_l[:], scalar1=-16.0, scalar2=None, op0=ALU.mult), reads=[("sp_l",)], writes=[("cv2",)])
    wg = A.alloc("wgr", [128, 8, 128], BF16)
    wi = A.alloc("wgi", [128, 8, 128], BF16)
    P.dma("pool", lambda e: e.dma_start(out=wg[:], in_=dr["a_w_rgate"][idx].rearrange("h i j -> i h j")), writes=[("wgr",)])
    P.dma("pool", lambda e: e.dma_start(out=wi[:], in_=dr["a_w_igate"][idx].rearrange("h i j -> i h j")), writes=[("wgi",)])
    wrec = [A.alloc("wrec", [128, 8, 128], BF16) for _ in range(2)]
    wgat = [A.alloc("wgat", [128, 8, 128], BF16) for _ in range(2)]
    RECp = [A.alloc("RECp", [128, 3 + S], F32) for _ in range(2)]
    nm = ["GX", "SQ", "SG", "CV", "R", "I", "AA", "OM", "H"]
    T = {n: [A.alloc(n, [128, TB], F32) for _ in range(2)] for n in nm}
    CVb = [A.alloc("CVb", [128, TB], BF16) for _ in range(2)]
    for q in range(2):
        P.op("pool", lambda e, q=q: e.memset(RECp[q][:, 0:3], 0.0), writes=[("RECp", q, -1)])
    it = 0
    for cc in range(8):
        q = cc % 2
        P.dma("pool", lambda e, cc=cc, q=q: e.dma_start(
            out=wrec[q][:], in_=dr["a_w_in"][idx][:, D + cc * 128:D + (cc + 1) * 128].rearrange("(k p) n -> p k n", p=128)),
            writes=[("wrec", q)])
        P.dma("pool", lambda e, cc=cc, q=q: e.dma_start(
            out=wgat[q][:], in_=dr["a_w_in"][idx][:, cc * 128:(cc + 1) * 128].rearrange("(k p) n -> p k n", p=128)),
            writes=[("wgat", q)])
        for tb in range(NTB):
            bk = next_bank(c)
            ps = c.ps[bk]
            for k in range(8):
                P.op("pe", lambda e, k=k, ps=ps, tb=tb, q=q: e.matmul(ps[:, :], lhsT=wrec[q][:, k, :], rhs=XT[:, k, tb * TB:(tb + 1) * TB],
                                                                   start=(k == 0), stop=(k == 7)),
                     reads=[("wrec", q)] + [("XT", i_) for i_ in range(tb * 4, tb * 4 + 4)], writes=[("ps", bk)])
            P.op("act", lambda e, ps=ps, tb=tb, q=q: e.activation(out=RECp[q][:, 3 + tb * TB:3 + (tb + 1) * TB], in_=ps[:, :], func=AF.Copy),
                 reads=[("ps", bk)], writes=[("RECp", q, tb)])
        for tb in range(NTB):
            b_ = it % 2
            it += 1
            t_ = {n: T[n][b_] for n in nm}
            k_ = lambda n: (n, b_)
            bk = next_bank(c)
            ps = c.ps[bk]
            for k in range(8):
                P.op("pe", lambda e, k=k, ps=ps, tb=tb, q=q: e.matmul(ps[:, :], lhsT=wgat[q][:, k, :], rhs=XT[:, k, tb * TB:(tb + 1) * TB],
                                                                   start=(k == 0), stop=(k == 7)),
                     reads=[("wgat", q)] + [("XT", i_) for i_ in range(tb * 4, tb * 4 + 4)], writes=[("ps", bk)])
            P.op("act", lambda e, ps=ps, t_=t_: e.activation(out=t_["GX"][:], in_=ps[:, :], func=AF.Copy), reads=[("ps", bk)], writes=[k_("GX")])
            P.op("act", lambda e, ps=ps, t_=t_: e.activation(out=t_["SQ"][:], in_=ps[:, :], func=AF.Square), reads=[("ps", bk)], writes=[k_("SQ")])
            P.op("dve", lambda e, t_=t_: e.tensor_scalar(out=t_["SQ"][:], in0=t_["SQ"][:], scalar1=0.044715, scalar2=1.0, op0=ALU.mult, op1=ALU.add),
                 reads=[k_("SQ")], writes=[k_("SQ")])
            P.op("dve", lambda e, t_=t_: e.tensor_tensor(out=t_["SQ"][:], in0=t_["SQ"][:], in1=t_["GX"][:], op=ALU.mult),
                 reads=[k_("SQ"), k_("GX")], writes=[k_("SQ")])
            P.op("act", lambda e, t_=t_: e.activation(out=t_["SG"][:], in_=t_["SQ"][:], func=AF.Sigmoid, scale=1.5957691216057308),
                 reads=[k_("SQ")], writes=[k_("SG")])
            P.op("pool", lambda e, t_=t_: e.tensor_tensor(out=t_["SG"][:], in0=t_["GX"][:], in1=t_["SG"][:], op=ALU.mult),
                 reads=[k_("GX"), k_("SG")], writes=[k_("SG")])
            rk = [("RECp", q, tb)] + ([("RECp", q, tb - 1)] if tb > 0 else [("RECp", q, -1)])
            P.op("dve", lambda e, t_=t_, tb=tb, q=q, cc=cc: e.tensor_scalar(
                out=t_["CV"][:], in0=RECp[q][:, tb * TB:tb * TB + TB], scalar1=pv[:, cc, 0:1], scalar2=pv[:, cc, 4:5], op0=ALU.mult, op1=ALU.add),
                reads=rk + [("pv",)], writes=[k_("CV")])
            for j in range(1, 4):
                P.op("dve", lambda e, t_=t_, tb=tb, q=q, cc=cc, j=j: e.scalar_tensor_tensor(
                    out=t_["CV"][:], in0=RECp[q][:, tb * TB + j:tb * TB + j + TB], scalar=pv[:, cc, j:j + 1], in1=t_["CV"][:], op0=ALU.mult, op1=ALU.add),
                    reads=rk + [("pv",), k_("CV")], writes=[k_("CV")])
            P.op("act", lambda e, t_=t_, b_=b_: e.activation(out=CVb[b_][:], in_=t_["CV"][:], func=AF.Copy), reads=[k_("CV")], writes=[("CVb", b_)])
            bkr = next_bank(c); bki = next_bank(c)
            P.op("pe", lambda e, b_=b_, cc=cc, bkr=bkr: e.matmul(c.ps[bkr][:, :], lhsT=wg[:, cc, :], rhs=CVb[b_][:], start=True, stop=True),
                 reads=[("wgr",), ("CVb", b_)], writes=[("ps", bkr)])
            P.op("pe", lambda e, b_=b_, cc=cc, bki=bki: e.matmul(c.ps[bki][:, :], lhsT=wi[:, cc, :], rhs=CVb[b_][:], start=True, stop=True),
                 reads=[("wgi",), ("CVb", b_)], writes=[("ps", bki)])
            P.op("act", lambda e, t_=t_, cc=cc, bkr=bkr: e.activation(out=t_["R"][:], in_=c.ps[bkr][:, :], func=AF.Sigmoid, bias=pv[:, cc, 5:6], scale=1.0),
                 reads=[("ps", bkr), ("pv",)], writes=[k_("R")])
            P.op("act", lambda e, t_=t_, cc=cc, bki=bki: e.activation(out=t_["I"][:], in_=c.ps[bki][:, :], func=AF.Sigmoid, bias=pv[:, cc, 6:7], scale=1.0),
                 reads=[("ps", bki), ("pv",)], writes=[k_("I")])
            P.op("act", lambda e, t_=t_, cc=cc: e.activation(out=t_["AA"][:], in_=t_["R"][:], func=AF.Exp, scale=cv1[:, cc:cc + 1]),
                 reads=[k_("R"), ("cv1",)], writes=[k_("AA")])
            P.op("act", lambda e, t_=t_, cc=cc: e.activation(out=t_["OM"][:], in_=t_["R"][:], func=AF.Exp, scale=cv2[:, cc:cc + 1]),
                 reads=[k_("R"), ("cv2",)], writes=[k_("OM")])
            P.op("dve", lambda e, t_=t_: e.tensor_scalar(out=t_["OM"][:], in0=t_["OM"][:], scalar1=-1.0, scalar2=1.0, op0=ALU.mult, op1=ALU.add),
                 reads=[k_("OM")], writes=[k_("OM")])
            P.op("act", lambda e, t_=t_: e.activation(out=t_["OM"][:], in_=t_["OM"][:], func=AF.Sqrt), reads=[k_("OM")], writes=[k_("OM")])
            P.op("pool", lambda e, t_=t_: e.tensor_tensor(out=t_["I"][:], in0=t_["I"][:], in1=t_["CV"][:], op=ALU.mult),
                 reads=[k_("I"), k_("CV")], writes=[k_("I")])
            P.op("dve", lambda e, t_=t_: e.tensor_tensor(out=t_["I"][:], in0=t_["I"][:], in1=t_["OM"][:], op=ALU.mult),
                 reads=[k_("I"), k_("OM")], writes=[k_("I")])
            if tb == 0:
                P.op("dve", lambda e, t_=t_: e.tensor_tensor_scan(out=t_["H"][:], data0=t_["AA"][:], data1=t_["I"][:], initial=0.0,
                                                                  op0=ALU.mult, op1=ALU.add),
                     reads=[k_("AA"), k_("I")], writes=[k_("H")])
            else:
                hp = T["H"][1 - b_]
                P.op("dve", lambda e, t_=t_, hp=hp: e.tensor_tensor_scan(out=t_["H"][:], data0=t_["AA"][:], data1=t_["I"][:],
                                                                         initial=hp[:, TB - 1:TB], op0=ALU.mult, op1=ALU.add),
                     reads=[k_("AA"), k_("I"), ("H", 1 - b_)], writes=[k_("H")])
            P.op("pool", lambda e, t_=t_, cc=cc, tb=tb: e.tensor_tensor(out=YT[:, cc, tb * TB:(tb + 1) * TB], in0=t_["SG"][:], in1=t_["H"][:], op=ALU.mult),
                 reads=[k_("SG"), k_("H")], writes=[("YT", cc)])
    P.barrier()
    A.reset(mark_after_yt)
    emit_mixer_epilogue(c, layer, YT, 8, dr["a_w_out"][idx])


RET_H = 4
RET_EPS = 1e-6


def ret_consts():
    f32 = np.float32
    log_gamma = np.log1p(-np.exp2(-5.0 - np.arange(RET_H, dtype=f32))).astype(f32)
    pos = np.arange(128, dtype=f32)
    rel = pos[:, None] - pos[None, :]
    intra = np.where(rel >= 0, np.exp(log_gamma[:, None, None] * np.maximum(rel, 0.0)), 0.0).astype(f32)
    dt = np.ascontiguousarray(intra.transpose(0, 2, 1))
    qd = np.exp(log_gamma[:, None] * (pos + 1.0)).astype(f32)
    kd = np.exp(log_gamma[:, None] * (127.0 - pos)).astype(f32)
    cd = np.exp(log_gamma * 128.0).astype(f32)
    return {"c_ret_dt": np.ascontiguousarray(dt.transpose(1, 0, 2)),
            "c_ret_qd": np.ascontiguousarray(np.tile(qd[None], (128, 1, 1))),
            "c_ret_kd": np.ascontiguousarray((kd.T / 16.0).astype(f32)),
            }, [float(v) for v in cd]


def phase_ret(c, layer, idx):
    A, P, dr = c.A, c.P, c.dr
    X, XT = c.X, c.XT
    _, CDV = ret_consts()
    DT = A.alloc("rDT", [128, RET_H, 128], F32)
    QD = A.alloc("rQD", [128, RET_H, 128], F32)
    KD = A.alloc("rKD", [128, RET_H], F32)
    eps6 = A.alloc("eps6", [128, 1], F32)
    P.dma("sp", lambda e: e.dma_start(out=DT[:], in_=dr["c_ret_dt"]), writes=[("rDT",)])
    P.dma("sp", lambda e: e.dma_start(out=QD[:], in_=dr["c_ret_qd"]), writes=[("rQD",)])
    P.dma("sp", lambda e: e.dma_start(out=KD[:], in_=dr["c_ret_kd"]), writes=[("rKD",)])
    P.op("dve", lambda e: e.memset(eps6[:], RET_EPS), writes=[("eps6",)])
    Wq = [A.alloc("Wq", [128, 8, 256], BF16) for _ in range(2)]
    Wk = [A.alloc("Wk", [128, 8, 256], BF16) for _ in range(2)]
    Wv = [A.alloc("Wv", [128, 8, 512], BF16) for _ in range(2)]
    Wg = [A.alloc("Wg", [128, 8, 512], BF16) for _ in range(2)]
    Wo = [A.alloc("Wo", [128, 4, D], BF16) for _ in range(2)]
    Sf = A.alloc("Sf", [128, 2, 512], F32)
    Sb = [A.alloc("Sb", [128, 2, 512], BF16) for _ in range(2)]
    qT = [A.alloc("qT", [128, 2, 128], BF16) for _ in range(2)]
    qdT = [A.alloc("qdT", [128, 2, 128], BF16) for _ in range(2)]
    kT = [A.alloc("kT", [128, 2, 128], BF16) for _ in range(2)]
    kdec = [A.alloc("kdec", [128, 256], BF16) for _ in range(2)]
    vc = [A.alloc("vc", [128, 512], BF16) for _ in range(2)]
    sgc = [A.alloc("sgc", [128, 512], F32) for _ in range(2)]
    PT = [A.alloc("PT", [128, 128], BF16) for _ in range(2)]
    qf = [A.alloc("qf", [128, 256], F32) for _ in range(2)]
    kf = [A.alloc("kf", [128, 256], F32) for _ in range(2)]
    ktf = [A.alloc("ktf", [128, 256], F32) for _ in range(2)]
    scf = [A.alloc("scf", [128, 128], F32) for _ in range(2)]
    osq = [A.alloc("osq", [128, 512], F32) for _ in range(2)]
    ms = [A.alloc("ms", [128, 1], F32) for _ in range(2)]
    sd = [A.alloc("rsd", [128, 1], F32) for _ in range(2)]
    rs = [A.alloc("rrs", [128, 1], F32) for _ in range(2)]
    yc = [A.alloc("yc", [128, 512], BF16) for _ in range(2)]
    yT = [A.alloc("yT", [128, 4, 128], BF16) for _ in range(2)]
    W = dr["b_w_in"][idx]
    QW = 1024
    thr = A.alloc("thr", [128, 1], BF16)

    def load_head(h):
        q = h % 2
        for (dst, col0, n, nm) in ((Wq[q], h * 256, 256, "Wq"), (Wk[q], QW + h * 256, 256, "Wk"),
                                   (Wv[q], 2 * QW + h * 512, 512, "Wv"), (Wg[q], 2 * QW + 2048 + h * 512, 512, "Wg")):
            P.dma("pool", lambda e, dst=dst, col0=col0, n=n: e.dma_start(
                out=dst[:], in_=W[:, col0:col0 + n].rearrange("(k p) n -> p k n", p=128)), reads=[("throttle",)], writes=[(nm, q)])
        P.dma("pool", lambda e, q=q, h=h: e.dma_start(
            out=Wo[q][:], in_=dr["b_w_out"][idx][h * 512:(h + 1) * 512, :].rearrange("(k p) n -> p k n", p=128)), reads=[("throttle",)], writes=[("Wo", q)])

    import os
    DBG = int(os.environ.get("RET_DBG", "99"))
    NH_ = int(os.environ.get("RET_NH", "4"))
    NI_ = int(os.environ.get("RET_NI", "16"))

    def ret_chunk(h, i, b_, q):
        if h >= NH_ or i >= NI_ or DBG < 1:
            return
        xtk = [("XT", i)]
        tok = slice(i * 128, (i + 1) * 128)
        bkq = next_bank(c); bkk = next_bank(c)
        for dc in range(2):
            for k in range(8):
                P.op("pe", lambda e, dc=dc, k=k, bkq=bkq: e.matmul(c.ps[bkq][:, dc * 128:(dc + 1) * 128], lhsT=Wq[q][:, k, dc * 128:(dc + 1) * 128],
                                                             rhs=XT[:, k, tok], start=(k == 0), stop=(k == 7)),
                     reads=[("Wq", q)] + xtk, writes=[("ps", bkq)])
        for dc in range(2):
            for k in range(8):
                P.op("pe", lambda e, dc=dc, k=k, bkk=bkk: e.matmul(c.ps[bkk][:, dc * 128:(dc + 1) * 128], lhsT=Wk[q][:, k, dc * 128:(dc + 1) * 128],
                                                             rhs=XT[:, k, tok], start=(k == 0), stop=(k == 7)),
                     reads=[("Wk", q)] + xtk, writes=[("ps", bkk)])
        P.op("act", lambda e, bkq=bkq, b_=b_: e.activation(out=qf[b_][:], in_=c.ps[bkq][:, 0:256], func=AF.Copy),
             reads=[("ps", bkq)], writes=[("qf", b_)])
        P.op("act", lambda e, bkk=bkk, b_=b_: e.activation(out=kf[b_][:], in_=c.ps[bkk][:, 0:256], func=AF.Copy),
             reads=[("ps", bkk)], writes=[("kf", b_)])
        P.op("pool", lambda e, b_=b_: e.tensor_copy(out=qT[b_][:].rearrange("p a b -> p (a b)"), in_=qf[b_][:]),
             reads=[("qf", b_)], writes=[("qT", b_)])
        for dc in range(2):
            P.op("dve", lambda e, dc=dc, b_=b_, h=h: e.tensor_tensor(out=qdT[b_][:, dc, :], in0=qf[b_][:, dc * 128:(dc + 1) * 128],
                                                                   in1=QD[:, h, :], op=ALU.mult),
                 reads=[("qf", b_), ("rQD",)], writes=[("qdT", b_)])
        P.op("pool", lambda e, b_=b_: e.tensor_scalar(out=kT[b_][:].rearrange("p a b -> p (a b)"), in0=kf[b_][:], scalar1=0.0625, scalar2=None, op0=ALU.mult),
             reads=[("kf", b_)], writes=[("kT", b_)])
        if DBG < 2:
            return
        bkt = next_bank(c)
        for k in range(8):
            P.op("pe", lambda e, k=k, bkt=bkt: e.matmul(c.ps[bkt][:, 0:256], lhsT=XT[:, k, tok], rhs=Wk[q][:, k, :], start=(k == 0), stop=(k == 7)),
                 reads=[("Wk", q)] + xtk, writes=[("ps", bkt)])
        P.op("act", lambda e, bkt=bkt, b_=b_: e.activation(out=ktf[b_][:], in_=c.ps[bkt][:, 0:256], func=AF.Copy),
             reads=[("ps", bkt)], writes=[("ktf", b_)])
        P.op("dve", lambda e, b_=b_, h=h: e.tensor_scalar(out=kdec[b_][:], in0=ktf[b_][:], scalar1=KD[:, h:h + 1], scalar2=None, op0=ALU.mult),
             reads=[("ktf", b_), ("rKD",)], writes=[("kdec", b_)])
        bkv = next_bank(c)
        for k in range(8):
            P.op("pe", lambda e, k=k, bkv=bkv: e.matmul(c.ps[bkv][:, :], lhsT=XT[:, k, tok], rhs=Wv[q][:, k, :], start=(k == 0), stop=(k == 7)),
                 reads=[("Wv", q)] + xtk, writes=[("ps", bkv)])
        P.op("act", lambda e, bkv=bkv, b_=b_: e.activation(out=vc[b_][:], in_=c.ps[bkv][:, :], func=AF.Copy), reads=[("ps", bkv)], writes=[("vc", b_)])
        bkg = next_bank(c)
        for k in range(8):
            P.op("pe", lambda e, k=k, bkg=bkg: e.matmul(c.ps[bkg][:, :], lhsT=XT[:, k, tok], rhs=Wg[q][:, k, :], start=(k == 0), stop=(k == 7)),
                 reads=[("Wg", q)] + xtk, writes=[("ps", bkg)])
        P.op("act", lambda e, bkg=bkg, b_=b_: e.activation(out=sgc[b_][:], in_=c.ps[bkg][:, :], func=AF.Sigmoid), reads=[("ps", bkg)], writes=[("sgc", b_)])
        P.op("dve", lambda e, bkg=bkg, b_=b_: e.tensor_tensor(out=sgc[b_][:], in0=c.ps[bkg][:, :], in1=sgc[b_][:], op=ALU.mult),
             reads=[("ps", bkg), ("sgc", b_)], writes=[("sgc", b_)])
        if DBG < 3:
            return
        bks = next_bank(c)
        for dc in range(2):
            P.op("pe", lambda e, dc=dc, bks=bks, b_=b_: e.matmul(c.ps[bks][:, 0:128], lhsT=kT[b_][:, dc, :], rhs=qT[b_][:, dc, :], start=(dc == 0), stop=(dc == 1)),
                 reads=[("kT", b_), ("qT", b_)], writes=[("ps", bks)])
        P.op("act", lambda e, bks=bks, b_=b_: e.activation(out=scf[b_][:], in_=c.ps[bks][:, 0:128], func=AF.Copy),
             reads=[("ps", bks)], writes=[("scf", b_)])
        P.op("dve", lambda e, b_=b_, h=h: e.tensor_tensor(out=PT[b_][:], in0=scf[b_][:], in1=DT[:, h, :], op=ALU.mult),
             reads=[("scf", b_), ("rDT",)], writes=[("PT", b_)])
        bko = next_bank(c)
        sbp = Sb[(i + 1) % 2]
        P.op("pe", lambda e, bko=bko, b_=b_: e.matmul(c.ps[bko][:, :], lhsT=PT[b_][:], rhs=vc[b_][:], start=True, stop=(i == 0)),
             reads=[("PT", b_), ("vc", b_)], writes=[("ps", bko)])
        if i > 0:
            for dc in range(2):
                P.op("pe", lambda e, dc=dc, bko=bko, b_=b_, sbp=sbp: e.matmul(c.ps[bko][:, :], lhsT=qdT[b_][:, dc, :], rhs=sbp[:, dc, :], start=False, stop=(dc == 1)),
                     reads=[("qdT", b_), ("Sb", (i + 1) % 2)], writes=[("ps", bko)])
        P.op("act", lambda e, bko=bko, b_=b_: e.activation(out=osq[b_][:], in_=c.ps[bko][:, :], func=AF.Square), reads=[("ps", bko)], writes=[("osq", b_)])
        P.op("dve", lambda e, b_=b_: e.reduce_sum(out=ms[b_][:], in_=osq[b_][:], axis=AX.X), reads=[("osq", b_)], writes=[("ms", b_)])
        P.op("act", lambda e, b_=b_: e.activation(out=sd[b_][:], in_=ms[b_][:], func=AF.Sqrt, bias=eps6[:], scale=1.0 / 512.0),
             reads=[("ms", b_), ("eps6",)], writes=[("rsd", b_)])
        P.op("dve", lambda e, b_=b_: e.reciprocal(out=rs[b_][:], in_=sd[b_][:]), reads=[("rsd", b_)], writes=[("rrs", b_)])
        P.op("dve", lambda e, bko=bko, b_=b_: e.scalar_tensor_tensor(out=osq[b_][:], in0=c.ps[bko][:, :], scalar=rs[b_][:, 0:1], in1=sgc[b_][:],
                                                                  op0=ALU.mult, op1=ALU.mult),
             reads=[("ps", bko), ("rrs", b_), ("sgc", b_), ("ms", b_)], writes=[("osq", b_)])
        P.op("pool", lambda e, b_=b_: e.tensor_copy(out=yc[b_][:], in_=osq[b_][:]), reads=[("osq", b_)], writes=[("yc", b_)])
        if DBG < 4:
            return
        if i < NT - 1:
            sbn = Sb[i % 2]
            for dc in range(2):
                bkS = next_bank(c)
                P.op("pe", lambda e, dc=dc, bkS=bkS, b_=b_: e.matmul(c.ps[bkS][:, :], lhsT=kdec[b_][:, dc * 128:(dc + 1) * 128], rhs=vc[b_][:], start=True, stop=True),
                     reads=[("kdec", b_), ("vc", b_)], writes=[("ps", bkS)])
                if i == 0:
                    P.op("dve", lambda e, dc=dc, bkS=bkS: e.tensor_copy(out=Sf[:, dc, :], in_=c.ps[bkS][:, :]), reads=[("ps", bkS)], writes=[("Sf", dc)])
                else:
                    P.op("dve", lambda e, dc=dc, bkS=bkS, h=h: e.scalar_tensor_tensor(out=Sf[:, dc, :], in0=Sf[:, dc, :], scalar=CDV[h], in1=c.ps[bkS][:, :],
                                                                                  op0=ALU.mult, op1=ALU.add),
                         reads=[("ps", bkS), ("Sf", dc)], writes=[("Sf", dc)])
                P.op("pool", lambda e, dc=dc, sbn=sbn: e.tensor_copy(out=sbn[:, dc, :], in_=Sf[:, dc, :]), reads=[("Sf", dc)], writes=[("Sb", i % 2)])
        if DBG < 5:
            return
        bky = next_bank(c)
        psb = c.ps[bky][:].bitcast(BF16)
        for fc in range(4):
            P.op("pe", lambda e, fc=fc, psb=psb, b_=b_: e.transpose(out=psb[:, fc * 128:(fc + 1) * 128], in_=yc[b_][:, fc * 128:(fc + 1) * 128], identity=c.ident_b[:]),
                 reads=[("yc", b_), ("ident_b",)], writes=[("ps", bky)])
        P.op("act", lambda e, psb=psb, b_=b_: e.activation(out=yT[b_][:].rearrange("p a b -> p (a b)"), in_=psb[:, 0:512], func=AF.Copy),
             reads=[("ps", bky)], writes=[("yT", b_)])
        for nh in range(2):
            bkx = next_bank(c)
            for fc in range(4):
                P.op("pe", lambda e, fc=fc, nh=nh, bkx=bkx, b_=b_: e.matmul(c.ps[bkx][:, :], lhsT=yT[b_][:, fc, :], rhs=Wo[q][:, fc, nh * 512:(nh + 1) * 512],
                                                                     start=(fc == 0), stop=(fc == 3)),
                     reads=[("yT", b_), ("Wo", q)], writes=[("ps", bkx)])
            xs = X[:, i, nh * 512:(nh + 1) * 512]
            if h == 0:
                P.op("dve", lambda e, xs=xs, bkx=bkx: e.scalar_tensor_tensor(out=xs, in0=xs, scalar=ALPHA, in1=c.ps[bkx][:, :], op0=ALU.mult, op1=ALU.add),
                     reads=[("ps", bkx), ("X", i)], writes=[("X", i)])
            else:
                P.op("dve", lambda e, xs=xs, bkx=bkx: e.tensor_tensor(out=xs, in0=xs, in1=c.ps[bkx][:, :], op=ALU.add),
                     reads=[("ps", bkx), ("X", i)], writes=[("X", i)])


    load_head(0)
    it = 0
    for h in range(RET_H):
        q = h % 2
        for i in range(NT):
            ret_chunk(h, i, it % 2, q)
            it += 1
            if i == 1 and h + 1 < RET_H:
                P.op("act", lambda e: e.activation(out=thr[:], in_=yT[(it - 1) % 2][:, 0, 0:1], func=AF.Copy),
                     reads=[("yT", (it - 1) % 2)], writes=[("throttle",)])
                load_head(h + 1)
    P.barrier()
    A.reset(c.phase_base)
    emit_ln_route_epilogue(c, layer)


def emit_ln_route_epilogue(c, layer):
    r = alloc_route(c, layer)
    g1, b1 = load_ln_params(c, layer, 0, "a")
    lt = alloc_ln_tmp(c)
    t = alloc_prep(c)
    for i in range(NT):
        emit_ln(c, lt, c.X[:, i, :], ("X", i), i, g1, b1, "a")
        emit_transpose_tile(c, t, i, write_xt=False)
        emit_route_tile(c, r, t, i)


ATT_PAT = ((128, 1), (512, 4), (2048, 16))
ATT_BIG = 1.0e9


def att_consts():
    u = np.arange(128)[:, None]
    j = np.arange(256)[None, :]
    steps = u + 128 - j
    valid = (steps >= 0) & (steps <= 128)
    neg = np.where(valid, -steps.astype(np.float32), -ATT_BIG).astype(np.float32)
    return {"c_att_neg": np.ascontiguousarray(neg)}


def phase_attn(c, layer, idx):
    A, P, dr = c.A, c.P, c.dr
    X, XT = c.X, c.XT
    nc = c.nc
    Wd = dr["c_w_in"][idx]
    NEG = A.alloc("aNEG", [128, 256], F32)
    P.dma("sp", lambda e: e.dma_start(out=NEG[:], in_=dr["c_att_neg"]), writes=[("aNEG",)])
    Wq = [A.alloc("aWq", [128, 8, 128], BF16) for _ in range(2)]
    Wk = [A.alloc("aWk", [128, 8, 128], BF16) for _ in range(2)]
    Wv = [A.alloc("aWv", [128, 8, 128], BF16) for _ in range(2)]
    qT = [A.alloc("aqT", [128, S], BF16) for _ in range(2)]
    kT = [A.alloc("akT", [128, S], BF16) for _ in range(2)]
    V = [A.alloc("aV", [128, NT, 128], BF16) for _ in range(2)]
    BI = [A.alloc("aBI", [128, 2, 256], F32) for _ in range(2)]
    Sb = [A.alloc("aSb", [128, 256], F32) for _ in range(2)]
    Pb = [A.alloc("aPb", [128, 256], BF16) for _ in range(2)]
    PT = [A.alloc("aPT", [128, 2, 128], BF16) for _ in range(2)]
    mx = [A.alloc("amx", [128, 1], F32) for _ in range(2)]
    nm = [A.alloc("anm", [128, 1], F32) for _ in range(2)]
    osb = [A.alloc("aosb", [128, 128], F32) for _ in range(2)]
    mst = [A.alloc("amst", [128, 2, 2], F32) for _ in range(2)]

    def load_w(g, hp, q):
        for (dst, s_, nm_) in ((Wq[q], 0, "aWq"), (Wk[q], 1, "aWk"), (Wv[q], 2, "aWv")):
            col0 = ((s_ * 3 + g) * 16 + 2 * hp) * 64
            P.dma("pool", lambda e, dst=dst, col0=col0: e.dma_start(
                out=dst[:], in_=Wd[:, col0:col0 + 128].rearrange("(k p) n -> p k n", p=128)), writes=[(nm_, q)])

    def tokslice(g, ut):
        d = ATT_PAT[g][1]
        nb = (S // d) // 128
        r, b = ut // nb, ut % nb
        st = 128 * b * d + r
        return slice(st, st + 127 * d + 1, d), b

    def proj(g, hp, q):
        for (Wt, dst, nm_) in ((Wq[q], qT[q], "aqT"), (Wk[q], kT[q], "akT")):
            for tb in range(4):
                bk = next_bank(c)
                for k in range(8):
                    P.op("pe", lambda e, k=k, bk=bk, Wt=Wt, tb=tb: e.matmul(c.ps[bk][:, :], lhsT=Wt[:, k, :], rhs=XT[:, k, tb * 512:(tb + 1) * 512],
                                                                      start=(k == 0), stop=(k == 7)),
                         reads=[(nm_.replace("qT", "Wq").replace("kT", "Wk"), q)] + [("XT", i_) for i_ in range(tb * 4, tb * 4 + 4)], writes=[("ps", bk)])
                P.op("act", lambda e, bk=bk, dst=dst, tb=tb: e.activation(out=dst[:, tb * 512:(tb + 1) * 512], in_=c.ps[bk][:, :], func=AF.Copy),
                     reads=[("ps", bk)], writes=[(nm_, q, tb)])
        for ut in range(NT):
            ts_, _ = tokslice(g, ut)
            bk = next_bank(c)
            for k in range(8):
                P.op("pe", lambda e, k=k, bk=bk, ts_=ts_: e.matmul(c.ps[bk][:, 0:128], lhsT=XT[:, k, ts_], rhs=Wv[q][:, k, :], start=(k == 0), stop=(k == 7)),
                     reads=[("aWv", q)] + [("XT", i_) for i_ in range(NT)], writes=[("ps", bk)])
            P.op("act", lambda e, bk=bk, ut=ut: e.activation(out=V[q][:, ut, :], in_=c.ps[bk][:, 0:128], func=AF.Copy),
                 reads=[("ps", bk)], writes=[("aV", q, ut)])
        d = ATT_PAT[g][1]
        for e_ in range(2):
            hh = 2 * hp + e_
            slope = float(2.0 ** (-8.0 * (hh + 1) / 16.0)) * d
            P.op("pool", lambda e, e_=e_, slope=slope: e.tensor_scalar(out=BI[q][:, e_, :], in0=NEG[:], scalar1=slope, scalar2=None, op0=ALU.mult),
                 reads=[("aNEG",)], writes=[("aBI", q, e_)])

    cnt = [0]

    def QK_KEYS(q):
        return [("aqT", q, tb) for tb in range(4)] + [("akT", q, tb) for tb in range(4)]

    def unit(g, hp, q, ut, bo):
        bkk_o = bo_bank[0]
        ts_, b = tokslice(g, ut)
        has_prev = b > 0
        if has_prev:
            tp_, _ = tokslice(g, ut - 1)
        for e_ in range(2):
            u_ = cnt[0] % 2
            cnt[0] += 1
            pr = slice(e_ * 64, (e_ + 1) * 64)
            lo = 0 if has_prev else 128
            bk = next_bank(c)
            if has_prev:
                P.op("pe", lambda e, bk=bk, pr=pr: e.matmul(c.ps[bk][:, 0:128], lhsT=qT[q][pr, ts_], rhs=kT[q][pr, tp_], start=True, stop=True),
                     reads=QK_KEYS(q), writes=[("ps", bk)])
            P.op("pe", lambda e, bk=bk, pr=pr: e.matmul(c.ps[bk][:, 128:256], lhsT=qT[q][pr, ts_], rhs=kT[q][pr, ts_], start=True, stop=True),
                 reads=QK_KEYS(q), writes=[("ps", bk)])
            P.op("dve", lambda e, bk=bk, u_=u_, e_=e_, lo=lo: e.scalar_tensor_tensor(out=Sb[u_][:, lo:256], in0=c.ps[bk][:, lo:256], scalar=0.125, in1=BI[q][:, e_, lo:256],
                                                                                op0=ALU.mult, op1=ALU.add),
                 reads=[("ps", bk), ("aBI", q, e_)], writes=[("aSb", u_)])
            P.op("dve", lambda e, u_=u_, lo=lo: e.reduce_max(out=mx[u_][:], in_=Sb[u_][:, lo:256], axis=AX.X), reads=[("aSb", u_)], writes=[("amx", u_)])
            P.op("pool", lambda e, u_=u_: e.tensor_scalar(out=nm[u_][:], in0=mx[u_][:], scalar1=-1.0, scalar2=None, op0=ALU.mult),
                 reads=[("amx", u_)], writes=[("anm", u_)])
            P.op("act", lambda e, u_=u_, lo=lo: e.activation(out=Pb[u_][:, lo:256], in_=Sb[u_][:, lo:256], func=AF.Exp, bias=nm[u_][:], scale=1.0),
                 reads=[("aSb", u_), ("anm", u_)], writes=[("aPb", u_)])
            P.op("dve", lambda e, u_=u_, lo=lo, e_=e_, bo=bo: e.reduce_sum(out=mst[bo][:, e_, 1:2], in_=Pb[u_][:, lo:256], axis=AX.X),
                 reads=[("aPb", u_)], writes=[("amst", bo, e_, 1)])
            P.op("pool", lambda e, u_=u_, e_=e_, bo=bo: e.tensor_copy(out=mst[bo][:, e_, 0:1], in_=mx[u_][:]), reads=[("amx", u_)], writes=[("amst", bo, e_, 0)])
            bkt = next_bank(c)
            psb = c.ps[bkt][:].bitcast(BF16)
            halves = (0, 1) if has_prev else (1,)
            for hf in halves:
                P.op("pe", lambda e, hf=hf, psb=psb, u_=u_: e.transpose(out=psb[:, hf * 128:(hf + 1) * 128], in_=Pb[u_][:, hf * 128:(hf + 1) * 128], identity=c.ident_b[:]),
                     reads=[("aPb", u_), ("ident_b",)], writes=[("ps", bkt)])
            P.op("act", lambda e, psb=psb, u_=u_, lo=lo: e.activation(out=PT[u_][:].rearrange("p a b -> p (a b)")[:, lo:256], in_=psb[:, lo:256], func=AF.Copy),
                 reads=[("ps", bkt)], writes=[("aPT", u_)])
            for n_, hf in enumerate(halves):
                vt = ut - 1 if hf == 0 else ut
                P.op("pe", lambda e, hf=hf, vt=vt, u_=u_, pr=pr, n_=n_: e.matmul(c.ps[bkk_o][:, pr], lhsT=PT[u_][:, hf, :], rhs=V[q][:, vt, pr],
                                                                            start=(n_ == 0), stop=(n_ == len(halves) - 1)),
                     reads=[("aPT", u_), ("aV", q, vt)], writes=[("ps", bkk_o)])
        P.op("dve", lambda e, bo=bo: e.tensor_copy(out=osb[bo][:], in_=c.ps[bkk_o][:, 0:128]), reads=[("ps", bkk_o)], writes=[("aosb", bo)])
        P.dma("sp", lambda e, bo=bo: e.dma_start(out=dr["att_o"][g, ts_, hp * 128:(hp + 1) * 128], in_=osb[bo][:]), reads=[("aosb", bo)], writes=[("att_o", g, hp, ut)])
        P.dma("sp", lambda e, bo=bo: e.dma_start(out=dr["att_ms"][g, ts_, 2 * hp:2 * hp + 2, :], in_=mst[bo][:]),
              reads=[("amst", bo, e2, t2) for e2 in range(2) for t2 in range(2)], writes=[("att_ms", g, hp, ut)])

    bo_bank = [0]
    import os
    NG_ = int(os.environ.get("ATT_NG", "3"))
    NHP_ = int(os.environ.get("ATT_NHP", "8"))
    combos = [(g, hp) for g in range(NG_) for hp in range(NHP_)]
    load_w(combos[0][0], combos[0][1], 0)
    uc = 0
    for n, (g, hp) in enumerate(combos):
        q = n % 2
        proj(g, hp, q)
        if n + 1 < len(combos):
            load_w(combos[n + 1][0], combos[n + 1][1], (n + 1) % 2)
        for ut in range(NT):
            bo_bank[0] = next_bank(c)
            unit(g, hp, q, ut, uc % 2)
            uc += 1
    P.barrier()
    A.reset(c.phase_base)
    YT = A.alloc("YT", [128, 8, S], BF16)
    mark = A.off
    Og = [[A.alloc("aOg", [128, D], F32) for _ in range(3)] for _ in range(2)]
    MS = [A.alloc("aMS", [128, 3, 16, 2], F32) for _ in range(2)]
    Mx = [A.alloc("aMx", [128, 16], F32) for _ in range(2)]
    Wt_ = [A.alloc("aWt", [128, 3, 16], F32) for _ in range(2)]
    Ws = [A.alloc("aWs", [128, 3, 16], F32) for _ in range(2)]
    Dn = [A.alloc("aDn", [128, 16], F32) for _ in range(2)]
    Yt = [A.alloc("aYt", [128, D], F32) for _ in range(2)]
    Yb = [A.alloc("aYb", [128, D], BF16) for _ in range(2)]

    def merge_tile(i, p):
        for g in range(3):
            P.dma("sp", lambda e, g=g: e.dma_start(out=Og[p][g][:], in_=dr["att_o"][g, i * 128:(i + 1) * 128, :]), writes=[("aOg", p, g)])
        P.dma("sp", lambda e: e.dma_start(out=MS[p][:], in_=dr["att_ms"][:, i * 128:(i + 1) * 128, :, :].rearrange("g p h t -> p g h t")), writes=[("aMS", p)])
        P.op("dve", lambda e: e.tensor_tensor(out=Mx[p][:], in0=MS[p][:, 0, :, 0], in1=MS[p][:, 1, :, 0], op=ALU.max), reads=[("aMS", p)], writes=[("aMx", p)])
        P.op("dve", lambda e: e.tensor_tensor(out=Mx[p][:], in0=Mx[p][:], in1=MS[p][:, 2, :, 0], op=ALU.max), reads=[("aMS", p), ("aMx", p)], writes=[("aMx", p)])
        for g in range(3):
            P.op("dve", lambda e, g=g: e.tensor_tensor(out=Wt_[p][:, g, :], in0=MS[p][:, g, :, 0], in1=Mx[p][:], op=ALU.subtract),
                 reads=[("aMS", p), ("aMx", p)], writes=[("aWt", p, g)])
        P.op("act", lambda e: e.activation(out=Wt_[p][:].rearrange("p a b -> p (a b)"), in_=Wt_[p][:].rearrange("p a b -> p (a b)"), func=AF.Exp), reads=[("aWt", p, g) for g in range(3)], writes=[("aWt", p)])
        P.op("dve", lambda e: e.tensor_tensor(out=Ws[p][:], in0=Wt_[p][:], in1=MS[p][:, :, :, 1], op=ALU.mult), reads=[("aWt", p), ("aMS", p)], writes=[("aWs", p)])
        P.op("dve", lambda e: e.tensor_tensor(out=Dn[p][:], in0=Ws[p][:, 0, :], in1=Ws[p][:, 1, :], op=ALU.add), reads=[("aWs", p)], writes=[("aDn", p)])
        P.op("dve", lambda e: e.tensor_tensor(out=Dn[p][:], in0=Dn[p][:], in1=Ws[p][:, 2, :], op=ALU.add), reads=[("aWs", p), ("aDn", p)], writes=[("aDn", p)])
        P.op("dve", lambda e: e.reciprocal(out=Dn[p][:], in_=Dn[p][:]), reads=[("aDn", p)], writes=[("aDn", p)])
        for g in range(3):
            P.op("dve", lambda e, g=g: e.tensor_tensor(out=Ws[p][:, g, :], in0=Wt_[p][:, g, :], in1=Dn[p][:], op=ALU.mult),
                 reads=[("aWt", p), ("aDn", p), ("aWs", p)], writes=[("aWs", p)])
        for g in range(3):
            eng = "dve" if g != 1 else "pool"
            P.op(eng, lambda e, g=g: e.tensor_tensor(out=Og[p][g][:].rearrange("p (h d) -> p h d", h=16), in0=Og[p][g][:].rearrange("p (h d) -> p h d", h=16),
                                                     in1=Ws[p][:, g, :].unsqueeze(2).to_broadcast([128, 16, 64]), op=ALU.mult),
                 reads=[("aOg", p, g), ("aWs", p)], writes=[("aOg", p, g)])
        P.op("pool", lambda e: e.tensor_tensor(out=Yt[p][:], in0=Og[p][0][:], in1=Og[p][1][:], op=ALU.add), reads=[("aOg", p, 0), ("aOg", p, 1)], writes=[("aYt", p)])
        P.op("dve", lambda e: e.tensor_tensor(out=Yb[p][:], in0=Yt[p][:], in1=Og[p][2][:], op=ALU.add), reads=[("aYt", p), ("aOg", p, 2)], writes=[("aYb", p)])
        for half in range(2):
            bk = next_bank(c)
            psb = c.ps[bk][:].bitcast(BF16)
            for q4 in range(4):
                ch = half * 4 + q4
                P.op("pe", lambda e, ch=ch, q4=q4, psb=psb: e.transpose(out=psb[:, q4 * 128:(q4 + 1) * 128], in_=Yb[p][:, ch * 128:(ch + 1) * 128], identity=c.ident_b[:]),
                     reads=[("aYb", p), ("ident_b",)], writes=[("ps", bk)])
            P.op("act", lambda e, half=half, psb=psb: e.activation(out=YT[:, half * 4:(half + 1) * 4, i * 128:(i + 1) * 128],
                                                                 in_=psb[:, 0:512].rearrange("p (a b) -> p a b", a=4), func=AF.Copy),
                 reads=[("ps", bk)], writes=[("YT", half * 4 + j) for j in range(4)])

    for i in range(NT):
        merge_tile(i, i % 2)
    P.barrier()
    A.reset(mark)
    emit_mixer_epilogue(c, layer, YT, 8, dr["c_w_out"][idx])


WEIGHT_NAMES = ["a_w_in", "a_conv_w", "a_conv_b", "a_w_rgate", "a_b_rgate", "a_w_igate", "a_b_igate", "a_lambda",
                "a_w_out", "b_w_in", "b_w_out", "c_w_in", "c_w_out", "ln_gain", "ln_bias", "moe_w_router",
                "moe_b_router", "moe_w_gu", "moe_b_gu", "moe_w_down", "moe_b_down"]


def make_consts():
    ident = np.eye(128, dtype=np.float32)
    tri = np.triu(np.ones((128, 128), dtype=np.float32), k=1)
    ec = np.tile((np.arange(NE, dtype=np.float32) * CAP)[None, :], (128, 1))
    d = {"c_ident": ident, "c_tri": tri, "c_ec": ec}
    d.update(ret_consts()[0])
    d.update(att_consts())
    return d


def run_plan(plan, x, weights, used=None):
    used = used if used is not None else WEIGHT_NAMES
    ws = {k: weights[k].shape for k in used}
    nc = build(plan, ws)
    consts = make_consts()
    in_maps = []
    for b in range(8):
        m = {"x": np.ascontiguousarray(x[b])}
        for k in used:
            m[k] = weights[k]
        m.update(consts)
        in_maps.append(m)
    res = run_bass_kernel_spmd(nc, in_maps, core_ids=list(range(8)))
    return np.stack([r["out"] for r in res.results], axis=0)


FULL_PLAN = [("prep",),
             ("rglru", 0, 0), ("moe", 0, False),
             ("ret", 1, 0), ("moe", 1, False),
             ("attn", 2, 0), ("moe", 2, False),
             ("rglru", 3, 1), ("moe", 3, False)]


def kernel(**inputs):
    x = np.asarray(inputs["x"], dtype=np.float32)
    weights = {k: np.ascontiguousarray(np.asarray(inputs[k], dtype=np.float32)) for k in WEIGHT_NAMES}
    out = run_plan(FULL_PLAN, x, weights)
    return out.astype(np.float32)
```

```python
import contextlib
import numpy as np
import concourse.bass as bass
import concourse.mybir as mybir
from concourse.bass_utils import run_bass_kernel_spmd

F32 = mybir.dt.float32
BF16 = mybir.dt.bfloat16
I32 = mybir.dt.int32
U8 = mybir.dt.uint8
AF = mybir.ActivationFunctionType
ALU = mybir.AluOpType
AX = mybir.AxisListType

D = 1024
S = 2048
NT = 16
NE = 32
CAP = 384
NST = CAP // 128
DEPTH = 4
ALPHA = (2.0 * DEPTH) ** 0.25
LN_EPS = 1e-5
ENGS = ("pe", "act", "dve", "pool", "sp")


class Op:
    __slots__ = ("eng", "fn", "deps", "is_dma", "sig", "cnt", "sem", "target")

    def __init__(s, eng, fn, is_dma):
        s.eng = eng; s.fn = fn; s.deps = []; s.is_dma = is_dma
        s.sig = False; s.cnt = 0; s.sem = None; s.target = 0


class Prog:
    def __init__(s, nc):
        s.nc = nc
        s.ops = {e: [] for e in ENGS}
        s.last_w = {}
        s.readers = {}
        s.n_dma_sems = {"sp": 16, "act": 8, "pool": 16}
        s.since_barrier = []

    def _add(s, eng, fn, reads, writes, is_dma):
        op = Op(eng, fn, is_dma)
        deps = set()
        for k in reads:
            w = s.last_w.get(k)
            if w is not None:
                deps.add(w)
        for k in writes:
            w = s.last_w.get(k)
            if w is not None and not (w.eng == eng and eng == "pe" and not w.is_dma and not is_dma):
                deps.add(w)
        for k in writes:
            for r in s.readers.get(k, ()):
                if r.eng == eng and not r.is_dma and not is_dma:
                    continue
                deps.add(r)
        op.deps = list(deps)
        for k in reads:
            s.readers.setdefault(k, []).append(op)
        for k in writes:
            s.last_w[k] = op
            s.readers[k] = []
        s.ops[eng].append(op)
        s.since_barrier.append(op)
        return op

    def op(s, eng, fn, reads=(), writes=()):
        return s._add(eng, fn, reads, writes, False)

    def dma(s, eng, fn, reads=(), writes=()):
        return s._add(eng, fn, reads, writes, True)

    def barrier(s):
        prev = s.since_barrier
        s.since_barrier = []
        lastc = {}
        dmas = []
        for op in prev:
            if op.fn is None:
                continue
            if op.is_dma:
                dmas.append(op)
            else:
                lastc[op.eng] = op
        deps = list(lastc.values()) + dmas
        for e in ENGS:
            v = Op(e, None, False)
            v.deps = list(deps)
            s.ops[e].append(v)
        s.last_w = {}
        s.readers = {}

    def emit(s, final_wait_ops=()):
        nc = s.nc
        for e in ENGS:
            for op in s.ops[e]:
                for d in op.deps:
                    if not d.is_dma:
                        d.sig = True
        for e in ENGS:
            c = 0
            for op in s.ops[e]:
                if not op.is_dma and op.sig and op.fn is not None:
                    c += 1
                    op.cnt = c
        stack = contextlib.ExitStack()
        prog_sem = {}
        for e in ("pe", "act", "dve", "pool"):
            prog_sem[e] = stack.enter_context(nc.semaphore("prog_" + e))
        for q in ("sp", "act", "pool"):
            sems = [stack.enter_context(nc.semaphore(f"dq_{q}_{i}")) for i in range(s.n_dma_sems[q])]
            cnts = [0] * len(sems)
            prev = [None] * len(sems)
            i = 0
            for op in s.ops[q]:
                if op.is_dma:
                    j = i % len(sems)
                    i += 1
                    cnts[j] += 16
                    op.sem = sems[j]
                    op.target = cnts[j]
                    if prev[j] is not None:
                        op.deps.append(prev[j])
                    prev[j] = op
        engobj = {"pe": nc.tensor, "act": nc.scalar, "dve": nc.vector, "pool": nc.gpsimd, "sp": nc.sync}
        finals = list(final_wait_ops)

        def run_engine(e):
            eng = engobj[e]
            waited = {}
            for op in s.ops[e]:
                need = {}
                for d in op.deps:
                    if d.is_dma:
                        key = ("d", id(d.sem)); val = d.target; sem = d.sem
                    else:
                        key = ("c", d.eng); val = d.cnt; sem = prog_sem[d.eng]
                    if val > need.get(key, (0, None))[0]:
                        need[key] = (val, sem)
                for key, (val, sem) in need.items():
                    if waited.get(key, 0) >= val:
                        continue
                    waited[key] = val
                    eng.wait_ge(sem, val)
                if op.fn is None:
                    continue
                ins = op.fn(eng)
                if op.is_dma:
                    ins.then_inc(op.sem, 16)
                elif op.sig:
                    ins.then_inc(prog_sem[e], 1)
            if e == "sp":
                for d in finals:
                    eng.wait_ge(d.sem, d.target)

        with stack:
            with nc.Block() as block:
                @block.tensor
                def _(t):
                    run_engine("pe")

                @block.scalar
                def _(t):
                    run_engine("act")

                @block.vector
                def _(t):
                    run_engine("dve")

                @block.gpsimd
                def _(t):
                    run_engine("pool")

                @block.sync
                def _(t):
                    run_engine("sp")


class Ctx:
    pass


class Arena:
    def __init__(s, nc, base, limit):
        s.nc = nc; s.base = base; s.limit = limit; s.off = base; s.n = 0

    def reset(s, off=None):
        s.off = s.base if off is None else off

    def alloc(s, name, shape, dtype):
        sz = int(np.prod(shape[1:])) * mybir.dt.size(dtype)
        sz = (sz + 63) // 64 * 64
        assert s.off + sz <= s.limit, (name, s.off, sz, s.limit)
        s.n += 1
        t = s.nc.alloc_sbuf_tensor_at(f"{name}_{s.n}", list(shape), dtype, offset=s.off)
        s.off += sz
        return t


def build(plan, weights_shapes):
    nc = bass.Bass("TRN2", target_bir_lowering=False)
    c = Ctx()
    c.nc = nc
    P = Prog(nc)
    c.P = P
    dr = {}
    dr["x"] = nc.dram_tensor("x", [S, D], F32, kind="ExternalInput").ap()
    for name, shp in weights_shapes.items():
        dr[name] = nc.dram_tensor(name, list(shp), F32, kind="ExternalInput").ap()
    dr["c_ident"] = nc.dram_tensor("c_ident", [128, 128], F32, kind="ExternalInput").ap()
    dr["c_tri"] = nc.dram_tensor("c_tri", [128, 128], F32, kind="ExternalInput").ap()
    dr["c_ec"] = nc.dram_tensor("c_ec", [128, NE], F32, kind="ExternalInput").ap()
    rc, _ = ret_consts()
    for k_, v_ in rc.items():
        dr[k_] = nc.dram_tensor(k_, list(v_.shape), F32, kind="ExternalInput").ap()
    for k_, v_ in att_consts().items():
        dr[k_] = nc.dram_tensor(k_, list(v_.shape), F32, kind="ExternalInput").ap()
    dr["att_o"] = nc.dram_tensor("att_o_scr", [3, S, D], F32, kind="Internal").ap()
    dr["att_ms"] = nc.dram_tensor("att_ms_scr", [3, S, 16, 2], F32, kind="Internal").ap()
    dr["out"] = nc.dram_tensor("out", [S, D], F32, kind="ExternalOutput").ap()
    dr["xg"] = nc.dram_tensor("xg_scr", [NE * CAP, D], BF16, kind="Internal").ap()
    dr["yy"] = nc.dram_tensor("yy_scr", [NE * CAP, D], F32, kind="Internal").ap()
    c.dr = dr

    slab = nc.alloc_sbuf_tensor("arena_slab", [128, 206 * 1024], U8)
    base = nc.lookup_mloc(slab).addr
    A = Arena(nc, base, base + 206 * 1024)
    c.A = A
    c.X = A.alloc("X", [128, NT, D], F32)
    c.XT = A.alloc("XT", [128, 8, S], BF16)
    c.xt_off = A.off - 8 * S * 2
    c.ident_f = A.alloc("ident_f", [128, 128], F32)
    c.ident_b = A.alloc("ident_b", [128, 128], BF16)
    c.tri_b = A.alloc("tri_b", [128, 128], BF16)
    c.ones_b = A.alloc("ones_b", [128, 128], BF16)
    c.ec = A.alloc("ec", [128, NE], F32)
    c.tri_f = A.alloc("tri_f", [128, 128], F32)
    c.eps_t = A.alloc("eps", [128, 1], F32)
    c.one_t = A.alloc("one", [128, 1], F32)
    c.ADRI = A.alloc("ADRI", [128, NT, 4], I32)
    c.G = A.alloc("G", [128, NT, 4], F32)
    c.phase_base = A.off
    c.ps = [nc.alloc_psum_tensor(f"ps{i}", [128, 512], F32) for i in range(8)]
    c.bank = 0

    X = c.X
    P.dma("sp", lambda e: e.dma_start(out=c.ident_f[:], in_=dr["c_ident"]), writes=[("ident_f",)])
    P.dma("sp", lambda e: e.dma_start(out=c.tri_f[:], in_=dr["c_tri"]), writes=[("tri_f",)])
    P.dma("sp", lambda e: e.dma_start(out=c.ec[:], in_=dr["c_ec"]), writes=[("ec",)])
    P.op("dve", lambda e: e.tensor_copy(out=c.ident_b[:], in_=c.ident_f[:]), reads=[("ident_f",)], writes=[("ident_b",)])
    P.op("dve", lambda e: e.tensor_copy(out=c.tri_b[:], in_=c.tri_f[:]), reads=[("tri_f",)], writes=[("tri_b",)])
    P.op("dve", lambda e: e.memset(c.ones_b[:], 1.0), writes=[("ones_b",)])
    P.op("dve", lambda e: e.memset(c.eps_t[:], LN_EPS), writes=[("eps",)])
    P.op("dve", lambda e: e.memset(c.one_t[:], 1.0), writes=[("one",)])
    for i in range(NT):
        P.dma("sp", lambda e, i=i: e.dma_start(out=X[:, i, :], in_=dr["x"][i * 128:(i + 1) * 128, :]),
              writes=[("X", i)])

    for ph in plan:
        A.reset(c.phase_base)
        if ph[0] == "prep":
            phase_prep(c)
        elif ph[0] == "moe":
            phase_moe(c, ph[1], do_route_prep=ph[2])
        elif ph[0] == "rglru":
            phase_rglru(c, ph[1], ph[2])
        elif ph[0] == "ret":
            phase_ret(c, ph[1], ph[2])
        elif ph[0] == "attn":
            phase_attn(c, ph[1], ph[2])
        else:
            raise ValueError(ph)
        P.barrier()

    outs = []
    for i in range(NT):
        outs.append(P.dma("sp", lambda e, i=i: e.dma_start(out=dr["out"][i * 128:(i + 1) * 128, :], in_=X[:, i, :]),
                          reads=[("X", i)]))
    P.emit(final_wait_ops=outs)
    return nc


def next_bank(c, n=8):
    b = c.bank % n
    c.bank = (c.bank + 1) % n
    return b


def load_ln_params(c, layer, which, tag):
    A, P, dr = c.A, c.P, c.dr
    g = A.alloc("lng" + tag, [128, D], F32)
    b = A.alloc("lnb" + tag, [128, D], F32)
    P.dma("sp", lambda e: e.dma_start(out=g[:], in_=dr["ln_gain"][layer, which, :].partition_broadcast(128)),
          writes=[("lng", tag)])
    P.dma("sp", lambda e: e.dma_start(out=b[:], in_=dr["ln_bias"][layer, which, :].partition_broadcast(128)),
          writes=[("lnb", tag)])
    return g, b


def alloc_ln_tmp(c):
    A = c.A
    t = Ctx()
    t.st = [A.alloc("lnst", [128, 2, 6], F32) for _ in range(2)]
    t.mv = [A.alloc("lnmv", [128, 2], F32) for _ in range(2)]
    t.sd = [A.alloc("lnsd", [128, 1], F32) for _ in range(2)]
    t.rs = [A.alloc("lnrs", [128, 1], F32) for _ in range(2)]
    t.xn = [A.alloc("lnxn", [128, D], F32) for _ in range(2)]
    return t


def emit_ln(c, t, Z, zkey, i, g, b, tag):
    P = c.P
    X = c.X
    p = i % 2
    st, mv, sd, rs, xn = t.st[p], t.mv[p], t.sd[p], t.rs[p], t.xn[p]
    for h in range(2):
        P.op("dve", lambda e, h=h: e.bn_stats(out=st[:, h, :], in_=Z[:, h * 512:(h + 1) * 512]),
             reads=[zkey], writes=[("lnst", p, h)])
    P.op("dve", lambda e: e.bn_aggr(out=mv[:], in_=st[:].rearrange("p a b -> p (a b)")),
         reads=[("lnst", p, 0), ("lnst", p, 1)], writes=[("lnmv", p)])
    P.op("act", lambda e: e.activation(out=sd[:], in_=mv[:, 1:2], func=AF.Sqrt, bias=c.eps_t[:], scale=1.0),
         reads=[("lnmv", p), ("eps",)], writes=[("lnsd", p)])
    P.op("dve", lambda e: e.reciprocal(out=rs[:], in_=sd[:]), reads=[("lnsd", p)], writes=[("lnrs", p)])
    P.op("dve", lambda e: e.tensor_scalar(out=xn[:], in0=Z, scalar1=mv[:, 0:1], scalar2=rs[:, 0:1],
                                          op0=ALU.subtract, op1=ALU.mult),
         reads=[zkey, ("lnmv", p), ("lnrs", p)], writes=[("lnxn", p)])
    P.op("dve", lambda e: e.tensor_tensor(out=xn[:], in0=xn[:], in1=g[:], op=ALU.mult),
         reads=[("lnxn", p), ("lng", tag)], writes=[("lnxn", p)])
    P.op("dve", lambda e: e.tensor_tensor(out=X[:, i, :], in0=xn[:], in1=b[:], op=ALU.add),
         reads=[("lnxn", p), ("lnb", tag)], writes=[("X", i)])


def alloc_prep(c):
    A = c.A
    t = Ctx()
    t.xtf = [A.alloc("xtf", [128, 8, 128], F32) for _ in range(2)]
    return t


def emit_transpose_tile(c, t, i, write_xt=True):
    P = c.P
    X, XT = c.X, c.XT
    p = i % 2
    xtf = t.xtf[p]
    for half in range(2):
        bk = next_bank(c)
        ps = c.ps[bk]
        for q in range(4):
            ch = half * 4 + q
            P.op("pe", lambda e, ch=ch, q=q, ps=ps: e.transpose(out=ps[:, q * 128:(q + 1) * 128],
                                                               in_=X[:, i, ch * 128:(ch + 1) * 128],
                                                               identity=c.ident_f[:]),
                 reads=[("X", i), ("ident_f",)], writes=[("ps", bk)])
        P.op("act", lambda e, half=half, ps=ps: e.activation(
            out=xtf[:, half * 4:(half + 1) * 4, :], in_=ps[:].rearrange("p (a b) -> p a b", a=4), func=AF.Copy),
            reads=[("ps", bk)], writes=[("xtf", p, half)])
        if write_xt:
            P.op("act", lambda e, half=half, ps=ps: e.activation(
                out=XT[:, half * 4:(half + 1) * 4, i * 128:(i + 1) * 128], in_=ps[:].rearrange("p (a b) -> p a b", a=4), func=AF.Copy),
                reads=[("ps", bk)], writes=[("XT", i)])


def phase_prep(c):
    t = alloc_prep(c)
    zt = c.A.alloc("zt", [128, NST, D], BF16)
    c.P.op("pool", lambda e: e.memset(zt[:], 0.0), writes=[("zt",)])
    for e_ in range(NE):
        c.P.dma("sp", lambda e, e_=e_: e.dma_start(out=c.dr["xg"][e_ * CAP:(e_ + 1) * CAP, :].rearrange("(t p) d -> p t d", p=128), in_=zt[:]),
                reads=[("zt",)], writes=[("xg_zero", e_)])
    for i in range(NT):
        emit_transpose_tile(c, t, i)


def alloc_route(c, layer):
    A, P, dr = c.A, c.P, c.dr
    r = Ctx()
    r.wr = A.alloc("wr", [128, 8, NE], F32)
    r.br = A.alloc("br", [128, NE], F32)
    r.L = A.alloc("L", [128, NT, NE], F32)
    r.T8 = A.alloc("T8", [128, NT, 8], F32)
    r.M = A.alloc("M", [128, NT, NE], BF16)
    r.CUM = A.alloc("CUM", [128, NT, NE], BF16)
    r.At = [A.alloc("At", [128, NE], F32) for _ in range(2)]
    r.junk = A.alloc("junk", [128, 4, NE], F32)
    r.ADRF = A.alloc("ADRF", [128, NT, 4], F32)
    r.ADRI = c.ADRI
    r.G = c.G
    r.E4 = A.alloc("E4", [128, NT, 4], F32)
    r.nmx = A.alloc("nmx", [128, NT], F32)
    r.sm = A.alloc("sm", [128, NT], F32)
    r.rsm = A.alloc("rsm", [128, NT], F32)
    r.XB = [A.alloc("XB", [128, D], BF16) for _ in range(2)]
    P.dma("sp", lambda e: e.dma_start(out=r.wr[:], in_=dr["moe_w_router"][layer].rearrange("(k p) n -> p k n", p=128)),
          writes=[("wr",)])
    P.dma("sp", lambda e: e.dma_start(out=r.br[:], in_=dr["moe_b_router"][layer, :].partition_broadcast(128)),
          writes=[("br",)])
    return r


def emit_route_tile(c, r, t, i):
    P, dr = c.P, c.dr
    X = c.X
    p = i % 2
    xtf = t.xtf[p]
    bk = next_bank(c)
    ps = c.ps[bk]
    for k in range(8):
        P.op("pe", lambda e, k=k: e.matmul(ps[:, 0:NE], lhsT=xtf[:, k, :], rhs=r.wr[:, k, :], start=(k == 0), stop=(k == 7)),
             reads=[("xtf", p, k // 4), ("wr",)], writes=[("ps", bk)])
    P.op("dve", lambda e: e.tensor_tensor(out=r.L[:, i, :], in0=ps[:, 0:NE], in1=r.br[:], op=ALU.add),
         reads=[("ps", bk), ("br",)], writes=[("L", i)])
    P.op("dve", lambda e: e.max(out=r.T8[:, i, :], in_=r.L[:, i, :]), reads=[("L", i)], writes=[("T8", i)])
    P.op("dve", lambda e: e.tensor_scalar(out=r.M[:, i, :], in0=r.L[:, i, :], scalar1=r.T8[:, i, 3:4], scalar2=None,
                                          op0=ALU.is_ge),
         reads=[("L", i), ("T8", i)], writes=[("M", i)])
    bk2 = next_bank(c)
    ps2 = c.ps[bk2]
    P.op("pe", lambda e: e.matmul(ps2[:, 0:NE], lhsT=c.tri_b[:], rhs=r.M[:, i, :], start=True, stop=(i == 0)),
         reads=[("tri_b",), ("M", i)], writes=[("ps", bk2)])
    if i > 0:
        P.op("pe", lambda e: e.matmul(ps2[:, 0:NE], lhsT=c.ones_b[:], rhs=r.CUM[:, i - 1, :], start=False, stop=True),
             reads=[("ones_b",), ("CUM", i - 1)], writes=[("ps", bk2)])
        P.op("pool", lambda e: e.tensor_tensor(out=r.CUM[:, i, :], in0=r.CUM[:, i - 1, :], in1=r.M[:, i, :], op=ALU.add),
             reads=[("CUM", i - 1), ("M", i)], writes=[("CUM", i)])
    else:
        P.op("pool", lambda e: e.tensor_copy(out=r.CUM[:, 0, :], in_=r.M[:, 0, :]), reads=[("M", 0)], writes=[("CUM", 0)])
    At = r.At[p]
    P.op("dve", lambda e: e.tensor_tensor(out=At[:], in0=ps2[:, 0:NE], in1=c.ec[:], op=ALU.add),
         reads=[("ps", bk2), ("ec",)], writes=[("At", p)])
    for k in range(4):
        P.op("dve", lambda e, k=k: e.scalar_tensor_tensor(out=r.junk[:, k, :], in0=r.L[:, i, :], scalar=r.T8[:, i, k:k + 1],
                                                          in1=At[:], op0=ALU.is_equal, op1=ALU.mult),
             reads=[("L", i), ("T8", i), ("At", p)], writes=[("junk", k)])
    P.op("dve", lambda e: e.reduce_sum(out=r.ADRF[:, i, :], in_=r.junk[:], axis=AX.X),
         reads=[("junk", k) for k in range(4)], writes=[("ADRF", i)])
    P.op("dve", lambda e: e.tensor_copy(out=r.ADRI[:, i, :], in_=r.ADRF[:, i, :]),
         reads=[("ADRF", i)], writes=[("ADRI", i)])
    P.op("dve", lambda e: e.tensor_scalar(out=r.nmx[:, i:i + 1], in0=r.T8[:, i, 0:1], scalar1=-1.0, scalar2=None, op0=ALU.mult),
         reads=[("T8", i)], writes=[("nmx", i)])
    P.op("act", lambda e: e.activation(out=r.E4[:, i, :], in_=r.T8[:, i, 0:4], func=AF.Exp, bias=r.nmx[:, i:i + 1], scale=1.0),
         reads=[("T8", i), ("nmx", i)], writes=[("E4", i)])
    P.op("dve", lambda e: e.reduce_sum(out=r.sm[:, i:i + 1], in_=r.E4[:, i, :], axis=AX.X), reads=[("E4", i)], writes=[("sm", i)])
    P.op("dve", lambda e: e.reciprocal(out=r.rsm[:, i:i + 1], in_=r.sm[:, i:i + 1]), reads=[("sm", i)], writes=[("rsm", i)])
    P.op("dve", lambda e: e.tensor_scalar(out=r.G[:, i, :], in0=r.E4[:, i, :], scalar1=r.rsm[:, i:i + 1], scalar2=None, op0=ALU.mult),
         reads=[("E4", i), ("rsm", i)], writes=[("G", i)])
    XB = r.XB[p]
    P.op("act", lambda e: e.activation(out=XB[:], in_=X[:, i, :], func=AF.Copy), reads=[("X", i)], writes=[("XB", p)])
    for k in range(4):
        P.dma("pool", lambda e, k=k: e.indirect_dma_start(
            out=dr["xg"], out_offset=bass.IndirectOffsetOnAxis(ap=r.ADRI[:, i, k:k + 1], axis=0),
            in_=XB[:], in_offset=None),
            reads=[("XB", p), ("ADRI", i)], writes=[("xg_dram", i, k)])


def phase_moe(c, layer, do_route_prep):
    A, P, dr = c.A, c.P, c.dr
    X, XT = c.X, c.XT
    nc = c.nc
    r = alloc_route(c, layer) if do_route_prep else None
    t = alloc_prep(c)
    if do_route_prep:
        for i in range(NT):
            emit_transpose_tile(c, t, i, write_xt=False)
            emit_route_tile(c, r, t, i)
    mark = A.off
    RING = 6 if do_route_prep else 7
    wring = [A.alloc("wring", [128, 8, 512], BF16) for _ in range(RING)]
    bgu = A.alloc("bgu", [128, 16, NE], F32)
    braw = A.alloc("braw", [NE, 2 * D], F32)
    P.dma("sp", lambda e: e.dma_start(out=braw[:], in_=dr["moe_b_gu"][layer]), writes=[("braw",)])
    bkb = next_bank(c)
    for cc in range(16):
        P.op("pe", lambda e, cc=cc: e.transpose(out=c.ps[bkb][:, cc * NE:(cc + 1) * NE], in_=braw[:, cc * 128:(cc + 1) * 128],
                                                identity=c.ident_f[0:NE, 0:NE]),
             reads=[("braw",), ("ident_f",)], writes=[("ps", bkb)])
    P.op("dve", lambda e: e.tensor_copy(out=bgu[:].rearrange("p a b -> p (a b)"), in_=c.ps[bkb][:, :]),
         reads=[("ps", bkb)], writes=[("bgu",)])
    P.op("dve", lambda e: e.tensor_scalar(out=bgu[:, 8:16, :], in0=bgu[:, 8:16, :], scalar1=1.0, scalar2=None, op0=ALU.add),
         reads=[("bgu",)], writes=[("bgu",)])
    bd = [A.alloc("bd", [128, D], F32) for _ in range(2)]
    ytile = [A.alloc("ytile", [128, D], F32) for _ in range(2)]
    gt = [A.alloc("gt", [128, CAP], F32) for _ in range(2)]
    ut = [A.alloc("ut", [128, CAP], F32) for _ in range(2)]
    sg = [A.alloc("sg", [128, CAP], F32) for _ in range(2)]
    A2 = Arena(nc, c.xt_off, c.xt_off + 8 * S * 2)
    A2.n = 1000
    xgtok = [A2.alloc("xgtok", [128, NST, D], BF16) for _ in range(2)]
    xgT = [A2.alloc("xgT", [128, 8, CAP], BF16) for _ in range(2)]
    hT = A2.alloc("hT", [128, 8, CAP], BF16)

    pieces = []
    for e_ in range(NE):
        for pc in (0, 2, 1, 3):
            pieces.append((e_, "gu", pc))
        for pc in (0, 1):
            pieces.append((e_, "dn", pc))
    piece_slot = {}

    def issue_piece(n):
        if n >= len(pieces):
            return
        e_, kind, pc = pieces[n]
        slot = n % RING
        piece_slot[(e_, kind, pc)] = slot
        if kind == "gu":
            src = dr["moe_w_gu"][layer, e_, :, pc * 512:(pc + 1) * 512]
        else:
            src = dr["moe_w_down"][layer, e_, :, pc * 512:(pc + 1) * 512]
        P.dma("pool", lambda e, src=src, slot=slot: e.dma_start(out=wring[slot][:], in_=src.rearrange("(k p) n -> p k n", p=128)),
              writes=[("wring", slot)])

    PRE = RING - 1
    for n in range(PRE):
        issue_piece(n)
    nissued = [PRE]
    def ex_load(e_):
        pe2 = e_ % 2
        P.dma("sp", lambda e, e_=e_: e.dma_start(out=xgtok[e_ % 2][:], in_=dr["xg"][e_ * CAP:(e_ + 1) * CAP, :].rearrange("(t p) d -> p t d", p=128)),
              reads=[("xg_dram", i_, k_) for i_ in range(NT) for k_ in range(4)], writes=[("xgtok", e_ % 2)])
        P.dma("sp", lambda e, e_=e_, pe2=pe2: e.dma_start(out=bd[pe2][:], in_=dr["moe_b_down"][layer, e_, :].partition_broadcast(128)),
              writes=[("bd", pe2)])
        for st in range(NST):
            for half in range(2):
                bk = next_bank(c)
                psb = c.ps[bk][:].bitcast(BF16)
                for q in range(4):
                    ch = half * 4 + q
                    P.op("pe", lambda e, st=st, ch=ch, q=q, psb=psb: e.transpose(
                        out=psb[:, q * 128:(q + 1) * 128], in_=xgtok[pe2][:, st, ch * 128:(ch + 1) * 128], identity=c.ident_b[:]),
                        reads=[("xgtok", pe2), ("ident_b",)], writes=[("ps", bk)])
                P.op("act", lambda e, st=st, half=half, psb=psb, pe2=pe2: e.activation(
                    out=xgT[pe2][:, half * 4:(half + 1) * 4, st * 128:(st + 1) * 128],
                    in_=psb[:, 0:512].rearrange("p (a b) -> p a b", a=4), func=AF.Copy),
                    reads=[("ps", bk)], writes=[("xgT", pe2)])

    def ex_gu(e_):
        pe2 = e_ % 2
        for fc in range(8):
            pcg = fc // 4
            pcu = 2 + fc // 4
            lc = (fc % 4) * 128
            if fc % 4 == 0:
                pass
            sg_ = piece_slot[(e_, "gu", pcg)]
            su_ = piece_slot[(e_, "gu", pcu)]
            bkg = next_bank(c); bku = next_bank(c)
            psg, psu = c.ps[bkg], c.ps[bku]
            for k in range(8):
                P.op("pe", lambda e, k=k, sg_=sg_, lc=lc, psg=psg, pe2=pe2: e.matmul(
                    psg[:, 0:CAP], lhsT=wring[sg_][:, k, lc:lc + 128], rhs=xgT[pe2][:, k, :], start=(k == 0), stop=(k == 7)),
                    reads=[("wring", sg_), ("xgT", pe2)], writes=[("ps", bkg)])
            for k in range(8):
                P.op("pe", lambda e, k=k, su_=su_, lc=lc, psu=psu, pe2=pe2: e.matmul(
                    psu[:, 0:CAP], lhsT=wring[su_][:, k, lc:lc + 128], rhs=xgT[pe2][:, k, :], start=(k == 0), stop=(k == 7)),
                    reads=[("wring", su_), ("xgT", pe2)], writes=[("ps", bku)])
            pp = fc % 2
            P.op("dve", lambda e, psg=psg, fc=fc, pp=pp, e_=e_: e.tensor_scalar(
                out=gt[pp][:], in0=psg[:, 0:CAP], scalar1=bgu[:, fc, e_:e_ + 1], scalar2=7.0, op0=ALU.add, op1=ALU.min),
                reads=[("ps", bkg), ("bgu",)], writes=[("gt", pp)])
            P.op("dve", lambda e, psu=psu, fc=fc, pp=pp, e_=e_: e.tensor_scalar(
                out=ut[pp][:], in0=psu[:, 0:CAP], scalar1=bgu[:, 8 + fc, e_:e_ + 1], scalar2=8.0, op0=ALU.add, op1=ALU.min),
                reads=[("ps", bku), ("bgu",)], writes=[("ut", pp)])
            P.op("act", lambda e, pp=pp: e.activation(out=sg[pp][:], in_=gt[pp][:], func=AF.Silu, scale=1.702),
                 reads=[("gt", pp)], writes=[("sg", pp)])
            P.op("dve", lambda e, pp=pp, fc=fc: e.scalar_tensor_tensor(out=hT[:, fc, :], in0=ut[pp][:], scalar=-6.0, in1=sg[pp][:],
                                                                    op0=ALU.max, op1=ALU.mult),
                 reads=[("ut", pp), ("sg", pp)], writes=[("hT", fc)])
            if fc % 4 == 3:
                issue_piece(nissued[0]); issue_piece(nissued[0] + 1)
                nissued[0] += 2

    def ex_down(e_):
        pe2 = e_ % 2
        for st in range(NST):
            yp = st % 2
            for nh in range(2):
                sd_ = piece_slot[(e_, "dn", nh)]
                bk = next_bank(c)
                psd = c.ps[bk]
                for fc in range(8):
                    P.op("pe", lambda e, fc=fc, st=st, sd_=sd_, psd=psd: e.matmul(
                        psd[:, :], lhsT=hT[:, fc, st * 128:(st + 1) * 128], rhs=wring[sd_][:, fc, :], start=(fc == 0), stop=(fc == 7)),
                        reads=[("hT", fc), ("wring", sd_)], writes=[("ps", bk)])
                P.op("dve", lambda e, nh=nh, yp=yp, psd=psd, pe2=pe2: e.scalar_tensor_tensor(
                    out=ytile[yp][:, nh * 512:(nh + 1) * 512], in0=psd[:, :], scalar=1.0 / 1.702, in1=bd[pe2][:, nh * 512:(nh + 1) * 512],
                    op0=ALU.mult, op1=ALU.add),
                    reads=[("ps", bk), ("bd", pe2)], writes=[("ytile", yp, nh)])
            P.dma("sp", lambda e, e_=e_, st=st, yp=yp: e.dma_start(
                out=dr["yy"][e_ * CAP + st * 128:e_ * CAP + (st + 1) * 128, :], in_=ytile[yp][:]),
                reads=[("ytile", yp, 0), ("ytile", yp, 1)], writes=[("yy_dram", e_, st)])

    ex_load(0)
    for e_ in range(NE):
        ex_gu(e_)
        if e_ + 1 < NE:
            ex_load(e_ + 1)
        ex_down(e_)
        issue_piece(nissued[0]); issue_piece(nissued[0] + 1)
        nissued[0] += 2

    P.barrier()
    A.reset(mark)
    g2, b2 = load_ln_params(c, layer, 1, "b")
    lt = alloc_ln_tmp(c)
    NYK = 3 if do_route_prep else 4
    YK = [[A.alloc("YK", [128, D], F32) for _ in range(4)] for _ in range(NYK)]
    Z = [A.alloc("Z", [128, D], F32) for _ in range(2)]
    t2 = t
    for i in range(NT):
        p = i % 2
        py = i % NYK
        for k in range(4):
            P.dma("pool", lambda e, k=k, py=py, i=i: e.indirect_dma_start(
                out=YK[py][k][:], out_offset=None, in_=dr["yy"],
                in_offset=bass.IndirectOffsetOnAxis(ap=c.ADRI[:, i, k:k + 1], axis=0)),
                reads=[("ADRI", i)], writes=[("YK", py, k)])
        P.op("dve", lambda e, p=p, py=py, i=i: e.tensor_scalar(out=Z[p][:], in0=YK[py][0][:], scalar1=c.G[:, i, 0:1], scalar2=None, op0=ALU.mult),
             reads=[("YK", py, 0), ("G", i)], writes=[("Z", p)])
        for k in range(1, 4):
            P.op("dve", lambda e, p=p, py=py, i=i, k=k: e.scalar_tensor_tensor(
                out=Z[p][:], in0=YK[py][k][:], scalar=c.G[:, i, k:k + 1], in1=Z[p][:], op0=ALU.mult, op1=ALU.add),
                reads=[("YK", py, k), ("G", i), ("Z", p)], writes=[("Z", p)])
        P.op("dve", lambda e, p=p, i=i: e.scalar_tensor_tensor(
            out=Z[p][:], in0=X[:, i, :], scalar=ALPHA, in1=Z[p][:], op0=ALU.mult, op1=ALU.add),
            reads=[("X", i), ("Z", p)], writes=[("Z", p)])
        emit_ln(c, lt, Z[p][:], ("Z", p), i, g2, b2, "b")
        emit_transpose_tile(c, t2, i, write_xt=True)


def emit_mixer_epilogue(c, layer, YT, kc, wout_dram):
    A, P, dr = c.A, c.P, c.dr
    X = c.X
    wout = A.alloc("wout", [128, kc, D], BF16)
    for k2 in range(0, kc, 4):
        P.dma("pool", lambda e, k2=k2: e.dma_start(
            out=wout[:, k2:k2 + 4, :], in_=wout_dram[k2 * 128:(k2 + 4) * 128, :].rearrange("(k p) n -> p k n", p=128)),
            writes=[("wout", k2)])
    r = alloc_route(c, layer)
    g1, b1 = load_ln_params(c, layer, 0, "a")
    lt = alloc_ln_tmp(c)
    t = alloc_prep(c)
    Z = [A.alloc("Z", [128, D], F32) for _ in range(2)]
    for i in range(NT):
        p = i % 2
        for nh in range(2):
            bk = next_bank(c)
            ps = c.ps[bk]
            for k in range(kc):
                P.op("pe", lambda e, k=k, nh=nh, ps=ps, i=i: e.matmul(
                    ps[:, :], lhsT=YT[:, k, i * 128:(i + 1) * 128], rhs=wout[:, k, nh * 512:(nh + 1) * 512],
                    start=(k == 0), stop=(k == kc - 1)),
                    reads=[("YT", k), ("wout", (k // 4) * 4)], writes=[("ps", bk)])
            P.op("dve", lambda e, nh=nh, ps=ps, p=p, i=i: e.scalar_tensor_tensor(
                out=Z[p][:, nh * 512:(nh + 1) * 512], in0=X[:, i, nh * 512:(nh + 1) * 512], scalar=ALPHA, in1=ps[:, :],
                op0=ALU.mult, op1=ALU.add),
                reads=[("X", i), ("ps", bk)], writes=[("Z", p, nh)])
        P.op("dve", lambda e: e.engine_nop(), reads=[("Z", p, 0), ("Z", p, 1)], writes=[("Z", p)])
        emit_ln(c, lt, Z[p][:], ("Z", p), i, g1, b1, "a")
        emit_transpose_tile(c, t, i, write_xt=False)
        emit_route_tile(c, r, t, i)


def load_small_vecs(c, rows, nvec):
    A, P = c.A, c.P
    braw = A.alloc("svraw", [nvec, D], F32)
    pv = A.alloc("pv", [128, 8, nvec], F32)
    for j, ap in enumerate(rows):
        P.dma("sp", lambda e, j=j, ap=ap: e.dma_start(out=braw[j:j + 1, :], in_=ap.unsqueeze(0)), writes=[("svraw", j)])
    bk = next_bank(c)
    for cc in range(8):
        P.op("pe", lambda e, cc=cc: e.transpose(out=c.ps[bk][:, cc * nvec:(cc + 1) * nvec], in_=braw[:, cc * 128:(cc + 1) * 128],
                                                identity=c.ident_f[0:nvec, 0:nvec]),
             reads=[("svraw", j) for j in range(nvec)] + [("ident_f",)], writes=[("ps", bk)])
    P.op("dve", lambda e: e.tensor_copy(out=pv[:].rearrange("p a b -> p (a b)"), in_=c.ps[bk][:, 0:8 * nvec]),
         reads=[("ps", bk)], writes=[("pv",)])
    return pv


def phase_rglru(c, layer, idx):
    A, P, dr = c.A, c.P, c.dr
    X, XT = c.X, c.XT
    TB = 512
    NTB = S // TB
    YT = A.alloc("YT", [128, 8, S], BF16)
    mark_after_yt = A.off
    pv = load_small_vecs(c, [dr["a_conv_w"][idx, 0], dr["a_conv_w"][idx, 1], dr["a_conv_w"][idx, 2], dr["a_conv_w"][idx, 3],
                             dr["a_conv_b"][idx], dr["a_b_rgate"][idx], dr["a_b_igate"][idx], dr["a_lambda"][idx]], 8)
    cv1 = A.alloc("cv1", [128, 8], F32)
    cv2 = A.alloc("cv2", [128, 8], F32)
    sp_e = A.alloc("sp_e", [128, 8], F32)
    sp_l = A.alloc("sp_l", [128, 8], F32)
    P.op("act", lambda e: e.activation(out=sp_e[:], in_=pv[:, :, 7], func=AF.Exp, scale=-1.0), reads=[("pv",)], writes=[("sp_e",)])
    P.op("act", lambda e: e.activation(out=sp_l[:], in_=sp_e[:], func=AF.Ln, bias=c.one_t[:], scale=1.0),
         reads=[("sp_e",), ("one",)], writes=[("sp_l",)])
    P.op("dve", lambda e: e.tensor_scalar(out=cv1[:], in0=sp_l[:], scalar1=-8.0, scalar2=None, op0=ALU.mult), reads=[("sp_l",)], writes=[("cv1",)])
    P.op("dve", lambda e: e.tensor_scalar(out=cv2[:], in0=sp_l[:], scalar1=-16.0, scalar2=None, op0=ALU.mult), reads=[("sp_l",)], writes=[("cv2",)])
    wg = A.alloc("wgr", [128, 8, 128], BF16)
    wi = A.alloc("wgi", [128, 8, 128], BF16)
    P.dma("pool", lambda e: e.dma_start(out=wg[:], in_=dr["a_w_rgate"][idx].rearrange("h i j -> i h j")), writes=[("wgr",)])
    P.dma("pool", lambda e: e.dma_start(out=wi[:], in_=dr["a_w_igate"][idx].rearrange("h i j -> i h j")), writes=[("wgi",)])
    wrec = [A.alloc("wrec", [128, 8, 128], BF16) for _ in range(2)]
    wgat = [A.alloc("wgat", [128, 8, 128], BF16) for _ in range(2)]
    RECp = [A.alloc("RECp", [128, 3 + S], F32) for _ in range(2)]
    nm = ["GX", "SQ", "SG", "CV", "R", "I", "AA", "OM", "H"]
    T = {n: [A.alloc(n, [128, TB], F32) for _ in range(2)] for n in nm}
    CVb = [A.alloc("CVb", [128, TB], BF16) for _ in range(2)]
    for q in range(2):
        P.op("pool", lambda e, q=q: e.memset(RECp[q][:, 0:3], 0.0), writes=[("RECp", q, -1)])
    it = 0
    for cc in range(8):
        q = cc % 2
        P.dma("pool", lambda e, cc=cc, q=q: e.dma_start(
            out=wrec[q][:], in_=dr["a_w_in"][idx][:, D + cc * 128:D + (cc + 1) * 128].rearrange("(k p) n -> p k n", p=128)),
            writes=[("wrec", q)])
        P.dma("pool", lambda e, cc=cc, q=q: e.dma_start(
            out=wgat[q][:], in_=dr["a_w_in"][idx][:, cc * 128:(cc + 1) * 128].rearrange("(k p) n -> p k n", p=128)),
            writes=[("wgat", q)])
        for tb in range(NTB):
            bk = next_bank(c)
            ps = c.ps[bk]
            for k in range(8):
                P.op("pe", lambda e, k=k, ps=ps, tb=tb, q=q: e.matmul(ps[:, :], lhsT=wrec[q][:, k, :], rhs=XT[:, k, tb * TB:(tb + 1) * TB],
                                                                   start=(k == 0), stop=(k == 7)),
                     reads=[("wrec", q)] + [("XT", i_) for i_ in range(tb * 4, tb * 4 + 4)], writes=[("ps", bk)])
            P.op("act", lambda e, ps=ps, tb=tb, q=q: e.activation(out=RECp[q][:, 3 + tb * TB:3 + (tb + 1) * TB], in_=ps[:, :], func=AF.Copy),
                 reads=[("ps", bk)], writes=[("RECp", q, tb)])
        for tb in range(NTB):
            b_ = it % 2
            it += 1
            t_ = {n: T[n][b_] for n in nm}
            k_ = lambda n: (n, b_)
            bk = next_bank(c)
            ps = c.ps[bk]
            for k in range(8):
                P.op("pe", lambda e, k=k, ps=ps, tb=tb, q=q: e.matmul(ps[:, :], lhsT=wgat[q][:, k, :], rhs=XT[:, k, tb * TB:(tb + 1) * TB],
                                                                   start=(k == 0), stop=(k == 7)),
                     reads=[("wgat", q)] + [("XT", i_) for i_ in range(tb * 4, tb * 4 + 4)], writes=[("ps", bk)])
            P.op("act", lambda e, ps=ps, t_=t_: e.activation(out=t_["GX"][:], in_=ps[:, :], func=AF.Copy), reads=[("ps", bk)], writes=[k_("GX")])
            P.op("act", lambda e, ps=ps, t_=t_: e.activation(out=t_["SQ"][:], in_=ps[:, :], func=AF.Square), reads=[("ps", bk)], writes=[k_("SQ")])
            P.op("dve", lambda e, t_=t_: e.tensor_scalar(out=t_["SQ"][:], in0=t_["SQ"][:], scalar1=0.044715, scalar2=1.0, op0=ALU.mult, op1=ALU.add),
                 reads=[k_("SQ")], writes=[k_("SQ")])
            P.op("dve", lambda e, t_=t_: e.tensor_tensor(out=t_["SQ"][:], in0=t_["SQ"][:], in1=t_["GX"][:], op=ALU.mult),
                 reads=[k_("SQ"), k_("GX")], writes=[k_("SQ")])
            P.op("act", lambda e, t_=t_: e.activation(out=t_["SG"][:], in_=t_["SQ"][:], func=AF.Sigmoid, scale=1.5957691216057308),
                 reads=[k_("SQ")], writes=[k_("SG")])
            P.op("pool", lambda e, t_=t_: e.tensor_tensor(out=t_["SG"][:], in0=t_["GX"][:], in1=t_["SG"][:], op=ALU.mult),
                 reads=[k_("GX"), k_("SG")], writes=[k_("SG")])
            rk = [("RECp", q, tb)] + ([("RECp", q, tb - 1)] if tb > 0 else [("RECp", q, -1)])
            P.op("dve", lambda e, t_=t_, tb=tb, q=q, cc=cc: e.tensor_scalar(
                out=t_["CV"][:], in0=RECp[q][:, tb * TB:tb * TB + TB], scalar1=pv[:, cc, 0:1], scalar2=pv[:, cc, 4:5], op0=ALU.mult, op1=ALU.add),
                reads=rk + [("pv",)], writes=[k_("CV")])
            for j in range(1, 4):
                P.op("dve", lambda e, t_=t_, tb=tb, q=q, cc=cc, j=j: e.scalar_tensor_tensor(
                    out=t_["CV"][:], in0=RECp[q][:, tb * TB + j:tb * TB + j + TB], scalar=pv[:, cc, j:j + 1], in1=t_["CV"][:], op0=ALU.mult, op1=ALU.add),
                    reads=rk + [("pv",), k_("CV")], writes=[k_("CV")])
            P.op("act", lambda e, t_=t_, b_=b_: e.activation(out=CVb[b_][:], in_=t_["CV"][:], func=AF.Copy), reads=[k_("CV")], writes=[("CVb", b_)])
            bkr = next_bank(c); bki = next_bank(c)
            P.op("pe", lambda e, b_=b_, cc=cc, bkr=bkr: e.matmul(c.ps[bkr][:, :], lhsT=wg[:, cc, :], rhs=CVb[b_][:], start=True, stop=True),
                 reads=[("wgr",), ("CVb", b_)], writes=[("ps", bkr)])
            P.op("pe", lambda e, b_=b_, cc=cc, bki=bki: e.matmul(c.ps[bki][:, :], lhsT=wi[:, cc, :], rhs=CVb[b_][:], start=True, stop=True),
                 reads=[("wgi",), ("CVb", b_)], writes=[("ps", bki)])
            P.op("act", lambda e, t_=t_, cc=cc, bkr=bkr: e.activation(out=t_["R"][:], in_=c.ps[bkr][:, :], func=AF.Sigmoid, bias=pv[:, cc, 5:6], scale=1.0),
                 reads=[("ps", bkr), ("pv",)], writes=[k_("R")])
            P.op("act", lambda e, t_=t_, cc=cc, bki=bki: e.activation(out=t_["I"][:], in_=c.ps[bki][:, :], func=AF.Sigmoid, bias=pv[:, cc, 6:7], scale=1.0),
                 reads=[("ps", bki), ("pv",)], writes=[k_("I")])
            P.op("act", lambda e, t_=t_, cc=cc: e.activation(out=t_["AA"][:], in_=t_["R"][:], func=AF.Exp, scale=cv1[:, cc:cc + 1]),
                 reads=[k_("R"), ("cv1",)], writes=[k_("AA")])
            P.op("act", lambda e, t_=t_, cc=cc: e.activation(out=t_["OM"][:], in_=t_["R"][:], func=AF.Exp, scale=cv2[:, cc:cc + 1]),
                 reads=[k_("R"), ("cv2",)], writes=[k_("OM")])
            P.op("dve", lambda e, t_=t_: e.tensor_scalar(out=t_["OM"][:], in0=t_["OM"][:], scalar1=-1.0, scalar2=1.0, op0=ALU.mult, op1=ALU.add),
                 reads=[k_("OM")], writes=[k_("OM")])
            P.op("act", lambda e, t_=t_: e.activation(out=t_["OM"][:], in_=t_["OM"][:], func=AF.Sqrt), reads=[k_("OM")], writes=[k_("OM")])
            P.op("pool", lambda e, t_=t_: e.tensor_tensor(out=t_["I"][:], in0=t_["I"][:], in1=t_["CV"][:], op=ALU.mult),
                 reads=[k_("I"), k_("CV")], writes=[k_("I")])
            P.op("dve", lambda e, t_=t_: e.tensor_tensor(out=t_["I"][:], in0=t_["I"][:], in1=t_["OM"][:], op=ALU.mult),
                 reads=[k_("I"), k_("OM")], writes=[k_("I")])
            if tb == 0:
                P.op("dve", lambda e, t_=t_: e.tensor_tensor_scan(out=t_["H"][:], data0=t_["AA"][:], data1=t_["I"][:], initial=0.0,
                                                                  op0=ALU.mult, op1=ALU.add),
                     reads=[k_("AA"), k_("I")], writes=[k_("H")])
            else:
                hp = T["H"][1 - b_]
                P.op("dve", lambda e, t_=t_, hp=hp: e.tensor_tensor_scan(out=t_["H"][:], data0=t_["AA"][:], data1=t_["I"][:],
                                                                         initial=hp[:, TB - 1:TB], op0=ALU.mult, op1=ALU.add),
                     reads=[k_("AA"), k_("I"), ("H", 1 - b_)], writes=[k_("H")])
            P.op("pool", lambda e, t_=t_, cc=cc, tb=tb: e.tensor_tensor(out=YT[:, cc, tb * TB:(tb + 1) * TB], in0=t_["SG"][:], in1=t_["H"][:], op=ALU.mult),
                 reads=[k_("SG"), k_("H")], writes=[("YT", cc)])
    P.barrier()
    A.reset(mark_after_yt)
    emit_mixer_epilogue(c, layer, YT, 8, dr["a_w_out"][idx])


RET_H = 4
RET_EPS = 1e-6


def ret_consts():
    f32 = np.float32
    log_gamma = np.log1p(-np.exp2(-5.0 - np.arange(RET_H, dtype=f32))).astype(f32)
    pos = np.arange(128, dtype=f32)
    rel = pos[:, None] - pos[None, :]
    intra = np.where(rel >= 0, np.exp(log_gamma[:, None, None] * np.maximum(rel, 0.0)), 0.0).astype(f32)
    dt = np.ascontiguousarray(intra.transpose(0, 2, 1))
    qd = np.exp(log_gamma[:, None] * (pos + 1.0)).astype(f32)
    kd = np.exp(log_gamma[:, None] * (127.0 - pos)).astype(f32)
    cd = np.exp(log_gamma * 128.0).astype(f32)
    return {"c_ret_dt": np.ascontiguousarray(dt.transpose(1, 0, 2)),
            "c_ret_qd": np.ascontiguousarray(np.tile(qd[None], (128, 1, 1))),
            "c_ret_kd": np.ascontiguousarray((kd.T / 16.0).astype(f32)),
            }, [float(v) for v in cd]


def phase_ret(c, layer, idx):
    A, P, dr = c.A, c.P, c.dr
    X, XT = c.X, c.XT
    _, CDV = ret_consts()
    DT = A.alloc("rDT", [128, RET_H, 128], F32)
    QD = A.alloc("rQD", [128, RET_H, 128], F32)
    KD = A.alloc("rKD", [128, RET_H], F32)
    eps6 = A.alloc("eps6", [128, 1], F32)
    P.dma("sp", lambda e: e.dma_start(out=DT[:], in_=dr["c_ret_dt"]), writes=[("rDT",)])
    P.dma("sp", lambda e: e.dma_start(out=QD[:], in_=dr["c_ret_qd"]), writes=[("rQD",)])
    P.dma("sp", lambda e: e.dma_start(out=KD[:], in_=dr["c_ret_kd"]), writes=[("rKD",)])
    P.op("dve", lambda e: e.memset(eps6[:], RET_EPS), writes=[("eps6",)])
    Wq = [A.alloc("Wq", [128, 8, 256], BF16) for _ in range(2)]
    Wk = [A.alloc("Wk", [128, 8, 256], BF16) for _ in range(2)]
    Wv = [A.alloc("Wv", [128, 8, 512], BF16) for _ in range(2)]
    Wg = [A.alloc("Wg", [128, 8, 512], BF16) for _ in range(2)]
    Wo = [A.alloc("Wo", [128, 4, D], BF16) for _ in range(2)]
    Sf = A.alloc("Sf", [128, 2, 512], F32)
    Sb = [A.alloc("Sb", [128, 2, 512], BF16) for _ in range(2)]
    qT = [A.alloc("qT", [128, 2, 128], BF16) for _ in range(2)]
    qdT = [A.alloc("qdT", [128, 2, 128], BF16) for _ in range(2)]
    kT = [A.alloc("kT", [128, 2, 128], BF16) for _ in range(2)]
    kdec = [A.alloc("kdec", [128, 256], BF16) for _ in range(2)]
    vc = [A.alloc("vc", [128, 512], BF16) for _ in range(2)]
    sgc = [A.alloc("sgc", [128, 512], F32) for _ in range(2)]
    PT = [A.alloc("PT", [128, 128], BF16) for _ in range(2)]
    qf = [A.alloc("qf", [128, 256], F32) for _ in range(2)]
    kf = [A.alloc("kf", [128, 256], F32) for _ in range(2)]
    ktf = [A.alloc("ktf", [128, 256], F32) for _ in range(2)]
    scf = [A.alloc("scf", [128, 128], F32) for _ in range(2)]
    osq = [A.alloc("osq", [128, 512], F32) for _ in range(2)]
    ms = [A.alloc("ms", [128, 1], F32) for _ in range(2)]
    sd = [A.alloc("rsd", [128, 1], F32) for _ in range(2)]
    rs = [A.alloc("rrs", [128, 1], F32) for _ in range(2)]
    yc = [A.alloc("yc", [128, 512], BF16) for _ in range(2)]
    yT = [A.alloc("yT", [128, 4, 128], BF16) for _ in range(2)]
    W = dr["b_w_in"][idx]
    QW = 1024
    thr = A.alloc("thr", [128, 1], BF16)

    def load_head(h):
        q = h % 2
        for (dst, col0, n, nm) in ((Wq[q], h * 256, 256, "Wq"), (Wk[q], QW + h * 256, 256, "Wk"),
                                   (Wv[q], 2 * QW + h * 512, 512, "Wv"), (Wg[q], 2 * QW + 2048 + h * 512, 512, "Wg")):
            P.dma("pool", lambda e, dst=dst, col0=col0, n=n: e.dma_start(
                out=dst[:], in_=W[:, col0:col0 + n].rearrange("(k p) n -> p k n", p=128)), reads=[("throttle",)], writes=[(nm, q)])
        P.dma("pool", lambda e, q=q, h=h: e.dma_start(
            out=Wo[q][:], in_=dr["b_w_out"][idx][h * 512:(h + 1) * 512, :].rearrange("(k p) n -> p k n", p=128)), reads=[("throttle",)], writes=[("Wo", q)])

    import os
    DBG = int(os.environ.get("RET_DBG", "99"))
    NH_ = int(os.environ.get("RET_NH", "4"))
    NI_ = int(os.environ.get("RET_NI", "16"))

    def ret_chunk(h, i, b_, q):
        if h >= NH_ or i >= NI_ or DBG < 1:
            return
        xtk = [("XT", i)]
        tok = slice(i * 128, (i + 1) * 128)
        bkq = next_bank(c); bkk = next_bank(c)
        for dc in range(2):
            for k in range(8):
                P.op("pe", lambda e, dc=dc, k=k, bkq=bkq: e.matmul(c.ps[bkq][:, dc * 128:(dc + 1) * 128], lhsT=Wq[q][:, k, dc * 128:(dc + 1) * 128],
                                                             rhs=XT[:, k, tok], start=(k == 0), stop=(k == 7)),
                     reads=[("Wq", q)] + xtk, writes=[("ps", bkq)])
        for dc in range(2):
            for k in range(8):
                P.op("pe", lambda e, dc=dc, k=k, bkk=bkk: e.matmul(c.ps[bkk][:, dc * 128:(dc + 1) * 128], lhsT=Wk[q][:, k, dc * 128:(dc + 1) * 128],
                                                             rhs=XT[:, k, tok], start=(k == 0), stop=(k == 7)),
                     reads=[("Wk", q)] + xtk, writes=[("ps", bkk)])
        P.op("act", lambda e, bkq=bkq, b_=b_: e.activation(out=qf[b_][:], in_=c.ps[bkq][:, 0:256], func=AF.Copy),
             reads=[("ps", bkq)], writes=[("qf", b_)])
        P.op("act", lambda e, bkk=bkk, b_=b_: e.activation(out=kf[b_][:], in_=c.ps[bkk][:, 0:256], func=AF.Copy),
             reads=[("ps", bkk)], writes=[("kf", b_)])
        P.op("pool", lambda e, b_=b_: e.tensor_copy(out=qT[b_][:].rearrange("p a b -> p (a b)"), in_=qf[b_][:]),
             reads=[("qf", b_)], writes=[("qT", b_)])
        for dc in range(2):
            P.op("dve", lambda e, dc=dc, b_=b_, h=h: e.tensor_tensor(out=qdT[b_][:, dc, :], in0=qf[b_][:, dc * 128:(dc + 1) * 128],
                                                                   in1=QD[:, h, :], op=ALU.mult),
                 reads=[("qf", b_), ("rQD",)], writes=[("qdT", b_)])
        P.op("pool", lambda e, b_=b_: e.tensor_scalar(out=kT[b_][:].rearrange("p a b -> p (a b)"), in0=kf[b_][:], scalar1=0.0625, scalar2=None, op0=ALU.mult),
             reads=[("kf", b_)], writes=[("kT", b_)])
        if DBG < 2:
            return
        bkt = next_bank(c)
        for k in range(8):
            P.op("pe", lambda e, k=k, bkt=bkt: e.matmul(c.ps[bkt][:, 0:256], lhsT=XT[:, k, tok], rhs=Wk[q][:, k, :], start=(k == 0), stop=(k == 7)),
                 reads=[("Wk", q)] + xtk, writes=[("ps", bkt)])
        P.op("act", lambda e, bkt=bkt, b_=b_: e.activation(out=ktf[b_][:], in_=c.ps[bkt][:, 0:256], func=AF.Copy),
             reads=[("ps", bkt)], writes=[("ktf", b_)])
        P.op("dve", lambda e, b_=b_, h=h: e.tensor_scalar(out=kdec[b_][:], in0=ktf[b_][:], scalar1=KD[:, h:h + 1], scalar2=None, op0=ALU.mult),
             reads=[("ktf", b_), ("rKD",)], writes=[("kdec", b_)])
        bkv = next_bank(c)
        for k in range(8):
            P.op("pe", lambda e, k=k, bkv=bkv: e.matmul(c.ps[bkv][:, :], lhsT=XT[:, k, tok], rhs=Wv[q][:, k, :], start=(k == 0), stop=(k == 7)),
                 reads=[("Wv", q)] + xtk, writes=[("ps", bkv)])
        P.op("act", lambda e, bkv=bkv, b_=b_: e.activation(out=vc[b_][:], in_=c.ps[bkv][:, :], func=AF.Copy), reads=[("ps", bkv)], writes=[("vc", b_)])
        bkg = next_bank(c)
        for k in range(8):
            P.op("pe", lambda e, k=k, bkg=bkg: e.matmul(c.ps[bkg][:, :], lhsT=XT[:, k, tok], rhs=Wg[q][:, k, :], start=(k == 0), stop=(k == 7)),
                 reads=[("Wg", q)] + xtk, writes=[("ps", bkg)])
        P.op("act", lambda e, bkg=bkg, b_=b_: e.activation(out=sgc[b_][:], in_=c.ps[bkg][:, :], func=AF.Sigmoid), reads=[("ps", bkg)], writes=[("sgc", b_)])
        P.op("dve", lambda e, bkg=bkg, b_=b_: e.tensor_tensor(out=sgc[b_][:], in0=c.ps[bkg][:, :], in1=sgc[b_][:], op=ALU.mult),
             reads=[("ps", bkg), ("sgc", b_)], writes=[("sgc", b_)])
        if DBG < 3:
            return
        bks = next_bank(c)
        for dc in range(2):
            P.op("pe", lambda e, dc=dc, bks=bks, b_=b_: e.matmul(c.ps[bks][:, 0:128], lhsT=kT[b_][:, dc, :], rhs=qT[b_][:, dc, :], start=(dc == 0), stop=(dc == 1)),
                 reads=[("kT", b_), ("qT", b_)], writes=[("ps", bks)])
        P.op("act", lambda e, bks=bks, b_=b_: e.activation(out=scf[b_][:], in_=c.ps[bks][:, 0:128], func=AF.Copy),
             reads=[("ps", bks)], writes=[("scf", b_)])
        P.op("dve", lambda e, b_=b_, h=h: e.tensor_tensor(out=PT[b_][:], in0=scf[b_][:], in1=DT[:, h, :], op=ALU.mult),
             reads=[("scf", b_), ("rDT",)], writes=[("PT", b_)])
        bko = next_bank(c)
        sbp = Sb[(i + 1) % 2]
        P.op("pe", lambda e, bko=bko, b_=b_: e.matmul(c.ps[bko][:, :], lhsT=PT[b_][:], rhs=vc[b_][:], start=True, stop=(i == 0)),
             reads=[("PT", b_), ("vc", b_)], writes=[("ps", bko)])
        if i > 0:
            for dc in range(2):
                P.op("pe", lambda e, dc=dc, bko=bko, b_=b_, sbp=sbp: e.matmul(c.ps[bko][:, :], lhsT=qdT[b_][:, dc, :], rhs=sbp[:, dc, :], start=False, stop=(dc == 1)),
                     reads=[("qdT", b_), ("Sb", (i + 1) % 2)], writes=[("ps", bko)])
        P.op("act", lambda e, bko=bko, b_=b_: e.activation(out=osq[b_][:], in_=c.ps[bko][:, :], func=AF.Square), reads=[("ps", bko)], writes=[("osq", b_)])
        P.op("dve", lambda e, b_=b_: e.reduce_sum(out=ms[b_][:], in_=osq[b_][:], axis=AX.X), reads=[("osq", b_)], writes=[("ms", b_)])
        P.op("act", lambda e, b_=b_: e.activation(out=sd[b_][:], in_=ms[b_][:], func=AF.Sqrt, bias=eps6[:], scale=1.0 / 512.0),
             reads=[("ms", b_), ("eps6",)], writes=[("rsd", b_)])
        P.op("dve", lambda e, b_=b_: e.reciprocal(out=rs[b_][:], in_=sd[b_][:]), reads=[("rsd", b_)], writes=[("rrs", b_)])
        P.op("dve", lambda e, bko=bko, b_=b_: e.scalar_tensor_tensor(out=osq[b_][:], in0=c.ps[bko][:, :], scalar=rs[b_][:, 0:1], in1=sgc[b_][:],
                                                                  op0=ALU.mult, op1=ALU.mult),
             reads=[("ps", bko), ("rrs", b_), ("sgc", b_), ("ms", b_)], writes=[("osq", b_)])
        P.op("pool", lambda e, b_=b_: e.tensor_copy(out=yc[b_][:], in_=osq[b_][:]), reads=[("osq", b_)], writes=[("yc", b_)])
        if DBG < 4:
            return
        if i < NT - 1:
            sbn = Sb[i % 2]
            for dc in range(2):
                bkS = next_bank(c)
                P.op("pe", lambda e, dc=dc, bkS=bkS, b_=b_: e.matmul(c.ps[bkS][:, :], lhsT=kdec[b_][:, dc * 128:(dc + 1) * 128], rhs=vc[b_][:], start=True, stop=True),
                     reads=[("kdec", b_), ("vc", b_)], writes=[("ps", bkS)])
                if i == 0:
                    P.op("dve", lambda e, dc=dc, bkS=bkS: e.tensor_copy(out=Sf[:, dc, :], in_=c.ps[bkS][:, :]), reads=[("ps", bkS)], writes=[("Sf", dc)])
                else:
                    P.op("dve", lambda e, dc=dc, bkS=bkS, h=h: e.scalar_tensor_tensor(out=Sf[:, dc, :], in0=Sf[:, dc, :], scalar=CDV[h], in1=c.ps[bkS][:, :],
                                                                                  op0=ALU.mult, op1=ALU.add),
                         reads=[("ps", bkS), ("Sf", dc)], writes=[("Sf", dc)])
                P.op("pool", lambda e, dc=dc, sbn=sbn: e.tensor_copy(out=sbn[:, dc, :], in_=Sf[:, dc, :]), reads=[("Sf", dc)], writes=[("Sb", i % 2)])
        if DBG < 5:
            return
        bky = next_bank(c)
        psb = c.ps[bky][:].bitcast(BF16)
        for fc in range(4):
            P.op("pe", lambda e, fc=fc, psb=psb, b_=b_: e.transpose(out=psb[:, fc * 128:(fc + 1) * 128], in_=yc[b_][:, fc * 128:(fc + 1) * 128], identity=c.ident_b[:]),
                 reads=[("yc", b_), ("ident_b",)], writes=[("ps", bky)])
        P.op("act", lambda e, psb=psb, b_=b_: e.activation(out=yT[b_][:].rearrange("p a b -> p (a b)"), in_=psb[:, 0:512], func=AF.Copy),
             reads=[("ps", bky)], writes=[("yT", b_)])
        for nh in range(2):
            bkx = next_bank(c)
            for fc in range(4):
                P.op("pe", lambda e, fc=fc, nh=nh, bkx=bkx, b_=b_: e.matmul(c.ps[bkx][:, :], lhsT=yT[b_][:, fc, :], rhs=Wo[q][:, fc, nh * 512:(nh + 1) * 512],
                                                                     start=(fc == 0), stop=(fc == 3)),
                     reads=[("yT", b_), ("Wo", q)], writes=[("ps", bkx)])
            xs = X[:, i, nh * 512:(nh + 1) * 512]
            if h == 0:
                P.op("dve", lambda e, xs=xs, bkx=bkx: e.scalar_tensor_tensor(out=xs, in0=xs, scalar=ALPHA, in1=c.ps[bkx][:, :], op0=ALU.mult, op1=ALU.add),
                     reads=[("ps", bkx), ("X", i)], writes=[("X", i)])
            else:
                P.op("dve", lambda e, xs=xs, bkx=bkx: e.tensor_tensor(out=xs, in0=xs, in1=c.ps[bkx][:, :], op=ALU.add),
                     reads=[("ps", bkx), ("X", i)], writes=[("X", i)])


    load_head(0)
    it = 0
    for h in range(RET_H):
        q = h % 2
        for i in range(NT):
            ret_chunk(h, i, it % 2, q)
            it += 1
            if i == 1 and h + 1 < RET_H:
                P.op("act", lambda e: e.activation(out=thr[:], in_=yT[(it - 1) % 2][:, 0, 0:1], func=AF.Copy),
                     reads=[("yT", (it - 1) % 2)], writes=[("throttle",)])
                load_head(h + 1)
    P.barrier()
    A.reset(c.phase_base)
    emit_ln_route_epilogue(c, layer)


def emit_ln_route_epilogue(c, layer):
    r = alloc_route(c, layer)
    g1, b1 = load_ln_params(c, layer, 0, "a")
    lt = alloc_ln_tmp(c)
    t = alloc_prep(c)
    for i in range(NT):
        emit_ln(c, lt, c.X[:, i, :], ("X", i), i, g1, b1, "a")
        emit_transpose_tile(c, t, i, write_xt=False)
        emit_route_tile(c, r, t, i)


ATT_PAT = ((128, 1), (512, 4), (2048, 16))
ATT_BIG = 1.0e9


def att_consts():
    u = np.arange(128)[:, None]
    j = np.arange(256)[None, :]
    steps = u + 128 - j
    valid = (steps >= 0) & (steps <= 128)
    neg = np.where(valid, -steps.astype(np.float32), -ATT_BIG).astype(np.float32)
    return {"c_att_neg": np.ascontiguousarray(neg)}


def phase_attn(c, layer, idx):
    A, P, dr = c.A, c.P, c.dr
    X, XT = c.X, c.XT
    nc = c.nc
    Wd = dr["c_w_in"][idx]
    NEG = A.alloc("aNEG", [128, 256], F32)
    P.dma("sp", lambda e: e.dma_start(out=NEG[:], in_=dr["c_att_neg"]), writes=[("aNEG",)])
    Wq = [A.alloc("aWq", [128, 8, 128], BF16) for _ in range(2)]
    Wk = [A.alloc("aWk", [128, 8, 128], BF16) for _ in range(2)]
    Wv = [A.alloc("aWv", [128, 8, 128], BF16) for _ in range(2)]
    qT = [A.alloc("aqT", [128, S], BF16) for _ in range(2)]
    kT = [A.alloc("akT", [128, S], BF16) for _ in range(2)]
    V = [A.alloc("aV", [128, NT, 128], BF16) for _ in range(2)]
    BI = [A.alloc("aBI", [128, 2, 256], F32) for _ in range(2)]
    Sb = [A.alloc("aSb", [128, 256], F32) for _ in range(4)]
    Pb = [A.alloc("aPb", [128, 256], BF16) for _ in range(4)]
    PT = [A.alloc("aPT", [128, 2, 128], BF16) for _ in range(2)]
    mx = [A.alloc("amx", [128, 1], F32) for _ in range(2)]
    nm = [A.alloc("anm", [128, 1], F32) for _ in range(4)]
    osb = [A.alloc("aosb", [128, 128], F32) for _ in range(2)]
    mst = [A.alloc("amst", [128, 2, 2], F32) for _ in range(4)]

    def load_w(g, hp, q):
        for (dst, s_, nm_) in ((Wq[q], 0, "aWq"), (Wk[q], 1, "aWk"), (Wv[q], 2, "aWv")):
            col0 = ((s_ * 3 + g) * 16 + 2 * hp) * 64
            P.dma("pool", lambda e, dst=dst, col0=col0: e.dma_start(
                out=dst[:], in_=Wd[:, col0:col0 + 128].rearrange("(k p) n -> p k n", p=128)), writes=[(nm_, q)])

    def tokslice(g, ut):
        d = ATT_PAT[g][1]
        nb = (S // d) // 128
        r, b = ut // nb, ut % nb
        st = 128 * b * d + r
        return slice(st, st + 127 * d + 1, d), b

    def proj(g, hp, q):
        for (Wt, dst, nm_) in ((Wq[q], qT[q], "aqT"), (Wk[q], kT[q], "akT")):
            for tb in range(4):
                bk = next_bank(c)
                for k in range(8):
                    P.op("pe", lambda e, k=k, bk=bk, Wt=Wt, tb=tb: e.matmul(c.ps[bk][:, :], lhsT=Wt[:, k, :], rhs=XT[:, k, tb * 512:(tb + 1) * 512],
                                                                      start=(k == 0), stop=(k == 7)),
                         reads=[(nm_.replace("qT", "Wq").replace("kT", "Wk"), q)] + [("XT", i_) for i_ in range(tb * 4, tb * 4 + 4)], writes=[("ps", bk)])
                P.op("act", lambda e, bk=bk, dst=dst, tb=tb: e.activation(out=dst[:, tb * 512:(tb + 1) * 512], in_=c.ps[bk][:, :], func=AF.Copy),
                     reads=[("ps", bk)], writes=[(nm_, q, tb)])
        for ut in range(NT):
            ts_, _ = tokslice(g, ut)
            bk = next_bank(c)
            for k in range(8):
                P.op("pe", lambda e, k=k, bk=bk, ts_=ts_: e.matmul(c.ps[bk][:, 0:128], lhsT=XT[:, k, ts_], rhs=Wv[q][:, k, :], start=(k == 0), stop=(k == 7)),
                     reads=[("aWv", q)] + [("XT", i_) for i_ in range(NT)], writes=[("ps", bk)])
            P.op("act", lambda e, bk=bk, ut=ut: e.activation(out=V[q][:, ut, :], in_=c.ps[bk][:, 0:128], func=AF.Copy),
                 reads=[("ps", bk)], writes=[("aV", q, ut)])
        d = ATT_PAT[g][1]
        for e_ in range(2):
            hh = 2 * hp + e_
            slope = float(2.0 ** (-8.0 * (hh + 1) / 16.0)) * d
            P.op("pool", lambda e, e_=e_, slope=slope: e.tensor_scalar(out=BI[q][:, e_, :], in0=NEG[:], scalar1=slope, scalar2=None, op0=ALU.mult),
                 reads=[("aNEG",)], writes=[("aBI", q, e_)])

    def QK_KEYS(q):
        return [("aqT", q, tb) for tb in range(4)] + [("akT", q, tb) for tb in range(4)]

    NSL = 4

    def stageA(g, hp, q, ut, e_, sl, bo):
        ts_, b = tokslice(g, ut)
        has_prev = b > 0
        pr = slice(e_ * 64, (e_ + 1) * 64)
        lo = 0 if has_prev else 128
        bk = next_bank(c, 6)
        if has_prev:
            tp_, _ = tokslice(g, ut - 1)
            P.op("pe", lambda e: e.matmul(c.ps[bk][:, 0:128], lhsT=qT[q][pr, ts_], rhs=kT[q][pr, tp_], start=True, stop=True),
                 reads=QK_KEYS(q), writes=[("ps", bk)])
        P.op("pe", lambda e: e.matmul(c.ps[bk][:, 128:256], lhsT=qT[q][pr, ts_], rhs=kT[q][pr, ts_], start=True, stop=True),
             reads=QK_KEYS(q), writes=[("ps", bk)])
        P.op("dve", lambda e: e.scalar_tensor_tensor(out=Sb[sl][:, lo:256], in0=c.ps[bk][:, lo:256], scalar=0.125, in1=BI[q][:, e_, lo:256],
                                                    op0=ALU.mult, op1=ALU.add),
             reads=[("ps", bk), ("aBI", q, e_)], writes=[("aSb", sl)])
        P.op("dve", lambda e: e.reduce_max(out=mst[bo][:, e_, 0:1], in_=Sb[sl][:, lo:256], axis=AX.X), reads=[("aSb", sl)], writes=[("amst", bo, e_, 0)])
        P.op("pool", lambda e: e.tensor_scalar(out=nm[sl][:], in0=mst[bo][:, e_, 0:1], scalar1=-1.0, scalar2=None, op0=ALU.mult),
             reads=[("amst", bo, e_, 0)], writes=[("anm", sl)])
        P.op("act", lambda e: e.activation(out=Pb[sl][:, lo:256], in_=Sb[sl][:, lo:256], func=AF.Exp, bias=nm[sl][:], scale=1.0),
             reads=[("aSb", sl), ("anm", sl)], writes=[("aPb", sl)])

    def stageA2(g, hp, q, ut, e_, sl, bo):
        ts_, b = tokslice(g, ut)
        lo = 0 if b > 0 else 128
        P.op("dve", lambda e: e.reduce_sum(out=mst[bo][:, e_, 1:2], in_=Pb[sl][:, lo:256], axis=AX.X),
             reads=[("aPb", sl)], writes=[("amst", bo, e_, 1)])

    def stageB1(g, hp, q, ut, e_, sl, bo):
        ts_, b = tokslice(g, ut)
        has_prev = b > 0
        lo = 0 if has_prev else 128
        halves = (0, 1) if has_prev else (1,)
        pt = PT[sl % 2]
        bkt = next_bank(c, 6)
        psb = c.ps[bkt][:].bitcast(BF16)
        for hf in halves:
            P.op("pe", lambda e, hf=hf: e.transpose(out=psb[:, hf * 128:(hf + 1) * 128], in_=Pb[sl][:, hf * 128:(hf + 1) * 128], identity=c.ident_b[:]),
                 reads=[("aPb", sl), ("ident_b",)], writes=[("ps", bkt)])
        P.op("act", lambda e: e.activation(out=pt[:].rearrange("p a b -> p (a b)")[:, lo:256], in_=psb[:, lo:256], func=AF.Copy),
             reads=[("ps", bkt)], writes=[("aPT", sl % 2)])

    def stageB(g, hp, q, ut, e_, sl, bo, obank):
        ts_, b = tokslice(g, ut)
        has_prev = b > 0
        pr = slice(e_ * 64, (e_ + 1) * 64)
        lo = 0 if has_prev else 128
        halves = (0, 1) if has_prev else (1,)
        pt = PT[sl % 2]
        for n_, hf in enumerate(halves):
            vt = ut - 1 if hf == 0 else ut
            P.op("pe", lambda e, hf=hf, vt=vt, n_=n_: e.matmul(c.ps[obank][:, pr], lhsT=pt[:, hf, :], rhs=V[q][:, vt, pr],
                                                            start=(n_ == 0), stop=(n_ == len(halves) - 1)),
                 reads=[("aPT", sl % 2), ("aV", q, vt)], writes=[("ps", obank)])
        if e_ == 1:
            ob = osb[bo % 2]
            P.op("dve", lambda e: e.tensor_copy(out=ob[:], in_=c.ps[obank][:, 0:128]), reads=[("ps", obank)], writes=[("aosb", bo % 2)])
            P.dma("sp", lambda e: e.dma_start(out=dr["att_o"][g, ts_, hp * 128:(hp + 1) * 128], in_=ob[:]), reads=[("aosb", bo % 2)], writes=[("att_o", g, hp, ut)])
            P.dma("sp", lambda e: e.dma_start(out=dr["att_ms"][g, ts_, 2 * hp:2 * hp + 2, :], in_=mst[bo][:]),
                  reads=[("amst", bo, e2, t2) for e2 in range(2) for t2 in range(2)], writes=[("att_ms", g, hp, ut)])

    import os
    NG_ = int(os.environ.get("ATT_NG", "3"))
    NHP_ = int(os.environ.get("ATT_NHP", "8"))
    LOOK = 3
    combos = [(g, hp) for g in range(NG_) for hp in range(NHP_)]
    load_w(combos[0][0], combos[0][1], 0)
    gcount = 0
    for n, (g, hp) in enumerate(combos):
        q = n % 2
        proj(g, hp, q)
        if n + 1 < len(combos):
            load_w(combos[n + 1][0], combos[n + 1][1], (n + 1) % 2)
        units = [(ut, e_) for ut in range(NT) for e_ in range(2)]
        def args(j):
            ut, e_ = units[j]
            gc = gcount + j
            return (g, hp, q, ut, e_, gc % NSL, (gc // 2) % NSL)
        nu = len(units)
        for j in range(-3, nu):
            if 0 <= j + 3 < nu:
                stageA(*args(j + 3))
            if 0 <= j + 1 < nu:
                stageA2(*args(j + 1))
                stageB1(*args(j + 1))
            if 0 <= j < nu:
                stageB(*args(j), 6 + ((gcount + j) // 2) % 2)
        gcount += len(units)
    P.barrier()
    A.reset(c.phase_base)
    YT = A.alloc("YT", [128, 8, S], BF16)
    mark = A.off
    Og = [[A.alloc("aOg", [128, D], F32) for _ in range(3)] for _ in range(2)]
    MS = [A.alloc("aMS", [128, 3, 16, 2], F32) for _ in range(2)]
    Mx = [A.alloc("aMx", [128, 16], F32) for _ in range(2)]
    Wt_ = [A.alloc("aWt", [128, 3, 16], F32) for _ in range(2)]
    Ws = [A.alloc("aWs", [128, 3, 16], F32) for _ in range(2)]
    Dn = [A.alloc("aDn", [128, 16], F32) for _ in range(2)]
    Yt = [A.alloc("aYt", [128, D], F32) for _ in range(2)]
    Yb = [A.alloc("aYb", [128, D], BF16) for _ in range(2)]

    def merge_tile(i, p):
        for g in range(3):
            P.dma("sp", lambda e, g=g: e.dma_start(out=Og[p][g][:], in_=dr["att_o"][g, i * 128:(i + 1) * 128, :]), writes=[("aOg", p, g)])
        P.dma("sp", lambda e: e.dma_start(out=MS[p][:], in_=dr["att_ms"][:, i * 128:(i + 1) * 128, :, :].rearrange("g p h t -> p g h t")), writes=[("aMS", p)])
        P.op("dve", lambda e: e.tensor_tensor(out=Mx[p][:], in0=MS[p][:, 0, :, 0], in1=MS[p][:, 1, :, 0], op=ALU.max), reads=[("aMS", p)], writes=[("aMx", p)])
        P.op("dve", lambda e: e.tensor_tensor(out=Mx[p][:], in0=Mx[p][:], in1=MS[p][:, 2, :, 0], op=ALU.max), reads=[("aMS", p), ("aMx", p)], writes=[("aMx", p)])
        for g in range(3):
            P.op("dve", lambda e, g=g: e.tensor_tensor(out=Wt_[p][:, g, :], in0=MS[p][:, g, :, 0], in1=Mx[p][:], op=ALU.subtract),
                 reads=[("aMS", p), ("aMx", p)], writes=[("aWt", p, g)])
        P.op("act", lambda e: e.activation(out=Wt_[p][:].rearrange("p a b -> p (a b)"), in_=Wt_[p][:].rearrange("p a b -> p (a b)"), func=AF.Exp), reads=[("aWt", p, g) for g in range(3)], writes=[("aWt", p)])
        P.op("dve", lambda e: e.tensor_tensor(out=Ws[p][:], in0=Wt_[p][:], in1=MS[p][:, :, :, 1], op=ALU.mult), reads=[("aWt", p), ("aMS", p)], writes=[("aWs", p)])
        P.op("dve", lambda e: e.tensor_tensor(out=Dn[p][:], in0=Ws[p][:, 0, :], in1=Ws[p][:, 1, :], op=ALU.add), reads=[("aWs", p)], writes=[("aDn", p)])
        P.op("dve", lambda e: e.tensor_tensor(out=Dn[p][:], in0=Dn[p][:], in1=Ws[p][:, 2, :], op=ALU.add), reads=[("aWs", p), ("aDn", p)], writes=[("aDn", p)])
        P.op("dve", lambda e: e.reciprocal(out=Dn[p][:], in_=Dn[p][:]), reads=[("aDn", p)], writes=[("aDn", p)])
        for g in range(3):
            P.op("dve", lambda e, g=g: e.tensor_tensor(out=Ws[p][:, g, :], in0=Wt_[p][:, g, :], in1=Dn[p][:], op=ALU.mult),
                 reads=[("aWt", p), ("aDn", p), ("aWs", p)], writes=[("aWs", p)])
        for g in range(3):
            eng = "dve" if g != 1 else "pool"
            P.op(eng, lambda e, g=g: e.tensor_tensor(out=Og[p][g][:].rearrange("p (h d) -> p h d", h=16), in0=Og[p][g][:].rearrange("p (h d) -> p h d", h=16),
                                                     in1=Ws[p][:, g, :].unsqueeze(2).to_broadcast([128, 16, 64]), op=ALU.mult),
                 reads=[("aOg", p, g), ("aWs", p)], writes=[("aOg", p, g)])
        P.op("pool", lambda e: e.tensor_tensor(out=Yt[p][:], in0=Og[p][0][:], in1=Og[p][1][:], op=ALU.add), reads=[("aOg", p, 0), ("aOg", p, 1)], writes=[("aYt", p)])
        P.op("dve", lambda e: e.tensor_tensor(out=Yb[p][:], in0=Yt[p][:], in1=Og[p][2][:], op=ALU.add), reads=[("aYt", p), ("aOg", p, 2)], writes=[("aYb", p)])
        for half in range(2):
            bk = next_bank(c)
            psb = c.ps[bk][:].bitcast(BF16)
            for q4 in range(4):
                ch = half * 4 + q4
                P.op("pe", lambda e, ch=ch, q4=q4, psb=psb: e.transpose(out=psb[:, q4 * 128:(q4 + 1) * 128], in_=Yb[p][:, ch * 128:(ch + 1) * 128], identity=c.ident_b[:]),
                     reads=[("aYb", p), ("ident_b",)], writes=[("ps", bk)])
            P.op("act", lambda e, half=half, psb=psb: e.activation(out=YT[:, half * 4:(half + 1) * 4, i * 128:(i + 1) * 128],
                                                                 in_=psb[:, 0:512].rearrange("p (a b) -> p a b", a=4), func=AF.Copy),
                 reads=[("ps", bk)], writes=[("YT", half * 4 + j) for j in range(4)])

    for i in range(NT):
        merge_tile(i, i % 2)
    P.barrier()
    A.reset(mark)
    emit_mixer_epilogue(c, layer, YT, 8, dr["c_w_out"][idx])


WEIGHT_NAMES = ["a_w_in", "a_conv_w", "a_conv_b", "a_w_rgate", "a_b_rgate", "a_w_igate", "a_b_igate", "a_lambda",
                "a_w_out", "b_w_in", "b_w_out", "c_w_in", "c_w_out", "ln_gain", "ln_bias", "moe_w_router",
                "moe_b_router", "moe_w_gu", "moe_b_gu", "moe_w_down", "moe_b_down"]


def make_consts():
    ident = np.eye(128, dtype=np.float32)
    tri = np.triu(np.ones((128, 128), dtype=np.float32), k=1)
    ec = np.tile((np.arange(NE, dtype=np.float32) * CAP)[None, :], (128, 1))
    d = {"c_ident": ident, "c_tri": tri, "c_ec": ec}
    d.update(ret_consts()[0])
    d.update(att_consts())
    return d


def run_plan(plan, x, weights, used=None):
    used = used if used is not None else WEIGHT_NAMES
    ws = {k: weights[k].shape for k in used}
    nc = build(plan, ws)
    consts = make_consts()
    in_maps = []
    for b in range(8):
        m = {"x": np.ascontiguousarray(x[b])}
        for k in used:
            m[k] = weights[k]
        m.update(consts)
        in_maps.append(m)
    res = run_bass_kernel_spmd(nc, in_maps, core_ids=list(range(8)))
    return np.stack([r["out"] for r in res.results], axis=0)


FULL_PLAN = [("prep",),
             ("rglru", 0, 0), ("moe", 0, False),
             ("ret", 1, 0), ("moe", 1, False),
             ("attn", 2, 0), ("moe", 2, False),
             ("rglru", 3, 1), ("moe", 3, False)]


def kernel(**inputs):
    x = np.asarray(inputs["x"], dtype=np.float32)
    weights = {k: np.ascontiguousarray(np.asarray(inputs[k], dtype=np.float32)) for k in WEIGHT_NAMES}
    out = run_plan(FULL_PLAN, x, weights)
    return out.astype(np.float32)
```

```python
import contextlib
import numpy as np
import concourse.bass as bass
import concourse.mybir as mybir
from concourse.bass_utils import run_bass_kernel_spmd

F32 = mybir.dt.float32
BF16 = mybir.dt.bfloat16
I32 = mybir.dt.int32
U8 = mybir.dt.uint8
AF = mybir.ActivationFunctionType
ALU = mybir.AluOpType
AX = mybir.AxisListType

D = 1024
S = 2048
NT = 16
NE = 32
CAP = 384
NST = CAP // 128
DEPTH = 4
ALPHA = (2.0 * DEPTH) ** 0.25
LN_EPS = 1e-5
ENGS = ("pe", "act", "dve", "pool", "sp")


class Op:
    __slots__ = ("eng", "fn", "deps", "is_dma", "sig", "cnt", "sem", "target")

    def __init__(s, eng, fn, is_dma):
        s.eng = eng; s.fn = fn; s.deps = []; s.is_dma = is_dma
        s.sig = False; s.cnt = 0; s.sem = None; s.target = 0


class Prog:
    def __init__(s, nc):
        s.nc = nc
        s.ops = {e: [] for e in ENGS}
        s.last_w = {}
        s.readers = {}
        s.n_dma_sems = {"sp": 16, "act": 8, "pool": 16}
        s.since_barrier = []

    def _add(s, eng, fn, reads, writes, is_dma):
        op = Op(eng, fn, is_dma)
        deps = set()
        for k in reads:
            w = s.last_w.get(k)
            if w is not None:
                deps.add(w)
        for k in writes:
            w = s.last_w.get(k)
            if w is not None and not (w.eng == eng and eng == "pe" and not w.is_dma and not is_dma):
                deps.add(w)
        for k in writes:
            for r in s.readers.get(k, ()):
                if r.eng == eng and not r.is_dma and not is_dma:
                    continue
                deps.add(r)
        op.deps = list(deps)
        for k in reads:
            s.readers.setdefault(k, []).append(op)
        for k in writes:
            s.last_w[k] = op
            s.readers[k] = []
        s.ops[eng].append(op)
        s.since_barrier.append(op)
        return op

    def op(s, eng, fn, reads=(), writes=()):
        return s._add(eng, fn, reads, writes, False)

    def dma(s, eng, fn, reads=(), writes=()):
        return s._add(eng, fn, reads, writes, True)

    def barrier(s):
        prev = s.since_barrier
        s.since_barrier = []
        lastc = {}
        dmas = []
        for op in prev:
            if op.fn is None:
                continue
            if op.is_dma:
                dmas.append(op)
            else:
                lastc[op.eng] = op
        deps = list(lastc.values()) + dmas
        for e in ENGS:
            v = Op(e, None, False)
            v.deps = list(deps)
            s.ops[e].append(v)
        s.last_w = {}
        s.readers = {}

    def emit(s, final_wait_ops=()):
        nc = s.nc
        for e in ENGS:
            for op in s.ops[e]:
                for d in op.deps:
                    if not d.is_dma:
                        d.sig = True
        for e in ENGS:
            c = 0
            for op in s.ops[e]:
                if not op.is_dma and op.sig and op.fn is not None:
                    c += 1
                    op.cnt = c
        stack = contextlib.ExitStack()
        prog_sem = {}
        for e in ("pe", "act", "dve", "pool"):
            prog_sem[e] = stack.enter_context(nc.semaphore("prog_" + e))
        for q in ("sp", "act", "pool"):
            sems = [stack.enter_context(nc.semaphore(f"dq_{q}_{i}")) for i in range(s.n_dma_sems[q])]
            cnts = [0] * len(sems)
            prev = [None] * len(sems)
            i = 0
            for op in s.ops[q]:
                if op.is_dma:
                    j = i % len(sems)
                    i += 1
                    cnts[j] += 16
                    op.sem = sems[j]
                    op.target = cnts[j]
                    if prev[j] is not None:
                        op.deps.append(prev[j])
                    prev[j] = op
        engobj = {"pe": nc.tensor, "act": nc.scalar, "dve": nc.vector, "pool": nc.gpsimd, "sp": nc.sync}
        finals = list(final_wait_ops)

        def run_engine(e):
            eng = engobj[e]
            waited = {}
            for op in s.ops[e]:
                need = {}
                for d in op.deps:
                    if d.is_dma:
                        key = ("d", id(d.sem)); val = d.target; sem = d.sem
                    else:
                        key = ("c", d.eng); val = d.cnt; sem = prog_sem[d.eng]
                    if val > need.get(key, (0, None))[0]:
                        need[key] = (val, sem)
                for key, (val, sem) in need.items():
                    if waited.get(key, 0) >= val:
                        continue
                    waited[key] = val
                    eng.wait_ge(sem, val)
                if op.fn is None:
                    continue
                ins = op.fn(eng)
                if op.is_dma:
                    ins.then_inc(op.sem, 16)
                elif op.sig:
                    ins.then_inc(prog_sem[e], 1)
            if e == "sp":
                for d in finals:
                    eng.wait_ge(d.sem, d.target)

        with stack:
            with nc.Block() as block:
                @block.tensor
                def _(t):
                    run_engine("pe")

                @block.scalar
                def _(t):
                    run_engine("act")

                @block.vector
                def _(t):
                    run_engine("dve")

                @block.gpsimd
                def _(t):
                    run_engine("pool")

                @block.sync
                def _(t):
                    run_engine("sp")


class Ctx:
    pass


class Arena:
    def __init__(s, nc, base, limit):
        s.nc = nc; s.base = base; s.limit = limit; s.off = base; s.n = 0

    def reset(s, off=None):
        s.off = s.base if off is None else off

    def alloc(s, name, shape, dtype):
        sz = int(np.prod(shape[1:])) * mybir.dt.size(dtype)
        sz = (sz + 63) // 64 * 64
        assert s.off + sz <= s.limit, (name, s.off, sz, s.limit)
        s.n += 1
        t = s.nc.alloc_sbuf_tensor_at(f"{name}_{s.n}", list(shape), dtype, offset=s.off)
        s.off += sz
        return t


def build(plan, weights_shapes):
    nc = bass.Bass("TRN2", target_bir_lowering=False)
    c = Ctx()
    c.nc = nc
    P = Prog(nc)
    c.P = P
    dr = {}
    dr["x"] = nc.dram_tensor("x", [S, D], F32, kind="ExternalInput").ap()
    for name, shp in weights_shapes.items():
        dr[name] = nc.dram_tensor(name, list(shp), F32, kind="ExternalInput").ap()
    dr["c_ident"] = nc.dram_tensor("c_ident", [128, 128], F32, kind="ExternalInput").ap()
    dr["c_tri"] = nc.dram_tensor("c_tri", [128, 128], F32, kind="ExternalInput").ap()
    dr["c_ec"] = nc.dram_tensor("c_ec", [128, NE], F32, kind="ExternalInput").ap()
    rc, _ = ret_consts()
    for k_, v_ in rc.items():
        dr[k_] = nc.dram_tensor(k_, list(v_.shape), F32, kind="ExternalInput").ap()
    for k_, v_ in att_consts().items():
        dr[k_] = nc.dram_tensor(k_, list(v_.shape), F32, kind="ExternalInput").ap()
    dr["att_o"] = nc.dram_tensor("att_o_scr", [3, S, D], F32, kind="Internal").ap()
    dr["att_ms"] = nc.dram_tensor("att_ms_scr", [3, S, 16, 2], F32, kind="Internal").ap()
    dr["out"] = nc.dram_tensor("out", [S, D], F32, kind="ExternalOutput").ap()
    dr["xg"] = nc.dram_tensor("xg_scr", [NE * CAP, D], BF16, kind="Internal").ap()
    dr["yy"] = nc.dram_tensor("yy_scr", [NE * CAP, D], F32, kind="Internal").ap()
    c.dr = dr

    slab = nc.alloc_sbuf_tensor("arena_slab", [128, 206 * 1024], U8)
    base = nc.lookup_mloc(slab).addr
    A = Arena(nc, base, base + 206 * 1024)
    c.A = A
    c.X = A.alloc("X", [128, NT, D], F32)
    c.XT = A.alloc("XT", [128, 8, S], BF16)
    c.xt_off = A.off - 8 * S * 2
    c.ident_f = A.alloc("ident_f", [128, 128], F32)
    c.ident_b = A.alloc("ident_b", [128, 128], BF16)
    c.tri_b = A.alloc("tri_b", [128, 128], BF16)
    c.ones_b = A.alloc("ones_b", [128, 128], BF16)
    c.ec = A.alloc("ec", [128, NE], F32)
    c.tri_f = A.alloc("tri_f", [128, 128], F32)
    c.eps_t = A.alloc("eps", [128, 1], F32)
    c.one_t = A.alloc("one", [128, 1], F32)
    c.ADRI = A.alloc("ADRI", [128, NT, 4], I32)
    c.G = A.alloc("G", [128, NT, 4], F32)
    c.phase_base = A.off
    c.ps = [nc.alloc_psum_tensor(f"ps{i}", [128, 512], F32) for i in range(8)]
    c.bank = 0

    X = c.X
    P.dma("sp", lambda e: e.dma_start(out=c.ident_f[:], in_=dr["c_ident"]), writes=[("ident_f",)])
    P.dma("sp", lambda e: e.dma_start(out=c.tri_f[:], in_=dr["c_tri"]), writes=[("tri_f",)])
    P.dma("sp", lambda e: e.dma_start(out=c.ec[:], in_=dr["c_ec"]), writes=[("ec",)])
    P.op("dve", lambda e: e.tensor_copy(out=c.ident_b[:], in_=c.ident_f[:]), reads=[("ident_f",)], writes=[("ident_b",)])
    P.op("dve", lambda e: e.tensor_copy(out=c.tri_b[:], in_=c.tri_f[:]), reads=[("tri_f",)], writes=[("tri_b",)])
    P.op("dve", lambda e: e.memset(c.ones_b[:], 1.0), writes=[("ones_b",)])
    P.op("dve", lambda e: e.memset(c.eps_t[:], LN_EPS), writes=[("eps",)])
    P.op("dve", lambda e: e.memset(c.one_t[:], 1.0), writes=[("one",)])
    for i in range(NT):
        P.dma("sp", lambda e, i=i: e.dma_start(out=X[:, i, :], in_=dr["x"][i * 128:(i + 1) * 128, :]),
              writes=[("X", i)])

    for ph in plan:
        A.reset(c.phase_base)
        if ph[0] == "prep":
            phase_prep(c)
        elif ph[0] == "moe":
            phase_moe(c, ph[1], do_route_prep=ph[2])
        elif ph[0] == "rglru":
            phase_rglru(c, ph[1], ph[2])
        elif ph[0] == "ret":
            phase_ret(c, ph[1], ph[2])
        elif ph[0] == "attn":
            phase_attn(c, ph[1], ph[2])
        else:
            raise ValueError(ph)
        P.barrier()

    outs = []
    for i in range(NT):
        outs.append(P.dma("sp", lambda e, i=i: e.dma_start(out=dr["out"][i * 128:(i + 1) * 128, :], in_=X[:, i, :]),
                          reads=[("X", i)]))
    P.emit(final_wait_ops=outs)
    return nc


def next_bank(c, n=8):
    b = c.bank % n
    c.bank = (c.bank + 1) % n
    return b


def load_ln_params(c, layer, which, tag):
    A, P, dr = c.A, c.P, c.dr
    g = A.alloc("lng" + tag, [128, D], F32)
    b = A.alloc("lnb" + tag, [128, D], F32)
    P.dma("sp", lambda e: e.dma_start(out=g[:], in_=dr["ln_gain"][layer, which, :].partition_broadcast(128)),
          writes=[("lng", tag)])
    P.dma("sp", lambda e: e.dma_start(out=b[:], in_=dr["ln_bias"][layer, which, :].partition_broadcast(128)),
          writes=[("lnb", tag)])
    return g, b


def alloc_ln_tmp(c):
    A = c.A
    t = Ctx()
    t.st = [A.alloc("lnst", [128, 2, 6], F32) for _ in range(2)]
    t.mv = [A.alloc("lnmv", [128, 2], F32) for _ in range(2)]
    t.sd = [A.alloc("lnsd", [128, 1], F32) for _ in range(2)]
    t.rs = [A.alloc("lnrs", [128, 1], F32) for _ in range(2)]
    t.xn = [A.alloc("lnxn", [128, D], F32) for _ in range(2)]
    return t


def emit_ln(c, t, Z, zkey, i, g, b, tag):
    P = c.P
    X = c.X
    p = i % 2
    st, mv, sd, rs, xn = t.st[p], t.mv[p], t.sd[p], t.rs[p], t.xn[p]
    for h in range(2):
        P.op("dve", lambda e, h=h: e.bn_stats(out=st[:, h, :], in_=Z[:, h * 512:(h + 1) * 512]),
             reads=[zkey], writes=[("lnst", p, h)])
    P.op("dve", lambda e: e.bn_aggr(out=mv[:], in_=st[:].rearrange("p a b -> p (a b)")),
         reads=[("lnst", p, 0), ("lnst", p, 1)], writes=[("lnmv", p)])
    P.op("act", lambda e: e.activation(out=sd[:], in_=mv[:, 1:2], func=AF.Sqrt, bias=c.eps_t[:], scale=1.0),
         reads=[("lnmv", p), ("eps",)], writes=[("lnsd", p)])
    P.op("dve", lambda e: e.reciprocal(out=rs[:], in_=sd[:]), reads=[("lnsd", p)], writes=[("lnrs", p)])
    P.op("dve", lambda e: e.tensor_scalar(out=xn[:], in0=Z, scalar1=mv[:, 0:1], scalar2=rs[:, 0:1],
                                          op0=ALU.subtract, op1=ALU.mult),
         reads=[zkey, ("lnmv", p), ("lnrs", p)], writes=[("lnxn", p)])
    P.op("dve", lambda e: e.tensor_tensor(out=xn[:], in0=xn[:], in1=g[:], op=ALU.mult),
         reads=[("lnxn", p), ("lng", tag)], writes=[("lnxn", p)])
    P.op("dve", lambda e: e.tensor_tensor(out=X[:, i, :], in0=xn[:], in1=b[:], op=ALU.add),
         reads=[("lnxn", p), ("lnb", tag)], writes=[("X", i)])


def alloc_prep(c):
    A = c.A
    t = Ctx()
    t.xtf = [A.alloc("xtf", [128, 8, 128], F32) for _ in range(2)]
    return t


def emit_transpose_tile(c, t, i, write_xt=True):
    P = c.P
    X, XT = c.X, c.XT
    p = i % 2
    xtf = t.xtf[p]
    for half in range(2):
        bk = next_bank(c)
        ps = c.ps[bk]
        for q in range(4):
            ch = half * 4 + q
            P.op("pe", lambda e, ch=ch, q=q, ps=ps: e.transpose(out=ps[:, q * 128:(q + 1) * 128],
                                                               in_=X[:, i, ch * 128:(ch + 1) * 128],
                                                               identity=c.ident_f[:]),
                 reads=[("X", i), ("ident_f",)], writes=[("ps", bk)])
        P.op("act", lambda e, half=half, ps=ps: e.activation(
            out=xtf[:, half * 4:(half + 1) * 4, :], in_=ps[:].rearrange("p (a b) -> p a b", a=4), func=AF.Copy),
            reads=[("ps", bk)], writes=[("xtf", p, half)])
        if write_xt:
            P.op("act", lambda e, half=half, ps=ps: e.activation(
                out=XT[:, half * 4:(half + 1) * 4, i * 128:(i + 1) * 128], in_=ps[:].rearrange("p (a b) -> p a b", a=4), func=AF.Copy),
                reads=[("ps", bk)], writes=[("XT", i)])


def phase_prep(c):
    t = alloc_prep(c)
    zt = c.A.alloc("zt", [128, NST, D], BF16)
    c.P.op("pool", lambda e: e.memset(zt[:], 0.0), writes=[("zt",)])
    for e_ in range(NE):
        c.P.dma("sp", lambda e, e_=e_: e.dma_start(out=c.dr["xg"][e_ * CAP:(e_ + 1) * CAP, :].rearrange("(t p) d -> p t d", p=128), in_=zt[:]),
                reads=[("zt",)], writes=[("xg_zero", e_)])
    for i in range(NT):
        emit_transpose_tile(c, t, i)


def alloc_route(c, layer):
    A, P, dr = c.A, c.P, c.dr
    r = Ctx()
    r.wr = A.alloc("wr", [128, 8, NE], F32)
    r.br = A.alloc("br", [128, NE], F32)
    r.L = A.alloc("L", [128, NT, NE], F32)
    r.T8 = A.alloc("T8", [128, NT, 8], F32)
    r.M = A.alloc("M", [128, NT, NE], BF16)
    r.CUM = A.alloc("CUM", [128, NT, NE], BF16)
    r.At = [A.alloc("At", [128, NE], F32) for _ in range(2)]
    r.junk = A.alloc("junk", [128, 4, NE], F32)
    r.ADRF = A.alloc("ADRF", [128, NT, 4], F32)
    r.ADRI = c.ADRI
    r.G = c.G
    r.E4 = A.alloc("E4", [128, NT, 4], F32)
    r.nmx = A.alloc("nmx", [128, NT], F32)
    r.sm = A.alloc("sm", [128, NT], F32)
    r.rsm = A.alloc("rsm", [128, NT], F32)
    r.XB = [A.alloc("XB", [128, D], BF16) for _ in range(2)]
    P.dma("sp", lambda e: e.dma_start(out=r.wr[:], in_=dr["moe_w_router"][layer].rearrange("(k p) n -> p k n", p=128)),
          writes=[("wr",)])
    P.dma("sp", lambda e: e.dma_start(out=r.br[:], in_=dr["moe_b_router"][layer, :].partition_broadcast(128)),
          writes=[("br",)])
    return r


def emit_route_tile1(c, r, t, i):
    P, dr = c.P, c.dr
    X = c.X
    p = i % 2
    xtf = t.xtf[p]
    bk = next_bank(c)
    ps = c.ps[bk]
    for k in range(8):
        P.op("pe", lambda e, k=k: e.matmul(ps[:, 0:NE], lhsT=xtf[:, k, :], rhs=r.wr[:, k, :], start=(k == 0), stop=(k == 7)),
             reads=[("xtf", p, k // 4), ("wr",)], writes=[("ps", bk)])
    P.op("dve", lambda e: e.tensor_tensor(out=r.L[:, i, :], in0=ps[:, 0:NE], in1=r.br[:], op=ALU.add),
         reads=[("ps", bk), ("br",)], writes=[("L", i)])
    P.op("dve", lambda e: e.max(out=r.T8[:, i, :], in_=r.L[:, i, :]), reads=[("L", i)], writes=[("T8", i)])
    P.op("dve", lambda e: e.tensor_scalar(out=r.M[:, i, :], in0=r.L[:, i, :], scalar1=r.T8[:, i, 3:4], scalar2=None,
                                          op0=ALU.is_ge),
         reads=[("L", i), ("T8", i)], writes=[("M", i)])


def emit_route_tile(c, r, t, i):
    emit_route_tile1(c, r, t, i)
    emit_route_tile2(c, r, t, i)


def emit_route_tile2(c, r, t, i):
    P, dr = c.P, c.dr
    X = c.X
    p = i % 2
    bk2 = next_bank(c)
    ps2 = c.ps[bk2]
    P.op("pe", lambda e: e.matmul(ps2[:, 0:NE], lhsT=c.tri_b[:], rhs=r.M[:, i, :], start=True, stop=(i == 0)),
         reads=[("tri_b",), ("M", i)], writes=[("ps", bk2)])
    if i > 0:
        P.op("pe", lambda e: e.matmul(ps2[:, 0:NE], lhsT=c.ones_b[:], rhs=r.CUM[:, i - 1, :], start=False, stop=True),
             reads=[("ones_b",), ("CUM", i - 1)], writes=[("ps", bk2)])
        P.op("pool", lambda e: e.tensor_tensor(out=r.CUM[:, i, :], in0=r.CUM[:, i - 1, :], in1=r.M[:, i, :], op=ALU.add),
             reads=[("CUM", i - 1), ("M", i)], writes=[("CUM", i)])
    else:
        P.op("pool", lambda e: e.tensor_copy(out=r.CUM[:, 0, :], in_=r.M[:, 0, :]), reads=[("M", 0)], writes=[("CUM", 0)])
    At = r.At[p]
    P.op("dve", lambda e: e.tensor_tensor(out=At[:], in0=ps2[:, 0:NE], in1=c.ec[:], op=ALU.add),
         reads=[("ps", bk2), ("ec",)], writes=[("At", p)])
    for k in range(4):
        P.op("dve", lambda e, k=k: e.scalar_tensor_tensor(out=r.junk[:, k, :], in0=r.L[:, i, :], scalar=r.T8[:, i, k:k + 1],
                                                          in1=At[:], op0=ALU.is_equal, op1=ALU.mult),
             reads=[("L", i), ("T8", i), ("At", p)], writes=[("junk", k)])
    P.op("dve", lambda e: e.reduce_sum(out=r.ADRF[:, i, :], in_=r.junk[:], axis=AX.X),
         reads=[("junk", k) for k in range(4)], writes=[("ADRF", i)])
    P.op("dve", lambda e: e.tensor_copy(out=r.ADRI[:, i, :], in_=r.ADRF[:, i, :]),
         reads=[("ADRF", i)], writes=[("ADRI", i)])
    P.op("dve", lambda e: e.tensor_scalar(out=r.nmx[:, i:i + 1], in0=r.T8[:, i, 0:1], scalar1=-1.0, scalar2=None, op0=ALU.mult),
         reads=[("T8", i)], writes=[("nmx", i)])
    P.op("act", lambda e: e.activation(out=r.E4[:, i, :], in_=r.T8[:, i, 0:4], func=AF.Exp, bias=r.nmx[:, i:i + 1], scale=1.0),
         reads=[("T8", i), ("nmx", i)], writes=[("E4", i)])
    P.op("dve", lambda e: e.reduce_sum(out=r.sm[:, i:i + 1], in_=r.E4[:, i, :], axis=AX.X), reads=[("E4", i)], writes=[("sm", i)])
    P.op("dve", lambda e: e.reciprocal(out=r.rsm[:, i:i + 1], in_=r.sm[:, i:i + 1]), reads=[("sm", i)], writes=[("rsm", i)])
    P.op("dve", lambda e: e.tensor_scalar(out=r.G[:, i, :], in0=r.E4[:, i, :], scalar1=r.rsm[:, i:i + 1], scalar2=None, op0=ALU.mult),
         reads=[("E4", i), ("rsm", i)], writes=[("G", i)])
    XB = r.XB[p]
    P.op("act", lambda e: e.activation(out=XB[:], in_=X[:, i, :], func=AF.Copy), reads=[("X", i)], writes=[("XB", p)])
    for k in range(4):
        P.dma("pool", lambda e, k=k: e.indirect_dma_start(
            out=dr["xg"], out_offset=bass.IndirectOffsetOnAxis(ap=r.ADRI[:, i, k:k + 1], axis=0),
            in_=XB[:], in_offset=None),
            reads=[("XB", p), ("ADRI", i)], writes=[("xg_dram", i, k)])


def phase_moe(c, layer, do_route_prep):
    A, P, dr = c.A, c.P, c.dr
    X, XT = c.X, c.XT
    nc = c.nc
    r = alloc_route(c, layer) if do_route_prep else None
    t = alloc_prep(c)
    if do_route_prep:
        for i in range(NT):
            emit_transpose_tile(c, t, i, write_xt=False)
            emit_route_tile(c, r, t, i)
    mark = A.off
    RING = 6 if do_route_prep else 7
    wring = [A.alloc("wring", [128, 8, 512], BF16) for _ in range(RING)]
    bgu = A.alloc("bgu", [128, 16, NE], F32)
    braw = A.alloc("braw", [NE, 2 * D], F32)
    P.dma("sp", lambda e: e.dma_start(out=braw[:], in_=dr["moe_b_gu"][layer]), writes=[("braw",)])
    bkb = next_bank(c)
    for cc in range(16):
        P.op("pe", lambda e, cc=cc: e.transpose(out=c.ps[bkb][:, cc * NE:(cc + 1) * NE], in_=braw[:, cc * 128:(cc + 1) * 128],
                                                identity=c.ident_f[0:NE, 0:NE]),
             reads=[("braw",), ("ident_f",)], writes=[("ps", bkb)])
    P.op("dve", lambda e: e.tensor_copy(out=bgu[:].rearrange("p a b -> p (a b)"), in_=c.ps[bkb][:, :]),
         reads=[("ps", bkb)], writes=[("bgu",)])
    P.op("dve", lambda e: e.tensor_scalar(out=bgu[:, 8:16, :], in0=bgu[:, 8:16, :], scalar1=1.0, scalar2=None, op0=ALU.add),
         reads=[("bgu",)], writes=[("bgu",)])
    bd = [A.alloc("bd", [128, D], F32) for _ in range(2)]
    ytile = [A.alloc("ytile", [128, D], F32) for _ in range(2)]
    gt = [A.alloc("gt", [128, CAP], F32) for _ in range(2)]
    ut = [A.alloc("ut", [128, CAP], F32) for _ in range(2)]
    sg = [A.alloc("sg", [128, CAP], F32) for _ in range(2)]
    A2 = Arena(nc, c.xt_off, c.xt_off + 8 * S * 2)
    A2.n = 1000
    xgtok = [A2.alloc("xgtok", [128, NST, D], BF16) for _ in range(2)]
    xgT = [A2.alloc("xgT", [128, 8, CAP], BF16) for _ in range(2)]
    hT = A2.alloc("hT", [128, 8, CAP], BF16)

    pieces = []
    for e_ in range(NE):
        for pc in (0, 2, 1, 3):
            pieces.append((e_, "gu", pc))
        for pc in (0, 1):
            pieces.append((e_, "dn", pc))
    piece_slot = {}

    def issue_piece(n):
        if n >= len(pieces):
            return
        e_, kind, pc = pieces[n]
        slot = n % RING
        piece_slot[(e_, kind, pc)] = slot
        if kind == "gu":
            src = dr["moe_w_gu"][layer, e_, :, pc * 512:(pc + 1) * 512]
        else:
            src = dr["moe_w_down"][layer, e_, :, pc * 512:(pc + 1) * 512]
        P.dma("pool", lambda e, src=src, slot=slot: e.dma_start(out=wring[slot][:], in_=src.rearrange("(k p) n -> p k n", p=128)),
              writes=[("wring", slot)])

    PRE = RING - 1
    for n in range(PRE):
        issue_piece(n)
    nissued = [PRE]
    def ex_load(e_):
        pe2 = e_ % 2
        P.dma("sp", lambda e, e_=e_: e.dma_start(out=xgtok[e_ % 2][:], in_=dr["xg"][e_ * CAP:(e_ + 1) * CAP, :].rearrange("(t p) d -> p t d", p=128)),
              reads=[("xg_dram", i_, k_) for i_ in range(NT) for k_ in range(4)], writes=[("xgtok", e_ % 2)])
        P.dma("sp", lambda e, e_=e_, pe2=pe2: e.dma_start(out=bd[pe2][:], in_=dr["moe_b_down"][layer, e_, :].partition_broadcast(128)),
              writes=[("bd", pe2)])
        for st in range(NST):
            for half in range(2):
                bk = next_bank(c)
                psb = c.ps[bk][:].bitcast(BF16)
                for q in range(4):
                    ch = half * 4 + q
                    P.op("pe", lambda e, st=st, ch=ch, q=q, psb=psb: e.transpose(
                        out=psb[:, q * 128:(q + 1) * 128], in_=xgtok[pe2][:, st, ch * 128:(ch + 1) * 128], identity=c.ident_b[:]),
                        reads=[("xgtok", pe2), ("ident_b",)], writes=[("ps", bk)])
                P.op("act", lambda e, st=st, half=half, psb=psb, pe2=pe2: e.activation(
                    out=xgT[pe2][:, half * 4:(half + 1) * 4, st * 128:(st + 1) * 128],
                    in_=psb[:, 0:512].rearrange("p (a b) -> p a b", a=4), func=AF.Copy),
                    reads=[("ps", bk)], writes=[("xgT", pe2)])

    def ex_gu(e_):
        pe2 = e_ % 2
        for fc in range(8):
            pcg = fc // 4
            pcu = 2 + fc // 4
            lc = (fc % 4) * 128
            if fc % 4 == 0:
                pass
            sg_ = piece_slot[(e_, "gu", pcg)]
            su_ = piece_slot[(e_, "gu", pcu)]
            bkg = next_bank(c); bku = next_bank(c)
            psg, psu = c.ps[bkg], c.ps[bku]
            for k in range(8):
                P.op("pe", lambda e, k=k, sg_=sg_, lc=lc, psg=psg, pe2=pe2: e.matmul(
                    psg[:, 0:CAP], lhsT=wring[sg_][:, k, lc:lc + 128], rhs=xgT[pe2][:, k, :], start=(k == 0), stop=(k == 7)),
                    reads=[("wring", sg_), ("xgT", pe2)], writes=[("ps", bkg)])
            for k in range(8):
                P.op("pe", lambda e, k=k, su_=su_, lc=lc, psu=psu, pe2=pe2: e.matmul(
                    psu[:, 0:CAP], lhsT=wring[su_][:, k, lc:lc + 128], rhs=xgT[pe2][:, k, :], start=(k == 0), stop=(k == 7)),
                    reads=[("wring", su_), ("xgT", pe2)], writes=[("ps", bku)])
            pp = fc % 2
            P.op("dve", lambda e, psg=psg, fc=fc, pp=pp, e_=e_: e.tensor_scalar(
                out=gt[pp][:], in0=psg[:, 0:CAP], scalar1=bgu[:, fc, e_:e_ + 1], scalar2=7.0, op0=ALU.add, op1=ALU.min),
                reads=[("ps", bkg), ("bgu",)], writes=[("gt", pp)])
            P.op("dve", lambda e, psu=psu, fc=fc, pp=pp, e_=e_: e.tensor_scalar(
                out=ut[pp][:], in0=psu[:, 0:CAP], scalar1=bgu[:, 8 + fc, e_:e_ + 1], scalar2=8.0, op0=ALU.add, op1=ALU.min),
                reads=[("ps", bku), ("bgu",)], writes=[("ut", pp)])
            P.op("act", lambda e, pp=pp: e.activation(out=sg[pp][:], in_=gt[pp][:], func=AF.Silu, scale=1.702),
                 reads=[("gt", pp)], writes=[("sg", pp)])
            P.op("dve", lambda e, pp=pp, fc=fc: e.scalar_tensor_tensor(out=hT[:, fc, :], in0=ut[pp][:], scalar=-6.0, in1=sg[pp][:],
                                                                    op0=ALU.max, op1=ALU.mult),
                 reads=[("ut", pp), ("sg", pp)], writes=[("hT", fc)])
            if fc % 4 == 3:
                issue_piece(nissued[0]); issue_piece(nissued[0] + 1)
                nissued[0] += 2

    def ex_down(e_):
        pe2 = e_ % 2
        for st in range(NST):
            yp = st % 2
            for nh in range(2):
                sd_ = piece_slot[(e_, "dn", nh)]
                bk = next_bank(c)
                psd = c.ps[bk]
                for fc in range(8):
                    P.op("pe", lambda e, fc=fc, st=st, sd_=sd_, psd=psd: e.matmul(
                        psd[:, :], lhsT=hT[:, fc, st * 128:(st + 1) * 128], rhs=wring[sd_][:, fc, :], start=(fc == 0), stop=(fc == 7)),
                        reads=[("hT", fc), ("wring", sd_)], writes=[("ps", bk)])
                P.op("dve", lambda e, nh=nh, yp=yp, psd=psd, pe2=pe2: e.scalar_tensor_tensor(
                    out=ytile[yp][:, nh * 512:(nh + 1) * 512], in0=psd[:, :], scalar=1.0 / 1.702, in1=bd[pe2][:, nh * 512:(nh + 1) * 512],
                    op0=ALU.mult, op1=ALU.add),
                    reads=[("ps", bk), ("bd", pe2)], writes=[("ytile", yp, nh)])
            P.dma("sp", lambda e, e_=e_, st=st, yp=yp: e.dma_start(
                out=dr["yy"][e_ * CAP + st * 128:e_ * CAP + (st + 1) * 128, :], in_=ytile[yp][:]),
                reads=[("ytile", yp, 0), ("ytile", yp, 1)], writes=[("yy_dram", e_, st)])

    ex_load(0)
    for e_ in range(NE):
        ex_gu(e_)
        if e_ + 1 < NE:
            ex_load(e_ + 1)
        ex_down(e_)
        issue_piece(nissued[0]); issue_piece(nissued[0] + 1)
        nissued[0] += 2

    P.barrier()
    A.reset(mark)
    g2, b2 = load_ln_params(c, layer, 1, "b")
    lt = alloc_ln_tmp(c)
    NYK = 3 if do_route_prep else 4
    YK = [[A.alloc("YK", [128, D], F32) for _ in range(4)] for _ in range(NYK)]
    Z = [A.alloc("Z", [128, D], F32) for _ in range(2)]
    t2 = t
    for i in range(NT):
        p = i % 2
        py = i % NYK
        for k in range(4):
            P.dma("pool", lambda e, k=k, py=py, i=i: e.indirect_dma_start(
                out=YK[py][k][:], out_offset=None, in_=dr["yy"],
                in_offset=bass.IndirectOffsetOnAxis(ap=c.ADRI[:, i, k:k + 1], axis=0)),
                reads=[("ADRI", i)], writes=[("YK", py, k)])
        P.op("dve", lambda e, p=p, py=py, i=i: e.tensor_scalar(out=Z[p][:], in0=YK[py][0][:], scalar1=c.G[:, i, 0:1], scalar2=None, op0=ALU.mult),
             reads=[("YK", py, 0), ("G", i)], writes=[("Z", p)])
        for k in range(1, 4):
            P.op("dve", lambda e, p=p, py=py, i=i, k=k: e.scalar_tensor_tensor(
                out=Z[p][:], in0=YK[py][k][:], scalar=c.G[:, i, k:k + 1], in1=Z[p][:], op0=ALU.mult, op1=ALU.add),
                reads=[("YK", py, k), ("G", i), ("Z", p)], writes=[("Z", p)])
        P.op("dve", lambda e, p=p, i=i: e.scalar_tensor_tensor(
            out=Z[p][:], in0=X[:, i, :], scalar=ALPHA, in1=Z[p][:], op0=ALU.mult, op1=ALU.add),
            reads=[("X", i), ("Z", p)], writes=[("Z", p)])
        emit_ln(c, lt, Z[p][:], ("Z", p), i, g2, b2, "b")
        if i >= 1:
            emit_transpose_tile(c, t2, i - 1, write_xt=True)
    emit_transpose_tile(c, t2, NT - 1, write_xt=True)


def emit_mixer_epilogue(c, layer, YT, kc, wout_dram):
    A, P, dr = c.A, c.P, c.dr
    X = c.X
    wout = A.alloc("wout", [128, kc, D], BF16)
    for k2 in range(0, kc, 4):
        P.dma("pool", lambda e, k2=k2: e.dma_start(
            out=wout[:, k2:k2 + 4, :], in_=wout_dram[k2 * 128:(k2 + 4) * 128, :].rearrange("(k p) n -> p k n", p=128)),
            writes=[("wout", k2)])
    r = alloc_route(c, layer)
    g1, b1 = load_ln_params(c, layer, 0, "a")
    lt = alloc_ln_tmp(c)
    t = alloc_prep(c)
    Z = [A.alloc("Z", [128, D], F32) for _ in range(2)]
    for i in range(NT):
        p = i % 2
        for nh in range(2):
            bk = next_bank(c)
            ps = c.ps[bk]
            for k in range(kc):
                P.op("pe", lambda e, k=k, nh=nh, ps=ps, i=i: e.matmul(
                    ps[:, :], lhsT=YT[:, k, i * 128:(i + 1) * 128], rhs=wout[:, k, nh * 512:(nh + 1) * 512],
                    start=(k == 0), stop=(k == kc - 1)),
                    reads=[("YT", k), ("wout", (k // 4) * 4)], writes=[("ps", bk)])
            P.op("dve", lambda e, nh=nh, ps=ps, p=p, i=i: e.scalar_tensor_tensor(
                out=Z[p][:, nh * 512:(nh + 1) * 512], in0=X[:, i, nh * 512:(nh + 1) * 512], scalar=ALPHA, in1=ps[:, :],
                op0=ALU.mult, op1=ALU.add),
                reads=[("X", i), ("ps", bk)], writes=[("Z", p, nh)])
        P.op("dve", lambda e: e.engine_nop(), reads=[("Z", p, 0), ("Z", p, 1)], writes=[("Z", p)])
        emit_ln(c, lt, Z[p][:], ("Z", p), i, g1, b1, "a")
        if i >= 1:
            emit_transpose_tile(c, t, i - 1, write_xt=False)
            emit_route_tile1(c, r, t, i - 1)
        if i >= 2:
            emit_route_tile2(c, r, t, i - 2)
    emit_transpose_tile(c, t, NT - 1, write_xt=False)
    emit_route_tile1(c, r, t, NT - 1)
    emit_route_tile2(c, r, t, NT - 2)
    emit_route_tile2(c, r, t, NT - 1)


def load_small_vecs(c, rows, nvec):
    A, P = c.A, c.P
    braw = A.alloc("svraw", [nvec, D], F32)
    pv = A.alloc("pv", [128, 8, nvec], F32)
    for j, ap in enumerate(rows):
        P.dma("sp", lambda e, j=j, ap=ap: e.dma_start(out=braw[j:j + 1, :], in_=ap.unsqueeze(0)), writes=[("svraw", j)])
    bk = next_bank(c)
    for cc in range(8):
        P.op("pe", lambda e, cc=cc: e.transpose(out=c.ps[bk][:, cc * nvec:(cc + 1) * nvec], in_=braw[:, cc * 128:(cc + 1) * 128],
                                                identity=c.ident_f[0:nvec, 0:nvec]),
             reads=[("svraw", j) for j in range(nvec)] + [("ident_f",)], writes=[("ps", bk)])
    P.op("dve", lambda e: e.tensor_copy(out=pv[:].rearrange("p a b -> p (a b)"), in_=c.ps[bk][:, 0:8 * nvec]),
         reads=[("ps", bk)], writes=[("pv",)])
    return pv


def phase_rglru(c, layer, idx):
    A, P, dr = c.A, c.P, c.dr
    X, XT = c.X, c.XT
    TB = 512
    NTB = S // TB
    YT = A.alloc("YT", [128, 8, S], BF16)
    mark_after_yt = A.off
    pv = load_small_vecs(c, [dr["a_conv_w"][idx, 0], dr["a_conv_w"][idx, 1], dr["a_conv_w"][idx, 2], dr["a_conv_w"][idx, 3],
                             dr["a_conv_b"][idx], dr["a_b_rgate"][idx], dr["a_b_igate"][idx], dr["a_lambda"][idx]], 8)
    cv1 = A.alloc("cv1", [128, 8], F32)
    cv2 = A.alloc("cv2", [128, 8], F32)
    sp_e = A.alloc("sp_e", [128, 8], F32)
    sp_l = A.alloc("sp_l", [128, 8], F32)
    P.op("act", lambda e: e.activation(out=sp_e[:], in_=pv[:, :, 7], func=AF.Exp, scale=-1.0), reads=[("pv",)], writes=[("sp_e",)])
    P.op("act", lambda e: e.activation(out=sp_l[:], in_=sp_e[:], func=AF.Ln, bias=c.one_t[:], scale=1.0),
         reads=[("sp_e",), ("one",)], writes=[("sp_l",)])
    P.op("dve", lambda e: e.tensor_scalar(out=cv1[:], in0=sp_l[:], scalar1=-8.0, scalar2=None, op0=ALU.mult), reads=[("sp_l",)], writes=[("cv1",)])
    P.op("dve", lambda e: e.tensor_scalar(out=cv2[:], in0=sp_l[:], scalar1=-16.0, scalar2=None, op0=ALU.mult), reads=[("sp_l",)], writes=[("cv2",)])
    wg = A.alloc("wgr", [128, 8, 128], BF16)
    wi = A.alloc("wgi", [128, 8, 128], BF16)
    P.dma("pool", lambda e: e.dma_start(out=wg[:], in_=dr["a_w_rgate"][idx].rearrange("h i j -> i h j")), writes=[("wgr",)])
    P.dma("pool", lambda e: e.dma_start(out=wi[:], in_=dr["a_w_igate"][idx].rearrange("h i j -> i h j")), writes=[("wgi",)])
    wrec = [A.alloc("wrec", [128, 8, 128], BF16) for _ in range(2)]
    wgat = [A.alloc("wgat", [128, 8, 128], BF16) for _ in range(2)]
    RECp = [A.alloc("RECp", [128, 3 + S], F32) for _ in range(2)]
    nm = ["GX", "SQ", "SG", "CV", "R", "I", "AA", "OM", "H"]
    T = {n: [A.alloc(n, [128, TB], F32) for _ in range(2)] for n in nm}
    CVb = [A.alloc("CVb", [128, TB], BF16) for _ in range(2)]
    for q in range(2):
        P.op("pool", lambda e, q=q: e.memset(RECp[q][:, 0:3], 0.0), writes=[("RECp", q, -1)])
    it = 0
    for cc in range(8):
        q = cc % 2
        P.dma("pool", lambda e, cc=cc, q=q: e.dma_start(
            out=wrec[q][:], in_=dr["a_w_in"][idx][:, D + cc * 128:D + (cc + 1) * 128].rearrange("(k p) n -> p k n", p=128)),
            writes=[("wrec", q)])
        P.dma("pool", lambda e, cc=cc, q=q: e.dma_start(
            out=wgat[q][:], in_=dr["a_w_in"][idx][:, cc * 128:(cc + 1) * 128].rearrange("(k p) n -> p k n", p=128)),
            writes=[("wgat", q)])
        for tb in range(NTB):
            bk = next_bank(c)
            ps = c.ps[bk]
            for k in range(8):
                P.op("pe", lambda e, k=k, ps=ps, tb=tb, q=q: e.matmul(ps[:, :], lhsT=wrec[q][:, k, :], rhs=XT[:, k, tb * TB:(tb + 1) * TB],
                                                                   start=(k == 0), stop=(k == 7)),
                     reads=[("wrec", q)] + [("XT", i_) for i_ in range(tb * 4, tb * 4 + 4)], writes=[("ps", bk)])
            P.op("act", lambda e, ps=ps, tb=tb, q=q: e.activation(out=RECp[q][:, 3 + tb * TB:3 + (tb + 1) * TB], in_=ps[:, :], func=AF.Copy),
                 reads=[("ps", bk)], writes=[("RECp", q, tb)])
        for tb in range(NTB):
            b_ = it % 2
            it += 1
            t_ = {n: T[n][b_] for n in nm}
            k_ = lambda n: (n, b_)
            bk = next_bank(c)
            ps = c.ps[bk]
            for k in range(8):
                P.op("pe", lambda e, k=k, ps=ps, tb=tb, q=q: e.matmul(ps[:, :], lhsT=wgat[q][:, k, :], rhs=XT[:, k, tb * TB:(tb + 1) * TB],
                                                                   start=(k == 0), stop=(k == 7)),
                     reads=[("wgat", q)] + [("XT", i_) for i_ in range(tb * 4, tb * 4 + 4)], writes=[("ps", bk)])
            P.op("act", lambda e, ps=ps, t_=t_: e.activation(out=t_["GX"][:], in_=ps[:, :], func=AF.Copy), reads=[("ps", bk)], writes=[k_("GX")])
            P.op("act", lambda e, ps=ps, t_=t_: e.activation(out=t_["SQ"][:], in_=ps[:, :], func=AF.Square), reads=[("ps", bk)], writes=[k_("SQ")])
            P.op("dve", lambda e, t_=t_: e.tensor_scalar(out=t_["SQ"][:], in0=t_["SQ"][:], scalar1=0.044715, scalar2=1.0, op0=ALU.mult, op1=ALU.add),
                 reads=[k_("SQ")], writes=[k_("SQ")])
            P.op("dve", lambda e, t_=t_: e.tensor_tensor(out=t_["SQ"][:], in0=t_["SQ"][:], in1=t_["GX"][:], op=ALU.mult),
                 reads=[k_("SQ"), k_("GX")], writes=[k_("SQ")])
            P.op("act", lambda e, t_=t_: e.activation(out=t_["SG"][:], in_=t_["SQ"][:], func=AF.Sigmoid, scale=1.5957691216057308),
                 reads=[k_("SQ")], writes=[k_("SG")])
            P.op("pool", lambda e, t_=t_: e.tensor_tensor(out=t_["SG"][:], in0=t_["GX"][:], in1=t_["SG"][:], op=ALU.mult),
                 reads=[k_("GX"), k_("SG")], writes=[k_("SG")])
            rk = [("RECp", q, tb)] + ([("RECp", q, tb - 1)] if tb > 0 else [("RECp", q, -1)])
            P.op("dve", lambda e, t_=t_, tb=tb, q=q, cc=cc: e.tensor_scalar(
                out=t_["CV"][:], in0=RECp[q][:, tb * TB:tb * TB + TB], scalar1=pv[:, cc, 0:1], scalar2=pv[:, cc, 4:5], op0=ALU.mult, op1=ALU.add),
                reads=rk + [("pv",)], writes=[k_("CV")])
            for j in range(1, 4):
                P.op("dve", lambda e, t_=t_, tb=tb, q=q, cc=cc, j=j: e.scalar_tensor_tensor(
                    out=t_["CV"][:], in0=RECp[q][:, tb * TB + j:tb * TB + j + TB], scalar=pv[:, cc, j:j + 1], in1=t_["CV"][:], op0=ALU.mult, op1=ALU.add),
                    reads=rk + [("pv",), k_("CV")], writes=[k_("CV")])
            P.op("act", lambda e, t_=t_, b_=b_: e.activation(out=CVb[b_][:], in_=t_["CV"][:], func=AF.Copy), reads=[k_("CV")], writes=[("CVb", b_)])
            bkr = next_bank(c); bki = next_bank(c)
            P.op("pe", lambda e, b_=b_, cc=cc, bkr=bkr: e.matmul(c.ps[bkr][:, :], lhsT=wg[:, cc, :], rhs=CVb[b_][:], start=True, stop=True),
                 reads=[("wgr",), ("CVb", b_)], writes=[("ps", bkr)])
            P.op("pe", lambda e, b_=b_, cc=cc, bki=bki: e.matmul(c.ps[bki][:, :], lhsT=wi[:, cc, :], rhs=CVb[b_][:], start=True, stop=True),
                 reads=[("wgi",), ("CVb", b_)], writes=[("ps", bki)])
            P.op("act", lambda e, t_=t_, cc=cc, bkr=bkr: e.activation(out=t_["R"][:], in_=c.ps[bkr][:, :], func=AF.Sigmoid, bias=pv[:, cc, 5:6], scale=1.0),
                 reads=[("ps", bkr), ("pv",)], writes=[k_("R")])
            P.op("act", lambda e, t_=t_, cc=cc, bki=bki: e.activation(out=t_["I"][:], in_=c.ps[bki][:, :], func=AF.Sigmoid, bias=pv[:, cc, 6:7], scale=1.0),
                 reads=[("ps", bki), ("pv",)], writes=[k_("I")])
            P.op("act", lambda e, t_=t_, cc=cc: e.activation(out=t_["AA"][:], in_=t_["R"][:], func=AF.Exp, scale=cv1[:, cc:cc + 1]),
                 reads=[k_("R"), ("cv1",)], writes=[k_("AA")])
            P.op("act", lambda e, t_=t_, cc=cc: e.activation(out=t_["OM"][:], in_=t_["R"][:], func=AF.Exp, scale=cv2[:, cc:cc + 1]),
                 reads=[k_("R"), ("cv2",)], writes=[k_("OM")])
            P.op("dve", lambda e, t_=t_: e.tensor_scalar(out=t_["OM"][:], in0=t_["OM"][:], scalar1=-1.0, scalar2=1.0, op0=ALU.mult, op1=ALU.add),
                 reads=[k_("OM")], writes=[k_("OM")])
            P.op("act", lambda e, t_=t_: e.activation(out=t_["OM"][:], in_=t_["OM"][:], func=AF.Sqrt), reads=[k_("OM")], writes=[k_("OM")])
            P.op("pool", lambda e, t_=t_: e.tensor_tensor(out=t_["I"][:], in0=t_["I"][:], in1=t_["CV"][:], op=ALU.mult),
                 reads=[k_("I"), k_("CV")], writes=[k_("I")])
            P.op("dve", lambda e, t_=t_: e.tensor_tensor(out=t_["I"][:], in0=t_["I"][:], in1=t_["OM"][:], op=ALU.mult),
                 reads=[k_("I"), k_("OM")], writes=[k_("I")])
            if tb == 0:
                P.op("dve", lambda e, t_=t_: e.tensor_tensor_scan(out=t_["H"][:], data0=t_["AA"][:], data1=t_["I"][:], initial=0.0,
                                                                  op0=ALU.mult, op1=ALU.add),
                     reads=[k_("AA"), k_("I")], writes=[k_("H")])
            else:
                hp = T["H"][1 - b_]
                P.op("dve", lambda e, t_=t_, hp=hp: e.tensor_tensor_scan(out=t_["H"][:], data0=t_["AA"][:], data1=t_["I"][:],
                                                                         initial=hp[:, TB - 1:TB], op0=ALU.mult, op1=ALU.add),
                     reads=[k_("AA"), k_("I"), ("H", 1 - b_)], writes=[k_("H")])
            P.op("pool", lambda e, t_=t_, cc=cc, tb=tb: e.tensor_tensor(out=YT[:, cc, tb * TB:(tb + 1) * TB], in0=t_["SG"][:], in1=t_["H"][:], op=ALU.mult),
                 reads=[k_("SG"), k_("H")], writes=[("YT", cc)])
    P.barrier()
    A.reset(mark_after_yt)
    emit_mixer_epilogue(c, layer, YT, 8, dr["a_w_out"][idx])


RET_H = 4
RET_EPS = 1e-6


def ret_consts():
    f32 = np.float32
    log_gamma = np.log1p(-np.exp2(-5.0 - np.arange(RET_H, dtype=f32))).astype(f32)
    pos = np.arange(128, dtype=f32)
    rel = pos[:, None] - pos[None, :]
    intra = np.where(rel >= 0, np.exp(log_gamma[:, None, None] * np.maximum(rel, 0.0)), 0.0).astype(f32)
    dt = np.ascontiguousarray(intra.transpose(0, 2, 1))
    qd = np.exp(log_gamma[:, None] * (pos + 1.0)).astype(f32)
    kd = np.exp(log_gamma[:, None] * (127.0 - pos)).astype(f32)
    cd = np.exp(log_gamma * 128.0).astype(f32)
    return {"c_ret_dt": np.ascontiguousarray(dt.transpose(1, 0, 2)),
            "c_ret_qd": np.ascontiguousarray(np.tile(qd[None], (128, 1, 1))),
            "c_ret_kd": np.ascontiguousarray((kd.T / 16.0).astype(f32)),
            }, [float(v) for v in cd]


def phase_ret(c, layer, idx):
    A, P, dr = c.A, c.P, c.dr
    X, XT = c.X, c.XT
    _, CDV = ret_consts()
    DT = A.alloc("rDT", [128, RET_H, 128], F32)
    QD = A.alloc("rQD", [128, RET_H, 128], F32)
    KD = A.alloc("rKD", [128, RET_H], F32)
    eps6 = A.alloc("eps6", [128, 1], F32)
    P.dma("sp", lambda e: e.dma_start(out=DT[:], in_=dr["c_ret_dt"]), writes=[("rDT",)])
    P.dma("sp", lambda e: e.dma_start(out=QD[:], in_=dr["c_ret_qd"]), writes=[("rQD",)])
    P.dma("sp", lambda e: e.dma_start(out=KD[:], in_=dr["c_ret_kd"]), writes=[("rKD",)])
    P.op("dve", lambda e: e.memset(eps6[:], RET_EPS), writes=[("eps6",)])
    Wq = [A.alloc("Wq", [128, 8, 256], BF16) for _ in range(2)]
    Wk = [A.alloc("Wk", [128, 8, 256], BF16) for _ in range(2)]
    Wv = [A.alloc("Wv", [128, 8, 512], BF16) for _ in range(2)]
    Wg = [A.alloc("Wg", [128, 8, 512], BF16) for _ in range(2)]
    Wo = [A.alloc("Wo", [128, 4, D], BF16) for _ in range(2)]
    Sf = A.alloc("Sf", [128, 2, 512], F32)
    Sb = [A.alloc("Sb", [128, 2, 512], BF16) for _ in range(2)]
    qT = [A.alloc("qT", [128, 2, 128], BF16) for _ in range(2)]
    qdT = [A.alloc("qdT", [128, 2, 128], BF16) for _ in range(2)]
    kT = [A.alloc("kT", [128, 2, 128], BF16) for _ in range(2)]
    kdec = [A.alloc("kdec", [128, 256], BF16) for _ in range(2)]
    vc = [A.alloc("vc", [128, 512], BF16) for _ in range(2)]
    sgc = [A.alloc("sgc", [128, 512], F32) for _ in range(2)]
    PT = [A.alloc("PT", [128, 128], BF16) for _ in range(2)]
    qf = [A.alloc("qf", [128, 256], F32) for _ in range(2)]
    kf = [A.alloc("kf", [128, 256], F32) for _ in range(2)]
    ktf = [A.alloc("ktf", [128, 256], F32) for _ in range(2)]
    scf = [A.alloc("scf", [128, 128], F32) for _ in range(2)]
    osq = [A.alloc("osq", [128, 512], F32) for _ in range(2)]
    ms = [A.alloc("ms", [128, 1], F32) for _ in range(2)]
    sd = [A.alloc("rsd", [128, 1], F32) for _ in range(2)]
    rs = [A.alloc("rrs", [128, 1], F32) for _ in range(2)]
    yc = [A.alloc("yc", [128, 512], BF16) for _ in range(2)]
    yT = [A.alloc("yT", [128, 4, 128], BF16) for _ in range(2)]
    W = dr["b_w_in"][idx]
    QW = 1024
    thr = A.alloc("thr", [128, 1], BF16)

    def load_head(h):
        q = h % 2
        for (dst, col0, n, nm) in ((Wq[q], h * 256, 256, "Wq"), (Wk[q], QW + h * 256, 256, "Wk"),
                                   (Wv[q], 2 * QW + h * 512, 512, "Wv"), (Wg[q], 2 * QW + 2048 + h * 512, 512, "Wg")):
            P.dma("pool", lambda e, dst=dst, col0=col0, n=n: e.dma_start(
                out=dst[:], in_=W[:, col0:col0 + n].rearrange("(k p) n -> p k n", p=128)), reads=[("throttle",)], writes=[(nm, q)])
        P.dma("pool", lambda e, q=q, h=h: e.dma_start(
            out=Wo[q][:], in_=dr["b_w_out"][idx][h * 512:(h + 1) * 512, :].rearrange("(k p) n -> p k n", p=128)), reads=[("throttle",)], writes=[("Wo", q)])

    import os
    DBG = int(os.environ.get("RET_DBG", "99"))
    NH_ = int(os.environ.get("RET_NH", "4"))
    NI_ = int(os.environ.get("RET_NI", "16"))

    def ret_P(h, i, b_, q):
        if h >= NH_ or i >= NI_ or DBG < 1:
            return
        xtk = [("XT", i)]
        tok = slice(i * 128, (i + 1) * 128)
        bkq = next_bank(c); bkk = next_bank(c)
        for dc in range(2):
            for k in range(8):
                P.op("pe", lambda e, dc=dc, k=k, bkq=bkq: e.matmul(c.ps[bkq][:, dc * 128:(dc + 1) * 128], lhsT=Wq[q][:, k, dc * 128:(dc + 1) * 128],
                                                             rhs=XT[:, k, tok], start=(k == 0), stop=(k == 7)),
                     reads=[("Wq", q)] + xtk, writes=[("ps", bkq)])
        for dc in range(2):
            for k in range(8):
                P.op("pe", lambda e, dc=dc, k=k, bkk=bkk: e.matmul(c.ps[bkk][:, dc * 128:(dc + 1) * 128], lhsT=Wk[q][:, k, dc * 128:(dc + 1) * 128],
                                                             rhs=XT[:, k, tok], start=(k == 0), stop=(k == 7)),
                     reads=[("Wk", q)] + xtk, writes=[("ps", bkk)])
        P.op("act", lambda e, bkq=bkq, b_=b_: e.activation(out=qf[b_][:], in_=c.ps[bkq][:, 0:256], func=AF.Copy),
             reads=[("ps", bkq)], writes=[("qf", b_)])
        P.op("act", lambda e, bkk=bkk, b_=b_: e.activation(out=kf[b_][:], in_=c.ps[bkk][:, 0:256], func=AF.Copy),
             reads=[("ps", bkk)], writes=[("kf", b_)])
        P.op("pool", lambda e, b_=b_: e.tensor_copy(out=qT[b_][:].rearrange("p a b -> p (a b)"), in_=qf[b_][:]),
             reads=[("qf", b_)], writes=[("qT", b_)])
        for dc in range(2):
            P.op("dve", lambda e, dc=dc, b_=b_, h=h: e.tensor_tensor(out=qdT[b_][:, dc, :], in0=qf[b_][:, dc * 128:(dc + 1) * 128],
                                                                   in1=QD[:, h, :], op=ALU.mult),
                 reads=[("qf", b_), ("rQD",)], writes=[("qdT", b_)])
        P.op("pool", lambda e, b_=b_: e.tensor_scalar(out=kT[b_][:].rearrange("p a b -> p (a b)"), in0=kf[b_][:], scalar1=0.0625, scalar2=None, op0=ALU.mult),
             reads=[("kf", b_)], writes=[("kT", b_)])
        if DBG < 2:
            return
        bkt = next_bank(c)
        for k in range(8):
            P.op("pe", lambda e, k=k, bkt=bkt: e.matmul(c.ps[bkt][:, 0:256], lhsT=XT[:, k, tok], rhs=Wk[q][:, k, :], start=(k == 0), stop=(k == 7)),
                 reads=[("Wk", q)] + xtk, writes=[("ps", bkt)])
        P.op("act", lambda e, bkt=bkt, b_=b_: e.activation(out=ktf[b_][:], in_=c.ps[bkt][:, 0:256], func=AF.Copy),
             reads=[("ps", bkt)], writes=[("ktf", b_)])
        P.op("dve", lambda e, b_=b_, h=h: e.tensor_scalar(out=kdec[b_][:], in0=ktf[b_][:], scalar1=KD[:, h:h + 1], scalar2=None, op0=ALU.mult),
             reads=[("ktf", b_), ("rKD",)], writes=[("kdec", b_)])
        bkv = next_bank(c)
        for k in range(8):
            P.op("pe", lambda e, k=k, bkv=bkv: e.matmul(c.ps[bkv][:, :], lhsT=XT[:, k, tok], rhs=Wv[q][:, k, :], start=(k == 0), stop=(k == 7)),
                 reads=[("Wv", q)] + xtk, writes=[("ps", bkv)])
        P.op("act", lambda e, bkv=bkv, b_=b_: e.activation(out=vc[b_][:], in_=c.ps[bkv][:, :], func=AF.Copy), reads=[("ps", bkv)], writes=[("vc", b_)])
        bkg = next_bank(c)
        for k in range(8):
            P.op("pe", lambda e, k=k, bkg=bkg: e.matmul(c.ps[bkg][:, :], lhsT=XT[:, k, tok], rhs=Wg[q][:, k, :], start=(k == 0), stop=(k == 7)),
                 reads=[("Wg", q)] + xtk, writes=[("ps", bkg)])
        P.op("act", lambda e, bkg=bkg, b_=b_: e.activation(out=sgc[b_][:], in_=c.ps[bkg][:, :], func=AF.Sigmoid), reads=[("ps", bkg)], writes=[("sgc", b_)])
        P.op("dve", lambda e, bkg=bkg, b_=b_: e.tensor_tensor(out=sgc[b_][:], in0=c.ps[bkg][:, :], in1=sgc[b_][:], op=ALU.mult),
             reads=[("ps", bkg), ("sgc", b_)], writes=[("sgc", b_)])

    def ret_S(h, i, b_, q):
        xtk = [("XT", i)]
        tok = slice(i * 128, (i + 1) * 128)
        bks = next_bank(c)
        for dc in range(2):
            P.op("pe", lambda e, dc=dc, bks=bks, b_=b_: e.matmul(c.ps[bks][:, 0:128], lhsT=kT[b_][:, dc, :], rhs=qT[b_][:, dc, :], start=(dc == 0), stop=(dc == 1)),
                 reads=[("kT", b_), ("qT", b_)], writes=[("ps", bks)])
        P.op("act", lambda e, bks=bks, b_=b_: e.activation(out=scf[b_][:], in_=c.ps[bks][:, 0:128], func=AF.Copy),
             reads=[("ps", bks)], writes=[("scf", b_)])
        P.op("dve", lambda e, b_=b_, h=h: e.tensor_tensor(out=PT[b_][:], in0=scf[b_][:], in1=DT[:, h, :], op=ALU.mult),
             reads=[("scf", b_), ("rDT",)], writes=[("PT", b_)])
        if i < NT - 1:
            sbn = Sb[i % 2]
            for dc in range(2):
                bkS = next_bank(c)
                P.op("pe", lambda e, dc=dc, bkS=bkS, b_=b_: e.matmul(c.ps[bkS][:, :], lhsT=kdec[b_][:, dc * 128:(dc + 1) * 128], rhs=vc[b_][:], start=True, stop=True),
                     reads=[("kdec", b_), ("vc", b_)], writes=[("ps", bkS)])
                if i == 0:
                    P.op("dve", lambda e, dc=dc, bkS=bkS: e.tensor_copy(out=Sf[:, dc, :], in_=c.ps[bkS][:, :]), reads=[("ps", bkS)], writes=[("Sf", dc)])
                else:
                    P.op("dve", lambda e, dc=dc, bkS=bkS, h=h: e.scalar_tensor_tensor(out=Sf[:, dc, :], in0=Sf[:, dc, :], scalar=CDV[h], in1=c.ps[bkS][:, :],
                                                                                  op0=ALU.mult, op1=ALU.add),
                         reads=[("ps", bkS), ("Sf", dc)], writes=[("Sf", dc)])
                P.op("pool", lambda e, dc=dc, sbn=sbn: e.tensor_copy(out=sbn[:, dc, :], in_=Sf[:, dc, :]), reads=[("Sf", dc)], writes=[("Sb", i % 2)])
        bko = next_bank(c)
        sbp = Sb[(i + 1) % 2]
        P.op("pe", lambda e, bko=bko, b_=b_: e.matmul(c.ps[bko][:, :], lhsT=PT[b_][:], rhs=vc[b_][:], start=True, stop=(i == 0)),
             reads=[("PT", b_), ("vc", b_)], writes=[("ps", bko)])
        if i > 0:
            for dc in range(2):
                P.op("pe", lambda e, dc=dc, bko=bko, b_=b_, sbp=sbp: e.matmul(c.ps[bko][:, :], lhsT=qdT[b_][:, dc, :], rhs=sbp[:, dc, :], start=False, stop=(dc == 1)),
                     reads=[("qdT", b_), ("Sb", (i + 1) % 2)], writes=[("ps", bko)])
        P.op("act", lambda e, bko=bko, b_=b_: e.activation(out=osq[b_][:], in_=c.ps[bko][:, :], func=AF.Square), reads=[("ps", bko)], writes=[("osq", b_)])
        P.op("dve", lambda e, b_=b_: e.reduce_sum(out=ms[b_][:], in_=osq[b_][:], axis=AX.X), reads=[("osq", b_)], writes=[("ms", b_)])
        P.op("act", lambda e, b_=b_: e.activation(out=sd[b_][:], in_=ms[b_][:], func=AF.Sqrt, bias=eps6[:], scale=1.0 / 512.0),
             reads=[("ms", b_), ("eps6",)], writes=[("rsd", b_)])
        P.op("dve", lambda e, b_=b_: e.reciprocal(out=rs[b_][:], in_=sd[b_][:]), reads=[("rsd", b_)], writes=[("rrs", b_)])
        P.op("dve", lambda e, bko=bko, b_=b_: e.scalar_tensor_tensor(out=osq[b_][:], in0=c.ps[bko][:, :], scalar=rs[b_][:, 0:1], in1=sgc[b_][:],
                                                                  op0=ALU.mult, op1=ALU.mult),
             reads=[("ps", bko), ("rrs", b_), ("sgc", b_), ("ms", b_)], writes=[("osq", b_)])
        P.op("pool", lambda e, b_=b_: e.tensor_copy(out=yc[b_][:], in_=osq[b_][:]), reads=[("osq", b_)], writes=[("yc", b_)])

    def ret_Y(h, i, b_, q):
        xtk = [("XT", i)]
        tok = slice(i * 128, (i + 1) * 128)
        bky = next_bank(c)
        psb = c.ps[bky][:].bitcast(BF16)
        for fc in range(4):
            P.op("pe", lambda e, fc=fc, psb=psb, b_=b_: e.transpose(out=psb[:, fc * 128:(fc + 1) * 128], in_=yc[b_][:, fc * 128:(fc + 1) * 128], identity=c.ident_b[:]),
                 reads=[("yc", b_), ("ident_b",)], writes=[("ps", bky)])
        P.op("act", lambda e, psb=psb, b_=b_: e.activation(out=yT[b_][:].rearrange("p a b -> p (a b)"), in_=psb[:, 0:512], func=AF.Copy),
             reads=[("ps", bky)], writes=[("yT", b_)])
        for nh in range(2):
            bkx = next_bank(c)
            for fc in range(4):
                P.op("pe", lambda e, fc=fc, nh=nh, bkx=bkx, b_=b_: e.matmul(c.ps[bkx][:, :], lhsT=yT[b_][:, fc, :], rhs=Wo[q][:, fc, nh * 512:(nh + 1) * 512],
                                                                     start=(fc == 0), stop=(fc == 3)),
                     reads=[("yT", b_), ("Wo", q)], writes=[("ps", bkx)])
            xs = X[:, i, nh * 512:(nh + 1) * 512]
            if h == 0:
                P.op("dve", lambda e, xs=xs, bkx=bkx: e.scalar_tensor_tensor(out=xs, in0=xs, scalar=ALPHA, in1=c.ps[bkx][:, :], op0=ALU.mult, op1=ALU.add),
                     reads=[("ps", bkx), ("X", i)], writes=[("X", i)])
            else:
                P.op("dve", lambda e, xs=xs, bkx=bkx: e.tensor_tensor(out=xs, in0=xs, in1=c.ps[bkx][:, :], op=ALU.add),
                     reads=[("ps", bkx), ("X", i)], writes=[("X", i)])


    load_head(0)
    it = 0
    for h in range(RET_H):
        q = h % 2
        for i in range(NT + 2):
            if i < NT:
                ret_P(h, i, i % 2, q)
            if 1 <= i <= NT:
                ret_S(h, i - 1, (i - 1) % 2, q)
            if i >= 2:
                ret_Y(h, i - 2, (i - 2) % 2, q)
            it += 1
            if i == 4 and h + 1 < RET_H:
                P.op("act", lambda e: e.activation(out=thr[:], in_=yT[0][:, 0, 0:1], func=AF.Copy),
                     reads=[("yT", 0)], writes=[("throttle",)])
                load_head(h + 1)
    P.barrier()
    A.reset(c.phase_base)
    emit_ln_route_epilogue(c, layer)


def emit_ln_route_epilogue(c, layer):
    r = alloc_route(c, layer)
    g1, b1 = load_ln_params(c, layer, 0, "a")
    lt = alloc_ln_tmp(c)
    t = alloc_prep(c)
    for i in range(NT):
        emit_ln(c, lt, c.X[:, i, :], ("X", i), i, g1, b1, "a")
        if i >= 1:
            emit_transpose_tile(c, t, i - 1, write_xt=False)
            emit_route_tile1(c, r, t, i - 1)
        if i >= 2:
            emit_route_tile2(c, r, t, i - 2)
    emit_transpose_tile(c, t, NT - 1, write_xt=False)
    emit_route_tile1(c, r, t, NT - 1)
    emit_route_tile2(c, r, t, NT - 2)
    emit_route_tile2(c, r, t, NT - 1)


ATT_PAT = ((128, 1), (512, 4), (2048, 16))
ATT_BIG = 1.0e9


def att_consts():
    u = np.arange(128)[:, None]
    j = np.arange(256)[None, :]
    steps = u + 128 - j
    valid = (steps >= 0) & (steps <= 128)
    neg = np.where(valid, -steps.astype(np.float32), -ATT_BIG).astype(np.float32)
    return {"c_att_neg": np.ascontiguousarray(neg)}


def phase_attn(c, layer, idx):
    A, P, dr = c.A, c.P, c.dr
    X, XT = c.X, c.XT
    nc = c.nc
    Wd = dr["c_w_in"][idx]
    NEG = A.alloc("aNEG", [128, 256], F32)
    P.dma("sp", lambda e: e.dma_start(out=NEG[:], in_=dr["c_att_neg"]), writes=[("aNEG",)])
    Wq = [A.alloc("aWq", [128, 8, 128], BF16) for _ in range(2)]
    Wk = [A.alloc("aWk", [128, 8, 128], BF16) for _ in range(2)]
    Wv = [A.alloc("aWv", [128, 8, 128], BF16) for _ in range(2)]
    qT = [A.alloc("aqT", [128, S], BF16) for _ in range(2)]
    kT = [A.alloc("akT", [128, S], BF16) for _ in range(2)]
    V = [A.alloc("aV", [128, NT, 128], BF16) for _ in range(2)]
    BI = [A.alloc("aBI", [128, 2, 256], F32) for _ in range(2)]
    Sb = [A.alloc("aSb", [128, 256], F32) for _ in range(4)]
    Pb = [A.alloc("aPb", [128, 256], BF16) for _ in range(4)]
    PT = [A.alloc("aPT", [128, 2, 128], BF16) for _ in range(2)]
    mx = [A.alloc("amx", [128, 1], F32) for _ in range(2)]
    nm = [A.alloc("anm", [128, 1], F32) for _ in range(4)]
    osb = [A.alloc("aosb", [128, 128], F32) for _ in range(2)]
    mst = [A.alloc("amst", [128, 2, 2], F32) for _ in range(4)]

    def load_w(g, hp, q):
        for (dst, s_, nm_) in ((Wq[q], 0, "aWq"), (Wk[q], 1, "aWk"), (Wv[q], 2, "aWv")):
            col0 = ((s_ * 3 + g) * 16 + 2 * hp) * 64
            P.dma("pool", lambda e, dst=dst, col0=col0: e.dma_start(
                out=dst[:], in_=Wd[:, col0:col0 + 128].rearrange("(k p) n -> p k n", p=128)), writes=[(nm_, q)])

    def tokslice(g, ut):
        d = ATT_PAT[g][1]
        nb = (S // d) // 128
        r, b = ut // nb, ut % nb
        st = 128 * b * d + r
        return slice(st, st + 127 * d + 1, d), b

    def proj(g, hp, q):
        for (Wt, dst, nm_) in ((Wq[q], qT[q], "aqT"), (Wk[q], kT[q], "akT")):
            for tb in range(4):
                bk = next_bank(c)
                for k in range(8):
                    P.op("pe", lambda e, k=k, bk=bk, Wt=Wt, tb=tb: e.matmul(c.ps[bk][:, :], lhsT=Wt[:, k, :], rhs=XT[:, k, tb * 512:(tb + 1) * 512],
                                                                      start=(k == 0), stop=(k == 7)),
                         reads=[(nm_.replace("qT", "Wq").replace("kT", "Wk"), q)] + [("XT", i_) for i_ in range(tb * 4, tb * 4 + 4)], writes=[("ps", bk)])
                P.op("act", lambda e, bk=bk, dst=dst, tb=tb: e.activation(out=dst[:, tb * 512:(tb + 1) * 512], in_=c.ps[bk][:, :], func=AF.Copy),
                     reads=[("ps", bk)], writes=[(nm_, q, tb)])
        for ut in range(NT):
            ts_, _ = tokslice(g, ut)
            bk = next_bank(c)
            for k in range(8):
                P.op("pe", lambda e, k=k, bk=bk, ts_=ts_: e.matmul(c.ps[bk][:, 0:128], lhsT=XT[:, k, ts_], rhs=Wv[q][:, k, :], start=(k == 0), stop=(k == 7)),
                     reads=[("aWv", q)] + [("XT", i_) for i_ in range(NT)], writes=[("ps", bk)])
            P.op("act", lambda e, bk=bk, ut=ut: e.activation(out=V[q][:, ut, :], in_=c.ps[bk][:, 0:128], func=AF.Copy),
                 reads=[("ps", bk)], writes=[("aV", q, ut)])
        d = ATT_PAT[g][1]
        for e_ in range(2):
            hh = 2 * hp + e_
            slope = float(2.0 ** (-8.0 * (hh + 1) / 16.0)) * d
            P.op("pool", lambda e, e_=e_, slope=slope: e.tensor_scalar(out=BI[q][:, e_, :], in0=NEG[:], scalar1=slope, scalar2=None, op0=ALU.mult),
                 reads=[("aNEG",)], writes=[("aBI", q, e_)])

    def QK_KEYS(q):
        return [("aqT", q, tb) for tb in range(4)] + [("akT", q, tb) for tb in range(4)]

    NSL = 4

    def stageA(g, hp, q, ut, e_, sl, bo):
        ts_, b = tokslice(g, ut)
        has_prev = b > 0
        pr = slice(e_ * 64, (e_ + 1) * 64)
        lo = 0 if has_prev else 128
        bk = next_bank(c, 6)
        if has_prev:
            tp_, _ = tokslice(g, ut - 1)
            P.op("pe", lambda e: e.matmul(c.ps[bk][:, 0:128], lhsT=qT[q][pr, ts_], rhs=kT[q][pr, tp_], start=True, stop=True),
                 reads=QK_KEYS(q), writes=[("ps", bk)])
        P.op("pe", lambda e: e.matmul(c.ps[bk][:, 128:256], lhsT=qT[q][pr, ts_], rhs=kT[q][pr, ts_], start=True, stop=True),
             reads=QK_KEYS(q), writes=[("ps", bk)])
        P.op("dve", lambda e: e.scalar_tensor_tensor(out=Sb[sl][:, lo:256], in0=c.ps[bk][:, lo:256], scalar=0.125, in1=BI[q][:, e_, lo:256],
                                                    op0=ALU.mult, op1=ALU.add),
             reads=[("ps", bk), ("aBI", q, e_)], writes=[("aSb", sl)])
        P.op("dve", lambda e: e.reduce_max(out=mst[bo][:, e_, 0:1], in_=Sb[sl][:, lo:256], axis=AX.X), reads=[("aSb", sl)], writes=[("amst", bo, e_, 0)])
        P.op("pool", lambda e: e.tensor_scalar(out=nm[sl][:], in0=mst[bo][:, e_, 0:1], scalar1=-1.0, scalar2=None, op0=ALU.mult),
             reads=[("amst", bo, e_, 0)], writes=[("anm", sl)])
        P.op("act", lambda e: e.activation(out=Pb[sl][:, lo:256], in_=Sb[sl][:, lo:256], func=AF.Exp, bias=nm[sl][:], scale=1.0),
             reads=[("aSb", sl), ("anm", sl)], writes=[("aPb", sl)])

    def stageA2(g, hp, q, ut, e_, sl, bo):
        ts_, b = tokslice(g, ut)
        lo = 0 if b > 0 else 128
        P.op("dve", lambda e: e.reduce_sum(out=mst[bo][:, e_, 1:2], in_=Pb[sl][:, lo:256], axis=AX.X),
             reads=[("aPb", sl)], writes=[("amst", bo, e_, 1)])

    def stageB1(g, hp, q, ut, e_, sl, bo):
        ts_, b = tokslice(g, ut)
        has_prev = b > 0
        lo = 0 if has_prev else 128
        halves = (0, 1) if has_prev else (1,)
        pt = PT[sl % 2]
        bkt = next_bank(c, 6)
        psb = c.ps[bkt][:].bitcast(BF16)
        for hf in halves:
            P.op("pe", lambda e, hf=hf: e.transpose(out=psb[:, hf * 128:(hf + 1) * 128], in_=Pb[sl][:, hf * 128:(hf + 1) * 128], identity=c.ident_b[:]),
                 reads=[("aPb", sl), ("ident_b",)], writes=[("ps", bkt)])
        P.op("act", lambda e: e.activation(out=pt[:].rearrange("p a b -> p (a b)")[:, lo:256], in_=psb[:, lo:256], func=AF.Copy),
             reads=[("ps", bkt)], writes=[("aPT", sl % 2)])

    def stageB(g, hp, q, ut, e_, sl, bo, obank):
        ts_, b = tokslice(g, ut)
        has_prev = b > 0
        pr = slice(e_ * 64, (e_ + 1) * 64)
        lo = 0 if has_prev else 128
        halves = (0, 1) if has_prev else (1,)
        pt = PT[sl % 2]
        for n_, hf in enumerate(halves):
            vt = ut - 1 if hf == 0 else ut
            P.op("pe", lambda e, hf=hf, vt=vt, n_=n_: e.matmul(c.ps[obank][:, pr], lhsT=pt[:, hf, :], rhs=V[q][:, vt, pr],
                                                            start=(n_ == 0), stop=(n_ == len(halves) - 1)),
                 reads=[("aPT", sl % 2), ("aV", q, vt)], writes=[("ps", obank)])
        if e_ == 1:
            ob = osb[bo % 2]
            P.op("dve", lambda e: e.tensor_copy(out=ob[:], in_=c.ps[obank][:, 0:128]), reads=[("ps", obank)], writes=[("aosb", bo % 2)])
            P.dma("sp", lambda e: e.dma_start(out=dr["att_o"][g, ts_, hp * 128:(hp + 1) * 128], in_=ob[:]), reads=[("aosb", bo % 2)], writes=[("att_o", g, hp, ut)])
            P.dma("sp", lambda e: e.dma_start(out=dr["att_ms"][g, ts_, 2 * hp:2 * hp + 2, :], in_=mst[bo][:]),
                  reads=[("amst", bo, e2, t2) for e2 in range(2) for t2 in range(2)], writes=[("att_ms", g, hp, ut)])

    import os
    NG_ = int(os.environ.get("ATT_NG", "3"))
    NHP_ = int(os.environ.get("ATT_NHP", "8"))
    LOOK = 3
    combos = [(g, hp) for g in range(NG_) for hp in range(NHP_)]
    load_w(combos[0][0], combos[0][1], 0)
    gcount = 0
    for n, (g, hp) in enumerate(combos):
        q = n % 2
        proj(g, hp, q)
        if n + 1 < len(combos):
            load_w(combos[n + 1][0], combos[n + 1][1], (n + 1) % 2)
        units = [(ut, e_) for ut in range(NT) for e_ in range(2)]
        def args(j):
            ut, e_ = units[j]
            gc = gcount + j
            return (g, hp, q, ut, e_, gc % NSL, (gc // 2) % NSL)
        nu = len(units)
        for j in range(-3, nu):
            if 0 <= j + 3 < nu:
                stageA(*args(j + 3))
            if 0 <= j + 1 < nu:
                stageA2(*args(j + 1))
                stageB1(*args(j + 1))
            if 0 <= j < nu:
                stageB(*args(j), 6 + ((gcount + j) // 2) % 2)
        gcount += len(units)
    P.barrier()
    A.reset(c.phase_base)
    YT = A.alloc("YT", [128, 8, S], BF16)
    mark = A.off
    Og = [[A.alloc("aOg", [128, D], F32) for _ in range(3)] for _ in range(2)]
    MS = [A.alloc("aMS", [128, 3, 16, 2], F32) for _ in range(2)]
    Mx = [A.alloc("aMx", [128, 16], F32) for _ in range(2)]
    Wt_ = [A.alloc("aWt", [128, 3, 16], F32) for _ in range(2)]
    Ws = [A.alloc("aWs", [128, 3, 16], F32) for _ in range(2)]
    Dn = [A.alloc("aDn", [128, 16], F32) for _ in range(2)]
    Yt = [A.alloc("aYt", [128, D], F32) for _ in range(2)]
    Yb = [A.alloc("aYb", [128, D], BF16) for _ in range(2)]

    def merge_tile(i, p):
        for g in range(3):
            P.dma("sp", lambda e, g=g: e.dma_start(out=Og[p][g][:], in_=dr["att_o"][g, i * 128:(i + 1) * 128, :]), writes=[("aOg", p, g)])
        P.dma("sp", lambda e: e.dma_start(out=MS[p][:], in_=dr["att_ms"][:, i * 128:(i + 1) * 128, :, :].rearrange("g p h t -> p g h t")), writes=[("aMS", p)])
        P.op("dve", lambda e: e.tensor_tensor(out=Mx[p][:], in0=MS[p][:, 0, :, 0], in1=MS[p][:, 1, :, 0], op=ALU.max), reads=[("aMS", p)], writes=[("aMx", p)])
        P.op("dve", lambda e: e.tensor_tensor(out=Mx[p][:], in0=Mx[p][:], in1=MS[p][:, 2, :, 0], op=ALU.max), reads=[("aMS", p), ("aMx", p)], writes=[("aMx", p)])
        for g in range(3):
            P.op("dve", lambda e, g=g: e.tensor_tensor(out=Wt_[p][:, g, :], in0=MS[p][:, g, :, 0], in1=Mx[p][:], op=ALU.subtract),
                 reads=[("aMS", p), ("aMx", p)], writes=[("aWt", p, g)])
        P.op("act", lambda e: e.activation(out=Wt_[p][:].rearrange("p a b -> p (a b)"), in_=Wt_[p][:].rearrange("p a b -> p (a b)"), func=AF.Exp), reads=[("aWt", p, g) for g in range(3)], writes=[("aWt", p)])
        P.op("dve", lambda e: e.tensor_tensor(out=Ws[p][:], in0=Wt_[p][:], in1=MS[p][:, :, :, 1], op=ALU.mult), reads=[("aWt", p), ("aMS", p)], writes=[("aWs", p)])
        P.op("dve", lambda e: e.tensor_tensor(out=Dn[p][:], in0=Ws[p][:, 0, :], in1=Ws[p][:, 1, :], op=ALU.add), reads=[("aWs", p)], writes=[("aDn", p)])
        P.op("dve", lambda e: e.tensor_tensor(out=Dn[p][:], in0=Dn[p][:], in1=Ws[p][:, 2, :], op=ALU.add), reads=[("aWs", p), ("aDn", p)], writes=[("aDn", p)])
        P.op("dve", lambda e: e.reciprocal(out=Dn[p][:], in_=Dn[p][:]), reads=[("aDn", p)], writes=[("aDn", p)])
        for g in range(3):
            P.op("dve", lambda e, g=g: e.tensor_tensor(out=Ws[p][:, g, :], in0=Wt_[p][:, g, :], in1=Dn[p][:], op=ALU.mult),
                 reads=[("aWt", p), ("aDn", p), ("aWs", p)], writes=[("aWs", p)])
        for g in range(3):
            eng = "dve" if g != 1 else "pool"
            P.op(eng, lambda e, g=g: e.tensor_tensor(out=Og[p][g][:].rearrange("p (h d) -> p h d", h=16), in0=Og[p][g][:].rearrange("p (h d) -> p h d", h=16),
                                                     in1=Ws[p][:, g, :].unsqueeze(2).to_broadcast([128, 16, 64]), op=ALU.mult),
                 reads=[("aOg", p, g), ("aWs", p)], writes=[("aOg", p, g)])
        P.op("pool", lambda e: e.tensor_tensor(out=Yt[p][:], in0=Og[p][0][:], in1=Og[p][1][:], op=ALU.add), reads=[("aOg", p, 0), ("aOg", p, 1)], writes=[("aYt", p)])
        P.op("dve", lambda e: e.tensor_tensor(out=Yb[p][:], in0=Yt[p][:], in1=Og[p][2][:], op=ALU.add), reads=[("aYt", p), ("aOg", p, 2)], writes=[("aYb", p)])
        for half in range(2):
            bk = next_bank(c)
            psb = c.ps[bk][:].bitcast(BF16)
            for q4 in range(4):
                ch = half * 4 + q4
                P.op("pe", lambda e, ch=ch, q4=q4, psb=psb: e.transpose(out=psb[:, q4 * 128:(q4 + 1) * 128], in_=Yb[p][:, ch * 128:(ch + 1) * 128], identity=c.ident_b[:]),
                     reads=[("aYb", p), ("ident_b",)], writes=[("ps", bk)])
            P.op("act", lambda e, half=half, psb=psb: e.activation(out=YT[:, half * 4:(half + 1) * 4, i * 128:(i + 1) * 128],
                                                                 in_=psb[:, 0:512].rearrange("p (a b) -> p a b", a=4), func=AF.Copy),
                 reads=[("ps", bk)], writes=[("YT", half * 4 + j) for j in range(4)])

    for i in range(NT):
        merge_tile(i, i % 2)
    P.barrier()
    A.reset(mark)
    emit_mixer_epilogue(c, layer, YT, 8, dr["c_w_out"][idx])


WEIGHT_NAMES = ["a_w_in", "a_conv_w", "a_conv_b", "a_w_rgate", "a_b_rgate", "a_w_igate", "a_b_igate", "a_lambda",
                "a_w_out", "b_w_in", "b_w_out", "c_w_in", "c_w_out", "ln_gain", "ln_bias", "moe_w_router",
                "moe_b_router", "moe_w_gu", "moe_b_gu", "moe_w_down", "moe_b_down"]


def make_consts():
    ident = np.eye(128, dtype=np.float32)
    tri = np.triu(np.ones((128, 128), dtype=np.float32), k=1)
    ec = np.tile((np.arange(NE, dtype=np.float32) * CAP)[None, :], (128, 1))
    d = {"c_ident": ident, "c_tri": tri, "c_ec": ec}
    d.update(ret_consts()[0])
    d.update(att_consts())
    return d


def run_plan(plan, x, weights, used=None):
    used = used if used is not None else WEIGHT_NAMES
    ws = {k: weights[k].shape for k in used}
    nc = build(plan, ws)
    consts = make_consts()
    in_maps = []
    for b in range(8):
        m = {"x": np.ascontiguousarray(x[b])}
        for k in used:
            m[k] = weights[k]
        m.update(consts)
        in_maps.append(m)
    res = run_bass_kernel_spmd(nc, in_maps, core_ids=list(range(8)))
    return np.stack([r["out"] for r in res.results], axis=0)


FULL_PLAN = [("prep",),
             ("rglru", 0, 0), ("moe", 0, False),
             ("ret", 1, 0), ("moe", 1, False),
             ("attn", 2, 0), ("moe", 2, False),
             ("rglru", 3, 1), ("moe", 3, False)]


def kernel(**inputs):
    x = np.asarray(inputs["x"], dtype=np.float32)
    weights = {k: np.ascontiguousarray(np.asarray(inputs[k], dtype=np.float32)) for k in WEIGHT_NAMES}
    out = run_plan(FULL_PLAN, x, weights)
    return out.astype(np.float32)
```

```python
import contextlib
import numpy as np
import concourse.bass as bass
import concourse.mybir as mybir
from concourse.bass_utils import run_bass_kernel_spmd

F32 = mybir.dt.float32
BF16 = mybir.dt.bfloat16
I32 = mybir.dt.int32
U8 = mybir.dt.uint8
AF = mybir.ActivationFunctionType
ALU = mybir.AluOpType
AX = mybir.AxisListType

D = 1024
S = 2048
NT = 16
NE = 32
CAP = 384
NST = CAP // 128
DEPTH = 4
ALPHA = (2.0 * DEPTH) ** 0.25
LN_EPS = 1e-5
ENGS = ("pe", "act", "dve", "pool", "sp")


class Op:
    __slots__ = ("eng", "fn", "deps", "is_dma", "sig", "cnt", "sem", "target")

    def __init__(s, eng, fn, is_dma):
        s.eng = eng; s.fn = fn; s.deps = []; s.is_dma = is_dma
        s.sig = False; s.cnt = 0; s.sem = None; s.target = 0


class Prog:
    def __init__(s, nc):
        s.nc = nc
        s.ops = {e: [] for e in ENGS}
        s.last_w = {}
        s.readers = {}
        s.n_dma_sems = {"sp": 16, "act": 8, "pool": 16}
        s.since_barrier = []

    def _add(s, eng, fn, reads, writes, is_dma):
        op = Op(eng, fn, is_dma)
        deps = set()
        for k in reads:
            w = s.last_w.get(k)
            if w is not None:
                deps.add(w)
        for k in writes:
            w = s.last_w.get(k)
            if w is not None and not (w.eng == eng and eng == "pe" and not w.is_dma and not is_dma):
                deps.add(w)
        for k in writes:
            for r in s.readers.get(k, ()):
                if r.eng == eng and not r.is_dma and not is_dma:
                    continue
                deps.add(r)
        op.deps = list(deps)
        for k in reads:
            s.readers.setdefault(k, []).append(op)
        for k in writes:
            s.last_w[k] = op
            s.readers[k] = []
        s.ops[eng].append(op)
        s.since_barrier.append(op)
        return op

    def op(s, eng, fn, reads=(), writes=()):
        return s._add(eng, fn, reads, writes, False)

    def dma(s, eng, fn, reads=(), writes=()):
        return s._add(eng, fn, reads, writes, True)

    def barrier(s):
        prev = s.since_barrier
        s.since_barrier = []
        lastc = {}
        dmas = []
        for op in prev:
            if op.fn is None:
                continue
            if op.is_dma:
                dmas.append(op)
            else:
                lastc[op.eng] = op
        deps = list(lastc.values()) + dmas
        for e in ENGS:
            v = Op(e, None, False)
            v.deps = list(deps)
            s.ops[e].append(v)
        s.last_w = {}
        s.readers = {}

    def emit(s, final_wait_ops=()):
        nc = s.nc
        for e in ENGS:
            for op in s.ops[e]:
                for d in op.deps:
                    if not d.is_dma:
                        d.sig = True
        for e in ENGS:
            c = 0
            for op in s.ops[e]:
                if not op.is_dma and op.sig and op.fn is not None:
                    c += 1
                    op.cnt = c
        stack = contextlib.ExitStack()
        prog_sem = {}
        for e in ("pe", "act", "dve", "pool"):
            prog_sem[e] = stack.enter_context(nc.semaphore("prog_" + e))
        for q in ("sp", "act", "pool"):
            sems = [stack.enter_context(nc.semaphore(f"dq_{q}_{i}")) for i in range(s.n_dma_sems[q])]
            cnts = [0] * len(sems)
            prev = [None] * len(sems)
            i = 0
            for op in s.ops[q]:
                if op.is_dma:
                    j = i % len(sems)
                    i += 1
                    cnts[j] += 16
                    op.sem = sems[j]
                    op.target = cnts[j]
                    if prev[j] is not None:
                        op.deps.append(prev[j])
                    prev[j] = op
        engobj = {"pe": nc.tensor, "act": nc.scalar, "dve": nc.vector, "pool": nc.gpsimd, "sp": nc.sync}
        finals = list(final_wait_ops)

        def run_engine(e):
            eng = engobj[e]
            waited = {}
            for op in s.ops[e]:
                need = {}
                for d in op.deps:
                    if d.is_dma:
                        key = ("d", id(d.sem)); val = d.target; sem = d.sem
                    else:
                        key = ("c", d.eng); val = d.cnt; sem = prog_sem[d.eng]
                    if val > need.get(key, (0, None))[0]:
                        need[key] = (val, sem)
                for key, (val, sem) in need.items():
                    if waited.get(key, 0) >= val:
                        continue
                    waited[key] = val
                    eng.wait_ge(sem, val)
                if op.fn is None:
                    continue
                ins = op.fn(eng)
                if op.is_dma:
                    ins.then_inc(op.sem, 16)
                elif op.sig:
                    ins.then_inc(prog_sem[e], 1)
            if e == "sp":
                for d in finals:
                    eng.wait_ge(d.sem, d.target)

        with stack:
            with nc.Block() as block:
                @block.tensor
                def _(t):
                    run_engine("pe")

                @block.scalar
                def _(t):
                    run_engine("act")

                @block.vector
                def _(t):
                    run_engine("dve")

                @block.gpsimd
                def _(t):
                    run_engine("pool")

                @block.sync
                def _(t):
                    run_engine("sp")


class Ctx:
    pass


class Arena:
    def __init__(s, nc, base, limit):
        s.nc = nc; s.base = base; s.limit = limit; s.off = base; s.n = 0

    def reset(s, off=None):
        s.off = s.base if off is None else off

    def alloc(s, name, shape, dtype):
        sz = int(np.prod(shape[1:])) * mybir.dt.size(dtype)
        sz = (sz + 63) // 64 * 64
        assert s.off + sz <= s.limit, (name, s.off, sz, s.limit)
        s.n += 1
        t = s.nc.alloc_sbuf_tensor_at(f"{name}_{s.n}", list(shape), dtype, offset=s.off)
        s.off += sz
        return t


def build(plan, weights_shapes):
    nc = bass.Bass("TRN2", target_bir_lowering=False)
    c = Ctx()
    c.nc = nc
    P = Prog(nc)
    c.P = P
    dr = {}
    dr["x"] = nc.dram_tensor("x", [S, D], F32, kind="ExternalInput").ap()
    for name, shp in weights_shapes.items():
        dr[name] = nc.dram_tensor(name, list(shp), F32, kind="ExternalInput").ap()
    dr["c_ident"] = nc.dram_tensor("c_ident", [128, 128], F32, kind="ExternalInput").ap()
    dr["c_tri"] = nc.dram_tensor("c_tri", [128, 128], F32, kind="ExternalInput").ap()
    dr["c_ec"] = nc.dram_tensor("c_ec", [128, NE], F32, kind="ExternalInput").ap()
    rc, _ = ret_consts()
    for k_, v_ in rc.items():
        dr[k_] = nc.dram_tensor(k_, list(v_.shape), F32, kind="ExternalInput").ap()
    for k_, v_ in att_consts().items():
        dr[k_] = nc.dram_tensor(k_, list(v_.shape), F32, kind="ExternalInput").ap()
    dr["att_o"] = nc.dram_tensor("att_o_scr", [3, S, D], F32, kind="Internal").ap()
    dr["att_ms"] = nc.dram_tensor("att_ms_scr", [3, S, 16, 2], F32, kind="Internal").ap()
    dr["out"] = nc.dram_tensor("out", [S, D], F32, kind="ExternalOutput").ap()
    dr["xg"] = nc.dram_tensor("xg_scr", [NE * CAP, D], BF16, kind="Internal").ap()
    dr["yy"] = nc.dram_tensor("yy_scr", [NE * CAP, D], F32, kind="Internal").ap()
    c.dr = dr

    slab = nc.alloc_sbuf_tensor("arena_slab", [128, 206 * 1024], U8)
    base = nc.lookup_mloc(slab).addr
    A = Arena(nc, base, base + 206 * 1024)
    c.A = A
    c.X = A.alloc("X", [128, NT, D], F32)
    c.XT = A.alloc("XT", [128, 8, S], BF16)
    c.xt_off = A.off - 8 * S * 2
    c.ident_f = A.alloc("ident_f", [128, 128], F32)
    c.ident_b = A.alloc("ident_b", [128, 128], BF16)
    c.tri_b = A.alloc("tri_b", [128, 128], BF16)
    c.ones_b = A.alloc("ones_b", [128, 128], BF16)
    c.ec = A.alloc("ec", [128, NE], F32)
    c.tri_f = A.alloc("tri_f", [128, 128], F32)
    c.eps_t = A.alloc("eps", [128, 1], F32)
    c.one_t = A.alloc("one", [128, 1], F32)
    c.ADRI = A.alloc("ADRI", [128, NT, 4], I32)
    c.G = A.alloc("G", [128, NT, 4], F32)
    c.phase_base = A.off
    c.ps = [nc.alloc_psum_tensor(f"ps{i}", [128, 512], F32) for i in range(8)]
    c.bank = 0

    X = c.X
    P.dma("sp", lambda e: e.dma_start(out=c.ident_f[:], in_=dr["c_ident"]), writes=[("ident_f",)])
    P.dma("sp", lambda e: e.dma_start(out=c.tri_f[:], in_=dr["c_tri"]), writes=[("tri_f",)])
    P.dma("sp", lambda e: e.dma_start(out=c.ec[:], in_=dr["c_ec"]), writes=[("ec",)])
    P.op("dve", lambda e: e.tensor_copy(out=c.ident_b[:], in_=c.ident_f[:]), reads=[("ident_f",)], writes=[("ident_b",)])
    P.op("dve", lambda e: e.tensor_copy(out=c.tri_b[:], in_=c.tri_f[:]), reads=[("tri_f",)], writes=[("tri_b",)])
    P.op("dve", lambda e: e.memset(c.ones_b[:], 1.0), writes=[("ones_b",)])
    P.op("dve", lambda e: e.memset(c.eps_t[:], LN_EPS), writes=[("eps",)])
    P.op("dve", lambda e: e.memset(c.one_t[:], 1.0), writes=[("one",)])
    for i in range(NT):
        P.dma("sp", lambda e, i=i: e.dma_start(out=X[:, i, :], in_=dr["x"][i * 128:(i + 1) * 128, :]),
              writes=[("X", i)])

    for ph in plan:
        A.reset(c.phase_base)
        if ph[0] == "prep":
            phase_prep(c)
        elif ph[0] == "moe":
            phase_moe(c, ph[1], do_route_prep=ph[2])
        elif ph[0] == "rglru":
            phase_rglru(c, ph[1], ph[2])
        elif ph[0] == "ret":
            phase_ret(c, ph[1], ph[2])
        elif ph[0] == "attn":
            phase_attn(c, ph[1], ph[2])
        else:
            raise ValueError(ph)
        P.barrier()

    outs = []
    for i in range(NT):
        outs.append(P.dma("sp", lambda e, i=i: e.dma_start(out=dr["out"][i * 128:(i + 1) * 128, :], in_=X[:, i, :]),
                          reads=[("X", i)]))
    P.emit(final_wait_ops=outs)
    return nc


def next_bank(c, n=8):
    b = c.bank % n
    c.bank = (c.bank + 1) % n
    return b


def load_ln_params(c, layer, which, tag):
    A, P, dr = c.A, c.P, c.dr
    g = A.alloc("lng" + tag, [128, D], F32)
    b = A.alloc("lnb" + tag, [128, D], F32)
    P.dma("sp", lambda e: e.dma_start(out=g[:], in_=dr["ln_gain"][layer, which, :].partition_broadcast(128)),
          writes=[("lng", tag)])
    P.dma("sp", lambda e: e.dma_start(out=b[:], in_=dr["ln_bias"][layer, which, :].partition_broadcast(128)),
          writes=[("lnb", tag)])
    return g, b


def alloc_ln_tmp(c):
    A = c.A
    t = Ctx()
    t.st = [A.alloc("lnst", [128, 2, 6], F32) for _ in range(2)]
    t.mv = [A.alloc("lnmv", [128, 2], F32) for _ in range(2)]
    t.sd = [A.alloc("lnsd", [128, 1], F32) for _ in range(2)]
    t.rs = [A.alloc("lnrs", [128, 1], F32) for _ in range(2)]
    t.xn = [A.alloc("lnxn", [128, D], F32) for _ in range(2)]
    return t


def emit_ln(c, t, Z, zkey, i, g, b, tag):
    P = c.P
    X = c.X
    p = i % 2
    st, mv, sd, rs, xn = t.st[p], t.mv[p], t.sd[p], t.rs[p], t.xn[p]
    for h in range(2):
        P.op("dve", lambda e, h=h: e.bn_stats(out=st[:, h, :], in_=Z[:, h * 512:(h + 1) * 512]),
             reads=[zkey], writes=[("lnst", p, h)])
    P.op("dve", lambda e: e.bn_aggr(out=mv[:], in_=st[:].rearrange("p a b -> p (a b)")),
         reads=[("lnst", p, 0), ("lnst", p, 1)], writes=[("lnmv", p)])
    P.op("act", lambda e: e.activation(out=sd[:], in_=mv[:, 1:2], func=AF.Sqrt, bias=c.eps_t[:], scale=1.0),
         reads=[("lnmv", p), ("eps",)], writes=[("lnsd", p)])
    P.op("dve", lambda e: e.reciprocal(out=rs[:], in_=sd[:]), reads=[("lnsd", p)], writes=[("lnrs", p)])
    P.op("dve", lambda e: e.tensor_scalar(out=xn[:], in0=Z, scalar1=mv[:, 0:1], scalar2=rs[:, 0:1],
                                          op0=ALU.subtract, op1=ALU.mult),
         reads=[zkey, ("lnmv", p), ("lnrs", p)], writes=[("lnxn", p)])
    P.op("dve", lambda e: e.tensor_tensor(out=xn[:], in0=xn[:], in1=g[:], op=ALU.mult),
         reads=[("lnxn", p), ("lng", tag)], writes=[("lnxn", p)])
    P.op("dve", lambda e: e.tensor_tensor(out=X[:, i, :], in0=xn[:], in1=b[:], op=ALU.add),
         reads=[("lnxn", p), ("lnb", tag)], writes=[("X", i)])


def alloc_prep(c):
    A = c.A
    t = Ctx()
    t.xtf = [A.alloc("xtf", [128, 8, 128], F32) for _ in range(2)]
    return t


def emit_transpose_tile(c, t, i, write_xt=True):
    P = c.P
    X, XT = c.X, c.XT
    p = i % 2
    xtf = t.xtf[p]
    for half in range(2):
        bk = next_bank(c)
        ps = c.ps[bk]
        for q in range(4):
            ch = half * 4 + q
            P.op("pe", lambda e, ch=ch, q=q, ps=ps: e.transpose(out=ps[:, q * 128:(q + 1) * 128],
                                                               in_=X[:, i, ch * 128:(ch + 1) * 128],
                                                               identity=c.ident_f[:]),
                 reads=[("X", i), ("ident_f",)], writes=[("ps", bk)])
        P.op("act", lambda e, half=half, ps=ps: e.activation(
            out=xtf[:, half * 4:(half + 1) * 4, :], in_=ps[:].rearrange("p (a b) -> p a b", a=4), func=AF.Copy),
            reads=[("ps", bk)], writes=[("xtf", p, half)])
        if write_xt:
            P.op("act", lambda e, half=half, ps=ps: e.activation(
                out=XT[:, half * 4:(half + 1) * 4, i * 128:(i + 1) * 128], in_=ps[:].rearrange("p (a b) -> p a b", a=4), func=AF.Copy),
                reads=[("ps", bk)], writes=[("XT", i)])


def phase_prep(c):
    t = alloc_prep(c)
    zt = c.A.alloc("zt", [128, NST, D], BF16)
    c.P.op("pool", lambda e: e.memset(zt[:], 0.0), writes=[("zt",)])
    for e_ in range(NE):
        c.P.dma("sp", lambda e, e_=e_: e.dma_start(out=c.dr["xg"][e_ * CAP:(e_ + 1) * CAP, :].rearrange("(t p) d -> p t d", p=128), in_=zt[:]),
                reads=[("zt",)], writes=[("xg_zero", e_)])
    for i in range(NT):
        emit_transpose_tile(c, t, i)


def alloc_route(c, layer):
    A, P, dr = c.A, c.P, c.dr
    r = Ctx()
    r.wr = A.alloc("wr", [128, 8, NE], F32)
    r.br = A.alloc("br", [128, NE], F32)
    r.L = A.alloc("L", [128, NT, NE], F32)
    r.T8 = A.alloc("T8", [128, NT, 8], F32)
    r.M = A.alloc("M", [128, NT, NE], BF16)
    r.CUM = A.alloc("CUM", [128, NT, NE], BF16)
    r.At = [A.alloc("At", [128, NE], F32) for _ in range(2)]
    r.junk = A.alloc("junk", [128, 4, NE], F32)
    r.ADRF = A.alloc("ADRF", [128, NT, 4], F32)
    r.ADRI = c.ADRI
    r.G = c.G
    r.E4 = A.alloc("E4", [128, NT, 4], F32)
    r.nmx = A.alloc("nmx", [128, NT], F32)
    r.sm = A.alloc("sm", [128, NT], F32)
    r.rsm = A.alloc("rsm", [128, NT], F32)
    r.XB = [A.alloc("XB", [128, D], BF16) for _ in range(2)]
    P.dma("sp", lambda e: e.dma_start(out=r.wr[:], in_=dr["moe_w_router"][layer].rearrange("(k p) n -> p k n", p=128)),
          writes=[("wr",)])
    P.dma("sp", lambda e: e.dma_start(out=r.br[:], in_=dr["moe_b_router"][layer, :].partition_broadcast(128)),
          writes=[("br",)])
    return r


def emit_route_tile1(c, r, t, i):
    P, dr = c.P, c.dr
    X = c.X
    p = i % 2
    xtf = t.xtf[p]
    bk = next_bank(c)
    ps = c.ps[bk]
    for k in range(8):
        P.op("pe", lambda e, k=k: e.matmul(ps[:, 0:NE], lhsT=xtf[:, k, :], rhs=r.wr[:, k, :], start=(k == 0), stop=(k == 7)),
             reads=[("xtf", p, k // 4), ("wr",)], writes=[("ps", bk)])
    P.op("dve", lambda e: e.tensor_tensor(out=r.L[:, i, :], in0=ps[:, 0:NE], in1=r.br[:], op=ALU.add),
         reads=[("ps", bk), ("br",)], writes=[("L", i)])
    P.op("dve", lambda e: e.max(out=r.T8[:, i, :], in_=r.L[:, i, :]), reads=[("L", i)], writes=[("T8", i)])
    P.op("dve", lambda e: e.tensor_scalar(out=r.M[:, i, :], in0=r.L[:, i, :], scalar1=r.T8[:, i, 3:4], scalar2=None,
                                          op0=ALU.is_ge),
         reads=[("L", i), ("T8", i)], writes=[("M", i)])


def emit_route_tile(c, r, t, i):
    emit_route_tile1(c, r, t, i)
    emit_route_tile2(c, r, t, i)


def emit_route_tile2(c, r, t, i):
    P, dr = c.P, c.dr
    X = c.X
    p = i % 2
    bk2 = next_bank(c)
    ps2 = c.ps[bk2]
    P.op("pe", lambda e: e.matmul(ps2[:, 0:NE], lhsT=c.tri_b[:], rhs=r.M[:, i, :], start=True, stop=(i == 0)),
         reads=[("tri_b",), ("M", i)], writes=[("ps", bk2)])
    if i > 0:
        P.op("pe", lambda e: e.matmul(ps2[:, 0:NE], lhsT=c.ones_b[:], rhs=r.CUM[:, i - 1, :], start=False, stop=True),
             reads=[("ones_b",), ("CUM", i - 1)], writes=[("ps", bk2)])
        P.op("pool", lambda e: e.tensor_tensor(out=r.CUM[:, i, :], in0=r.CUM[:, i - 1, :], in1=r.M[:, i, :], op=ALU.add),
             reads=[("CUM", i - 1), ("M", i)], writes=[("CUM", i)])
    else:
        P.op("pool", lambda e: e.tensor_copy(out=r.CUM[:, 0, :], in_=r.M[:, 0, :]), reads=[("M", 0)], writes=[("CUM", 0)])
    At = r.At[p]
    P.op("dve", lambda e: e.tensor_tensor(out=At[:], in0=ps2[:, 0:NE], in1=c.ec[:], op=ALU.add),
         reads=[("ps", bk2), ("ec",)], writes=[("At", p)])
    for k in range(4):
        P.op("dve", lambda e, k=k: e.scalar_tensor_tensor(out=r.junk[:, k, :], in0=r.L[:, i, :], scalar=r.T8[:, i, k:k + 1],
                                                          in1=At[:], op0=ALU.is_equal, op1=ALU.mult),
             reads=[("L", i), ("T8", i), ("At", p)], writes=[("junk", k)])
    P.op("dve", lambda e: e.reduce_sum(out=r.ADRF[:, i, :], in_=r.junk[:], axis=AX.X),
         reads=[("junk", k) for k in range(4)], writes=[("ADRF", i)])
    P.op("dve", lambda e: e.tensor_copy(out=r.ADRI[:, i, :], in_=r.ADRF[:, i, :]),
         reads=[("ADRF", i)], writes=[("ADRI", i)])
    P.op("dve", lambda e: e.tensor_scalar(out=r.nmx[:, i:i + 1], in0=r.T8[:, i, 0:1], scalar1=-1.0, scalar2=None, op0=ALU.mult),
         reads=[("T8", i)], writes=[("nmx", i)])
    P.op("act", lambda e: e.activation(out=r.E4[:, i, :], in_=r.T8[:, i, 0:4], func=AF.Exp, bias=r.nmx[:, i:i + 1], scale=1.0),
         reads=[("T8", i), ("nmx", i)], writes=[("E4", i)])
    P.op("dve", lambda e: e.reduce_sum(out=r.sm[:, i:i + 1], in_=r.E4[:, i, :], axis=AX.X), reads=[("E4", i)], writes=[("sm", i)])
    P.op("dve", lambda e: e.reciprocal(out=r.rsm[:, i:i + 1], in_=r.sm[:, i:i + 1]), reads=[("sm", i)], writes=[("rsm", i)])
    P.op("dve", lambda e: e.tensor_scalar(out=r.G[:, i, :], in0=r.E4[:, i, :], scalar1=r.rsm[:, i:i + 1], scalar2=None, op0=ALU.mult),
         reads=[("E4", i), ("rsm", i)], writes=[("G", i)])
    XB = r.XB[p]
    P.op("act", lambda e: e.activation(out=XB[:], in_=X[:, i, :], func=AF.Copy), reads=[("X", i)], writes=[("XB", p)])
    for k in range(4):
        P.dma("pool", lambda e, k=k: e.indirect_dma_start(
            out=dr["xg"], out_offset=bass.IndirectOffsetOnAxis(ap=r.ADRI[:, i, k:k + 1], axis=0),
            in_=XB[:], in_offset=None),
            reads=[("XB", p), ("ADRI", i)], writes=[("xg_dram", i, k)])


def phase_moe(c, layer, do_route_prep):
    A, P, dr = c.A, c.P, c.dr
    X, XT = c.X, c.XT
    nc = c.nc
    r = alloc_route(c, layer) if do_route_prep else None
    t = alloc_prep(c)
    if do_route_prep:
        for i in range(NT):
            emit_transpose_tile(c, t, i, write_xt=False)
            emit_route_tile(c, r, t, i)
    mark = A.off
    RING = 6 if do_route_prep else 7
    wring = [A.alloc("wring", [128, 8, 512], BF16) for _ in range(RING)]
    bgu = A.alloc("bgu", [128, 16, NE], F32)
    braw = A.alloc("braw", [NE, 2 * D], F32)
    P.dma("sp", lambda e: e.dma_start(out=braw[:], in_=dr["moe_b_gu"][layer]), writes=[("braw",)])
    bkb = next_bank(c)
    for cc in range(16):
        P.op("pe", lambda e, cc=cc: e.transpose(out=c.ps[bkb][:, cc * NE:(cc + 1) * NE], in_=braw[:, cc * 128:(cc + 1) * 128],
                                                identity=c.ident_f[0:NE, 0:NE]),
             reads=[("braw",), ("ident_f",)], writes=[("ps", bkb)])
    P.op("dve", lambda e: e.tensor_copy(out=bgu[:].rearrange("p a b -> p (a b)"), in_=c.ps[bkb][:, :]),
         reads=[("ps", bkb)], writes=[("bgu",)])
    P.op("dve", lambda e: e.tensor_scalar(out=bgu[:, 8:16, :], in0=bgu[:, 8:16, :], scalar1=1.0, scalar2=None, op0=ALU.add),
         reads=[("bgu",)], writes=[("bgu",)])
    bd = [A.alloc("bd", [128, D], F32) for _ in range(2)]
    ytile = [A.alloc("ytile", [128, D], F32) for _ in range(2)]
    gt = [A.alloc("gt", [128, CAP], F32) for _ in range(2)]
    ut = [A.alloc("ut", [128, CAP], F32) for _ in range(2)]
    sg = [A.alloc("sg", [128, CAP], F32) for _ in range(2)]
    A2 = Arena(nc, c.xt_off, c.xt_off + 8 * S * 2)
    A2.n = 1000
    xgtok = [A2.alloc("xgtok", [128, NST, D], BF16) for _ in range(2)]
    xgT = [A2.alloc("xgT", [128, 8, CAP], BF16) for _ in range(2)]
    hT = A2.alloc("hT", [128, 8, CAP], BF16)

    pieces = []
    for e_ in range(NE):
        for pc in (0, 2, 1, 3):
            pieces.append((e_, "gu", pc))
        for pc in (0, 1):
            pieces.append((e_, "dn", pc))
    piece_slot = {}

    def issue_piece(n):
        if n >= len(pieces):
            return
        e_, kind, pc = pieces[n]
        slot = n % RING
        piece_slot[(e_, kind, pc)] = slot
        if kind == "gu":
            src = dr["moe_w_gu"][layer, e_, :, pc * 512:(pc + 1) * 512]
        else:
            src = dr["moe_w_down"][layer, e_, :, pc * 512:(pc + 1) * 512]
        P.dma("pool", lambda e, src=src, slot=slot: e.dma_start(out=wring[slot][:], in_=src.rearrange("(k p) n -> p k n", p=128)),
              writes=[("wring", slot)])

    PRE = RING - 1
    for n in range(PRE):
        issue_piece(n)
    nissued = [PRE]
    def ex_load(e_):
        pe2 = e_ % 2
        P.dma("sp", lambda e, e_=e_: e.dma_start(out=xgtok[e_ % 2][:], in_=dr["xg"][e_ * CAP:(e_ + 1) * CAP, :].rearrange("(t p) d -> p t d", p=128)),
              reads=[("xg_dram", i_, k_) for i_ in range(NT) for k_ in range(4)], writes=[("xgtok", e_ % 2)])
        P.dma("sp", lambda e, e_=e_, pe2=pe2: e.dma_start(out=bd[pe2][:], in_=dr["moe_b_down"][layer, e_, :].partition_broadcast(128)),
              writes=[("bd", pe2)])
        for st in range(NST):
            for half in range(2):
                bk = next_bank(c)
                psb = c.ps[bk][:].bitcast(BF16)
                for q in range(4):
                    ch = half * 4 + q
                    P.op("pe", lambda e, st=st, ch=ch, q=q, psb=psb: e.transpose(
                        out=psb[:, q * 128:(q + 1) * 128], in_=xgtok[pe2][:, st, ch * 128:(ch + 1) * 128], identity=c.ident_b[:]),
                        reads=[("xgtok", pe2), ("ident_b",)], writes=[("ps", bk)])
                P.op("act", lambda e, st=st, half=half, psb=psb, pe2=pe2: e.activation(
                    out=xgT[pe2][:, half * 4:(half + 1) * 4, st * 128:(st + 1) * 128],
                    in_=psb[:, 0:512].rearrange("p (a b) -> p a b", a=4), func=AF.Copy),
                    reads=[("ps", bk)], writes=[("xgT", pe2)])

    def ex_gu(e_):
        pe2 = e_ % 2
        for fc in range(8):
            pcg = fc // 4
            pcu = 2 + fc // 4
            lc = (fc % 4) * 128
            if fc % 4 == 0:
                pass
            sg_ = piece_slot[(e_, "gu", pcg)]
            su_ = piece_slot[(e_, "gu", pcu)]
            bkg = next_bank(c); bku = next_bank(c)
            psg, psu = c.ps[bkg], c.ps[bku]
            for k in range(8):
                P.op("pe", lambda e, k=k, sg_=sg_, lc=lc, psg=psg, pe2=pe2: e.matmul(
                    psg[:, 0:CAP], lhsT=wring[sg_][:, k, lc:lc + 128], rhs=xgT[pe2][:, k, :], start=(k == 0), stop=(k == 7)),
                    reads=[("wring", sg_), ("xgT", pe2)], writes=[("ps", bkg)])
            for k in range(8):
                P.op("pe", lambda e, k=k, su_=su_, lc=lc, psu=psu, pe2=pe2: e.matmul(
                    psu[:, 0:CAP], lhsT=wring[su_][:, k, lc:lc + 128], rhs=xgT[pe2][:, k, :], start=(k == 0), stop=(k == 7)),
                    reads=[("wring", su_), ("xgT", pe2)], writes=[("ps", bku)])
            pp = fc % 2
            P.op("dve", lambda e, psg=psg, fc=fc, pp=pp, e_=e_: e.tensor_scalar(
                out=gt[pp][:], in0=psg[:, 0:CAP], scalar1=bgu[:, fc, e_:e_ + 1], scalar2=7.0, op0=ALU.add, op1=ALU.min),
                reads=[("ps", bkg), ("bgu",)], writes=[("gt", pp)])
            P.op("dve", lambda e, psu=psu, fc=fc, pp=pp, e_=e_: e.tensor_scalar(
                out=ut[pp][:], in0=psu[:, 0:CAP], scalar1=bgu[:, 8 + fc, e_:e_ + 1], scalar2=8.0, op0=ALU.add, op1=ALU.min),
                reads=[("ps", bku), ("bgu",)], writes=[("ut", pp)])
            P.op("act", lambda e, pp=pp: e.activation(out=sg[pp][:], in_=gt[pp][:], func=AF.Silu, scale=1.702),
                 reads=[("gt", pp)], writes=[("sg", pp)])
            P.op("dve", lambda e, pp=pp, fc=fc: e.scalar_tensor_tensor(out=hT[:, fc, :], in0=ut[pp][:], scalar=-6.0, in1=sg[pp][:],
                                                                    op0=ALU.max, op1=ALU.mult),
                 reads=[("ut", pp), ("sg", pp)], writes=[("hT", fc)])
            if fc % 4 == 3:
                issue_piece(nissued[0]); issue_piece(nissued[0] + 1)
                nissued[0] += 2

    def ex_down(e_):
        pe2 = e_ % 2
        for st in range(NST):
            yp = st % 2
            for nh in range(2):
                sd_ = piece_slot[(e_, "dn", nh)]
                bk = next_bank(c)
                psd = c.ps[bk]
                for fc in range(8):
                    P.op("pe", lambda e, fc=fc, st=st, sd_=sd_, psd=psd: e.matmul(
                        psd[:, :], lhsT=hT[:, fc, st * 128:(st + 1) * 128], rhs=wring[sd_][:, fc, :], start=(fc == 0), stop=(fc == 7)),
                        reads=[("hT", fc), ("wring", sd_)], writes=[("ps", bk)])
                P.op("dve", lambda e, nh=nh, yp=yp, psd=psd, pe2=pe2: e.scalar_tensor_tensor(
                    out=ytile[yp][:, nh * 512:(nh + 1) * 512], in0=psd[:, :], scalar=1.0 / 1.702, in1=bd[pe2][:, nh * 512:(nh + 1) * 512],
                    op0=ALU.mult, op1=ALU.add),
                    reads=[("ps", bk), ("bd", pe2)], writes=[("ytile", yp, nh)])
            P.dma("sp", lambda e, e_=e_, st=st, yp=yp: e.dma_start(
                out=dr["yy"][e_ * CAP + st * 128:e_ * CAP + (st + 1) * 128, :], in_=ytile[yp][:]),
                reads=[("ytile", yp, 0), ("ytile", yp, 1)], writes=[("yy_dram", e_, st)])

    ex_load(0)
    for e_ in range(NE):
        ex_gu(e_)
        if e_ + 1 < NE:
            ex_load(e_ + 1)
        ex_down(e_)
        issue_piece(nissued[0]); issue_piece(nissued[0] + 1)
        nissued[0] += 2

    P.barrier()
    A.reset(mark)
    g2, b2 = load_ln_params(c, layer, 1, "b")
    lt = alloc_ln_tmp(c)
    NYK = 3 if do_route_prep else 4
    YK = [[A.alloc("YK", [128, D], F32) for _ in range(4)] for _ in range(NYK)]
    Z = [A.alloc("Z", [128, D], F32) for _ in range(2)]
    t2 = t
    for i in range(NT):
        p = i % 2
        py = i % NYK
        for k in range(4):
            P.dma("pool", lambda e, k=k, py=py, i=i: e.indirect_dma_start(
                out=YK[py][k][:], out_offset=None, in_=dr["yy"],
                in_offset=bass.IndirectOffsetOnAxis(ap=c.ADRI[:, i, k:k + 1], axis=0)),
                reads=[("ADRI", i)], writes=[("YK", py, k)])
        P.op("dve", lambda e, p=p, py=py, i=i: e.tensor_scalar(out=Z[p][:], in0=YK[py][0][:], scalar1=c.G[:, i, 0:1], scalar2=None, op0=ALU.mult),
             reads=[("YK", py, 0), ("G", i)], writes=[("Z", p)])
        for k in range(1, 4):
            P.op("dve", lambda e, p=p, py=py, i=i, k=k: e.scalar_tensor_tensor(
                out=Z[p][:], in0=YK[py][k][:], scalar=c.G[:, i, k:k + 1], in1=Z[p][:], op0=ALU.mult, op1=ALU.add),
                reads=[("YK", py, k), ("G", i), ("Z", p)], writes=[("Z", p)])
        P.op("dve", lambda e, p=p, i=i: e.scalar_tensor_tensor(
            out=Z[p][:], in0=X[:, i, :], scalar=ALPHA, in1=Z[p][:], op0=ALU.mult, op1=ALU.add),
            reads=[("X", i), ("Z", p)], writes=[("Z", p)])
        emit_ln(c, lt, Z[p][:], ("Z", p), i, g2, b2, "b")
        if i >= 1:
            emit_transpose_tile(c, t2, i - 1, write_xt=True)
    emit_transpose_tile(c, t2, NT - 1, write_xt=True)


def emit_mixer_epilogue(c, layer, YT, kc, wout_dram):
    A, P, dr = c.A, c.P, c.dr
    X = c.X
    wout = A.alloc("wout", [128, kc, D], BF16)
    for k2 in range(0, kc, 4):
        P.dma("pool", lambda e, k2=k2: e.dma_start(
            out=wout[:, k2:k2 + 4, :], in_=wout_dram[k2 * 128:(k2 + 4) * 128, :].rearrange("(k p) n -> p k n", p=128)),
            writes=[("wout", k2)])
    r = alloc_route(c, layer)
    g1, b1 = load_ln_params(c, layer, 0, "a")
    lt = alloc_ln_tmp(c)
    t = alloc_prep(c)
    Z = [A.alloc("Z", [128, D], F32) for _ in range(2)]
    for i in range(NT):
        p = i % 2
        for nh in range(2):
            bk = next_bank(c)
            ps = c.ps[bk]
            for k in range(kc):
                P.op("pe", lambda e, k=k, nh=nh, ps=ps, i=i: e.matmul(
                    ps[:, :], lhsT=YT[:, k, i * 128:(i + 1) * 128], rhs=wout[:, k, nh * 512:(nh + 1) * 512],
                    start=(k == 0), stop=(k == kc - 1)),
                    reads=[("YT", k), ("wout", (k // 4) * 4)], writes=[("ps", bk)])
            P.op("dve", lambda e, nh=nh, ps=ps, p=p, i=i: e.scalar_tensor_tensor(
                out=Z[p][:, nh * 512:(nh + 1) * 512], in0=X[:, i, nh * 512:(nh + 1) * 512], scalar=ALPHA, in1=ps[:, :],
                op0=ALU.mult, op1=ALU.add),
                reads=[("X", i), ("ps", bk)], writes=[("Z", p, nh)])
        P.op("dve", lambda e: e.engine_nop(), reads=[("Z", p, 0), ("Z", p, 1)], writes=[("Z", p)])
        emit_ln(c, lt, Z[p][:], ("Z", p), i, g1, b1, "a")
        if i >= 1:
            emit_transpose_tile(c, t, i - 1, write_xt=False)
            emit_route_tile1(c, r, t, i - 1)
        if i >= 2:
            emit_route_tile2(c, r, t, i - 2)
    emit_transpose_tile(c, t, NT - 1, write_xt=False)
    emit_route_tile1(c, r, t, NT - 1)
    emit_route_tile2(c, r, t, NT - 2)
    emit_route_tile2(c, r, t, NT - 1)


def load_small_vecs(c, rows, nvec):
    A, P = c.A, c.P
    braw = A.alloc("svraw", [nvec, D], F32)
    pv = A.alloc("pv", [128, 8, nvec], F32)
    for j, ap in enumerate(rows):
        P.dma("sp", lambda e, j=j, ap=ap: e.dma_start(out=braw[j:j + 1, :], in_=ap.unsqueeze(0)), writes=[("svraw", j)])
    bk = next_bank(c)
    for cc in range(8):
        P.op("pe", lambda e, cc=cc: e.transpose(out=c.ps[bk][:, cc * nvec:(cc + 1) * nvec], in_=braw[:, cc * 128:(cc + 1) * 128],
                                                identity=c.ident_f[0:nvec, 0:nvec]),
             reads=[("svraw", j) for j in range(nvec)] + [("ident_f",)], writes=[("ps", bk)])
    P.op("dve", lambda e: e.tensor_copy(out=pv[:].rearrange("p a b -> p (a b)"), in_=c.ps[bk][:, 0:8 * nvec]),
         reads=[("ps", bk)], writes=[("pv",)])
    return pv


def phase_rglru(c, layer, idx):
    A, P, dr = c.A, c.P, c.dr
    X, XT = c.X, c.XT
    TB = 512
    NTB = S // TB
    YT = A.alloc("YT", [128, 8, S], BF16)
    mark_after_yt = A.off
    pv = load_small_vecs(c, [dr["a_conv_w"][idx, 0], dr["a_conv_w"][idx, 1], dr["a_conv_w"][idx, 2], dr["a_conv_w"][idx, 3],
                             dr["a_conv_b"][idx], dr["a_b_rgate"][idx], dr["a_b_igate"][idx], dr["a_lambda"][idx]], 8)
    cv1 = A.alloc("cv1", [128, 8], F32)
    cv2 = A.alloc("cv2", [128, 8], F32)
    sp_e = A.alloc("sp_e", [128, 8], F32)
    sp_l = A.alloc("sp_l", [128, 8], F32)
    P.op("act", lambda e: e.activation(out=sp_e[:], in_=pv[:, :, 7], func=AF.Exp, scale=-1.0), reads=[("pv",)], writes=[("sp_e",)])
    P.op("act", lambda e: e.activation(out=sp_l[:], in_=sp_e[:], func=AF.Ln, bias=c.one_t[:], scale=1.0),
         reads=[("sp_e",), ("one",)], writes=[("sp_l",)])
    P.op("dve", lambda e: e.tensor_scalar(out=cv1[:], in0=sp_l[:], scalar1=-8.0, scalar2=None, op0=ALU.mult), reads=[("sp_l",)], writes=[("cv1",)])
    P.op("dve", lambda e: e.tensor_scalar(out=cv2[:], in0=sp_l[:], scalar1=-16.0, scalar2=None, op0=ALU.mult), reads=[("sp_l",)], writes=[("cv2",)])
    wg = A.alloc("wgr", [128, 8, 128], BF16)
    wi = A.alloc("wgi", [128, 8, 128], BF16)
    P.dma("pool", lambda e: e.dma_start(out=wg[:], in_=dr["a_w_rgate"][idx].rearrange("h i j -> i h j")), writes=[("wgr",)])
    P.dma("pool", lambda e: e.dma_start(out=wi[:], in_=dr["a_w_igate"][idx].rearrange("h i j -> i h j")), writes=[("wgi",)])
    wrec = [A.alloc("wrec", [128, 8, 128], BF16) for _ in range(2)]
    wgat = [A.alloc("wgat", [128, 8, 128], BF16) for _ in range(2)]
    RECp = [A.alloc("RECp", [128, 3 + S], F32) for _ in range(2)]
    nm = ["GX", "SQ", "SG", "CV", "R", "I", "AA", "OM", "H"]
    T = {n: [A.alloc(n, [128, TB], F32) for _ in range(2)] for n in nm}
    CVb = [A.alloc("CVb", [128, TB], BF16) for _ in range(2)]
    for q in range(2):
        P.op("pool", lambda e, q=q: e.memset(RECp[q][:, 0:3], 0.0), writes=[("RECp", q, -1)])
    def chunk_pre(cc):
        q = cc % 2
        P.dma("pool", lambda e, cc=cc, q=q: e.dma_start(
            out=wrec[q][:], in_=dr["a_w_in"][idx][:, D + cc * 128:D + (cc + 1) * 128].rearrange("(k p) n -> p k n", p=128)),
            writes=[("wrec", q)])
        P.dma("pool", lambda e, cc=cc, q=q: e.dma_start(
            out=wgat[q][:], in_=dr["a_w_in"][idx][:, cc * 128:(cc + 1) * 128].rearrange("(k p) n -> p k n", p=128)),
            writes=[("wgat", q)])
        for tb in range(NTB):
            bk = next_bank(c)
            ps = c.ps[bk]
            for k in range(8):
                P.op("pe", lambda e, k=k, ps=ps, tb=tb, q=q: e.matmul(ps[:, :], lhsT=wrec[q][:, k, :], rhs=XT[:, k, tb * TB:(tb + 1) * TB],
                                                                   start=(k == 0), stop=(k == 7)),
                     reads=[("wrec", q)] + [("XT", i_) for i_ in range(tb * 4, tb * 4 + 4)], writes=[("ps", bk)])
            P.op("act", lambda e, ps=ps, tb=tb, q=q: e.activation(out=RECp[q][:, 3 + tb * TB:3 + (tb + 1) * TB], in_=ps[:, :], func=AF.Copy),
                 reads=[("ps", bk)], writes=[("RECp", q, tb)])

    def blk_G(cc, tb, b_, q):
        t_ = {n: T[n][b_] for n in nm}
        k_ = lambda n: (n, b_)
        bk = next_bank(c)
        ps = c.ps[bk]
        for k in range(8):
            P.op("pe", lambda e, k=k, ps=ps, tb=tb, q=q: e.matmul(ps[:, :], lhsT=wgat[q][:, k, :], rhs=XT[:, k, tb * TB:(tb + 1) * TB],
                                                               start=(k == 0), stop=(k == 7)),
                 reads=[("wgat", q)] + [("XT", i_) for i_ in range(tb * 4, tb * 4 + 4)], writes=[("ps", bk)])
        P.op("act", lambda e, ps=ps, t_=t_: e.activation(out=t_["GX"][:], in_=ps[:, :], func=AF.Copy), reads=[("ps", bk)], writes=[k_("GX")])
        P.op("act", lambda e, ps=ps, t_=t_: e.activation(out=t_["SQ"][:], in_=ps[:, :], func=AF.Square), reads=[("ps", bk)], writes=[k_("SQ")])
        P.op("dve", lambda e, t_=t_: e.tensor_scalar(out=t_["SQ"][:], in0=t_["SQ"][:], scalar1=0.044715, scalar2=1.0, op0=ALU.mult, op1=ALU.add),
             reads=[k_("SQ")], writes=[k_("SQ")])
        P.op("dve", lambda e, t_=t_: e.tensor_tensor(out=t_["SQ"][:], in0=t_["SQ"][:], in1=t_["GX"][:], op=ALU.mult),
             reads=[k_("SQ"), k_("GX")], writes=[k_("SQ")])
        P.op("act", lambda e, t_=t_: e.activation(out=t_["SG"][:], in_=t_["SQ"][:], func=AF.Sigmoid, scale=1.5957691216057308),
             reads=[k_("SQ")], writes=[k_("SG")])
        P.op("pool", lambda e, t_=t_: e.tensor_tensor(out=t_["SG"][:], in0=t_["GX"][:], in1=t_["SG"][:], op=ALU.mult),
             reads=[k_("GX"), k_("SG")], writes=[k_("SG")])

    def blk_R(cc, tb, b_, q):
        t_ = {n: T[n][b_] for n in nm}
        k_ = lambda n: (n, b_)
        rk = [("RECp", q, tb)] + ([("RECp", q, tb - 1)] if tb > 0 else [("RECp", q, -1)])
        P.op("dve", lambda e, t_=t_, tb=tb, q=q, cc=cc: e.tensor_scalar(
            out=t_["CV"][:], in0=RECp[q][:, tb * TB:tb * TB + TB], scalar1=pv[:, cc, 0:1], scalar2=pv[:, cc, 4:5], op0=ALU.mult, op1=ALU.add),
            reads=rk + [("pv",)], writes=[k_("CV")])
        for j in range(1, 4):
            P.op("dve", lambda e, t_=t_, tb=tb, q=q, cc=cc, j=j: e.scalar_tensor_tensor(
                out=t_["CV"][:], in0=RECp[q][:, tb * TB + j:tb * TB + j + TB], scalar=pv[:, cc, j:j + 1], in1=t_["CV"][:], op0=ALU.mult, op1=ALU.add),
                reads=rk + [("pv",), k_("CV")], writes=[k_("CV")])
        P.op("act", lambda e, t_=t_, b_=b_: e.activation(out=CVb[b_][:], in_=t_["CV"][:], func=AF.Copy), reads=[k_("CV")], writes=[("CVb", b_)])
        bkr = next_bank(c); bki = next_bank(c)
        P.op("pe", lambda e, b_=b_, cc=cc, bkr=bkr: e.matmul(c.ps[bkr][:, :], lhsT=wg[:, cc, :], rhs=CVb[b_][:], start=True, stop=True),
             reads=[("wgr",), ("CVb", b_)], writes=[("ps", bkr)])
        P.op("pe", lambda e, b_=b_, cc=cc, bki=bki: e.matmul(c.ps[bki][:, :], lhsT=wi[:, cc, :], rhs=CVb[b_][:], start=True, stop=True),
             reads=[("wgi",), ("CVb", b_)], writes=[("ps", bki)])
        P.op("act", lambda e, t_=t_, cc=cc, bkr=bkr: e.activation(out=t_["R"][:], in_=c.ps[bkr][:, :], func=AF.Sigmoid, bias=pv[:, cc, 5:6], scale=1.0),
             reads=[("ps", bkr), ("pv",)], writes=[k_("R")])
        P.op("act", lambda e, t_=t_, cc=cc, bki=bki: e.activation(out=t_["I"][:], in_=c.ps[bki][:, :], func=AF.Sigmoid, bias=pv[:, cc, 6:7], scale=1.0),
             reads=[("ps", bki), ("pv",)], writes=[k_("I")])
        P.op("act", lambda e, t_=t_, cc=cc: e.activation(out=t_["AA"][:], in_=t_["R"][:], func=AF.Exp, scale=cv1[:, cc:cc + 1]),
             reads=[k_("R"), ("cv1",)], writes=[k_("AA")])
        P.op("act", lambda e, t_=t_, cc=cc: e.activation(out=t_["OM"][:], in_=t_["R"][:], func=AF.Exp, scale=cv2[:, cc:cc + 1]),
             reads=[k_("R"), ("cv2",)], writes=[k_("OM")])
        P.op("dve", lambda e, t_=t_: e.tensor_scalar(out=t_["OM"][:], in0=t_["OM"][:], scalar1=-1.0, scalar2=1.0, op0=ALU.mult, op1=ALU.add),
             reads=[k_("OM")], writes=[k_("OM")])
        P.op("act", lambda e, t_=t_: e.activation(out=t_["OM"][:], in_=t_["OM"][:], func=AF.Sqrt), reads=[k_("OM")], writes=[k_("OM")])
        P.op("pool", lambda e, t_=t_: e.tensor_tensor(out=t_["I"][:], in0=t_["I"][:], in1=t_["CV"][:], op=ALU.mult),
             reads=[k_("I"), k_("CV")], writes=[k_("I")])
        P.op("dve", lambda e, t_=t_: e.tensor_tensor(out=t_["I"][:], in0=t_["I"][:], in1=t_["OM"][:], op=ALU.mult),
             reads=[k_("I"), k_("OM")], writes=[k_("I")])
        if tb == 0:
            P.op("dve", lambda e, t_=t_: e.tensor_tensor_scan(out=t_["H"][:], data0=t_["AA"][:], data1=t_["I"][:], initial=0.0,
                                                              op0=ALU.mult, op1=ALU.add),
                 reads=[k_("AA"), k_("I")], writes=[k_("H")])
        else:
            hp = T["H"][1 - b_]
            P.op("dve", lambda e, t_=t_, hp=hp: e.tensor_tensor_scan(out=t_["H"][:], data0=t_["AA"][:], data1=t_["I"][:],
                                                                     initial=hp[:, TB - 1:TB], op0=ALU.mult, op1=ALU.add),
                 reads=[k_("AA"), k_("I"), ("H", 1 - b_)], writes=[k_("H")])
        P.op("pool", lambda e, t_=t_, cc=cc, tb=tb: e.tensor_tensor(out=YT[:, cc, tb * TB:(tb + 1) * TB], in0=t_["SG"][:], in1=t_["H"][:], op=ALU.mult),
             reads=[k_("SG"), k_("H")], writes=[("YT", cc)])

    blocks = [(cc, tb, (cc * NTB + tb) % 2, cc % 2) for cc in range(8) for tb in range(NTB)]
    chunk_pre(0)
    blk_G(*blocks[0])
    for n_ in range(len(blocks)):
        if n_ + 1 < len(blocks):
            if blocks[n_ + 1][1] == 0:
                chunk_pre(blocks[n_ + 1][0])
            blk_G(*blocks[n_ + 1])
        blk_R(*blocks[n_])
    P.barrier()
    A.reset(mark_after_yt)
    emit_mixer_epilogue(c, layer, YT, 8, dr["a_w_out"][idx])


RET_H = 4
RET_EPS = 1e-6


def ret_consts():
    f32 = np.float32
    log_gamma = np.log1p(-np.exp2(-5.0 - np.arange(RET_H, dtype=f32))).astype(f32)
    pos = np.arange(128, dtype=f32)
    rel = pos[:, None] - pos[None, :]
    intra = np.where(rel >= 0, np.exp(log_gamma[:, None, None] * np.maximum(rel, 0.0)), 0.0).astype(f32)
    dt = np.ascontiguousarray(intra.transpose(0, 2, 1))
    qd = np.exp(log_gamma[:, None] * (pos + 1.0)).astype(f32)
    kd = np.exp(log_gamma[:, None] * (127.0 - pos)).astype(f32)
    cd = np.exp(log_gamma * 128.0).astype(f32)
    return {"c_ret_dt": np.ascontiguousarray(dt.transpose(1, 0, 2)),
            "c_ret_qd": np.ascontiguousarray(np.tile(qd[None], (128, 1, 1))),
            "c_ret_kd": np.ascontiguousarray((kd.T / 16.0).astype(f32)),
            }, [float(v) for v in cd]


def phase_ret(c, layer, idx):
    A, P, dr = c.A, c.P, c.dr
    X, XT = c.X, c.XT
    _, CDV = ret_consts()
    DT = A.alloc("rDT", [128, RET_H, 128], F32)
    QD = A.alloc("rQD", [128, RET_H, 128], F32)
    KD = A.alloc("rKD", [128, RET_H], F32)
    eps6 = A.alloc("eps6", [128, 1], F32)
    P.dma("sp", lambda e: e.dma_start(out=DT[:], in_=dr["c_ret_dt"]), writes=[("rDT",)])
    P.dma("sp", lambda e: e.dma_start(out=QD[:], in_=dr["c_ret_qd"]), writes=[("rQD",)])
    P.dma("sp", lambda e: e.dma_start(out=KD[:], in_=dr["c_ret_kd"]), writes=[("rKD",)])
    P.op("dve", lambda e: e.memset(eps6[:], RET_EPS), writes=[("eps6",)])
    Wq = [A.alloc("Wq", [128, 8, 256], BF16) for _ in range(2)]
    Wk = [A.alloc("Wk", [128, 8, 256], BF16) for _ in range(2)]
    Wv = [A.alloc("Wv", [128, 8, 512], BF16) for _ in range(2)]
    Wg = [A.alloc("Wg", [128, 8, 512], BF16) for _ in range(2)]
    Wo = [A.alloc("Wo", [128, 4, D], BF16) for _ in range(2)]
    Sf = A.alloc("Sf", [128, 2, 512], F32)
    Sb = [A.alloc("Sb", [128, 2, 512], BF16) for _ in range(2)]
    qT = [A.alloc("qT", [128, 2, 128], BF16) for _ in range(2)]
    qdT = [A.alloc("qdT", [128, 2, 128], BF16) for _ in range(2)]
    kT = [A.alloc("kT", [128, 2, 128], BF16) for _ in range(2)]
    kdec = [A.alloc("kdec", [128, 256], BF16) for _ in range(2)]
    vc = [A.alloc("vc", [128, 512], BF16) for _ in range(2)]
    sgc = [A.alloc("sgc", [128, 512], F32) for _ in range(2)]
    PT = [A.alloc("PT", [128, 128], BF16) for _ in range(2)]
    qf = [A.alloc("qf", [128, 256], F32) for _ in range(2)]
    kf = [A.alloc("kf", [128, 256], F32) for _ in range(2)]
    ktf = [A.alloc("ktf", [128, 256], F32) for _ in range(2)]
    scf = [A.alloc("scf", [128, 128], F32) for _ in range(2)]
    osq = [A.alloc("osq", [128, 512], F32) for _ in range(2)]
    ms = [A.alloc("ms", [128, 1], F32) for _ in range(2)]
    sd = [A.alloc("rsd", [128, 1], F32) for _ in range(2)]
    rs = [A.alloc("rrs", [128, 1], F32) for _ in range(2)]
    yc = [A.alloc("yc", [128, 512], BF16) for _ in range(2)]
    yT = [A.alloc("yT", [128, 4, 128], BF16) for _ in range(2)]
    W = dr["b_w_in"][idx]
    QW = 1024
    thr = A.alloc("thr", [128, 1], BF16)

    def load_head(h):
        q = h % 2
        for (dst, col0, n, nm) in ((Wq[q], h * 256, 256, "Wq"), (Wk[q], QW + h * 256, 256, "Wk"),
                                   (Wv[q], 2 * QW + h * 512, 512, "Wv"), (Wg[q], 2 * QW + 2048 + h * 512, 512, "Wg")):
            P.dma("pool", lambda e, dst=dst, col0=col0, n=n: e.dma_start(
                out=dst[:], in_=W[:, col0:col0 + n].rearrange("(k p) n -> p k n", p=128)), reads=[("throttle",)], writes=[(nm, q)])
        P.dma("pool", lambda e, q=q, h=h: e.dma_start(
            out=Wo[q][:], in_=dr["b_w_out"][idx][h * 512:(h + 1) * 512, :].rearrange("(k p) n -> p k n", p=128)), reads=[("throttle",)], writes=[("Wo", q)])

    import os
    DBG = int(os.environ.get("RET_DBG", "99"))
    NH_ = int(os.environ.get("RET_NH", "4"))
    NI_ = int(os.environ.get("RET_NI", "16"))

    def ret_P(h, i, b_, q):
        if h >= NH_ or i >= NI_ or DBG < 1:
            return
        xtk = [("XT", i)]
        tok = slice(i * 128, (i + 1) * 128)
        bkq = next_bank(c); bkk = next_bank(c)
        for dc in range(2):
            for k in range(8):
                P.op("pe", lambda e, dc=dc, k=k, bkq=bkq: e.matmul(c.ps[bkq][:, dc * 128:(dc + 1) * 128], lhsT=Wq[q][:, k, dc * 128:(dc + 1) * 128],
                                                             rhs=XT[:, k, tok], start=(k == 0), stop=(k == 7)),
                     reads=[("Wq", q)] + xtk, writes=[("ps", bkq)])
        for dc in range(2):
            for k in range(8):
                P.op("pe", lambda e, dc=dc, k=k, bkk=bkk: e.matmul(c.ps[bkk][:, dc * 128:(dc + 1) * 128], lhsT=Wk[q][:, k, dc * 128:(dc + 1) * 128],
                                                             rhs=XT[:, k, tok], start=(k == 0), stop=(k == 7)),
                     reads=[("Wk", q)] + xtk, writes=[("ps", bkk)])
        P.op("act", lambda e, bkq=bkq, b_=b_: e.activation(out=qf[b_][:], in_=c.ps[bkq][:, 0:256], func=AF.Copy),
             reads=[("ps", bkq)], writes=[("qf", b_)])
        P.op("act", lambda e, bkk=bkk, b_=b_: e.activation(out=kf[b_][:], in_=c.ps[bkk][:, 0:256], func=AF.Copy),
             reads=[("ps", bkk)], writes=[("kf", b_)])
        P.op("pool", lambda e, b_=b_: e.tensor_copy(out=qT[b_][:].rearrange("p a b -> p (a b)"), in_=qf[b_][:]),
             reads=[("qf", b_)], writes=[("qT", b_)])
        for dc in range(2):
            P.op("dve", lambda e, dc=dc, b_=b_, h=h: e.tensor_tensor(out=qdT[b_][:, dc, :], in0=qf[b_][:, dc * 128:(dc + 1) * 128],
                                                                   in1=QD[:, h, :], op=ALU.mult),
                 reads=[("qf", b_), ("rQD",)], writes=[("qdT", b_)])
        P.op("pool", lambda e, b_=b_: e.tensor_scalar(out=kT[b_][:].rearrange("p a b -> p (a b)"), in0=kf[b_][:], scalar1=0.0625, scalar2=None, op0=ALU.mult),
             reads=[("kf", b_)], writes=[("kT", b_)])
        if DBG < 2:
            return
        bkt = next_bank(c)
        for k in range(8):
            P.op("pe", lambda e, k=k, bkt=bkt: e.matmul(c.ps[bkt][:, 0:256], lhsT=XT[:, k, tok], rhs=Wk[q][:, k, :], start=(k == 0), stop=(k == 7)),
                 reads=[("Wk", q)] + xtk, writes=[("ps", bkt)])
        P.op("act", lambda e, bkt=bkt, b_=b_: e.activation(out=ktf[b_][:], in_=c.ps[bkt][:, 0:256], func=AF.Copy),
             reads=[("ps", bkt)], writes=[("ktf", b_)])
        P.op("dve", lambda e, b_=b_, h=h: e.tensor_scalar(out=kdec[b_][:], in0=ktf[b_][:], scalar1=KD[:, h:h + 1], scalar2=None, op0=ALU.mult),
             reads=[("ktf", b_), ("rKD",)], writes=[("kdec", b_)])
        bkv = next_bank(c)
        for k in range(8):
            P.op("pe", lambda e, k=k, bkv=bkv: e.matmul(c.ps[bkv][:, :], lhsT=XT[:, k, tok], rhs=Wv[q][:, k, :], start=(k == 0), stop=(k == 7)),
                 reads=[("Wv", q)] + xtk, writes=[("ps", bkv)])
        P.op("act", lambda e, bkv=bkv, b_=b_: e.activation(out=vc[b_][:], in_=c.ps[bkv][:, :], func=AF.Copy), reads=[("ps", bkv)], writes=[("vc", b_)])
        bkg = next_bank(c)
        for k in range(8):
            P.op("pe", lambda e, k=k, bkg=bkg: e.matmul(c.ps[bkg][:, :], lhsT=XT[:, k, tok], rhs=Wg[q][:, k, :], start=(k == 0), stop=(k == 7)),
                 reads=[("Wg", q)] + xtk, writes=[("ps", bkg)])
        P.op("act", lambda e, bkg=bkg, b_=b_: e.activation(out=sgc[b_][:], in_=c.ps[bkg][:, :], func=AF.Sigmoid), reads=[("ps", bkg)], writes=[("sgc", b_)])
        P.op("dve", lambda e, bkg=bkg, b_=b_: e.tensor_tensor(out=sgc[b_][:], in0=c.ps[bkg][:, :], in1=sgc[b_][:], op=ALU.mult),
             reads=[("ps", bkg), ("sgc", b_)], writes=[("sgc", b_)])

    def ret_S(h, i, b_, q):
        xtk = [("XT", i)]
        tok = slice(i * 128, (i + 1) * 128)
        bks = next_bank(c)
        for dc in range(2):
            P.op("pe", lambda e, dc=dc, bks=bks, b_=b_: e.matmul(c.ps[bks][:, 0:128], lhsT=kT[b_][:, dc, :], rhs=qT[b_][:, dc, :], start=(dc == 0), stop=(dc == 1)),
                 reads=[("kT", b_), ("qT", b_)], writes=[("ps", bks)])
        P.op("act", lambda e, bks=bks, b_=b_: e.activation(out=scf[b_][:], in_=c.ps[bks][:, 0:128], func=AF.Copy),
             reads=[("ps", bks)], writes=[("scf", b_)])
        P.op("dve", lambda e, b_=b_, h=h: e.tensor_tensor(out=PT[b_][:], in0=scf[b_][:], in1=DT[:, h, :], op=ALU.mult),
             reads=[("scf", b_), ("rDT",)], writes=[("PT", b_)])
        if i < NT - 1:
            sbn = Sb[i % 2]
            for dc in range(2):
                bkS = next_bank(c)
                P.op("pe", lambda e, dc=dc, bkS=bkS, b_=b_: e.matmul(c.ps[bkS][:, :], lhsT=kdec[b_][:, dc * 128:(dc + 1) * 128], rhs=vc[b_][:], start=True, stop=True),
                     reads=[("kdec", b_), ("vc", b_)], writes=[("ps", bkS)])
                if i == 0:
                    P.op("dve", lambda e, dc=dc, bkS=bkS: e.tensor_copy(out=Sf[:, dc, :], in_=c.ps[bkS][:, :]), reads=[("ps", bkS)], writes=[("Sf", dc)])
                else:
                    P.op("dve", lambda e, dc=dc, bkS=bkS, h=h: e.scalar_tensor_tensor(out=Sf[:, dc, :], in0=Sf[:, dc, :], scalar=CDV[h], in1=c.ps[bkS][:, :],
                                                                                  op0=ALU.mult, op1=ALU.add),
                         reads=[("ps", bkS), ("Sf", dc)], writes=[("Sf", dc)])
                P.op("pool", lambda e, dc=dc, sbn=sbn: e.tensor_copy(out=sbn[:, dc, :], in_=Sf[:, dc, :]), reads=[("Sf", dc)], writes=[("Sb", i % 2)])
        bko = next_bank(c)
        sbp = Sb[(i + 1) % 2]
        P.op("pe", lambda e, bko=bko, b_=b_: e.matmul(c.ps[bko][:, :], lhsT=PT[b_][:], rhs=vc[b_][:], start=True, stop=(i == 0)),
             reads=[("PT", b_), ("vc", b_)], writes=[("ps", bko)])
        if i > 0:
            for dc in range(2):
                P.op("pe", lambda e, dc=dc, bko=bko, b_=b_, sbp=sbp: e.matmul(c.ps[bko][:, :], lhsT=qdT[b_][:, dc, :], rhs=sbp[:, dc, :], start=False, stop=(dc == 1)),
                     reads=[("qdT", b_), ("Sb", (i + 1) % 2)], writes=[("ps", bko)])
        P.op("act", lambda e, bko=bko, b_=b_: e.activation(out=osq[b_][:], in_=c.ps[bko][:, :], func=AF.Square), reads=[("ps", bko)], writes=[("osq", b_)])
        P.op("dve", lambda e, b_=b_: e.reduce_sum(out=ms[b_][:], in_=osq[b_][:], axis=AX.X), reads=[("osq", b_)], writes=[("ms", b_)])
        P.op("act", lambda e, b_=b_: e.activation(out=sd[b_][:], in_=ms[b_][:], func=AF.Sqrt, bias=eps6[:], scale=1.0 / 512.0),
             reads=[("ms", b_), ("eps6",)], writes=[("rsd", b_)])
        P.op("dve", lambda e, b_=b_: e.reciprocal(out=rs[b_][:], in_=sd[b_][:]), reads=[("rsd", b_)], writes=[("rrs", b_)])
        P.op("dve", lambda e, bko=bko, b_=b_: e.scalar_tensor_tensor(out=osq[b_][:], in0=c.ps[bko][:, :], scalar=rs[b_][:, 0:1], in1=sgc[b_][:],
                                                                  op0=ALU.mult, op1=ALU.mult),
             reads=[("ps", bko), ("rrs", b_), ("sgc", b_), ("ms", b_)], writes=[("osq", b_)])
        P.op("pool", lambda e, b_=b_: e.tensor_copy(out=yc[b_][:], in_=osq[b_][:]), reads=[("osq", b_)], writes=[("yc", b_)])

    def ret_Y(h, i, b_, q):
        xtk = [("XT", i)]
        tok = slice(i * 128, (i + 1) * 128)
        bky = next_bank(c)
        psb = c.ps[bky][:].bitcast(BF16)
        for fc in range(4):
            P.op("pe", lambda e, fc=fc, psb=psb, b_=b_: e.transpose(out=psb[:, fc * 128:(fc + 1) * 128], in_=yc[b_][:, fc * 128:(fc + 1) * 128], identity=c.ident_b[:]),
                 reads=[("yc", b_), ("ident_b",)], writes=[("ps", bky)])
        P.op("act", lambda e, psb=psb, b_=b_: e.activation(out=yT[b_][:].rearrange("p a b -> p (a b)"), in_=psb[:, 0:512], func=AF.Copy),
             reads=[("ps", bky)], writes=[("yT", b_)])
        for nh in range(2):
            bkx = next_bank(c)
            for fc in range(4):
                P.op("pe", lambda e, fc=fc, nh=nh, bkx=bkx, b_=b_: e.matmul(c.ps[bkx][:, :], lhsT=yT[b_][:, fc, :], rhs=Wo[q][:, fc, nh * 512:(nh + 1) * 512],
                                                                     start=(fc == 0), stop=(fc == 3)),
                     reads=[("yT", b_), ("Wo", q)], writes=[("ps", bkx)])
            xs = X[:, i, nh * 512:(nh + 1) * 512]
            if h == 0:
                P.op("dve", lambda e, xs=xs, bkx=bkx: e.scalar_tensor_tensor(out=xs, in0=xs, scalar=ALPHA, in1=c.ps[bkx][:, :], op0=ALU.mult, op1=ALU.add),
                     reads=[("ps", bkx), ("X", i)], writes=[("X", i)])
            else:
                P.op("dve", lambda e, xs=xs, bkx=bkx: e.tensor_tensor(out=xs, in0=xs, in1=c.ps[bkx][:, :], op=ALU.add),
                     reads=[("ps", bkx), ("X", i)], writes=[("X", i)])


    load_head(0)
    it = 0
    for h in range(RET_H):
        q = h % 2
        for i in range(NT + 2):
            if i < NT:
                ret_P(h, i, i % 2, q)
            if 1 <= i <= NT:
                ret_S(h, i - 1, (i - 1) % 2, q)
            if i >= 2:
                ret_Y(h, i - 2, (i - 2) % 2, q)
            it += 1
            if i == 4 and h + 1 < RET_H:
                P.op("act", lambda e: e.activation(out=thr[:], in_=yT[0][:, 0, 0:1], func=AF.Copy),
                     reads=[("yT", 0)], writes=[("throttle",)])
                load_head(h + 1)
    P.barrier()
    A.reset(c.phase_base)
    emit_ln_route_epilogue(c, layer)


def emit_ln_route_epilogue(c, layer):
    r = alloc_route(c, layer)
    g1, b1 = load_ln_params(c, layer, 0, "a")
    lt = alloc_ln_tmp(c)
    t = alloc_prep(c)
    for i in range(NT):
        emit_ln(c, lt, c.X[:, i, :], ("X", i), i, g1, b1, "a")
        if i >= 1:
            emit_transpose_tile(c, t, i - 1, write_xt=False)
            emit_route_tile1(c, r, t, i - 1)
        if i >= 2:
            emit_route_tile2(c, r, t, i - 2)
    emit_transpose_tile(c, t, NT - 1, write_xt=False)
    emit_route_tile1(c, r, t, NT - 1)
    emit_route_tile2(c, r, t, NT - 2)
    emit_route_tile2(c, r, t, NT - 1)


ATT_PAT = ((128, 1), (512, 4), (2048, 16))
ATT_BIG = 1.0e9


def att_consts():
    u = np.arange(128)[:, None]
    j = np.arange(256)[None, :]
    steps = u + 128 - j
    valid = (steps >= 0) & (steps <= 128)
    neg = np.where(valid, -steps.astype(np.float32), -ATT_BIG).astype(np.float32)
    return {"c_att_neg": np.ascontiguousarray(neg)}


def phase_attn(c, layer, idx):
    A, P, dr = c.A, c.P, c.dr
    X, XT = c.X, c.XT
    nc = c.nc
    Wd = dr["c_w_in"][idx]
    NEG = A.alloc("aNEG", [128, 256], F32)
    P.dma("sp", lambda e: e.dma_start(out=NEG[:], in_=dr["c_att_neg"]), writes=[("aNEG",)])
    Wq = [A.alloc("aWq", [128, 8, 128], BF16) for _ in range(2)]
    Wk = [A.alloc("aWk", [128, 8, 128], BF16) for _ in range(2)]
    Wv = [A.alloc("aWv", [128, 8, 128], BF16) for _ in range(2)]
    qT = [A.alloc("aqT", [128, S], BF16) for _ in range(2)]
    kT = [A.alloc("akT", [128, S], BF16) for _ in range(2)]
    V = [A.alloc("aV", [128, NT, 128], BF16) for _ in range(2)]
    BI = [A.alloc("aBI", [128, 2, 256], F32) for _ in range(2)]
    Sb = [A.alloc("aSb", [128, 256], F32) for _ in range(4)]
    Pb = [A.alloc("aPb", [128, 256], BF16) for _ in range(4)]
    PT = [A.alloc("aPT", [128, 2, 128], BF16) for _ in range(2)]
    mx = [A.alloc("amx", [128, 1], F32) for _ in range(2)]
    nm = [A.alloc("anm", [128, 1], F32) for _ in range(4)]
    osb = [A.alloc("aosb", [128, 128], F32) for _ in range(2)]
    mst = [A.alloc("amst", [128, 2, 2], F32) for _ in range(4)]

    def load_w(g, hp, q):
        for (dst, s_, nm_) in ((Wq[q], 0, "aWq"), (Wk[q], 1, "aWk"), (Wv[q], 2, "aWv")):
            col0 = ((s_ * 3 + g) * 16 + 2 * hp) * 64
            P.dma("pool", lambda e, dst=dst, col0=col0: e.dma_start(
                out=dst[:], in_=Wd[:, col0:col0 + 128].rearrange("(k p) n -> p k n", p=128)), writes=[(nm_, q)])

    def tokslice(g, ut):
        d = ATT_PAT[g][1]
        nb = (S // d) // 128
        r, b = ut // nb, ut % nb
        st = 128 * b * d + r
        return slice(st, st + 127 * d + 1, d), b

    def proj(g, hp, q):
        for (Wt, dst, nm_) in ((Wq[q], qT[q], "aqT"), (Wk[q], kT[q], "akT")):
            for tb in range(4):
                bk = next_bank(c)
                for k in range(8):
                    P.op("pe", lambda e, k=k, bk=bk, Wt=Wt, tb=tb: e.matmul(c.ps[bk][:, :], lhsT=Wt[:, k, :], rhs=XT[:, k, tb * 512:(tb + 1) * 512],
                                                                      start=(k == 0), stop=(k == 7)),
                         reads=[(nm_.replace("qT", "Wq").replace("kT", "Wk"), q)] + [("XT", i_) for i_ in range(tb * 4, tb * 4 + 4)], writes=[("ps", bk)])
                P.op("act", lambda e, bk=bk, dst=dst, tb=tb: e.activation(out=dst[:, tb * 512:(tb + 1) * 512], in_=c.ps[bk][:, :], func=AF.Copy),
                     reads=[("ps", bk)], writes=[(nm_, q, tb)])
        for ut in range(NT):
            ts_, _ = tokslice(g, ut)
            bk = next_bank(c)
            for k in range(8):
                P.op("pe", lambda e, k=k, bk=bk, ts_=ts_: e.matmul(c.ps[bk][:, 0:128], lhsT=XT[:, k, ts_], rhs=Wv[q][:, k, :], start=(k == 0), stop=(k == 7)),
                     reads=[("aWv", q)] + [("XT", i_) for i_ in range(NT)], writes=[("ps", bk)])
            P.op("act", lambda e, bk=bk, ut=ut: e.activation(out=V[q][:, ut, :], in_=c.ps[bk][:, 0:128], func=AF.Copy),
                 reads=[("ps", bk)], writes=[("aV", q, ut)])
        d = ATT_PAT[g][1]
        for e_ in range(2):
            hh = 2 * hp + e_
            slope = float(2.0 ** (-8.0 * (hh + 1) / 16.0)) * d
            P.op("pool", lambda e, e_=e_, slope=slope: e.tensor_scalar(out=BI[q][:, e_, :], in0=NEG[:], scalar1=slope, scalar2=None, op0=ALU.mult),
                 reads=[("aNEG",)], writes=[("aBI", q, e_)])

    def QK_KEYS(q):
        return [("aqT", q, tb) for tb in range(4)] + [("akT", q, tb) for tb in range(4)]

    NSL = 4

    def stageA(g, hp, q, ut, e_, sl, bo):
        ts_, b = tokslice(g, ut)
        has_prev = b > 0
        pr = slice(e_ * 64, (e_ + 1) * 64)
        lo = 0 if has_prev else 128
        bk = next_bank(c, 6)
        if has_prev:
            tp_, _ = tokslice(g, ut - 1)
            P.op("pe", lambda e: e.matmul(c.ps[bk][:, 0:128], lhsT=qT[q][pr, ts_], rhs=kT[q][pr, tp_], start=True, stop=True),
                 reads=QK_KEYS(q), writes=[("ps", bk)])
        P.op("pe", lambda e: e.matmul(c.ps[bk][:, 128:256], lhsT=qT[q][pr, ts_], rhs=kT[q][pr, ts_], start=True, stop=True),
             reads=QK_KEYS(q), writes=[("ps", bk)])
        P.op("dve", lambda e: e.scalar_tensor_tensor(out=Sb[sl][:, lo:256], in0=c.ps[bk][:, lo:256], scalar=0.125, in1=BI[q][:, e_, lo:256],
                                                    op0=ALU.mult, op1=ALU.add),
             reads=[("ps", bk), ("aBI", q, e_)], writes=[("aSb", sl)])
        P.op("dve", lambda e: e.reduce_max(out=mst[bo][:, e_, 0:1], in_=Sb[sl][:, lo:256], axis=AX.X), reads=[("aSb", sl)], writes=[("amst", bo, e_, 0)])
        P.op("pool", lambda e: e.tensor_scalar(out=nm[sl][:], in0=mst[bo][:, e_, 0:1], scalar1=-1.0, scalar2=None, op0=ALU.mult),
             reads=[("amst", bo, e_, 0)], writes=[("anm", sl)])
        P.op("act", lambda e: e.activation(out=Pb[sl][:, lo:256], in_=Sb[sl][:, lo:256], func=AF.Exp, bias=nm[sl][:], scale=1.0),
             reads=[("aSb", sl), ("anm", sl)], writes=[("aPb", sl)])

    def stageA2(g, hp, q, ut, e_, sl, bo):
        ts_, b = tokslice(g, ut)
        lo = 0 if b > 0 else 128
        P.op("dve", lambda e: e.reduce_sum(out=mst[bo][:, e_, 1:2], in_=Pb[sl][:, lo:256], axis=AX.X),
             reads=[("aPb", sl)], writes=[("amst", bo, e_, 1)])

    def stageB1(g, hp, q, ut, e_, sl, bo):
        ts_, b = tokslice(g, ut)
        has_prev = b > 0
        lo = 0 if has_prev else 128
        halves = (0, 1) if has_prev else (1,)
        pt = PT[sl % 2]
        bkt = next_bank(c, 6)
        psb = c.ps[bkt][:].bitcast(BF16)
        for hf in halves:
            P.op("pe", lambda e, hf=hf: e.transpose(out=psb[:, hf * 128:(hf + 1) * 128], in_=Pb[sl][:, hf * 128:(hf + 1) * 128], identity=c.ident_b[:]),
                 reads=[("aPb", sl), ("ident_b",)], writes=[("ps", bkt)])
        P.op("act", lambda e: e.activation(out=pt[:].rearrange("p a b -> p (a b)")[:, lo:256], in_=psb[:, lo:256], func=AF.Copy),
             reads=[("ps", bkt)], writes=[("aPT", sl % 2)])

    def stageB(g, hp, q, ut, e_, sl, bo, obank):
        ts_, b = tokslice(g, ut)
        has_prev = b > 0
        pr = slice(e_ * 64, (e_ + 1) * 64)
        lo = 0 if has_prev else 128
        halves = (0, 1) if has_prev else (1,)
        pt = PT[sl % 2]
        for n_, hf in enumerate(halves):
            vt = ut - 1 if hf == 0 else ut
            P.op("pe", lambda e, hf=hf, vt=vt, n_=n_: e.matmul(c.ps[obank][:, pr], lhsT=pt[:, hf, :], rhs=V[q][:, vt, pr],
                                                            start=(n_ == 0), stop=(n_ == len(halves) - 1)),
                 reads=[("aPT", sl % 2), ("aV", q, vt)], writes=[("ps", obank)])
        if e_ == 1:
            ob = osb[bo % 2]
            P.op("dve", lambda e: e.tensor_copy(out=ob[:], in_=c.ps[obank][:, 0:128]), reads=[("ps", obank)], writes=[("aosb", bo % 2)])
            P.dma("sp", lambda e: e.dma_start(out=dr["att_o"][g, ts_, hp * 128:(hp + 1) * 128], in_=ob[:]), reads=[("aosb", bo % 2)], writes=[("att_o", g, hp, ut)])
            P.dma("sp", lambda e: e.dma_start(out=dr["att_ms"][g, ts_, 2 * hp:2 * hp + 2, :], in_=mst[bo][:]),
                  reads=[("amst", bo, e2, t2) for e2 in range(2) for t2 in range(2)], writes=[("att_ms", g, hp, ut)])

    import os
    NG_ = int(os.environ.get("ATT_NG", "3"))
    NHP_ = int(os.environ.get("ATT_NHP", "8"))
    LOOK = 3
    combos = [(g, hp) for g in range(NG_) for hp in range(NHP_)]
    load_w(combos[0][0], combos[0][1], 0)
    gcount = 0
    for n, (g, hp) in enumerate(combos):
        q = n % 2
        proj(g, hp, q)
        if n + 1 < len(combos):
            load_w(combos[n + 1][0], combos[n + 1][1], (n + 1) % 2)
        units = [(ut, e_) for ut in range(NT) for e_ in range(2)]
        def args(j):
            ut, e_ = units[j]
            gc = gcount + j
            return (g, hp, q, ut, e_, gc % NSL, (gc // 2) % NSL)
        nu = len(units)
        for j in range(-3, nu):
            if 0 <= j + 3 < nu:
                stageA(*args(j + 3))
            if 0 <= j + 1 < nu:
                stageA2(*args(j + 1))
                stageB1(*args(j + 1))
            if 0 <= j < nu:
                stageB(*args(j), 6 + ((gcount + j) // 2) % 2)
        gcount += len(units)
    P.barrier()
    A.reset(c.phase_base)
    YT = A.alloc("YT", [128, 8, S], BF16)
    mark = A.off
    Og = [[A.alloc("aOg", [128, D], F32) for _ in range(3)] for _ in range(2)]
    MS = [A.alloc("aMS", [128, 3, 16, 2], F32) for _ in range(2)]
    Mx = [A.alloc("aMx", [128, 16], F32) for _ in range(2)]
    Wt_ = [A.alloc("aWt", [128, 3, 16], F32) for _ in range(2)]
    Ws = [A.alloc("aWs", [128, 3, 16], F32) for _ in range(2)]
    Dn = [A.alloc("aDn", [128, 16], F32) for _ in range(2)]
    Yt = [A.alloc("aYt", [128, D], F32) for _ in range(2)]
    Yb = [A.alloc("aYb", [128, D], BF16) for _ in range(2)]

    def merge_tile(i, p):
        for g in range(3):
            P.dma("sp", lambda e, g=g: e.dma_start(out=Og[p][g][:], in_=dr["att_o"][g, i * 128:(i + 1) * 128, :]), writes=[("aOg", p, g)])
        P.dma("sp", lambda e: e.dma_start(out=MS[p][:], in_=dr["att_ms"][:, i * 128:(i + 1) * 128, :, :].rearrange("g p h t -> p g h t")), writes=[("aMS", p)])
        P.op("dve", lambda e: e.tensor_tensor(out=Mx[p][:], in0=MS[p][:, 0, :, 0], in1=MS[p][:, 1, :, 0], op=ALU.max), reads=[("aMS", p)], writes=[("aMx", p)])
        P.op("dve", lambda e: e.tensor_tensor(out=Mx[p][:], in0=Mx[p][:], in1=MS[p][:, 2, :, 0], op=ALU.max), reads=[("aMS", p), ("aMx", p)], writes=[("aMx", p)])
        for g in range(3):
            P.op("dve", lambda e, g=g: e.tensor_tensor(out=Wt_[p][:, g, :], in0=MS[p][:, g, :, 0], in1=Mx[p][:], op=ALU.subtract),
                 reads=[("aMS", p), ("aMx", p)], writes=[("aWt", p, g)])
        P.op("act", lambda e: e.activation(out=Wt_[p][:].rearrange("p a b -> p (a b)"), in_=Wt_[p][:].rearrange("p a b -> p (a b)"), func=AF.Exp), reads=[("aWt", p, g) for g in range(3)], writes=[("aWt", p)])
        P.op("dve", lambda e: e.tensor_tensor(out=Ws[p][:], in0=Wt_[p][:], in1=MS[p][:, :, :, 1], op=ALU.mult), reads=[("aWt", p), ("aMS", p)], writes=[("aWs", p)])
        P.op("dve", lambda e: e.tensor_tensor(out=Dn[p][:], in0=Ws[p][:, 0, :], in1=Ws[p][:, 1, :], op=ALU.add), reads=[("aWs", p)], writes=[("aDn", p)])
        P.op("dve", lambda e: e.tensor_tensor(out=Dn[p][:], in0=Dn[p][:], in1=Ws[p][:, 2, :], op=ALU.add), reads=[("aWs", p), ("aDn", p)], writes=[("aDn", p)])
        P.op("dve", lambda e: e.reciprocal(out=Dn[p][:], in_=Dn[p][:]), reads=[("aDn", p)], writes=[("aDn", p)])
        for g in range(3):
            P.op("dve", lambda e, g=g: e.tensor_tensor(out=Ws[p][:, g, :], in0=Wt_[p][:, g, :], in1=Dn[p][:], op=ALU.mult),
                 reads=[("aWt", p), ("aDn", p), ("aWs", p)], writes=[("aWs", p)])
        for g in range(3):
            eng = "dve" if g != 1 else "pool"
            P.op(eng, lambda e, g=g: e.tensor_tensor(out=Og[p][g][:].rearrange("p (h d) -> p h d", h=16), in0=Og[p][g][:].rearrange("p (h d) -> p h d", h=16),
                                                     in1=Ws[p][:, g, :].unsqueeze(2).to_broadcast([128, 16, 64]), op=ALU.mult),
                 reads=[("aOg", p, g), ("aWs", p)], writes=[("aOg", p, g)])
        P.op("pool", lambda e: e.tensor_tensor(out=Yt[p][:], in0=Og[p][0][:], in1=Og[p][1][:], op=ALU.add), reads=[("aOg", p, 0), ("aOg", p, 1)], writes=[("aYt", p)])
        P.op("dve", lambda e: e.tensor_tensor(out=Yb[p][:], in0=Yt[p][:], in1=Og[p][2][:], op=ALU.add), reads=[("aYt", p), ("aOg", p, 2)], writes=[("aYb", p)])
        for half in range(2):
            bk = next_bank(c)
            psb = c.ps[bk][:].bitcast(BF16)
            for q4 in range(4):
                ch = half * 4 + q4
                P.op("pe", lambda e, ch=ch, q4=q4, psb=psb: e.transpose(out=psb[:, q4 * 128:(q4 + 1) * 128], in_=Yb[p][:, ch * 128:(ch + 1) * 128], identity=c.ident_b[:]),
                     reads=[("aYb", p), ("ident_b",)], writes=[("ps", bk)])
            P.op("act", lambda e, half=half, psb=psb: e.activation(out=YT[:, half * 4:(half + 1) * 4, i * 128:(i + 1) * 128],
                                                                 in_=psb[:, 0:512].rearrange("p (a b) -> p a b", a=4), func=AF.Copy),
                 reads=[("ps", bk)], writes=[("YT", half * 4 + j) for j in range(4)])

    for i in range(NT):
        merge_tile(i, i % 2)
    P.barrier()
    A.reset(mark)
    emit_mixer_epilogue(c, layer, YT, 8, dr["c_w_out"][idx])


WEIGHT_NAMES = ["a_w_in", "a_conv_w", "a_conv_b", "a_w_rgate", "a_b_rgate", "a_w_igate", "a_b_igate", "a_lambda",
                "a_w_out", "b_w_in", "b_w_out", "c_w_in", "c_w_out", "ln_gain", "ln_bias", "moe_w_router",
                "moe_b_router", "moe_w_gu", "moe_b_gu", "moe_w_down", "moe_b_down"]


def make_consts():
    ident = np.eye(128, dtype=np.float32)
    tri = np.triu(np.ones((128, 128), dtype=np.float32), k=1)
    ec = np.tile((np.arange(NE, dtype=np.float32) * CAP)[None, :], (128, 1))
    d = {"c_ident": ident, "c_tri": tri, "c_ec": ec}
    d.update(ret_consts()[0])
    d.update(att_consts())
    return d


def run_plan(plan, x, weights, used=None):
    used = used if used is not None else WEIGHT_NAMES
    ws = {k: weights[k].shape for k in used}
    nc = build(plan, ws)
    consts = make_consts()
    in_maps = []
    for b in range(8):
        m = {"x": np.ascontiguousarray(x[b])}
        for k in used:
            m[k] = weights[k]
        m.update(consts)
        in_maps.append(m)
    res = run_bass_kernel_spmd(nc, in_maps, core_ids=list(range(8)))
    return np.stack([r["out"] for r in res.results], axis=0)


FULL_PLAN = [("prep",),
             ("rglru", 0, 0), ("moe", 0, False),
             ("ret", 1, 0), ("moe", 1, False),
             ("attn", 2, 0), ("moe", 2, False),
             ("rglru", 3, 1), ("moe", 3, False)]


def kernel(**inputs):
    x = np.asarray(inputs["x"], dtype=np.float32)
    weights = {k: np.ascontiguousarray(np.asarray(inputs[k], dtype=np.float32)) for k in WEIGHT_NAMES}
    out = run_plan(FULL_PLAN, x, weights)
    return out.astype(np.float32)
```

```python
import contextlib
import numpy as np
import concourse.bass as bass
import concourse.mybir as mybir
from concourse.bass_utils import run_bass_kernel_spmd

F32 = mybir.dt.float32
BF16 = mybir.dt.bfloat16
I32 = mybir.dt.int32
U8 = mybir.dt.uint8
AF = mybir.ActivationFunctionType
ALU = mybir.AluOpType
AX = mybir.AxisListType

D = 1024
S = 2048
NT = 16
NE = 32
CAP = 384
NST = CAP // 128
DEPTH = 4
ALPHA = (2.0 * DEPTH) ** 0.25
LN_EPS = 1e-5
ENGS = ("pe", "act", "dve", "pool", "sp")


class Op:
    __slots__ = ("eng", "fn", "deps", "is_dma", "sig", "cnt", "sem", "target")

    def __init__(s, eng, fn, is_dma):
        s.eng = eng; s.fn = fn; s.deps = []; s.is_dma = is_dma
        s.sig = False; s.cnt = 0; s.sem = None; s.target = 0


class Prog:
    def __init__(s, nc):
        s.nc = nc
        s.ops = {e: [] for e in ENGS}
        s.last_w = {}
        s.readers = {}
        s.n_dma_sems = {"sp": 16, "act": 8, "pool": 16}
        s.since_barrier = []

    def _add(s, eng, fn, reads, writes, is_dma):
        op = Op(eng, fn, is_dma)
        deps = set()
        for k in reads:
            w = s.last_w.get(k)
            if w is not None:
                deps.add(w)
        for k in writes:
            w = s.last_w.get(k)
            if w is not None and not (w.eng == eng and eng == "pe" and not w.is_dma and not is_dma):
                deps.add(w)
        for k in writes:
            for r in s.readers.get(k, ()):
                if r.eng == eng and not r.is_dma and not is_dma:
                    continue
                deps.add(r)
        op.deps = list(deps)
        for k in reads:
            s.readers.setdefault(k, []).append(op)
        for k in writes:
            s.last_w[k] = op
            s.readers[k] = []
        s.ops[eng].append(op)
        s.since_barrier.append(op)
        return op

    def op(s, eng, fn, reads=(), writes=()):
        return s._add(eng, fn, reads, writes, False)

    def dma(s, eng, fn, reads=(), writes=()):
        return s._add(eng, fn, reads, writes, True)

    def barrier(s):
        prev = s.since_barrier
        s.since_barrier = []
        lastc = {}
        dmas = []
        for op in prev:
            if op.fn is None:
                continue
            if op.is_dma:
                dmas.append(op)
            else:
                lastc[op.eng] = op
        deps = list(lastc.values()) + dmas
        for e in ENGS:
            v = Op(e, None, False)
            v.deps = list(deps)
            s.ops[e].append(v)
        s.last_w = {}
        s.readers = {}

    def emit(s, final_wait_ops=()):
        nc = s.nc
        for e in ENGS:
            for op in s.ops[e]:
                for d in op.deps:
                    if not d.is_dma:
                        d.sig = True
        for e in ENGS:
            c = 0
            for op in s.ops[e]:
                if not op.is_dma and op.sig and op.fn is not None:
                    c += 1
                    op.cnt = c
        stack = contextlib.ExitStack()
        prog_sem = {}
        for e in ("pe", "act", "dve", "pool"):
            prog_sem[e] = stack.enter_context(nc.semaphore("prog_" + e))
        for q in ("sp", "act", "pool"):
            sems = [stack.enter_context(nc.semaphore(f"dq_{q}_{i}")) for i in range(s.n_dma_sems[q])]
            cnts = [0] * len(sems)
            prev = [None] * len(sems)
            i = 0
            for op in s.ops[q]:
                if op.is_dma:
                    j = i % len(sems)
                    i += 1
                    cnts[j] += 16
                    op.sem = sems[j]
                    op.target = cnts[j]
                    if prev[j] is not None:
                        op.deps.append(prev[j])
                    prev[j] = op
        engobj = {"pe": nc.tensor, "act": nc.scalar, "dve": nc.vector, "pool": nc.gpsimd, "sp": nc.sync}
        finals = list(final_wait_ops)

        def run_engine(e):
            eng = engobj[e]
            waited = {}
            for op in s.ops[e]:
                need = {}
                for d in op.deps:
                    if d.is_dma:
                        key = ("d", id(d.sem)); val = d.target; sem = d.sem
                    else:
                        key = ("c", d.eng); val = d.cnt; sem = prog_sem[d.eng]
                    if val > need.get(key, (0, None))[0]:
                        need[key] = (val, sem)
                for key, (val, sem) in need.items():
                    if waited.get(key, 0) >= val:
                        continue
                    waited[key] = val
                    eng.wait_ge(sem, val)
                if op.fn is None:
                    continue
                ins = op.fn(eng)
                if op.is_dma:
                    ins.then_inc(op.sem, 16)
                elif op.sig:
                    ins.then_inc(prog_sem[e], 1)
            if e == "sp":
                for d in finals:
                    eng.wait_ge(d.sem, d.target)

        with stack:
            with nc.Block() as block:
                @block.tensor
                def _(t):
                    run_engine("pe")

                @block.scalar
                def _(t):
                    run_engine("act")

                @block.vector
                def _(t):
                    run_engine("dve")

                @block.gpsimd
                def _(t):
                    run_engine("pool")

                @block.sync
                def _(t):
                    run_engine("sp")


class Ctx:
    pass


class Arena:
    def __init__(s, nc, base, limit):
        s.nc = nc; s.base = base; s.limit = limit; s.off = base; s.n = 0

    def reset(s, off=None):
        s.off = s.base if off is None else off

    def alloc(s, name, shape, dtype):
        sz = int(np.prod(shape[1:])) * mybir.dt.size(dtype)
        sz = (sz + 63) // 64 * 64
        assert s.off + sz <= s.limit, (name, s.off, sz, s.limit)
        s.n += 1
        t = s.nc.alloc_sbuf_tensor_at(f"{name}_{s.n}", list(shape), dtype, offset=s.off)
        s.off += sz
        return t


def build(plan, weights_shapes):
    nc = bass.Bass("TRN2", target_bir_lowering=False)
    c = Ctx()
    c.nc = nc
    P = Prog(nc)
    c.P = P
    dr = {}
    dr["x"] = nc.dram_tensor("x", [S, D], F32, kind="ExternalInput").ap()
    for name, shp in weights_shapes.items():
        dr[name] = nc.dram_tensor(name, list(shp), F32, kind="ExternalInput").ap()
    dr["c_ident"] = nc.dram_tensor("c_ident", [128, 128], F32, kind="ExternalInput").ap()
    dr["c_tri"] = nc.dram_tensor("c_tri", [128, 128], F32, kind="ExternalInput").ap()
    dr["c_ec"] = nc.dram_tensor("c_ec", [128, NE], F32, kind="ExternalInput").ap()
    rc, _ = ret_consts()
    for k_, v_ in rc.items():
        dr[k_] = nc.dram_tensor(k_, list(v_.shape), F32, kind="ExternalInput").ap()
    for k_, v_ in att_consts().items():
        dr[k_] = nc.dram_tensor(k_, list(v_.shape), F32, kind="ExternalInput").ap()
    dr["att_o"] = nc.dram_tensor("att_o_scr", [3, S, D], F32, kind="Internal").ap()
    dr["att_ms"] = nc.dram_tensor("att_ms_scr", [3, S, 16, 2], F32, kind="Internal").ap()
    dr["out"] = nc.dram_tensor("out", [S, D], F32, kind="ExternalOutput").ap()
    dr["xg"] = nc.dram_tensor("xg_scr", [NE * CAP, D], BF16, kind="Internal").ap()
    dr["yy"] = nc.dram_tensor("yy_scr", [NE * CAP, D], F32, kind="Internal").ap()
    c.dr = dr

    slab = nc.alloc_sbuf_tensor("arena_slab", [128, 206 * 1024], U8)
    base = nc.lookup_mloc(slab).addr
    A = Arena(nc, base, base + 206 * 1024)
    c.A = A
    c.X = A.alloc("X", [128, NT, D], F32)
    c.XT = A.alloc("XT", [128, 8, S], BF16)
    c.xt_off = A.off - 8 * S * 2
    c.ident_f = A.alloc("ident_f", [128, 128], F32)
    c.ident_b = A.alloc("ident_b", [128, 128], BF16)
    c.tri_b = A.alloc("tri_b", [128, 128], BF16)
    c.ones_b = A.alloc("ones_b", [128, 128], BF16)
    c.ec = A.alloc("ec", [128, NE], F32)
    c.tri_f = A.alloc("tri_f", [128, 128], F32)
    c.eps_t = A.alloc("eps", [128, 1], F32)
    c.one_t = A.alloc("one", [128, 1], F32)
    c.ADRI = A.alloc("ADRI", [128, NT, 4], I32)
    c.G = A.alloc("G", [128, NT, 4], F32)
    c.phase_base = A.off
    c.ps = [nc.alloc_psum_tensor(f"ps{i}", [128, 512], F32) for i in range(8)]
    c.bank = 0

    X = c.X
    P.dma("sp", lambda e: e.dma_start(out=c.ident_f[:], in_=dr["c_ident"]), writes=[("ident_f",)])
    P.dma("sp", lambda e: e.dma_start(out=c.tri_f[:], in_=dr["c_tri"]), writes=[("tri_f",)])
    P.dma("sp", lambda e: e.dma_start(out=c.ec[:], in_=dr["c_ec"]), writes=[("ec",)])
    P.op("dve", lambda e: e.tensor_copy(out=c.ident_b[:], in_=c.ident_f[:]), reads=[("ident_f",)], writes=[("ident_b",)])
    P.op("dve", lambda e: e.tensor_copy(out=c.tri_b[:], in_=c.tri_f[:]), reads=[("tri_f",)], writes=[("tri_b",)])
    P.op("dve", lambda e: e.memset(c.ones_b[:], 1.0), writes=[("ones_b",)])
    P.op("dve", lambda e: e.memset(c.eps_t[:], LN_EPS), writes=[("eps",)])
    P.op("dve", lambda e: e.memset(c.one_t[:], 1.0), writes=[("one",)])
    for i in range(NT):
        P.dma("sp", lambda e, i=i: e.dma_start(out=X[:, i, :], in_=dr["x"][i * 128:(i + 1) * 128, :]),
              writes=[("X", i)])

    for ph in plan:
        A.reset(c.phase_base)
        if ph[0] == "prep":
            phase_prep(c)
        elif ph[0] == "moe":
            phase_moe(c, ph[1], do_route_prep=ph[2])
        elif ph[0] == "rglru":
            phase_rglru(c, ph[1], ph[2])
        elif ph[0] == "ret":
            phase_ret(c, ph[1], ph[2])
        elif ph[0] == "attn":
            phase_attn(c, ph[1], ph[2])
        else:
            raise ValueError(ph)
        P.barrier()

    outs = []
    for i in range(NT):
        outs.append(P.dma("sp", lambda e, i=i: e.dma_start(out=dr["out"][i * 128:(i + 1) * 128, :], in_=X[:, i, :]),
                          reads=[("X", i)]))
    P.emit(final_wait_ops=outs)
    return nc


def next_bank(c, n=8):
    b = c.bank % n
    c.bank = (c.bank + 1) % n
    return b


def load_ln_params(c, layer, which, tag):
    A, P, dr = c.A, c.P, c.dr
    g = A.alloc("lng" + tag, [128, D], F32)
    b = A.alloc("lnb" + tag, [128, D], F32)
    P.dma("sp", lambda e: e.dma_start(out=g[:], in_=dr["ln_gain"][layer, which, :].partition_broadcast(128)),
          writes=[("lng", tag)])
    P.dma("sp", lambda e: e.dma_start(out=b[:], in_=dr["ln_bias"][layer, which, :].partition_broadcast(128)),
          writes=[("lnb", tag)])
    return g, b


def alloc_ln_tmp(c):
    A = c.A
    t = Ctx()
    t.st = [A.alloc("lnst", [128, 2, 6], F32) for _ in range(2)]
    t.mv = [A.alloc("lnmv", [128, 2], F32) for _ in range(2)]
    t.sd = [A.alloc("lnsd", [128, 1], F32) for _ in range(2)]
    t.rs = [A.alloc("lnrs", [128, 1], F32) for _ in range(2)]
    t.xn = [A.alloc("lnxn", [128, D], F32) for _ in range(2)]
    return t


def emit_ln(c, t, Z, zkey, i, g, b, tag):
    P = c.P
    X = c.X
    p = i % 2
    st, mv, sd, rs, xn = t.st[p], t.mv[p], t.sd[p], t.rs[p], t.xn[p]
    for h in range(2):
        P.op("dve", lambda e, h=h: e.bn_stats(out=st[:, h, :], in_=Z[:, h * 512:(h + 1) * 512]),
             reads=[zkey], writes=[("lnst", p, h)])
    P.op("dve", lambda e: e.bn_aggr(out=mv[:], in_=st[:].rearrange("p a b -> p (a b)")),
         reads=[("lnst", p, 0), ("lnst", p, 1)], writes=[("lnmv", p)])
    P.op("act", lambda e: e.activation(out=sd[:], in_=mv[:, 1:2], func=AF.Sqrt, bias=c.eps_t[:], scale=1.0),
         reads=[("lnmv", p), ("eps",)], writes=[("lnsd", p)])
    P.op("dve", lambda e: e.reciprocal(out=rs[:], in_=sd[:]), reads=[("lnsd", p)], writes=[("lnrs", p)])
    P.op("dve", lambda e: e.tensor_scalar(out=xn[:], in0=Z, scalar1=mv[:, 0:1], scalar2=rs[:, 0:1],
                                          op0=ALU.subtract, op1=ALU.mult),
         reads=[zkey, ("lnmv", p), ("lnrs", p)], writes=[("lnxn", p)])
    P.op("dve", lambda e: e.tensor_tensor(out=xn[:], in0=xn[:], in1=g[:], op=ALU.mult),
         reads=[("lnxn", p), ("lng", tag)], writes=[("lnxn", p)])
    P.op("dve", lambda e: e.tensor_tensor(out=X[:, i, :], in0=xn[:], in1=b[:], op=ALU.add),
         reads=[("lnxn", p), ("lnb", tag)], writes=[("X", i)])


def alloc_prep(c):
    A = c.A
    t = Ctx()
    t.xtf = [A.alloc("xtf", [128, 8, 128], F32) for _ in range(2)]
    return t


def emit_transpose_tile(c, t, i, write_xt=True):
    P = c.P
    X, XT = c.X, c.XT
    p = i % 2
    xtf = t.xtf[p]
    for half in range(2):
        bk = next_bank(c)
        ps = c.ps[bk]
        for q in range(4):
            ch = half * 4 + q
            P.op("pe", lambda e, ch=ch, q=q, ps=ps: e.transpose(out=ps[:, q * 128:(q + 1) * 128],
                                                               in_=X[:, i, ch * 128:(ch + 1) * 128],
                                                               identity=c.ident_f[:]),
                 reads=[("X", i), ("ident_f",)], writes=[("ps", bk)])
        P.op("act", lambda e, half=half, ps=ps: e.activation(
            out=xtf[:, half * 4:(half + 1) * 4, :], in_=ps[:].rearrange("p (a b) -> p a b", a=4), func=AF.Copy),
            reads=[("ps", bk)], writes=[("xtf", p, half)])
        if write_xt:
            P.op("act", lambda e, half=half, ps=ps: e.activation(
                out=XT[:, half * 4:(half + 1) * 4, i * 128:(i + 1) * 128], in_=ps[:].rearrange("p (a b) -> p a b", a=4), func=AF.Copy),
                reads=[("ps", bk)], writes=[("XT", i)])


def phase_prep(c):
    t = alloc_prep(c)
    zt = c.A.alloc("zt", [128, NST, D], BF16)
    c.P.op("pool", lambda e: e.memset(zt[:], 0.0), writes=[("zt",)])
    for e_ in range(NE):
        c.P.dma("sp", lambda e, e_=e_: e.dma_start(out=c.dr["xg"][e_ * CAP:(e_ + 1) * CAP, :].rearrange("(t p) d -> p t d", p=128), in_=zt[:]),
                reads=[("zt",)], writes=[("xg_zero", e_)])
    for i in range(NT):
        emit_transpose_tile(c, t, i)


def alloc_route(c, layer):
    A, P, dr = c.A, c.P, c.dr
    r = Ctx()
    r.wr = A.alloc("wr", [128, 8, NE], F32)
    r.br = A.alloc("br", [128, NE], F32)
    r.L = A.alloc("L", [128, NT, NE], F32)
    r.T8 = A.alloc("T8", [128, NT, 8], F32)
    r.M = A.alloc("M", [128, NT, NE], BF16)
    r.CUM = A.alloc("CUM", [128, NT, NE], BF16)
    r.At = [A.alloc("At", [128, NE], F32) for _ in range(2)]
    r.junk = A.alloc("junk", [128, 4, NE], F32)
    r.ADRF = A.alloc("ADRF", [128, NT, 4], F32)
    r.ADRI = c.ADRI
    r.G = c.G
    r.E4 = A.alloc("E4", [128, NT, 4], F32)
    r.nmx = A.alloc("nmx", [128, NT], F32)
    r.sm = A.alloc("sm", [128, NT], F32)
    r.rsm = A.alloc("rsm", [128, NT], F32)
    r.XB = [A.alloc("XB", [128, D], BF16) for _ in range(2)]
    P.dma("sp", lambda e: e.dma_start(out=r.wr[:], in_=dr["moe_w_router"][layer].rearrange("(k p) n -> p k n", p=128)),
          writes=[("wr",)])
    P.dma("sp", lambda e: e.dma_start(out=r.br[:], in_=dr["moe_b_router"][layer, :].partition_broadcast(128)),
          writes=[("br",)])
    return r


def emit_route_tile1(c, r, t, i):
    P, dr = c.P, c.dr
    X = c.X
    p = i % 2
    xtf = t.xtf[p]
    bk = next_bank(c)
    ps = c.ps[bk]
    for k in range(8):
        P.op("pe", lambda e, k=k: e.matmul(ps[:, 0:NE], lhsT=xtf[:, k, :], rhs=r.wr[:, k, :], start=(k == 0), stop=(k == 7)),
             reads=[("xtf", p, k // 4), ("wr",)], writes=[("ps", bk)])
    P.op("dve", lambda e: e.tensor_tensor(out=r.L[:, i, :], in0=ps[:, 0:NE], in1=r.br[:], op=ALU.add),
         reads=[("ps", bk), ("br",)], writes=[("L", i)])
    P.op("dve", lambda e: e.max(out=r.T8[:, i, :], in_=r.L[:, i, :]), reads=[("L", i)], writes=[("T8", i)])
    P.op("dve", lambda e: e.tensor_scalar(out=r.M[:, i, :], in0=r.L[:, i, :], scalar1=r.T8[:, i, 3:4], scalar2=None,
                                          op0=ALU.is_ge),
         reads=[("L", i), ("T8", i)], writes=[("M", i)])


def emit_route_tile(c, r, t, i):
    emit_route_tile1(c, r, t, i)
    emit_route_tile2(c, r, t, i)


def emit_route_tile2(c, r, t, i):
    P, dr = c.P, c.dr
    X = c.X
    p = i % 2
    bk2 = next_bank(c)
    ps2 = c.ps[bk2]
    P.op("pe", lambda e: e.matmul(ps2[:, 0:NE], lhsT=c.tri_b[:], rhs=r.M[:, i, :], start=True, stop=(i == 0)),
         reads=[("tri_b",), ("M", i)], writes=[("ps", bk2)])
    if i > 0:
        P.op("pe", lambda e: e.matmul(ps2[:, 0:NE], lhsT=c.ones_b[:], rhs=r.CUM[:, i - 1, :], start=False, stop=True),
             reads=[("ones_b",), ("CUM", i - 1)], writes=[("ps", bk2)])
        P.op("pool", lambda e: e.tensor_tensor(out=r.CUM[:, i, :], in0=r.CUM[:, i - 1, :], in1=r.M[:, i, :], op=ALU.add),
             reads=[("CUM", i - 1), ("M", i)], writes=[("CUM", i)])
    else:
        P.op("pool", lambda e: e.tensor_copy(out=r.CUM[:, 0, :], in_=r.M[:, 0, :]), reads=[("M", 0)], writes=[("CUM", 0)])
    At = r.At[p]
    P.op("dve", lambda e: e.tensor_tensor(out=At[:], in0=ps2[:, 0:NE], in1=c.ec[:], op=ALU.add),
         reads=[("ps", bk2), ("ec",)], writes=[("At", p)])
    for k in range(4):
        P.op("dve", lambda e, k=k: e.scalar_tensor_tensor(out=r.junk[:, k, :], in0=r.L[:, i, :], scalar=r.T8[:, i, k:k + 1],
                                                          in1=At[:], op0=ALU.is_equal, op1=ALU.mult),
             reads=[("L", i), ("T8", i), ("At", p)], writes=[("junk", k)])
    P.op("dve", lambda e: e.reduce_sum(out=r.ADRF[:, i, :], in_=r.junk[:], axis=AX.X),
         reads=[("junk", k) for k in range(4)], writes=[("ADRF", i)])
    P.op("dve", lambda e: e.tensor_copy(out=r.ADRI[:, i, :], in_=r.ADRF[:, i, :]),
         reads=[("ADRF", i)], writes=[("ADRI", i)])
    P.op("dve", lambda e: e.tensor_scalar(out=r.nmx[:, i:i + 1], in0=r.T8[:, i, 0:1], scalar1=-1.0, scalar2=None, op0=ALU.mult),
         reads=[("T8", i)], writes=[("nmx", i)])
    P.op("act", lambda e: e.activation(out=r.E4[:, i, :], in_=r.T8[:, i, 0:4], func=AF.Exp, bias=r.nmx[:, i:i + 1], scale=1.0),
         reads=[("T8", i), ("nmx", i)], writes=[("E4", i)])
    P.op("dve", lambda e: e.reduce_sum(out=r.sm[:, i:i + 1], in_=r.E4[:, i, :], axis=AX.X), reads=[("E4", i)], writes=[("sm", i)])
    P.op("dve", lambda e: e.reciprocal(out=r.rsm[:, i:i + 1], in_=r.sm[:, i:i + 1]), reads=[("sm", i)], writes=[("rsm", i)])
    P.op("dve", lambda e: e.tensor_scalar(out=r.G[:, i, :], in0=r.E4[:, i, :], scalar1=r.rsm[:, i:i + 1], scalar2=None, op0=ALU.mult),
         reads=[("E4", i), ("rsm", i)], writes=[("G", i)])
    XB = r.XB[p]
    P.op("act", lambda e: e.activation(out=XB[:], in_=X[:, i, :], func=AF.Copy), reads=[("X", i)], writes=[("XB", p)])
    for k in range(4):
        P.dma("pool", lambda e, k=k: e.indirect_dma_start(
            out=dr["xg"], out_offset=bass.IndirectOffsetOnAxis(ap=r.ADRI[:, i, k:k + 1], axis=0),
            in_=XB[:], in_offset=None),
            reads=[("XB", p), ("ADRI", i)], writes=[("xg_dram", i, k)])


def phase_moe(c, layer, do_route_prep):
    A, P, dr = c.A, c.P, c.dr
    X, XT = c.X, c.XT
    nc = c.nc
    r = alloc_route(c, layer) if do_route_prep else None
    t = alloc_prep(c)
    if do_route_prep:
        for i in range(NT):
            emit_transpose_tile(c, t, i, write_xt=False)
            emit_route_tile(c, r, t, i)
    mark = A.off
    RING = 6 if do_route_prep else 7
    wring = [A.alloc("wring", [128, 8, 512], BF16) for _ in range(RING)]
    bgu = A.alloc("bgu", [128, 16, NE], F32)
    braw = A.alloc("braw", [NE, 2 * D], F32)
    P.dma("sp", lambda e: e.dma_start(out=braw[:], in_=dr["moe_b_gu"][layer]), writes=[("braw",)])
    bkb = next_bank(c)
    for cc in range(16):
        P.op("pe", lambda e, cc=cc: e.transpose(out=c.ps[bkb][:, cc * NE:(cc + 1) * NE], in_=braw[:, cc * 128:(cc + 1) * 128],
                                                identity=c.ident_f[0:NE, 0:NE]),
             reads=[("braw",), ("ident_f",)], writes=[("ps", bkb)])
    P.op("dve", lambda e: e.tensor_copy(out=bgu[:].rearrange("p a b -> p (a b)"), in_=c.ps[bkb][:, :]),
         reads=[("ps", bkb)], writes=[("bgu",)])
    P.op("dve", lambda e: e.tensor_scalar(out=bgu[:, 8:16, :], in0=bgu[:, 8:16, :], scalar1=1.0, scalar2=None, op0=ALU.add),
         reads=[("bgu",)], writes=[("bgu",)])
    bd = [A.alloc("bd", [128, D], F32) for _ in range(2)]
    ytile = [A.alloc("ytile", [128, D], F32) for _ in range(2)]
    gt = [A.alloc("gt", [128, CAP], F32) for _ in range(2)]
    ut = [A.alloc("ut", [128, CAP], F32) for _ in range(2)]
    sg = [A.alloc("sg", [128, CAP], F32) for _ in range(2)]
    A2 = Arena(nc, c.xt_off, c.xt_off + 8 * S * 2)
    A2.n = 1000
    xgtok = [A2.alloc("xgtok", [128, NST, D], BF16) for _ in range(2)]
    xgT = [A2.alloc("xgT", [128, 8, CAP], BF16) for _ in range(2)]
    hT = A2.alloc("hT", [128, 8, CAP], BF16)

    pieces = []
    for e_ in range(NE):
        for pc in (0, 2, 1, 3):
            pieces.append((e_, "gu", pc))
        for pc in (0, 1):
            pieces.append((e_, "dn", pc))
    piece_slot = {}

    def issue_piece(n):
        if n >= len(pieces):
            return
        e_, kind, pc = pieces[n]
        slot = n % RING
        piece_slot[(e_, kind, pc)] = slot
        if kind == "gu":
            src = dr["moe_w_gu"][layer, e_, :, pc * 512:(pc + 1) * 512]
        else:
            src = dr["moe_w_down"][layer, e_, :, pc * 512:(pc + 1) * 512]
        P.dma("pool", lambda e, src=src, slot=slot: e.dma_start(out=wring[slot][:], in_=src.rearrange("(k p) n -> p k n", p=128)),
              writes=[("wring", slot)])

    PRE = RING - 1
    for n in range(PRE):
        issue_piece(n)
    nissued = [PRE]
    def ex_load(e_):
        pe2 = e_ % 2
        P.dma("sp", lambda e, e_=e_: e.dma_start(out=xgtok[e_ % 2][:], in_=dr["xg"][e_ * CAP:(e_ + 1) * CAP, :].rearrange("(t p) d -> p t d", p=128)),
              reads=[("xg_dram", i_, k_) for i_ in range(NT) for k_ in range(4)], writes=[("xgtok", e_ % 2)])
        P.dma("sp", lambda e, e_=e_, pe2=pe2: e.dma_start(out=bd[pe2][:], in_=dr["moe_b_down"][layer, e_, :].partition_broadcast(128)),
              writes=[("bd", pe2)])
        for st in range(NST):
            for half in range(2):
                bk = next_bank(c)
                psb = c.ps[bk][:].bitcast(BF16)
                for q in range(4):
                    ch = half * 4 + q
                    P.op("pe", lambda e, st=st, ch=ch, q=q, psb=psb: e.transpose(
                        out=psb[:, q * 128:(q + 1) * 128], in_=xgtok[pe2][:, st, ch * 128:(ch + 1) * 128], identity=c.ident_b[:]),
                        reads=[("xgtok", pe2), ("ident_b",)], writes=[("ps", bk)])
                P.op("act", lambda e, st=st, half=half, psb=psb, pe2=pe2: e.activation(
                    out=xgT[pe2][:, half * 4:(half + 1) * 4, st * 128:(st + 1) * 128],
                    in_=psb[:, 0:512].rearrange("p (a b) -> p a b", a=4), func=AF.Copy),
                    reads=[("ps", bk)], writes=[("xgT", pe2)])

    def ex_gu(e_):
        pe2 = e_ % 2
        for fc in range(8):
            pcg = fc // 4
            pcu = 2 + fc // 4
            lc = (fc % 4) * 128
            if fc % 4 == 0:
                pass
            sg_ = piece_slot[(e_, "gu", pcg)]
            su_ = piece_slot[(e_, "gu", pcu)]
            bkg = next_bank(c); bku = next_bank(c)
            psg, psu = c.ps[bkg], c.ps[bku]
            for k in range(8):
                P.op("pe", lambda e, k=k, sg_=sg_, lc=lc, psg=psg, pe2=pe2: e.matmul(
                    psg[:, 0:CAP], lhsT=wring[sg_][:, k, lc:lc + 128], rhs=xgT[pe2][:, k, :], start=(k == 0), stop=(k == 7)),
                    reads=[("wring", sg_), ("xgT", pe2)], writes=[("ps", bkg)])
            for k in range(8):
                P.op("pe", lambda e, k=k, su_=su_, lc=lc, psu=psu, pe2=pe2: e.matmul(
                    psu[:, 0:CAP], lhsT=wring[su_][:, k, lc:lc + 128], rhs=xgT[pe2][:, k, :], start=(k == 0), stop=(k == 7)),
                    reads=[("wring", su_), ("xgT", pe2)], writes=[("ps", bku)])
            pp = fc % 2
            P.op("dve", lambda e, psg=psg, fc=fc, pp=pp, e_=e_: e.tensor_scalar(
                out=gt[pp][:], in0=psg[:, 0:CAP], scalar1=bgu[:, fc, e_:e_ + 1], scalar2=7.0, op0=ALU.add, op1=ALU.min),
                reads=[("ps", bkg), ("bgu",)], writes=[("gt", pp)])
            P.op("dve", lambda e, psu=psu, fc=fc, pp=pp, e_=e_: e.tensor_scalar(
                out=ut[pp][:], in0=psu[:, 0:CAP], scalar1=bgu[:, 8 + fc, e_:e_ + 1], scalar2=8.0, op0=ALU.add, op1=ALU.min),
                reads=[("ps", bku), ("bgu",)], writes=[("ut", pp)])
            P.op("act", lambda e, pp=pp: e.activation(out=sg[pp][:], in_=gt[pp][:], func=AF.Silu, scale=1.702),
                 reads=[("gt", pp)], writes=[("sg", pp)])
            P.op("dve", lambda e, pp=pp, fc=fc: e.scalar_tensor_tensor(out=hT[:, fc, :], in0=ut[pp][:], scalar=-6.0, in1=sg[pp][:],
                                                                    op0=ALU.max, op1=ALU.mult),
                 reads=[("ut", pp), ("sg", pp)], writes=[("hT", fc)])
            if fc % 4 == 3:
                issue_piece(nissued[0]); issue_piece(nissued[0] + 1)
                nissued[0] += 2

    def ex_down(e_):
        pe2 = e_ % 2
        for st in range(NST):
            yp = st % 2
            for nh in range(2):
                sd_ = piece_slot[(e_, "dn", nh)]
                bk = next_bank(c)
                psd = c.ps[bk]
                for fc in range(8):
                    P.op("pe", lambda e, fc=fc, st=st, sd_=sd_, psd=psd: e.matmul(
                        psd[:, :], lhsT=hT[:, fc, st * 128:(st + 1) * 128], rhs=wring[sd_][:, fc, :], start=(fc == 0), stop=(fc == 7)),
                        reads=[("hT", fc), ("wring", sd_)], writes=[("ps", bk)])
                P.op("dve", lambda e, nh=nh, yp=yp, psd=psd, pe2=pe2: e.scalar_tensor_tensor(
                    out=ytile[yp][:, nh * 512:(nh + 1) * 512], in0=psd[:, :], scalar=1.0 / 1.702, in1=bd[pe2][:, nh * 512:(nh + 1) * 512],
                    op0=ALU.mult, op1=ALU.add),
                    reads=[("ps", bk), ("bd", pe2)], writes=[("ytile", yp, nh)])
            P.dma("sp", lambda e, e_=e_, st=st, yp=yp: e.dma_start(
                out=dr["yy"][e_ * CAP + st * 128:e_ * CAP + (st + 1) * 128, :], in_=ytile[yp][:]),
                reads=[("ytile", yp, 0), ("ytile", yp, 1)], writes=[("yy_dram", e_, st)])

    ex_load(0)
    for e_ in range(NE):
        ex_gu(e_)
        if e_ + 1 < NE:
            ex_load(e_ + 1)
        ex_down(e_)
        issue_piece(nissued[0]); issue_piece(nissued[0] + 1)
        nissued[0] += 2

    P.barrier()
    A.reset(mark)
    g2, b2 = load_ln_params(c, layer, 1, "b")
    lt = alloc_ln_tmp(c)
    NYK = 3 if do_route_prep else 4
    YK = [[A.alloc("YK", [128, D], F32) for _ in range(4)] for _ in range(NYK)]
    Z = [A.alloc("Z", [128, D], F32) for _ in range(2)]
    t2 = t
    for i in range(NT):
        p = i % 2
        py = i % NYK
        for k in range(4):
            P.dma("pool", lambda e, k=k, py=py, i=i: e.indirect_dma_start(
                out=YK[py][k][:], out_offset=None, in_=dr["yy"],
                in_offset=bass.IndirectOffsetOnAxis(ap=c.ADRI[:, i, k:k + 1], axis=0)),
                reads=[("ADRI", i)], writes=[("YK", py, k)])
        P.op("dve", lambda e, p=p, py=py, i=i: e.tensor_scalar(out=Z[p][:], in0=YK[py][0][:], scalar1=c.G[:, i, 0:1], scalar2=None, op0=ALU.mult),
             reads=[("YK", py, 0), ("G", i)], writes=[("Z", p)])
        for k in range(1, 4):
            P.op("dve", lambda e, p=p, py=py, i=i, k=k: e.scalar_tensor_tensor(
                out=Z[p][:], in0=YK[py][k][:], scalar=c.G[:, i, k:k + 1], in1=Z[p][:], op0=ALU.mult, op1=ALU.add),
                reads=[("YK", py, k), ("G", i), ("Z", p)], writes=[("Z", p)])
        P.op("dve", lambda e, p=p, i=i: e.scalar_tensor_tensor(
            out=Z[p][:], in0=X[:, i, :], scalar=ALPHA, in1=Z[p][:], op0=ALU.mult, op1=ALU.add),
            reads=[("X", i), ("Z", p)], writes=[("Z", p)])
        emit_ln(c, lt, Z[p][:], ("Z", p), i, g2, b2, "b")
        if i >= 1:
            emit_transpose_tile(c, t2, i - 1, write_xt=True)
    emit_transpose_tile(c, t2, NT - 1, write_xt=True)


def emit_mixer_epilogue(c, layer, YT, kc, wout_dram):
    A, P, dr = c.A, c.P, c.dr
    X = c.X
    wout = A.alloc("wout", [128, kc, D], BF16)
    for k2 in range(0, kc, 4):
        P.dma("pool", lambda e, k2=k2: e.dma_start(
            out=wout[:, k2:k2 + 4, :], in_=wout_dram[k2 * 128:(k2 + 4) * 128, :].rearrange("(k p) n -> p k n", p=128)),
            writes=[("wout", k2)])
    r = alloc_route(c, layer)
    g1, b1 = load_ln_params(c, layer, 0, "a")
    lt = alloc_ln_tmp(c)
    t = alloc_prep(c)
    Z = [A.alloc("Z", [128, D], F32) for _ in range(2)]
    for i in range(NT):
        p = i % 2
        for nh in range(2):
            bk = next_bank(c)
            ps = c.ps[bk]
            for k in range(kc):
                P.op("pe", lambda e, k=k, nh=nh, ps=ps, i=i: e.matmul(
                    ps[:, :], lhsT=YT[:, k, i * 128:(i + 1) * 128], rhs=wout[:, k, nh * 512:(nh + 1) * 512],
                    start=(k == 0), stop=(k == kc - 1)),
                    reads=[("YT", k), ("wout", (k // 4) * 4)], writes=[("ps", bk)])
            P.op("dve", lambda e, nh=nh, ps=ps, p=p, i=i: e.scalar_tensor_tensor(
                out=Z[p][:, nh * 512:(nh + 1) * 512], in0=X[:, i, nh * 512:(nh + 1) * 512], scalar=ALPHA, in1=ps[:, :],
                op0=ALU.mult, op1=ALU.add),
                reads=[("X", i), ("ps", bk)], writes=[("Z", p, nh)])
        P.op("dve", lambda e: e.engine_nop(), reads=[("Z", p, 0), ("Z", p, 1)], writes=[("Z", p)])
        emit_ln(c, lt, Z[p][:], ("Z", p), i, g1, b1, "a")
        if i >= 1:
            emit_transpose_tile(c, t, i - 1, write_xt=False)
            emit_route_tile1(c, r, t, i - 1)
        if i >= 2:
            emit_route_tile2(c, r, t, i - 2)
    emit_transpose_tile(c, t, NT - 1, write_xt=False)
    emit_route_tile1(c, r, t, NT - 1)
    emit_route_tile2(c, r, t, NT - 2)
    emit_route_tile2(c, r, t, NT - 1)


def load_small_vecs(c, rows, nvec):
    A, P = c.A, c.P
    braw = A.alloc("svraw", [nvec, D], F32)
    pv = A.alloc("pv", [128, 8, nvec], F32)
    for j, ap in enumerate(rows):
        P.dma("sp", lambda e, j=j, ap=ap: e.dma_start(out=braw[j:j + 1, :], in_=ap.unsqueeze(0)), writes=[("svraw", j)])
    bk = next_bank(c)
    for cc in range(8):
        P.op("pe", lambda e, cc=cc: e.transpose(out=c.ps[bk][:, cc * nvec:(cc + 1) * nvec], in_=braw[:, cc * 128:(cc + 1) * 128],
                                                identity=c.ident_f[0:nvec, 0:nvec]),
             reads=[("svraw", j) for j in range(nvec)] + [("ident_f",)], writes=[("ps", bk)])
    P.op("dve", lambda e: e.tensor_copy(out=pv[:].rearrange("p a b -> p (a b)"), in_=c.ps[bk][:, 0:8 * nvec]),
         reads=[("ps", bk)], writes=[("pv",)])
    return pv


def phase_rglru(c, layer, idx):
    A, P, dr = c.A, c.P, c.dr
    X, XT = c.X, c.XT
    TB = 512
    NTB = S // TB
    YT = A.alloc("YT", [128, 8, S], BF16)
    mark_after_yt = A.off
    pv = load_small_vecs(c, [dr["a_conv_w"][idx, 0], dr["a_conv_w"][idx, 1], dr["a_conv_w"][idx, 2], dr["a_conv_w"][idx, 3],
                             dr["a_conv_b"][idx], dr["a_b_rgate"][idx], dr["a_b_igate"][idx], dr["a_lambda"][idx]], 8)
    cv1 = A.alloc("cv1", [128, 8], F32)
    cv2 = A.alloc("cv2", [128, 8], F32)
    sp_e = A.alloc("sp_e", [128, 8], F32)
    sp_l = A.alloc("sp_l", [128, 8], F32)
    P.op("act", lambda e: e.activation(out=sp_e[:], in_=pv[:, :, 7], func=AF.Exp, scale=-1.0), reads=[("pv",)], writes=[("sp_e",)])
    P.op("act", lambda e: e.activation(out=sp_l[:], in_=sp_e[:], func=AF.Ln, bias=c.one_t[:], scale=1.0),
         reads=[("sp_e",), ("one",)], writes=[("sp_l",)])
    P.op("dve", lambda e: e.tensor_scalar(out=cv1[:], in0=sp_l[:], scalar1=-8.0, scalar2=None, op0=ALU.mult), reads=[("sp_l",)], writes=[("cv1",)])
    P.op("dve", lambda e: e.tensor_scalar(out=cv2[:], in0=sp_l[:], scalar1=-16.0, scalar2=None, op0=ALU.mult), reads=[("sp_l",)], writes=[("cv2",)])
    wg = A.alloc("wgr", [128, 8, 128], BF16)
    wi = A.alloc("wgi", [128, 8, 128], BF16)
    P.dma("pool", lambda e: e.dma_start(out=wg[:], in_=dr["a_w_rgate"][idx].rearrange("h i j -> i h j")), writes=[("wgr",)])
    P.dma("pool", lambda e: e.dma_start(out=wi[:], in_=dr["a_w_igate"][idx].rearrange("h i j -> i h j")), writes=[("wgi",)])
    wrec = [A.alloc("wrec", [128, 8, 128], BF16) for _ in range(2)]
    wgat = [A.alloc("wgat", [128, 8, 128], BF16) for _ in range(2)]
    RECp = [A.alloc("RECp", [128, 3 + S], F32) for _ in range(2)]
    nm = ["GX", "SQ", "SG", "CV", "R", "I", "AA", "OM", "H"]
    T = {n: [A.alloc(n, [128, TB], F32) for _ in range(2)] for n in nm}
    CVb = [A.alloc("CVb", [128, TB], BF16) for _ in range(2)]
    for q in range(2):
        P.op("pool", lambda e, q=q: e.memset(RECp[q][:, 0:3], 0.0), writes=[("RECp", q, -1)])
    def chunk_pre(cc):
        q = cc % 2
        P.dma("pool", lambda e, cc=cc, q=q: e.dma_start(
            out=wrec[q][:], in_=dr["a_w_in"][idx][:, D + cc * 128:D + (cc + 1) * 128].rearrange("(k p) n -> p k n", p=128)),
            writes=[("wrec", q)])
        P.dma("pool", lambda e, cc=cc, q=q: e.dma_start(
            out=wgat[q][:], in_=dr["a_w_in"][idx][:, cc * 128:(cc + 1) * 128].rearrange("(k p) n -> p k n", p=128)),
            writes=[("wgat", q)])
        for tb in range(NTB):
            bk = next_bank(c)
            ps = c.ps[bk]
            for k in range(8):
                P.op("pe", lambda e, k=k, ps=ps, tb=tb, q=q: e.matmul(ps[:, :], lhsT=wrec[q][:, k, :], rhs=XT[:, k, tb * TB:(tb + 1) * TB],
                                                                   start=(k == 0), stop=(k == 7)),
                     reads=[("wrec", q)] + [("XT", i_) for i_ in range(tb * 4, tb * 4 + 4)], writes=[("ps", bk)])
            P.op("act", lambda e, ps=ps, tb=tb, q=q: e.activation(out=RECp[q][:, 3 + tb * TB:3 + (tb + 1) * TB], in_=ps[:, :], func=AF.Copy),
                 reads=[("ps", bk)], writes=[("RECp", q, tb)])

    def blk_G(cc, tb, b_, q):
        t_ = {n: T[n][b_] for n in nm}
        k_ = lambda n: (n, b_)
        bk = next_bank(c)
        ps = c.ps[bk]
        for k in range(8):
            P.op("pe", lambda e, k=k, ps=ps, tb=tb, q=q: e.matmul(ps[:, :], lhsT=wgat[q][:, k, :], rhs=XT[:, k, tb * TB:(tb + 1) * TB],
                                                               start=(k == 0), stop=(k == 7)),
                 reads=[("wgat", q)] + [("XT", i_) for i_ in range(tb * 4, tb * 4 + 4)], writes=[("ps", bk)])
        P.op("act", lambda e, ps=ps, t_=t_: e.activation(out=t_["GX"][:], in_=ps[:, :], func=AF.Copy), reads=[("ps", bk)], writes=[k_("GX")])
        P.op("act", lambda e, ps=ps, t_=t_: e.activation(out=t_["SQ"][:], in_=ps[:, :], func=AF.Square), reads=[("ps", bk)], writes=[k_("SQ")])
        P.op("dve", lambda e, t_=t_: e.tensor_scalar(out=t_["SQ"][:], in0=t_["SQ"][:], scalar1=0.044715, scalar2=1.0, op0=ALU.mult, op1=ALU.add),
             reads=[k_("SQ")], writes=[k_("SQ")])
        P.op("dve", lambda e, t_=t_: e.tensor_tensor(out=t_["SQ"][:], in0=t_["SQ"][:], in1=t_["GX"][:], op=ALU.mult),
             reads=[k_("SQ"), k_("GX")], writes=[k_("SQ")])
        P.op("act", lambda e, t_=t_: e.activation(out=t_["SG"][:], in_=t_["SQ"][:], func=AF.Sigmoid, scale=1.5957691216057308),
             reads=[k_("SQ")], writes=[k_("SG")])
        P.op("pool", lambda e, t_=t_: e.tensor_tensor(out=t_["SG"][:], in0=t_["GX"][:], in1=t_["SG"][:], op=ALU.mult),
             reads=[k_("GX"), k_("SG")], writes=[k_("SG")])

    def blk_R(cc, tb, b_, q):
        t_ = {n: T[n][b_] for n in nm}
        k_ = lambda n: (n, b_)
        rk = [("RECp", q, tb)] + ([("RECp", q, tb - 1)] if tb > 0 else [("RECp", q, -1)])
        P.op("dve", lambda e, t_=t_, tb=tb, q=q, cc=cc: e.tensor_scalar(
            out=t_["CV"][:], in0=RECp[q][:, tb * TB:tb * TB + TB], scalar1=pv[:, cc, 0:1], scalar2=pv[:, cc, 4:5], op0=ALU.mult, op1=ALU.add),
            reads=rk + [("pv",)], writes=[k_("CV")])
        for j in range(1, 4):
            P.op("dve", lambda e, t_=t_, tb=tb, q=q, cc=cc, j=j: e.scalar_tensor_tensor(
                out=t_["CV"][:], in0=RECp[q][:, tb * TB + j:tb * TB + j + TB], scalar=pv[:, cc, j:j + 1], in1=t_["CV"][:], op0=ALU.mult, op1=ALU.add),
                reads=rk + [("pv",), k_("CV")], writes=[k_("CV")])
        P.op("act", lambda e, t_=t_, b_=b_: e.activation(out=CVb[b_][:], in_=t_["CV"][:], func=AF.Copy), reads=[k_("CV")], writes=[("CVb", b_)])
        bkr = next_bank(c); bki = next_bank(c)
        P.op("pe", lambda e, b_=b_, cc=cc, bkr=bkr: e.matmul(c.ps[bkr][:, :], lhsT=wg[:, cc, :], rhs=CVb[b_][:], start=True, stop=True),
             reads=[("wgr",), ("CVb", b_)], writes=[("ps", bkr)])
        P.op("pe", lambda e, b_=b_, cc=cc, bki=bki: e.matmul(c.ps[bki][:, :], lhsT=wi[:, cc, :], rhs=CVb[b_][:], start=True, stop=True),
             reads=[("wgi",), ("CVb", b_)], writes=[("ps", bki)])
        P.op("act", lambda e, t_=t_, cc=cc, bkr=bkr: e.activation(out=t_["R"][:], in_=c.ps[bkr][:, :], func=AF.Sigmoid, bias=pv[:, cc, 5:6], scale=1.0),
             reads=[("ps", bkr), ("pv",)], writes=[k_("R")])
        P.op("act", lambda e, t_=t_, cc=cc, bki=bki: e.activation(out=t_["I"][:], in_=c.ps[bki][:, :], func=AF.Sigmoid, bias=pv[:, cc, 6:7], scale=1.0),
             reads=[("ps", bki), ("pv",)], writes=[k_("I")])
        P.op("act", lambda e, t_=t_, cc=cc: e.activation(out=t_["AA"][:], in_=t_["R"][:], func=AF.Exp, scale=cv1[:, cc:cc + 1]),
             reads=[k_("R"), ("cv1",)], writes=[k_("AA")])
        P.op("act", lambda e, t_=t_, cc=cc: e.activation(out=t_["OM"][:], in_=t_["R"][:], func=AF.Exp, scale=cv2[:, cc:cc + 1]),
             reads=[k_("R"), ("cv2",)], writes=[k_("OM")])
        P.op("dve", lambda e, t_=t_: e.tensor_scalar(out=t_["OM"][:], in0=t_["OM"][:], scalar1=-1.0, scalar2=1.0, op0=ALU.mult, op1=ALU.add),
             reads=[k_("OM")], writes=[k_("OM")])
        P.op("act", lambda e, t_=t_: e.activation(out=t_["OM"][:], in_=t_["OM"][:], func=AF.Sqrt), reads=[k_("OM")], writes=[k_("OM")])
        P.op("pool", lambda e, t_=t_: e.tensor_tensor(out=t_["I"][:], in0=t_["I"][:], in1=t_["CV"][:], op=ALU.mult),
             reads=[k_("I"), k_("CV")], writes=[k_("I")])
        P.op("dve", lambda e, t_=t_: e.tensor_tensor(out=t_["I"][:], in0=t_["I"][:], in1=t_["OM"][:], op=ALU.mult),
             reads=[k_("I"), k_("OM")], writes=[k_("I")])
        if tb == 0:
            P.op("dve", lambda e, t_=t_: e.tensor_tensor_scan(out=t_["H"][:], data0=t_["AA"][:], data1=t_["I"][:], initial=0.0,
                                                              op0=ALU.mult, op1=ALU.add),
                 reads=[k_("AA"), k_("I")], writes=[k_("H")])
        else:
            hp = T["H"][1 - b_]
            P.op("dve", lambda e, t_=t_, hp=hp: e.tensor_tensor_scan(out=t_["H"][:], data0=t_["AA"][:], data1=t_["I"][:],
                                                                     initial=hp[:, TB - 1:TB], op0=ALU.mult, op1=ALU.add),
                 reads=[k_("AA"), k_("I"), ("H", 1 - b_)], writes=[k_("H")])
        P.op("pool", lambda e, t_=t_, cc=cc, tb=tb: e.tensor_tensor(out=YT[:, cc, tb * TB:(tb + 1) * TB], in0=t_["SG"][:], in1=t_["H"][:], op=ALU.mult),
             reads=[k_("SG"), k_("H")], writes=[("YT", cc)])

    blocks = [(cc, tb, (cc * NTB + tb) % 2, cc % 2) for cc in range(8) for tb in range(NTB)]
    chunk_pre(0)
    blk_G(*blocks[0])
    for n_ in range(len(blocks)):
        if n_ + 1 < len(blocks):
            if blocks[n_ + 1][1] == 0:
                chunk_pre(blocks[n_ + 1][0])
            blk_G(*blocks[n_ + 1])
        blk_R(*blocks[n_])
    P.barrier()
    A.reset(mark_after_yt)
    emit_mixer_epilogue(c, layer, YT, 8, dr["a_w_out"][idx])


RET_H = 4
RET_EPS = 1e-6


def ret_consts():
    f32 = np.float32
    log_gamma = np.log1p(-np.exp2(-5.0 - np.arange(RET_H, dtype=f32))).astype(f32)
    pos = np.arange(128, dtype=f32)
    rel = pos[:, None] - pos[None, :]
    intra = np.where(rel >= 0, np.exp(log_gamma[:, None, None] * np.maximum(rel, 0.0)), 0.0).astype(f32)
    dt = np.ascontiguousarray(intra.transpose(0, 2, 1))
    qd = np.exp(log_gamma[:, None] * (pos + 1.0)).astype(f32)
    kd = np.exp(log_gamma[:, None] * (127.0 - pos)).astype(f32)
    cd = np.exp(log_gamma * 128.0).astype(f32)
    return {"c_ret_dt": np.ascontiguousarray(dt.transpose(1, 0, 2)),
            "c_ret_qd": np.ascontiguousarray(np.tile(qd[None], (128, 1, 1))),
            "c_ret_kd": np.ascontiguousarray((kd.T / 16.0).astype(f32)),
            }, [float(v) for v in cd]


def phase_ret(c, layer, idx):
    A, P, dr = c.A, c.P, c.dr
    X, XT = c.X, c.XT
    _, CDV = ret_consts()
    DT = A.alloc("rDT", [128, RET_H, 128], F32)
    QD = A.alloc("rQD", [128, RET_H, 128], F32)
    KD = A.alloc("rKD", [128, RET_H], F32)
    eps6 = A.alloc("eps6", [128, 1], F32)
    P.dma("sp", lambda e: e.dma_start(out=DT[:], in_=dr["c_ret_dt"]), writes=[("rDT",)])
    P.dma("sp", lambda e: e.dma_start(out=QD[:], in_=dr["c_ret_qd"]), writes=[("rQD",)])
    P.dma("sp", lambda e: e.dma_start(out=KD[:], in_=dr["c_ret_kd"]), writes=[("rKD",)])
    P.op("dve", lambda e: e.memset(eps6[:], RET_EPS), writes=[("eps6",)])
    Wq = [A.alloc("Wq", [128, 8, 256], BF16) for _ in range(2)]
    Wk = [A.alloc("Wk", [128, 8, 256], BF16) for _ in range(2)]
    Wv = [A.alloc("Wv", [128, 8, 512], BF16) for _ in range(2)]
    Wg = [A.alloc("Wg", [128, 8, 512], BF16) for _ in range(2)]
    Wo = [A.alloc("Wo", [128, 4, D], BF16) for _ in range(2)]
    Sf = A.alloc("Sf", [128, 2, 512], F32)
    Sb = [A.alloc("Sb", [128, 2, 512], BF16) for _ in range(2)]
    qT = [A.alloc("qT", [128, 2, 128], BF16) for _ in range(2)]
    qdT = [A.alloc("qdT", [128, 2, 128], BF16) for _ in range(2)]
    kT = [A.alloc("kT", [128, 2, 128], BF16) for _ in range(2)]
    kdec = [A.alloc("kdec", [128, 256], BF16) for _ in range(2)]
    vc = [A.alloc("vc", [128, 512], BF16) for _ in range(2)]
    sgc = [A.alloc("sgc", [128, 512], F32) for _ in range(2)]
    PT = [A.alloc("PT", [128, 128], BF16) for _ in range(2)]
    qf = [A.alloc("qf", [128, 256], F32) for _ in range(2)]
    kf = [A.alloc("kf", [128, 256], F32) for _ in range(2)]
    ktf = [A.alloc("ktf", [128, 256], F32) for _ in range(2)]
    scf = [A.alloc("scf", [128, 128], F32) for _ in range(2)]
    osq = [A.alloc("osq", [128, 512], F32) for _ in range(2)]
    ms = [A.alloc("ms", [128, 1], F32) for _ in range(2)]
    sd = [A.alloc("rsd", [128, 1], F32) for _ in range(2)]
    rs = [A.alloc("rrs", [128, 1], F32) for _ in range(2)]
    yc = [A.alloc("yc", [128, 512], BF16) for _ in range(2)]
    yT = [A.alloc("yT", [128, 4, 128], BF16) for _ in range(2)]
    W = dr["b_w_in"][idx]
    QW = 1024
    thr = A.alloc("thr", [128, 1], BF16)

    def load_head(h):
        q = h % 2
        for (dst, col0, n, nm) in ((Wq[q], h * 256, 256, "Wq"), (Wk[q], QW + h * 256, 256, "Wk"),
                                   (Wv[q], 2 * QW + h * 512, 512, "Wv"), (Wg[q], 2 * QW + 2048 + h * 512, 512, "Wg")):
            P.dma("pool", lambda e, dst=dst, col0=col0, n=n: e.dma_start(
                out=dst[:], in_=W[:, col0:col0 + n].rearrange("(k p) n -> p k n", p=128)), reads=[("throttle",)], writes=[(nm, q)])
        P.dma("pool", lambda e, q=q, h=h: e.dma_start(
            out=Wo[q][:], in_=dr["b_w_out"][idx][h * 512:(h + 1) * 512, :].rearrange("(k p) n -> p k n", p=128)), reads=[("throttle",)], writes=[("Wo", q)])

    import os
    DBG = int(os.environ.get("RET_DBG", "99"))
    NH_ = int(os.environ.get("RET_NH", "4"))
    NI_ = int(os.environ.get("RET_NI", "16"))

    def ret_P(h, i, b_, q):
        if h >= NH_ or i >= NI_ or DBG < 1:
            return
        xtk = [("XT", i)]
        tok = slice(i * 128, (i + 1) * 128)
        bkq = next_bank(c); bkk = next_bank(c)
        for dc in range(2):
            for k in range(8):
                P.op("pe", lambda e, dc=dc, k=k, bkq=bkq: e.matmul(c.ps[bkq][:, dc * 128:(dc + 1) * 128], lhsT=Wq[q][:, k, dc * 128:(dc + 1) * 128],
                                                             rhs=XT[:, k, tok], start=(k == 0), stop=(k == 7)),
                     reads=[("Wq", q)] + xtk, writes=[("ps", bkq)])
        for dc in range(2):
            for k in range(8):
                P.op("pe", lambda e, dc=dc, k=k, bkk=bkk: e.matmul(c.ps[bkk][:, dc * 128:(dc + 1) * 128], lhsT=Wk[q][:, k, dc * 128:(dc + 1) * 128],
                                                             rhs=XT[:, k, tok], start=(k == 0), stop=(k == 7)),
                     reads=[("Wk", q)] + xtk, writes=[("ps", bkk)])
        P.op("act", lambda e, bkq=bkq, b_=b_: e.activation(out=qf[b_][:], in_=c.ps[bkq][:, 0:256], func=AF.Copy),
             reads=[("ps", bkq)], writes=[("qf", b_)])
        P.op("act", lambda e, bkk=bkk, b_=b_: e.activation(out=kf[b_][:], in_=c.ps[bkk][:, 0:256], func=AF.Copy),
             reads=[("ps", bkk)], writes=[("kf", b_)])
        P.op("pool", lambda e, b_=b_: e.tensor_copy(out=qT[b_][:].rearrange("p a b -> p (a b)"), in_=qf[b_][:]),
             reads=[("qf", b_)], writes=[("qT", b_)])
        for dc in range(2):
            P.op("dve", lambda e, dc=dc, b_=b_, h=h: e.tensor_tensor(out=qdT[b_][:, dc, :], in0=qf[b_][:, dc * 128:(dc + 1) * 128],
                                                                   in1=QD[:, h, :], op=ALU.mult),
                 reads=[("qf", b_), ("rQD",)], writes=[("qdT", b_)])
        P.op("pool", lambda e, b_=b_: e.tensor_scalar(out=kT[b_][:].rearrange("p a b -> p (a b)"), in0=kf[b_][:], scalar1=0.0625, scalar2=None, op0=ALU.mult),
             reads=[("kf", b_)], writes=[("kT", b_)])
        if DBG < 2:
            return
        bkt = next_bank(c)
        for k in range(8):
            P.op("pe", lambda e, k=k, bkt=bkt: e.matmul(c.ps[bkt][:, 0:256], lhsT=XT[:, k, tok], rhs=Wk[q][:, k, :], start=(k == 0), stop=(k == 7)),
                 reads=[("Wk", q)] + xtk, writes=[("ps", bkt)])
        P.op("act", lambda e, bkt=bkt, b_=b_: e.activation(out=ktf[b_][:], in_=c.ps[bkt][:, 0:256], func=AF.Copy),
             reads=[("ps", bkt)], writes=[("ktf", b_)])
        P.op("dve", lambda e, b_=b_, h=h: e.tensor_scalar(out=kdec[b_][:], in0=ktf[b_][:], scalar1=KD[:, h:h + 1], scalar2=None, op0=ALU.mult),
             reads=[("ktf", b_), ("rKD",)], writes=[("kdec", b_)])
        bkv = next_bank(c)
        for k in range(8):
            P.op("pe", lambda e, k=k, bkv=bkv: e.matmul(c.ps[bkv][:, :], lhsT=XT[:, k, tok], rhs=Wv[q][:, k, :], start=(k == 0), stop=(k == 7)),
                 reads=[("Wv", q)] + xtk, writes=[("ps", bkv)])
        P.op("act", lambda e, bkv=bkv, b_=b_: e.activation(out=vc[b_][:], in_=c.ps[bkv][:, :], func=AF.Copy), reads=[("ps", bkv)], writes=[("vc", b_)])
        bkg = next_bank(c)
        for k in range(8):
            P.op("pe", lambda e, k=k, bkg=bkg: e.matmul(c.ps[bkg][:, :], lhsT=XT[:, k, tok], rhs=Wg[q][:, k, :], start=(k == 0), stop=(k == 7)),
                 reads=[("Wg", q)] + xtk, writes=[("ps", bkg)])
        P.op("act", lambda e, bkg=bkg, b_=b_: e.activation(out=sgc[b_][:], in_=c.ps[bkg][:, :], func=AF.Sigmoid), reads=[("ps", bkg)], writes=[("sgc", b_)])
        P.op("dve", lambda e, bkg=bkg, b_=b_: e.tensor_tensor(out=sgc[b_][:], in0=c.ps[bkg][:, :], in1=sgc[b_][:], op=ALU.mult),
             reads=[("ps", bkg), ("sgc", b_)], writes=[("sgc", b_)])

    def ret_S(h, i, b_, q):
        xtk = [("XT", i)]
        tok = slice(i * 128, (i + 1) * 128)
        bks = next_bank(c)
        for dc in range(2):
            P.op("pe", lambda e, dc=dc, bks=bks, b_=b_: e.matmul(c.ps[bks][:, 0:128], lhsT=kT[b_][:, dc, :], rhs=qT[b_][:, dc, :], start=(dc == 0), stop=(dc == 1)),
                 reads=[("kT", b_), ("qT", b_)], writes=[("ps", bks)])
        P.op("act", lambda e, bks=bks, b_=b_: e.activation(out=scf[b_][:], in_=c.ps[bks][:, 0:128], func=AF.Copy),
             reads=[("ps", bks)], writes=[("scf", b_)])
        P.op("dve", lambda e, b_=b_, h=h: e.tensor_tensor(out=PT[b_][:], in0=scf[b_][:], in1=DT[:, h, :], op=ALU.mult),
             reads=[("scf", b_), ("rDT",)], writes=[("PT", b_)])
        if i < NT - 1:
            sbn = Sb[i % 2]
            for dc in range(2):
                bkS = next_bank(c)
                P.op("pe", lambda e, dc=dc, bkS=bkS, b_=b_: e.matmul(c.ps[bkS][:, :], lhsT=kdec[b_][:, dc * 128:(dc + 1) * 128], rhs=vc[b_][:], start=True, stop=True),
                     reads=[("kdec", b_), ("vc", b_)], writes=[("ps", bkS)])
                if i == 0:
                    P.op("dve", lambda e, dc=dc, bkS=bkS: e.tensor_copy(out=Sf[:, dc, :], in_=c.ps[bkS][:, :]), reads=[("ps", bkS)], writes=[("Sf", dc)])
                else:
                    P.op("dve", lambda e, dc=dc, bkS=bkS, h=h: e.scalar_tensor_tensor(out=Sf[:, dc, :], in0=Sf[:, dc, :], scalar=CDV[h], in1=c.ps[bkS][:, :],
                                                                                  op0=ALU.mult, op1=ALU.add),
                         reads=[("ps", bkS), ("Sf", dc)], writes=[("Sf", dc)])
                P.op("pool", lambda e, dc=dc, sbn=sbn: e.tensor_copy(out=sbn[:, dc, :], in_=Sf[:, dc, :]), reads=[("Sf", dc)], writes=[("Sb", i % 2)])
        bko = next_bank(c)
        sbp = Sb[(i + 1) % 2]
        P.op("pe", lambda e, bko=bko, b_=b_: e.matmul(c.ps[bko][:, :], lhsT=PT[b_][:], rhs=vc[b_][:], start=True, stop=(i == 0)),
             reads=[("PT", b_), ("vc", b_)], writes=[("ps", bko)])
        if i > 0:
            for dc in range(2):
                P.op("pe", lambda e, dc=dc, bko=bko, b_=b_, sbp=sbp: e.matmul(c.ps[bko][:, :], lhsT=qdT[b_][:, dc, :], rhs=sbp[:, dc, :], start=False, stop=(dc == 1)),
                     reads=[("qdT", b_), ("Sb", (i + 1) % 2)], writes=[("ps", bko)])
        P.op("act", lambda e, bko=bko, b_=b_: e.activation(out=osq[b_][:], in_=c.ps[bko][:, :], func=AF.Square), reads=[("ps", bko)], writes=[("osq", b_)])
        P.op("dve", lambda e, b_=b_: e.reduce_sum(out=ms[b_][:], in_=osq[b_][:], axis=AX.X), reads=[("osq", b_)], writes=[("ms", b_)])
        P.op("act", lambda e, b_=b_: e.activation(out=sd[b_][:], in_=ms[b_][:], func=AF.Sqrt, bias=eps6[:], scale=1.0 / 512.0),
             reads=[("ms", b_), ("eps6",)], writes=[("rsd", b_)])
        P.op("dve", lambda e, b_=b_: e.reciprocal(out=rs[b_][:], in_=sd[b_][:]), reads=[("rsd", b_)], writes=[("rrs", b_)])
        P.op("dve", lambda e, bko=bko, b_=b_: e.scalar_tensor_tensor(out=osq[b_][:], in0=c.ps[bko][:, :], scalar=rs[b_][:, 0:1], in1=sgc[b_][:],
                                                                  op0=ALU.mult, op1=ALU.mult),
             reads=[("ps", bko), ("rrs", b_), ("sgc", b_), ("ms", b_)], writes=[("osq", b_)])
        P.op("pool", lambda e, b_=b_: e.tensor_copy(out=yc[b_][:], in_=osq[b_][:]), reads=[("osq", b_)], writes=[("yc", b_)])

    def ret_Y(h, i, b_, q):
        xtk = [("XT", i)]
        tok = slice(i * 128, (i + 1) * 128)
        bky = next_bank(c)
        psb = c.ps[bky][:].bitcast(BF16)
        for fc in range(4):
            P.op("pe", lambda e, fc=fc, psb=psb, b_=b_: e.transpose(out=psb[:, fc * 128:(fc + 1) * 128], in_=yc[b_][:, fc * 128:(fc + 1) * 128], identity=c.ident_b[:]),
                 reads=[("yc", b_), ("ident_b",)], writes=[("ps", bky)])
        P.op("act", lambda e, psb=psb, b_=b_: e.activation(out=yT[b_][:].rearrange("p a b -> p (a b)"), in_=psb[:, 0:512], func=AF.Copy),
             reads=[("ps", bky)], writes=[("yT", b_)])
        for nh in range(2):
            bkx = next_bank(c)
            for fc in range(4):
                P.op("pe", lambda e, fc=fc, nh=nh, bkx=bkx, b_=b_: e.matmul(c.ps[bkx][:, :], lhsT=yT[b_][:, fc, :], rhs=Wo[q][:, fc, nh * 512:(nh + 1) * 512],
                                                                     start=(fc == 0), stop=(fc == 3)),
                     reads=[("yT", b_), ("Wo", q)], writes=[("ps", bkx)])
            xs = X[:, i, nh * 512:(nh + 1) * 512]
            if h == 0:
                P.op("dve", lambda e, xs=xs, bkx=bkx: e.scalar_tensor_tensor(out=xs, in0=xs, scalar=ALPHA, in1=c.ps[bkx][:, :], op0=ALU.mult, op1=ALU.add),
                     reads=[("ps", bkx), ("X", i)], writes=[("X", i)])
            else:
                P.op("dve", lambda e, xs=xs, bkx=bkx: e.tensor_tensor(out=xs, in0=xs, in1=c.ps[bkx][:, :], op=ALU.add),
                     reads=[("ps", bkx), ("X", i)], writes=[("X", i)])


    load_head(0)
    it = 0
    for h in range(RET_H):
        q = h % 2
        for i in range(NT + 2):
            if i < NT:
                ret_P(h, i, i % 2, q)
            if 1 <= i <= NT:
                ret_S(h, i - 1, (i - 1) % 2, q)
            if i >= 2:
                ret_Y(h, i - 2, (i - 2) % 2, q)
            it += 1
            if i == 4 and h + 1 < RET_H:
                P.op("act", lambda e: e.activation(out=thr[:], in_=yT[0][:, 0, 0:1], func=AF.Copy),
                     reads=[("yT", 0)], writes=[("throttle",)])
                load_head(h + 1)
    P.barrier()
    A.reset(c.phase_base)
    emit_ln_route_epilogue(c, layer)


def emit_ln_route_epilogue(c, layer):
    r = alloc_route(c, layer)
    g1, b1 = load_ln_params(c, layer, 0, "a")
    lt = alloc_ln_tmp(c)
    t = alloc_prep(c)
    for i in range(NT):
        emit_ln(c, lt, c.X[:, i, :], ("X", i), i, g1, b1, "a")
        if i >= 1:
            emit_transpose_tile(c, t, i - 1, write_xt=False)
            emit_route_tile1(c, r, t, i - 1)
        if i >= 2:
            emit_route_tile2(c, r, t, i - 2)
    emit_transpose_tile(c, t, NT - 1, write_xt=False)
    emit_route_tile1(c, r, t, NT - 1)
    emit_route_tile2(c, r, t, NT - 2)
    emit_route_tile2(c, r, t, NT - 1)


ATT_PAT = ((128, 1), (512, 4), (2048, 16))
ATT_BIG = 1.0e9


def att_consts():
    u = np.arange(128)[:, None]
    j = np.arange(256)[None, :]
    steps = u + 128 - j
    valid = (steps >= 0) & (steps <= 128)
    neg = np.where(valid, -steps.astype(np.float32), -ATT_BIG).astype(np.float32)
    return {"c_att_neg": np.ascontiguousarray(neg)}


def phase_attn(c, layer, idx):
    A, P, dr = c.A, c.P, c.dr
    X, XT = c.X, c.XT
    nc = c.nc
    Wd = dr["c_w_in"][idx]
    NEG = A.alloc("aNEG", [128, 256], F32)
    P.dma("sp", lambda e: e.dma_start(out=NEG[:], in_=dr["c_att_neg"]), writes=[("aNEG",)])
    Wq = [A.alloc("aWq", [128, 8, 128], BF16) for _ in range(2)]
    Wk = [A.alloc("aWk", [128, 8, 128], BF16) for _ in range(2)]
    Wv = [A.alloc("aWv", [128, 8, 128], BF16) for _ in range(2)]
    qT = [A.alloc("aqT", [128, S], BF16) for _ in range(2)]
    kT = [A.alloc("akT", [128, S], BF16) for _ in range(2)]
    V = [A.alloc("aV", [128, NT, 128], BF16) for _ in range(2)]
    BI = [A.alloc("aBI", [128, 2, 256], F32) for _ in range(2)]
    Sb = [A.alloc("aSb", [128, 256], F32) for _ in range(4)]
    Pb = [A.alloc("aPb", [128, 256], BF16) for _ in range(4)]
    PT = [A.alloc("aPT", [128, 2, 128], BF16) for _ in range(2)]
    mx = [A.alloc("amx", [128, 1], F32) for _ in range(2)]
    nm = [A.alloc("anm", [128, 1], F32) for _ in range(4)]
    osb = [A.alloc("aosb", [128, 128], F32) for _ in range(2)]
    mst = [A.alloc("amst", [128, 2, 2], F32) for _ in range(4)]

    def load_w(g, hp, q):
        for (dst, s_, nm_) in ((Wq[q], 0, "aWq"), (Wk[q], 1, "aWk"), (Wv[q], 2, "aWv")):
            col0 = ((s_ * 3 + g) * 16 + 2 * hp) * 64
            P.dma("pool", lambda e, dst=dst, col0=col0: e.dma_start(
                out=dst[:], in_=Wd[:, col0:col0 + 128].rearrange("(k p) n -> p k n", p=128)), writes=[(nm_, q)])

    def tokslice(g, ut):
        d = ATT_PAT[g][1]
        nb = (S // d) // 128
        r, b = ut // nb, ut % nb
        st = 128 * b * d + r
        return slice(st, st + 127 * d + 1, d), b

    def proj_slices(g, hp, q):
        sl = []
        for (Wt, dst, nm_, wn_) in ((Wq[q], qT[q], "aqT", "aWq"), (Wk[q], kT[q], "akT", "aWk")):
            for tb in range(4):
                def f(Wt=Wt, dst=dst, nm_=nm_, wn_=wn_, tb=tb):
                    bk = next_bank(c, 6)
                    for k in range(8):
                        P.op("pe", lambda e, k=k: e.matmul(c.ps[bk][:, :], lhsT=Wt[:, k, :], rhs=XT[:, k, tb * 512:(tb + 1) * 512],
                                                         start=(k == 0), stop=(k == 7)),
                             reads=[(wn_, q)] + [("XT", i_) for i_ in range(tb * 4, tb * 4 + 4)], writes=[("ps", bk)])
                    P.op("act", lambda e: e.activation(out=dst[:, tb * 512:(tb + 1) * 512], in_=c.ps[bk][:, :], func=AF.Copy),
                         reads=[("ps", bk)], writes=[(nm_, q, tb)])
                sl.append(f)
        for ut in range(NT):
            def f(ut=ut):
                ts_, _ = tokslice(g, ut)
                bk = next_bank(c, 6)
                for k in range(8):
                    P.op("pe", lambda e, k=k: e.matmul(c.ps[bk][:, 0:128], lhsT=XT[:, k, ts_], rhs=Wv[q][:, k, :], start=(k == 0), stop=(k == 7)),
                         reads=[("aWv", q)] + [("XT", i_) for i_ in range(NT)], writes=[("ps", bk)])
                P.op("act", lambda e: e.activation(out=V[q][:, ut, :], in_=c.ps[bk][:, 0:128], func=AF.Copy),
                     reads=[("ps", bk)], writes=[("aV", q, ut)])
            sl.append(f)
        def f():
            d = ATT_PAT[g][1]
            for e_ in range(2):
                hh = 2 * hp + e_
                slope = float(2.0 ** (-8.0 * (hh + 1) / 16.0)) * d
                P.op("pool", lambda e, e_=e_, slope=slope: e.tensor_scalar(out=BI[q][:, e_, :], in0=NEG[:], scalar1=slope, scalar2=None, op0=ALU.mult),
                     reads=[("aNEG",)], writes=[("aBI", q, e_)])
        sl.append(f)
        return sl

    def QK_KEYS(q):
        return [("aqT", q, tb) for tb in range(4)] + [("akT", q, tb) for tb in range(4)]

    NSL = 4

    def stageA(g, hp, q, ut, e_, sl, bo):
        ts_, b = tokslice(g, ut)
        has_prev = b > 0
        pr = slice(e_ * 64, (e_ + 1) * 64)
        lo = 0 if has_prev else 128
        bk = next_bank(c, 6)
        if has_prev:
            tp_, _ = tokslice(g, ut - 1)
            P.op("pe", lambda e: e.matmul(c.ps[bk][:, 0:128], lhsT=qT[q][pr, ts_], rhs=kT[q][pr, tp_], start=True, stop=True),
                 reads=QK_KEYS(q), writes=[("ps", bk)])
        P.op("pe", lambda e: e.matmul(c.ps[bk][:, 128:256], lhsT=qT[q][pr, ts_], rhs=kT[q][pr, ts_], start=True, stop=True),
             reads=QK_KEYS(q), writes=[("ps", bk)])
        P.op("dve", lambda e: e.scalar_tensor_tensor(out=Sb[sl][:, lo:256], in0=c.ps[bk][:, lo:256], scalar=0.125, in1=BI[q][:, e_, lo:256],
                                                    op0=ALU.mult, op1=ALU.add),
             reads=[("ps", bk), ("aBI", q, e_)], writes=[("aSb", sl)])
        P.op("dve", lambda e: e.reduce_max(out=mst[bo][:, e_, 0:1], in_=Sb[sl][:, lo:256], axis=AX.X), reads=[("aSb", sl)], writes=[("amst", bo, e_, 0)])
        P.op("pool", lambda e: e.tensor_scalar(out=nm[sl][:], in0=mst[bo][:, e_, 0:1], scalar1=-1.0, scalar2=None, op0=ALU.mult),
             reads=[("amst", bo, e_, 0)], writes=[("anm", sl)])
        P.op("act", lambda e: e.activation(out=Pb[sl][:, lo:256], in_=Sb[sl][:, lo:256], func=AF.Exp, bias=nm[sl][:], scale=1.0),
             reads=[("aSb", sl), ("anm", sl)], writes=[("aPb", sl)])

    def stageA2(g, hp, q, ut, e_, sl, bo):
        ts_, b = tokslice(g, ut)
        lo = 0 if b > 0 else 128
        P.op("dve", lambda e: e.reduce_sum(out=mst[bo][:, e_, 1:2], in_=Pb[sl][:, lo:256], axis=AX.X),
             reads=[("aPb", sl)], writes=[("amst", bo, e_, 1)])

    def stageB1(g, hp, q, ut, e_, sl, bo):
        ts_, b = tokslice(g, ut)
        has_prev = b > 0
        lo = 0 if has_prev else 128
        halves = (0, 1) if has_prev else (1,)
        pt = PT[sl % 2]
        bkt = next_bank(c, 6)
        psb = c.ps[bkt][:].bitcast(BF16)
        for hf in halves:
            P.op("pe", lambda e, hf=hf: e.transpose(out=psb[:, hf * 128:(hf + 1) * 128], in_=Pb[sl][:, hf * 128:(hf + 1) * 128], identity=c.ident_b[:]),
                 reads=[("aPb", sl), ("ident_b",)], writes=[("ps", bkt)])
        P.op("act", lambda e: e.activation(out=pt[:].rearrange("p a b -> p (a b)")[:, lo:256], in_=psb[:, lo:256], func=AF.Copy),
             reads=[("ps", bkt)], writes=[("aPT", sl % 2)])

    def stageB(g, hp, q, ut, e_, sl, bo, obank):
        ts_, b = tokslice(g, ut)
        has_prev = b > 0
        pr = slice(e_ * 64, (e_ + 1) * 64)
        lo = 0 if has_prev else 128
        halves = (0, 1) if has_prev else (1,)
        pt = PT[sl % 2]
        for n_, hf in enumerate(halves):
            vt = ut - 1 if hf == 0 else ut
            P.op("pe", lambda e, hf=hf, vt=vt, n_=n_: e.matmul(c.ps[obank][:, pr], lhsT=pt[:, hf, :], rhs=V[q][:, vt, pr],
                                                            start=(n_ == 0), stop=(n_ == len(halves) - 1)),
                 reads=[("aPT", sl % 2), ("aV", q, vt)], writes=[("ps", obank)])
        if e_ == 1:
            ob = osb[bo % 2]
            P.op("dve", lambda e: e.tensor_copy(out=ob[:], in_=c.ps[obank][:, 0:128]), reads=[("ps", obank)], writes=[("aosb", bo % 2)])
            P.dma("sp", lambda e: e.dma_start(out=dr["att_o"][g, ts_, hp * 128:(hp + 1) * 128], in_=ob[:]), reads=[("aosb", bo % 2)], writes=[("att_o", g, hp, ut)])
            P.dma("sp", lambda e: e.dma_start(out=dr["att_ms"][g, ts_, 2 * hp:2 * hp + 2, :], in_=mst[bo][:]),
                  reads=[("amst", bo, e2, t2) for e2 in range(2) for t2 in range(2)], writes=[("att_ms", g, hp, ut)])

    import os
    NG_ = int(os.environ.get("ATT_NG", "3"))
    NHP_ = int(os.environ.get("ATT_NHP", "8"))
    LOOK = 3
    combos = [(g, hp) for g in range(NG_) for hp in range(NHP_)]
    load_w(combos[0][0], combos[0][1], 0)
    for f_ in proj_slices(combos[0][0], combos[0][1], 0):
        f_()
    gcount = 0
    for n, (g, hp) in enumerate(combos):
        q = n % 2
        nxt = []
        if n + 1 < len(combos):
            load_w(combos[n + 1][0], combos[n + 1][1], (n + 1) % 2)
            nxt = proj_slices(combos[n + 1][0], combos[n + 1][1], (n + 1) % 2)
        units = [(ut, e_) for ut in range(NT) for e_ in range(2)]
        def args(j):
            ut, e_ = units[j]
            gc = gcount + j
            return (g, hp, q, ut, e_, gc % NSL, (gc // 2) % NSL)
        nu = len(units)
        for j in range(-3, nu):
            if 0 <= j + 3 < nu:
                stageA(*args(j + 3))
            if 0 <= j + 1 < nu:
                stageA2(*args(j + 1))
                stageB1(*args(j + 1))
            if 0 <= j < nu:
                stageB(*args(j), 6 + ((gcount + j) // 2) % 2)
            if j >= 3 and nxt:
                nxt.pop(0)()
        while nxt:
            nxt.pop(0)()
        gcount += len(units)
    P.barrier()
    A.reset(c.phase_base)
    YT = A.alloc("YT", [128, 8, S], BF16)
    mark = A.off
    Og = [[A.alloc("aOg", [128, D], F32) for _ in range(3)] for _ in range(2)]
    MS = [A.alloc("aMS", [128, 3, 16, 2], F32) for _ in range(2)]
    Mx = [A.alloc("aMx", [128, 16], F32) for _ in range(2)]
    Wt_ = [A.alloc("aWt", [128, 3, 16], F32) for _ in range(2)]
    Ws = [A.alloc("aWs", [128, 3, 16], F32) for _ in range(2)]
    Dn = [A.alloc("aDn", [128, 16], F32) for _ in range(2)]
    Yt = [A.alloc("aYt", [128, D], F32) for _ in range(2)]
    Yb = [A.alloc("aYb", [128, D], BF16) for _ in range(2)]

    def merge_tile(i, p):
        for g in range(3):
            P.dma("sp", lambda e, g=g: e.dma_start(out=Og[p][g][:], in_=dr["att_o"][g, i * 128:(i + 1) * 128, :]), writes=[("aOg", p, g)])
        P.dma("sp", lambda e: e.dma_start(out=MS[p][:], in_=dr["att_ms"][:, i * 128:(i + 1) * 128, :, :].rearrange("g p h t -> p g h t")), writes=[("aMS", p)])
        P.op("dve", lambda e: e.tensor_tensor(out=Mx[p][:], in0=MS[p][:, 0, :, 0], in1=MS[p][:, 1, :, 0], op=ALU.max), reads=[("aMS", p)], writes=[("aMx", p)])
        P.op("dve", lambda e: e.tensor_tensor(out=Mx[p][:], in0=Mx[p][:], in1=MS[p][:, 2, :, 0], op=ALU.max), reads=[("aMS", p), ("aMx", p)], writes=[("aMx", p)])
        for g in range(3):
            P.op("dve", lambda e, g=g: e.tensor_tensor(out=Wt_[p][:, g, :], in0=MS[p][:, g, :, 0], in1=Mx[p][:], op=ALU.subtract),
                 reads=[("aMS", p), ("aMx", p)], writes=[("aWt", p, g)])
        P.op("act", lambda e: e.activation(out=Wt_[p][:].rearrange("p a b -> p (a b)"), in_=Wt_[p][:].rearrange("p a b -> p (a b)"), func=AF.Exp), reads=[("aWt", p, g) for g in range(3)], writes=[("aWt", p)])
        P.op("dve", lambda e: e.tensor_tensor(out=Ws[p][:], in0=Wt_[p][:], in1=MS[p][:, :, :, 1], op=ALU.mult), reads=[("aWt", p), ("aMS", p)], writes=[("aWs", p)])
        P.op("dve", lambda e: e.tensor_tensor(out=Dn[p][:], in0=Ws[p][:, 0, :], in1=Ws[p][:, 1, :], op=ALU.add), reads=[("aWs", p)], writes=[("aDn", p)])
        P.op("dve", lambda e: e.tensor_tensor(out=Dn[p][:], in0=Dn[p][:], in1=Ws[p][:, 2, :], op=ALU.add), reads=[("aWs", p), ("aDn", p)], writes=[("aDn", p)])
        P.op("dve", lambda e: e.reciprocal(out=Dn[p][:], in_=Dn[p][:]), reads=[("aDn", p)], writes=[("aDn", p)])
        for g in range(3):
            P.op("dve", lambda e, g=g: e.tensor_tensor(out=Ws[p][:, g, :], in0=Wt_[p][:, g, :], in1=Dn[p][:], op=ALU.mult),
                 reads=[("aWt", p), ("aDn", p), ("aWs", p)], writes=[("aWs", p)])
        for g in range(3):
            eng = "dve" if g != 1 else "pool"
            P.op(eng, lambda e, g=g: e.tensor_tensor(out=Og[p][g][:].rearrange("p (h d) -> p h d", h=16), in0=Og[p][g][:].rearrange("p (h d) -> p h d", h=16),
                                                     in1=Ws[p][:, g, :].unsqueeze(2).to_broadcast([128, 16, 64]), op=ALU.mult),
                 reads=[("aOg", p, g), ("aWs", p)], writes=[("aOg", p, g)])
        P.op("pool", lambda e: e.tensor_tensor(out=Yt[p][:], in0=Og[p][0][:], in1=Og[p][1][:], op=ALU.add), reads=[("aOg", p, 0), ("aOg", p, 1)], writes=[("aYt", p)])
        P.op("dve", lambda e: e.tensor_tensor(out=Yb[p][:], in0=Yt[p][:], in1=Og[p][2][:], op=ALU.add), reads=[("aYt", p), ("aOg", p, 2)], writes=[("aYb", p)])
        for half in range(2):
            bk = next_bank(c)
            psb = c.ps[bk][:].bitcast(BF16)
            for q4 in range(4):
                ch = half * 4 + q4
                P.op("pe", lambda e, ch=ch, q4=q4, psb=psb: e.transpose(out=psb[:, q4 * 128:(q4 + 1) * 128], in_=Yb[p][:, ch * 128:(ch + 1) * 128], identity=c.ident_b[:]),
                     reads=[("aYb", p), ("ident_b",)], writes=[("ps", bk)])
            P.op("act", lambda e, half=half, psb=psb: e.activation(out=YT[:, half * 4:(half + 1) * 4, i * 128:(i + 1) * 128],
                                                                 in_=psb[:, 0:512].rearrange("p (a b) -> p a b", a=4), func=AF.Copy),
                 reads=[("ps", bk)], writes=[("YT", half * 4 + j) for j in range(4)])

    for i in range(NT):
        merge_tile(i, i % 2)
    P.barrier()
    A.reset(mark)
    emit_mixer_epilogue(c, layer, YT, 8, dr["c_w_out"][idx])


WEIGHT_NAMES = ["a_w_in", "a_conv_w", "a_conv_b", "a_w_rgate", "a_b_rgate", "a_w_igate", "a_b_igate", "a_lambda",
                "a_w_out", "b_w_in", "b_w_out", "c_w_in", "c_w_out", "ln_gain", "ln_bias", "moe_w_router",
                "moe_b_router", "moe_w_gu", "moe_b_gu", "moe_w_down", "moe_b_down"]


def make_consts():
    ident = np.eye(128, dtype=np.float32)
    tri = np.triu(np.ones((128, 128), dtype=np.float32), k=1)
    ec = np.tile((np.arange(NE, dtype=np.float32) * CAP)[None, :], (128, 1))
    d = {"c_ident": ident, "c_tri": tri, "c_ec": ec}
    d.update(ret_consts()[0])
    d.update(att_consts())
    return d


def run_plan(plan, x, weights, used=None):
    used = used if used is not None else WEIGHT_NAMES
    ws = {k: weights[k].shape for k in used}
    nc = build(plan, ws)
    consts = make_consts()
    in_maps = []
    for b in range(8):
        m = {"x": np.ascontiguousarray(x[b])}
        for k in used:
            m[k] = weights[k]
        m.update(consts)
        in_maps.append(m)
    res = run_bass_kernel_spmd(nc, in_maps, core_ids=list(range(8)))
    return np.stack([r["out"] for r in res.results], axis=0)


FULL_PLAN = [("prep",),
             ("rglru", 0, 0), ("moe", 0, False),
             ("ret", 1, 0), ("moe", 1, False),
             ("attn", 2, 0), ("moe", 2, False),
             ("rglru", 3, 1), ("moe", 3, False)]


def kernel(**inputs):
    x = np.asarray(inputs["x"], dtype=np.float32)
    weights = {k: np.ascontiguousarray(np.asarray(inputs[k], dtype=np.float32)) for k in WEIGHT_NAMES}
    out = run_plan(FULL_PLAN, x, weights)
    return out.astype(np.float32)
```

```python
import contextlib
import numpy as np
import concourse.bass as bass
import concourse.mybir as mybir
from concourse.bass_utils import run_bass_kernel_spmd

F32 = mybir.dt.float32
BF16 = mybir.dt.bfloat16
I32 = mybir.dt.int32
U8 = mybir.dt.uint8
AF = mybir.ActivationFunctionType
ALU = mybir.AluOpType
AX = mybir.AxisListType

D = 1024
S = 2048
NT = 16
NE = 32
CAP = 384
NST = CAP // 128
DEPTH = 4
ALPHA = (2.0 * DEPTH) ** 0.25
LN_EPS = 1e-5
ENGS = ("pe", "act", "dve", "pool", "sp")


class Op:
    __slots__ = ("eng", "fn", "deps", "is_dma", "sig", "cnt", "sem", "target")

    def __init__(s, eng, fn, is_dma):
        s.eng = eng; s.fn = fn; s.deps = []; s.is_dma = is_dma
        s.sig = False; s.cnt = 0; s.sem = None; s.target = 0


class Prog:
    def __init__(s, nc):
        s.nc = nc
        s.ops = {e: [] for e in ENGS}
        s.last_w = {}
        s.readers = {}
        s.n_dma_sems = {"sp": 16, "act": 8, "pool": 16}
        s.since_barrier = []

    def _add(s, eng, fn, reads, writes, is_dma):
        op = Op(eng, fn, is_dma)
        deps = set()
        for k in reads:
            w = s.last_w.get(k)
            if w is not None:
                deps.add(w)
        for k in writes:
            w = s.last_w.get(k)
            if w is not None and not (w.eng == eng and eng == "pe" and not w.is_dma and not is_dma):
                deps.add(w)
        for k in writes:
            for r in s.readers.get(k, ()):
                if r.eng == eng and not r.is_dma and not is_dma:
                    continue
                deps.add(r)
        op.deps = list(deps)
        for k in reads:
            s.readers.setdefault(k, []).append(op)
        for k in writes:
            s.last_w[k] = op
            s.readers[k] = []
        s.ops[eng].append(op)
        s.since_barrier.append(op)
        return op

    def op(s, eng, fn, reads=(), writes=()):
        return s._add(eng, fn, reads, writes, False)

    def dma(s, eng, fn, reads=(), writes=()):
        return s._add(eng, fn, reads, writes, True)

    def barrier(s):
        prev = s.since_barrier
        s.since_barrier = []
        lastc = {}
        dmas = []
        for op in prev:
            if op.fn is None:
                continue
            if op.is_dma:
                dmas.append(op)
            else:
                lastc[op.eng] = op
        deps = list(lastc.values()) + dmas
        for e in ENGS:
            v = Op(e, None, False)
            v.deps = list(deps)
            s.ops[e].append(v)
        s.last_w = {}
        s.readers = {}

    def emit(s, final_wait_ops=()):
        nc = s.nc
        for e in ENGS:
            for op in s.ops[e]:
                for d in op.deps:
                    if not d.is_dma:
                        d.sig = True
        for e in ENGS:
            c = 0
            for op in s.ops[e]:
                if not op.is_dma and op.sig and op.fn is not None:
                    c += 1
                    op.cnt = c
        stack = contextlib.ExitStack()
        prog_sem = {}
        for e in ("pe", "act", "dve", "pool"):
            prog_sem[e] = stack.enter_context(nc.semaphore("prog_" + e))
        for q in ("sp", "act", "pool"):
            sems = [stack.enter_context(nc.semaphore(f"dq_{q}_{i}")) for i in range(s.n_dma_sems[q])]
            cnts = [0] * len(sems)
            prev = [None] * len(sems)
            i = 0
            for op in s.ops[q]:
                if op.is_dma:
                    j = i % len(sems)
                    i += 1
                    cnts[j] += 16
                    op.sem = sems[j]
                    op.target = cnts[j]
                    if prev[j] is not None:
                        op.deps.append(prev[j])
                    prev[j] = op
        engobj = {"pe": nc.tensor, "act": nc.scalar, "dve": nc.vector, "pool": nc.gpsimd, "sp": nc.sync}
        finals = list(final_wait_ops)

        def run_engine(e):
            eng = engobj[e]
            waited = {}
            for op in s.ops[e]:
                need = {}
                for d in op.deps:
                    if d.is_dma:
                        key = ("d", id(d.sem)); val = d.target; sem = d.sem
                    else:
                        key = ("c", d.eng); val = d.cnt; sem = prog_sem[d.eng]
                    if val > need.get(key, (0, None))[0]:
                        need[key] = (val, sem)
                for key, (val, sem) in need.items():
                    if waited.get(key, 0) >= val:
                        continue
                    waited[key] = val
                    eng.wait_ge(sem, val)
                if op.fn is None:
                    continue
                ins = op.fn(eng)
                if op.is_dma:
                    ins.then_inc(op.sem, 16)
                elif op.sig:
                    ins.then_inc(prog_sem[e], 1)
            if e == "sp":
                for d in finals:
                    eng.wait_ge(d.sem, d.target)

        with stack:
            with nc.Block() as block:
                @block.tensor
                def _(t):
                    run_engine("pe")

                @block.scalar
                def _(t):
                    run_engine("act")

                @block.vector
                def _(t):
                    run_engine("dve")

                @block.gpsimd
                def _(t):
                    run_engine("pool")

                @block.sync
                def _(t):
                    run_engine("sp")


class Ctx:
    pass


class Arena:
    def __init__(s, nc, base, limit):
        s.nc = nc; s.base = base; s.limit = limit; s.off = base; s.n = 0

    def reset(s, off=None):
        s.off = s.base if off is None else off

    def alloc(s, name, shape, dtype):
        sz = int(np.prod(shape[1:])) * mybir.dt.size(dtype)
        sz = (sz + 63) // 64 * 64
        assert s.off + sz <= s.limit, (name, s.off, sz, s.limit)
        s.n += 1
        t = s.nc.alloc_sbuf_tensor_at(f"{name}_{s.n}", list(shape), dtype, offset=s.off)
        s.off += sz
        return t


def build(plan, weights_shapes):
    nc = bass.Bass("TRN2", target_bir_lowering=False)
    c = Ctx()
    c.nc = nc
    P = Prog(nc)
    c.P = P
    dr = {}
    dr["x"] = nc.dram_tensor("x", [S, D], F32, kind="ExternalInput").ap()
    for name, shp in weights_shapes.items():
        dr[name] = nc.dram_tensor(name, list(shp), F32, kind="ExternalInput").ap()
    dr["c_ident"] = nc.dram_tensor("c_ident", [128, 128], F32, kind="ExternalInput").ap()
    dr["c_tri"] = nc.dram_tensor("c_tri", [128, 128], F32, kind="ExternalInput").ap()
    dr["c_ec"] = nc.dram_tensor("c_ec", [128, NE], F32, kind="ExternalInput").ap()
    rc, _ = ret_consts()
    for k_, v_ in rc.items():
        dr[k_] = nc.dram_tensor(k_, list(v_.shape), F32, kind="ExternalInput").ap()
    for k_, v_ in att_consts().items():
        dr[k_] = nc.dram_tensor(k_, list(v_.shape), F32, kind="ExternalInput").ap()
    dr["att_o"] = nc.dram_tensor("att_o_scr", [3, S, D], F32, kind="Internal").ap()
    dr["att_ms"] = nc.dram_tensor("att_ms_scr", [3, S, 16, 2], F32, kind="Internal").ap()
    dr["out"] = nc.dram_tensor("out", [S, D], F32, kind="ExternalOutput").ap()
    dr["xg"] = nc.dram_tensor("xg_scr", [NE * CAP, D], BF16, kind="Internal").ap()
    dr["yy"] = nc.dram_tensor("yy_scr", [NE * CAP, D], F32, kind="Internal").ap()
    c.dr = dr

    slab = nc.alloc_sbuf_tensor("arena_slab", [128, 206 * 1024], U8)
    base = nc.lookup_mloc(slab).addr
    A = Arena(nc, base, base + 206 * 1024)
    c.A = A
    c.X = A.alloc("X", [128, NT, D], F32)
    c.XT = A.alloc("XT", [128, 8, S], BF16)
    c.xt_off = A.off - 8 * S * 2
    c.ident_f = A.alloc("ident_f", [128, 128], F32)
    c.ident_b = A.alloc("ident_b", [128, 128], BF16)
    c.tri_b = A.alloc("tri_b", [128, 128], BF16)
    c.ones_b = A.alloc("ones_b", [128, 128], BF16)
    c.ec = A.alloc("ec", [128, NE], F32)
    c.tri_f = A.alloc("tri_f", [128, 128], F32)
    c.eps_t = A.alloc("eps", [128, 1], F32)
    c.one_t = A.alloc("one", [128, 1], F32)
    c.ADRI = A.alloc("ADRI", [128, NT, 4], I32)
    c.G = A.alloc("G", [128, NT, 4], F32)
    c.phase_base = A.off
    c.ps = [nc.alloc_psum_tensor(f"ps{i}", [128, 512], F32) for i in range(8)]
    c.bank = 0

    X = c.X
    P.dma("sp", lambda e: e.dma_start(out=c.ident_f[:], in_=dr["c_ident"]), writes=[("ident_f",)])
    P.dma("sp", lambda e: e.dma_start(out=c.tri_f[:], in_=dr["c_tri"]), writes=[("tri_f",)])
    P.dma("sp", lambda e: e.dma_start(out=c.ec[:], in_=dr["c_ec"]), writes=[("ec",)])
    P.op("dve", lambda e: e.tensor_copy(out=c.ident_b[:], in_=c.ident_f[:]), reads=[("ident_f",)], writes=[("ident_b",)])
    P.op("dve", lambda e: e.tensor_copy(out=c.tri_b[:], in_=c.tri_f[:]), reads=[("tri_f",)], writes=[("tri_b",)])
    P.op("dve", lambda e: e.memset(c.ones_b[:], 1.0), writes=[("ones_b",)])
    P.op("dve", lambda e: e.memset(c.eps_t[:], LN_EPS), writes=[("eps",)])
    P.op("dve", lambda e: e.memset(c.one_t[:], 1.0), writes=[("one",)])
    for i in range(NT):
        P.dma("sp", lambda e, i=i: e.dma_start(out=X[:, i, :], in_=dr["x"][i * 128:(i + 1) * 128, :]),
              writes=[("X", i)])

    for ph in plan:
        A.reset(c.phase_base)
        if ph[0] == "prep":
            phase_prep(c)
        elif ph[0] == "moe":
            phase_moe(c, ph[1], do_route_prep=ph[2])
        elif ph[0] == "rglru":
            phase_rglru(c, ph[1], ph[2])
        elif ph[0] == "ret":
            phase_ret(c, ph[1], ph[2])
        elif ph[0] == "attn":
            phase_attn(c, ph[1], ph[2])
        else:
            raise ValueError(ph)
        P.barrier()

    outs = []
    for i in range(NT):
        outs.append(P.dma("sp", lambda e, i=i: e.dma_start(out=dr["out"][i * 128:(i + 1) * 128, :], in_=X[:, i, :]),
                          reads=[("X", i)]))
    P.emit(final_wait_ops=outs)
    return nc


def next_bank(c, n=8):
    b = c.bank % n
    c.bank = (c.bank + 1) % n
    return b


def load_ln_params(c, layer, which, tag):
    A, P, dr = c.A, c.P, c.dr
    g = A.alloc("lng" + tag, [128, D], F32)
    b = A.alloc("lnb" + tag, [128, D], F32)
    P.dma("sp", lambda e: e.dma_start(out=g[:], in_=dr["ln_gain"][layer, which, :].partition_broadcast(128)),
          writes=[("lng", tag)])
    P.dma("sp", lambda e: e.dma_start(out=b[:], in_=dr["ln_bias"][layer, which, :].partition_broadcast(128)),
          writes=[("lnb", tag)])
    return g, b


def alloc_ln_tmp(c):
    A = c.A
    t = Ctx()
    t.st = [A.alloc("lnst", [128, 2, 6], F32) for _ in range(2)]
    t.mv = [A.alloc("lnmv", [128, 2], F32) for _ in range(2)]
    t.sd = [A.alloc("lnsd", [128, 1], F32) for _ in range(2)]
    t.rs = [A.alloc("lnrs", [128, 1], F32) for _ in range(2)]
    t.xn = [A.alloc("lnxn", [128, D], F32) for _ in range(2)]
    return t


def emit_ln(c, t, Z, zkey, i, g, b, tag):
    P = c.P
    X = c.X
    p = i % 2
    st, mv, sd, rs, xn = t.st[p], t.mv[p], t.sd[p], t.rs[p], t.xn[p]
    for h in range(2):
        P.op("dve", lambda e, h=h: e.bn_stats(out=st[:, h, :], in_=Z[:, h * 512:(h + 1) * 512]),
             reads=[zkey], writes=[("lnst", p, h)])
    P.op("dve", lambda e: e.bn_aggr(out=mv[:], in_=st[:].rearrange("p a b -> p (a b)")),
         reads=[("lnst", p, 0), ("lnst", p, 1)], writes=[("lnmv", p)])
    P.op("act", lambda e: e.activation(out=sd[:], in_=mv[:, 1:2], func=AF.Sqrt, bias=c.eps_t[:], scale=1.0),
         reads=[("lnmv", p), ("eps",)], writes=[("lnsd", p)])
    P.op("dve", lambda e: e.reciprocal(out=rs[:], in_=sd[:]), reads=[("lnsd", p)], writes=[("lnrs", p)])
    P.op("dve", lambda e: e.tensor_scalar(out=xn[:], in0=Z, scalar1=mv[:, 0:1], scalar2=rs[:, 0:1],
                                          op0=ALU.subtract, op1=ALU.mult),
         reads=[zkey, ("lnmv", p), ("lnrs", p)], writes=[("lnxn", p)])
    P.op("dve", lambda e: e.tensor_tensor(out=xn[:], in0=xn[:], in1=g[:], op=ALU.mult),
         reads=[("lnxn", p), ("lng", tag)], writes=[("lnxn", p)])
    P.op("dve", lambda e: e.tensor_tensor(out=X[:, i, :], in0=xn[:], in1=b[:], op=ALU.add),
         reads=[("lnxn", p), ("lnb", tag)], writes=[("X", i)])


def alloc_prep(c):
    A = c.A
    t = Ctx()
    t.xtf = [A.alloc("xtf", [128, 8, 128], F32) for _ in range(2)]
    return t


def emit_transpose_tile(c, t, i, write_xt=True):
    P = c.P
    X, XT = c.X, c.XT
    p = i % 2
    xtf = t.xtf[p]
    for half in range(2):
        bk = next_bank(c)
        ps = c.ps[bk]
        for q in range(4):
            ch = half * 4 + q
            P.op("pe", lambda e, ch=ch, q=q, ps=ps: e.transpose(out=ps[:, q * 128:(q + 1) * 128],
                                                               in_=X[:, i, ch * 128:(ch + 1) * 128],
                                                               identity=c.ident_f[:]),
                 reads=[("X", i), ("ident_f",)], writes=[("ps", bk)])
        P.op("act", lambda e, half=half, ps=ps: e.activation(
            out=xtf[:, half * 4:(half + 1) * 4, :], in_=ps[:].rearrange("p (a b) -> p a b", a=4), func=AF.Copy),
            reads=[("ps", bk)], writes=[("xtf", p, half)])
        if write_xt:
            P.op("act", lambda e, half=half, ps=ps: e.activation(
                out=XT[:, half * 4:(half + 1) * 4, i * 128:(i + 1) * 128], in_=ps[:].rearrange("p (a b) -> p a b", a=4), func=AF.Copy),
                reads=[("ps", bk)], writes=[("XT", i)])


def phase_prep(c):
    t = alloc_prep(c)
    zt = c.A.alloc("zt", [128, NST, D], BF16)
    c.P.op("pool", lambda e: e.memset(zt[:], 0.0), writes=[("zt",)])
    for e_ in range(NE):
        c.P.dma("sp", lambda e, e_=e_: e.dma_start(out=c.dr["xg"][e_ * CAP:(e_ + 1) * CAP, :].rearrange("(t p) d -> p t d", p=128), in_=zt[:]),
                reads=[("zt",)], writes=[("xg_zero", e_)])
    for i in range(NT):
        emit_transpose_tile(c, t, i)


def alloc_route(c, layer):
    A, P, dr = c.A, c.P, c.dr
    r = Ctx()
    r.wr = A.alloc("wr", [128, 8, NE], F32)
    r.br = A.alloc("br", [128, NE], F32)
    r.L = A.alloc("L", [128, NT, NE], F32)
    r.T8 = A.alloc("T8", [128, NT, 8], F32)
    r.M = A.alloc("M", [128, NT, NE], BF16)
    r.CUM = A.alloc("CUM", [128, NT, NE], BF16)
    r.At = [A.alloc("At", [128, NE], F32) for _ in range(2)]
    r.junk = A.alloc("junk", [128, 4, NE], F32)
    r.ADRF = A.alloc("ADRF", [128, NT, 4], F32)
    r.ADRI = c.ADRI
    r.G = c.G
    r.E4 = A.alloc("E4", [128, NT, 4], F32)
    r.nmx = A.alloc("nmx", [128, NT], F32)
    r.sm = A.alloc("sm", [128, NT], F32)
    r.rsm = A.alloc("rsm", [128, NT], F32)
    r.XB = [A.alloc("XB", [128, D], BF16) for _ in range(2)]
    P.dma("sp", lambda e: e.dma_start(out=r.wr[:], in_=dr["moe_w_router"][layer].rearrange("(k p) n -> p k n", p=128)),
          writes=[("wr",)])
    P.dma("sp", lambda e: e.dma_start(out=r.br[:], in_=dr["moe_b_router"][layer, :].partition_broadcast(128)),
          writes=[("br",)])
    return r


def emit_route_tile1(c, r, t, i):
    P, dr = c.P, c.dr
    X = c.X
    p = i % 2
    xtf = t.xtf[p]
    bk = next_bank(c)
    ps = c.ps[bk]
    for k in range(8):
        P.op("pe", lambda e, k=k: e.matmul(ps[:, 0:NE], lhsT=xtf[:, k, :], rhs=r.wr[:, k, :], start=(k == 0), stop=(k == 7)),
             reads=[("xtf", p, k // 4), ("wr",)], writes=[("ps", bk)])
    P.op("dve", lambda e: e.tensor_tensor(out=r.L[:, i, :], in0=ps[:, 0:NE], in1=r.br[:], op=ALU.add),
         reads=[("ps", bk), ("br",)], writes=[("L", i)])
    P.op("dve", lambda e: e.max(out=r.T8[:, i, :], in_=r.L[:, i, :]), reads=[("L", i)], writes=[("T8", i)])
    P.op("dve", lambda e: e.tensor_scalar(out=r.M[:, i, :], in0=r.L[:, i, :], scalar1=r.T8[:, i, 3:4], scalar2=None,
                                          op0=ALU.is_ge),
         reads=[("L", i), ("T8", i)], writes=[("M", i)])


def emit_route_tile(c, r, t, i):
    emit_route_tile1(c, r, t, i)
    emit_route_tile2(c, r, t, i)


def emit_route_tile2(c, r, t, i):
    P, dr = c.P, c.dr
    X = c.X
    p = i % 2
    bk2 = next_bank(c)
    ps2 = c.ps[bk2]
    P.op("pe", lambda e: e.matmul(ps2[:, 0:NE], lhsT=c.tri_b[:], rhs=r.M[:, i, :], start=True, stop=(i == 0)),
         reads=[("tri_b",), ("M", i)], writes=[("ps", bk2)])
    if i > 0:
        P.op("pe", lambda e: e.matmul(ps2[:, 0:NE], lhsT=c.ones_b[:], rhs=r.CUM[:, i - 1, :], start=False, stop=True),
             reads=[("ones_b",), ("CUM", i - 1)], writes=[("ps", bk2)])
        P.op("pool", lambda e: e.tensor_tensor(out=r.CUM[:, i, :], in0=r.CUM[:, i - 1, :], in1=r.M[:, i, :], op=ALU.add),
             reads=[("CUM", i - 1), ("M", i)], writes=[("CUM", i)])
    else:
        P.op("pool", lambda e: e.tensor_copy(out=r.CUM[:, 0, :], in_=r.M[:, 0, :]), reads=[("M", 0)], writes=[("CUM", 0)])
    At = r.At[p]
    P.op("dve", lambda e: e.tensor_tensor(out=At[:], in0=ps2[:, 0:NE], in1=c.ec[:], op=ALU.add),
         reads=[("ps", bk2), ("ec",)], writes=[("At", p)])
    for k in range(4):
        P.op("dve", lambda e, k=k: e.scalar_tensor_tensor(out=r.junk[:, k, :], in0=r.L[:, i, :], scalar=r.T8[:, i, k:k + 1],
                                                          in1=At[:], op0=ALU.is_equal, op1=ALU.mult),
             reads=[("L", i), ("T8", i), ("At", p)], writes=[("junk", k)])
    P.op("dve", lambda e: e.reduce_sum(out=r.ADRF[:, i, :], in_=r.junk[:], axis=AX.X),
         reads=[("junk", k) for k in range(4)], writes=[("ADRF", i)])
    P.op("dve", lambda e: e.tensor_copy(out=r.ADRI[:, i, :], in_=r.ADRF[:, i, :]),
         reads=[("ADRF", i)], writes=[("ADRI", i)])
    P.op("dve", lambda e: e.tensor_scalar(out=r.nmx[:, i:i + 1], in0=r.T8[:, i, 0:1], scalar1=-1.0, scalar2=None, op0=ALU.mult),
         reads=[("T8", i)], writes=[("nmx", i)])
    P.op("act", lambda e: e.activation(out=r.E4[:, i, :], in_=r.T8[:, i, 0:4], func=AF.Exp, bias=r.nmx[:, i:i + 1], scale=1.0),
         reads=[("T8", i), ("nmx", i)], writes=[("E4", i)])
    P.op("dve", lambda e: e.reduce_sum(out=r.sm[:, i:i + 1], in_=r.E4[:, i, :], axis=AX.X), reads=[("E4", i)], writes=[("sm", i)])
    P.op("dve", lambda e: e.reciprocal(out=r.rsm[:, i:i + 1], in_=r.sm[:, i:i + 1]), reads=[("sm", i)], writes=[("rsm", i)])
    P.op("dve", lambda e: e.tensor_scalar(out=r.G[:, i, :], in0=r.E4[:, i, :], scalar1=r.rsm[:, i:i + 1], scalar2=None, op0=ALU.mult),
         reads=[("E4", i), ("rsm", i)], writes=[("G", i)])
    XB = r.XB[p]
    P.op("act", lambda e: e.activation(out=XB[:], in_=X[:, i, :], func=AF.Copy), reads=[("X", i)], writes=[("XB", p)])
    for k in range(4):
        P.dma("pool", lambda e, k=k: e.indirect_dma_start(
            out=dr["xg"], out_offset=bass.IndirectOffsetOnAxis(ap=r.ADRI[:, i, k:k + 1], axis=0),
            in_=XB[:], in_offset=None),
            reads=[("XB", p), ("ADRI", i)], writes=[("xg_dram", i, k)])


def phase_moe(c, layer, do_route_prep):
    A, P, dr = c.A, c.P, c.dr
    X, XT = c.X, c.XT
    nc = c.nc
    r = alloc_route(c, layer) if do_route_prep else None
    t = alloc_prep(c)
    if do_route_prep:
        for i in range(NT):
            emit_transpose_tile(c, t, i, write_xt=False)
            emit_route_tile(c, r, t, i)
    mark = A.off
    RING = 6 if do_route_prep else 7
    wring = [A.alloc("wring", [128, 8, 512], BF16) for _ in range(RING)]
    bgu = A.alloc("bgu", [128, 16, NE], F32)
    braw = A.alloc("braw", [NE, 2 * D], F32)
    P.dma("sp", lambda e: e.dma_start(out=braw[:], in_=dr["moe_b_gu"][layer]), writes=[("braw",)])
    bkb = next_bank(c)
    for cc in range(16):
        P.op("pe", lambda e, cc=cc: e.transpose(out=c.ps[bkb][:, cc * NE:(cc + 1) * NE], in_=braw[:, cc * 128:(cc + 1) * 128],
                                                identity=c.ident_f[0:NE, 0:NE]),
             reads=[("braw",), ("ident_f",)], writes=[("ps", bkb)])
    P.op("dve", lambda e: e.tensor_copy(out=bgu[:].rearrange("p a b -> p (a b)"), in_=c.ps[bkb][:, :]),
         reads=[("ps", bkb)], writes=[("bgu",)])
    P.op("dve", lambda e: e.tensor_scalar(out=bgu[:, 8:16, :], in0=bgu[:, 8:16, :], scalar1=1.0, scalar2=None, op0=ALU.add),
         reads=[("bgu",)], writes=[("bgu",)])
    bd = [A.alloc("bd", [128, D], F32) for _ in range(2)]
    ytile = [A.alloc("ytile", [128, D], F32) for _ in range(2)]
    gt = [A.alloc("gt", [128, CAP], F32) for _ in range(2)]
    ut = [A.alloc("ut", [128, CAP], F32) for _ in range(2)]
    sg = [A.alloc("sg", [128, CAP], F32) for _ in range(2)]
    A2 = Arena(nc, c.xt_off, c.xt_off + 8 * S * 2)
    A2.n = 1000
    xgtok = [A2.alloc("xgtok", [128, NST, D], BF16) for _ in range(2)]
    xgT = [A2.alloc("xgT", [128, 8, CAP], BF16) for _ in range(2)]
    hT = A2.alloc("hT", [128, 8, CAP], BF16)

    pieces = []
    for e_ in range(NE):
        for pc in (0, 2, 1, 3):
            pieces.append((e_, "gu", pc))
        for pc in (0, 1):
            pieces.append((e_, "dn", pc))
    piece_slot = {}

    def issue_piece(n):
        if n >= len(pieces):
            return
        e_, kind, pc = pieces[n]
        slot = n % RING
        piece_slot[(e_, kind, pc)] = slot
        if kind == "gu":
            src = dr["moe_w_gu"][layer, e_, :, pc * 512:(pc + 1) * 512]
            P.dma("pool", lambda e, src=src, slot=slot: e.dma_start(out=wring[slot][:], in_=src.rearrange("(k p) n -> p k n", p=128)),
                  writes=[("wring", slot)])
        else:
            src = dr["moe_w_down"][layer, e_, pc * 512:(pc + 1) * 512, :]
            P.dma("pool", lambda e, src=src, slot=slot: e.dma_start(
                out=wring[slot][:].rearrange("p k n -> p (k n)").rearrange("p (k n) -> p k n", k=4),
                in_=src.rearrange("(k p) n -> p k n", p=128)),
                writes=[("wring", slot)])

    PRE = RING - 1
    for n in range(PRE):
        issue_piece(n)
    nissued = [PRE]
    def ex_load(e_):
        pe2 = e_ % 2
        P.dma("sp", lambda e, e_=e_: e.dma_start(out=xgtok[e_ % 2][:], in_=dr["xg"][e_ * CAP:(e_ + 1) * CAP, :].rearrange("(t p) d -> p t d", p=128)),
              reads=[("xg_dram", i_, k_) for i_ in range(NT) for k_ in range(4)], writes=[("xgtok", e_ % 2)])
        P.dma("sp", lambda e, e_=e_, pe2=pe2: e.dma_start(out=bd[pe2][:], in_=dr["moe_b_down"][layer, e_, :].partition_broadcast(128)),
              writes=[("bd", pe2)])
        for st in range(NST):
            for half in range(2):
                bk = next_bank(c)
                psb = c.ps[bk][:].bitcast(BF16)
                for q in range(4):
                    ch = half * 4 + q
                    P.op("pe", lambda e, st=st, ch=ch, q=q, psb=psb: e.transpose(
                        out=psb[:, q * 128:(q + 1) * 128], in_=xgtok[pe2][:, st, ch * 128:(ch + 1) * 128], identity=c.ident_b[:]),
                        reads=[("xgtok", pe2), ("ident_b",)], writes=[("ps", bk)])
                P.op("act", lambda e, st=st, half=half, psb=psb, pe2=pe2: e.activation(
                    out=xgT[pe2][:, half * 4:(half + 1) * 4, st * 128:(st + 1) * 128],
                    in_=psb[:, 0:512].rearrange("p (a b) -> p a b", a=4), func=AF.Copy),
                    reads=[("ps", bk)], writes=[("xgT", pe2)])

    def ex_gu(e_):
        pe2 = e_ % 2
        for fc in range(8):
            pcg = fc // 4
            pcu = 2 + fc // 4
            lc = (fc % 4) * 128
            if fc % 4 == 0:
                pass
            sg_ = piece_slot[(e_, "gu", pcg)]
            su_ = piece_slot[(e_, "gu", pcu)]
            bkg = next_bank(c); bku = next_bank(c)
            psg, psu = c.ps[bkg], c.ps[bku]
            for k in range(8):
                P.op("pe", lambda e, k=k, sg_=sg_, lc=lc, psg=psg, pe2=pe2: e.matmul(
                    psg[:, 0:CAP], lhsT=wring[sg_][:, k, lc:lc + 128], rhs=xgT[pe2][:, k, :], start=(k == 0), stop=(k == 7)),
                    reads=[("wring", sg_), ("xgT", pe2)], writes=[("ps", bkg)])
            for k in range(8):
                P.op("pe", lambda e, k=k, su_=su_, lc=lc, psu=psu, pe2=pe2: e.matmul(
                    psu[:, 0:CAP], lhsT=wring[su_][:, k, lc:lc + 128], rhs=xgT[pe2][:, k, :], start=(k == 0), stop=(k == 7)),
                    reads=[("wring", su_), ("xgT", pe2)], writes=[("ps", bku)])
            pp = fc % 2
            P.op("dve", lambda e, psg=psg, fc=fc, pp=pp, e_=e_: e.tensor_scalar(
                out=gt[pp][:], in0=psg[:, 0:CAP], scalar1=bgu[:, fc, e_:e_ + 1], scalar2=7.0, op0=ALU.add, op1=ALU.min),
                reads=[("ps", bkg), ("bgu",)], writes=[("gt", pp)])
            P.op("dve", lambda e, psu=psu, fc=fc, pp=pp, e_=e_: e.tensor_scalar(
                out=ut[pp][:], in0=psu[:, 0:CAP], scalar1=bgu[:, 8 + fc, e_:e_ + 1], scalar2=8.0, op0=ALU.add, op1=ALU.min),
                reads=[("ps", bku), ("bgu",)], writes=[("ut", pp)])
            P.op("act", lambda e, pp=pp: e.activation(out=sg[pp][:], in_=gt[pp][:], func=AF.Silu, scale=1.702),
                 reads=[("gt", pp)], writes=[("sg", pp)])
            P.op("dve", lambda e, pp=pp, fc=fc: e.scalar_tensor_tensor(out=hT[:, fc, :], in0=ut[pp][:], scalar=-6.0, in1=sg[pp][:],
                                                                    op0=ALU.max, op1=ALU.mult),
                 reads=[("ut", pp), ("sg", pp)], writes=[("hT", fc)])
            if fc % 4 == 3:
                issue_piece(nissued[0]); issue_piece(nissued[0] + 1)
                nissued[0] += 2

    def ex_down(e_):
        pe2 = e_ % 2
        for st in range(NST):
            yp = st % 2
            for nh in range(2):
                bk = next_bank(c)
                psd = c.ps[bk]
                for fc in range(8):
                    sd_ = piece_slot[(e_, "dn", fc // 4)]
                    o_ = (fc % 4) * 1024 + nh * 512
                    P.op("pe", lambda e, fc=fc, st=st, sd_=sd_, psd=psd, o_=o_: e.matmul(
                        psd[:, :], lhsT=hT[:, fc, st * 128:(st + 1) * 128], rhs=wring[sd_][:].rearrange("p k n -> p (k n)")[:, o_:o_ + 512],
                        start=(fc == 0), stop=(fc == 7)),
                        reads=[("hT", fc), ("wring", sd_)], writes=[("ps", bk)])
                P.op("dve", lambda e, nh=nh, yp=yp, psd=psd, pe2=pe2: e.scalar_tensor_tensor(
                    out=ytile[yp][:, nh * 512:(nh + 1) * 512], in0=psd[:, :], scalar=1.0 / 1.702, in1=bd[pe2][:, nh * 512:(nh + 1) * 512],
                    op0=ALU.mult, op1=ALU.add),
                    reads=[("ps", bk), ("bd", pe2)], writes=[("ytile", yp, nh)])
            P.dma("sp", lambda e, e_=e_, st=st, yp=yp: e.dma_start(
                out=dr["yy"][e_ * CAP + st * 128:e_ * CAP + (st + 1) * 128, :], in_=ytile[yp][:]),
                reads=[("ytile", yp, 0), ("ytile", yp, 1)], writes=[("yy_dram", e_, st)])

    ex_load(0)
    for e_ in range(NE):
        ex_gu(e_)
        if e_ + 1 < NE:
            ex_load(e_ + 1)
        ex_down(e_)
        issue_piece(nissued[0]); issue_piece(nissued[0] + 1)
        nissued[0] += 2

    P.barrier()
    A.reset(mark)
    g2, b2 = load_ln_params(c, layer, 1, "b")
    lt = alloc_ln_tmp(c)
    NYK = 3 if do_route_prep else 4
    YK = [[A.alloc("YK", [128, D], F32) for _ in range(4)] for _ in range(NYK)]
    Z = [A.alloc("Z", [128, D], F32) for _ in range(2)]
    t2 = t
    for i in range(NT):
        p = i % 2
        py = i % NYK
        for k in range(4):
            P.dma("pool", lambda e, k=k, py=py, i=i: e.indirect_dma_start(
                out=YK[py][k][:], out_offset=None, in_=dr["yy"],
                in_offset=bass.IndirectOffsetOnAxis(ap=c.ADRI[:, i, k:k + 1], axis=0)),
                reads=[("ADRI", i)], writes=[("YK", py, k)])
        P.op("dve", lambda e, p=p, py=py, i=i: e.tensor_scalar(out=Z[p][:], in0=YK[py][0][:], scalar1=c.G[:, i, 0:1], scalar2=None, op0=ALU.mult),
             reads=[("YK", py, 0), ("G", i)], writes=[("Z", p)])
        for k in range(1, 4):
            P.op("dve", lambda e, p=p, py=py, i=i, k=k: e.scalar_tensor_tensor(
                out=Z[p][:], in0=YK[py][k][:], scalar=c.G[:, i, k:k + 1], in1=Z[p][:], op0=ALU.mult, op1=ALU.add),
                reads=[("YK", py, k), ("G", i), ("Z", p)], writes=[("Z", p)])
        P.op("dve", lambda e, p=p, i=i: e.scalar_tensor_tensor(
            out=Z[p][:], in0=X[:, i, :], scalar=ALPHA, in1=Z[p][:], op0=ALU.mult, op1=ALU.add),
            reads=[("X", i), ("Z", p)], writes=[("Z", p)])
        emit_ln(c, lt, Z[p][:], ("Z", p), i, g2, b2, "b")
        if i >= 1:
            emit_transpose_tile(c, t2, i - 1, write_xt=True)
    emit_transpose_tile(c, t2, NT - 1, write_xt=True)


def emit_mixer_epilogue(c, layer, YT, kc, wout_dram):
    A, P, dr = c.A, c.P, c.dr
    X = c.X
    wout = A.alloc("wout", [128, kc, D], BF16)
    for k2 in range(0, kc, 4):
        P.dma("pool", lambda e, k2=k2: e.dma_start(
            out=wout[:, k2:k2 + 4, :], in_=wout_dram[k2 * 128:(k2 + 4) * 128, :].rearrange("(k p) n -> p k n", p=128)),
            writes=[("wout", k2)])
    r = alloc_route(c, layer)
    g1, b1 = load_ln_params(c, layer, 0, "a")
    lt = alloc_ln_tmp(c)
    t = alloc_prep(c)
    Z = [A.alloc("Z", [128, D], F32) for _ in range(2)]
    for i in range(NT):
        p = i % 2
        for nh in range(2):
            bk = next_bank(c)
            ps = c.ps[bk]
            for k in range(kc):
                P.op("pe", lambda e, k=k, nh=nh, ps=ps, i=i: e.matmul(
                    ps[:, :], lhsT=YT[:, k, i * 128:(i + 1) * 128], rhs=wout[:, k, nh * 512:(nh + 1) * 512],
                    start=(k == 0), stop=(k == kc - 1)),
                    reads=[("YT", k), ("wout", (k // 4) * 4)], writes=[("ps", bk)])
            P.op("dve", lambda e, nh=nh, ps=ps, p=p, i=i: e.scalar_tensor_tensor(
                out=Z[p][:, nh * 512:(nh + 1) * 512], in0=X[:, i, nh * 512:(nh + 1) * 512], scalar=ALPHA, in1=ps[:, :],
                op0=ALU.mult, op1=ALU.add),
                reads=[("X", i), ("ps", bk)], writes=[("Z", p, nh)])
        P.op("dve", lambda e: e.engine_nop(), reads=[("Z", p, 0), ("Z", p, 1)], writes=[("Z", p)])
        emit_ln(c, lt, Z[p][:], ("Z", p), i, g1, b1, "a")
        if i >= 1:
            emit_transpose_tile(c, t, i - 1, write_xt=False)
            emit_route_tile1(c, r, t, i - 1)
        if i >= 2:
            emit_route_tile2(c, r, t, i - 2)
    emit_transpose_tile(c, t, NT - 1, write_xt=False)
    emit_route_tile1(c, r, t, NT - 1)
    emit_route_tile2(c, r, t, NT - 2)
    emit_route_tile2(c, r, t, NT - 1)


def load_small_vecs(c, rows, nvec):
    A, P = c.A, c.P
    braw = A.alloc("svraw", [nvec, D], F32)
    pv = A.alloc("pv", [128, 8, nvec], F32)
    for j, ap in enumerate(rows):
        P.dma("sp", lambda e, j=j, ap=ap: e.dma_start(out=braw[j:j + 1, :], in_=ap.unsqueeze(0)), writes=[("svraw", j)])
    bk = next_bank(c)
    for cc in range(8):
        P.op("pe", lambda e, cc=cc: e.transpose(out=c.ps[bk][:, cc * nvec:(cc + 1) * nvec], in_=braw[:, cc * 128:(cc + 1) * 128],
                                                identity=c.ident_f[0:nvec, 0:nvec]),
             reads=[("svraw", j) for j in range(nvec)] + [("ident_f",)], writes=[("ps", bk)])
    P.op("dve", lambda e: e.tensor_copy(out=pv[:].rearrange("p a b -> p (a b)"), in_=c.ps[bk][:, 0:8 * nvec]),
         reads=[("ps", bk)], writes=[("pv",)])
    return pv


def phase_rglru(c, layer, idx):
    A, P, dr = c.A, c.P, c.dr
    X, XT = c.X, c.XT
    TB = 512
    NTB = S // TB
    YT = A.alloc("YT", [128, 8, S], BF16)
    mark_after_yt = A.off
    pv = load_small_vecs(c, [dr["a_conv_w"][idx, 0], dr["a_conv_w"][idx, 1], dr["a_conv_w"][idx, 2], dr["a_conv_w"][idx, 3],
                             dr["a_conv_b"][idx], dr["a_b_rgate"][idx], dr["a_b_igate"][idx], dr["a_lambda"][idx]], 8)
    cv1 = A.alloc("cv1", [128, 8], F32)
    cv2 = A.alloc("cv2", [128, 8], F32)
    sp_e = A.alloc("sp_e", [128, 8], F32)
    sp_l = A.alloc("sp_l", [128, 8], F32)
    P.op("act", lambda e: e.activation(out=sp_e[:], in_=pv[:, :, 7], func=AF.Exp, scale=-1.0), reads=[("pv",)], writes=[("sp_e",)])
    P.op("act", lambda e: e.activation(out=sp_l[:], in_=sp_e[:], func=AF.Ln, bias=c.one_t[:], scale=1.0),
         reads=[("sp_e",), ("one",)], writes=[("sp_l",)])
    P.op("dve", lambda e: e.tensor_scalar(out=cv1[:], in0=sp_l[:], scalar1=-8.0, scalar2=None, op0=ALU.mult), reads=[("sp_l",)], writes=[("cv1",)])
    P.op("dve", lambda e: e.tensor_scalar(out=cv2[:], in0=sp_l[:], scalar1=-16.0, scalar2=None, op0=ALU.mult), reads=[("sp_l",)], writes=[("cv2",)])
    wg = A.alloc("wgr", [128, 8, 128], BF16)
    wi = A.alloc("wgi", [128, 8, 128], BF16)
    P.dma("pool", lambda e: e.dma_start(out=wg[:], in_=dr["a_w_rgate"][idx].rearrange("h i j -> i h j")), writes=[("wgr",)])
    P.dma("pool", lambda e: e.dma_start(out=wi[:], in_=dr["a_w_igate"][idx].rearrange("h i j -> i h j")), writes=[("wgi",)])
    wrec = [A.alloc("wrec", [128, 8, 128], BF16) for _ in range(2)]
    wgat = [A.alloc("wgat", [128, 8, 128], BF16) for _ in range(2)]
    RECp = [A.alloc("RECp", [128, 3 + S], F32) for _ in range(2)]
    nm = ["GX", "SQ", "SG", "CV", "R", "I", "AA", "OM", "H"]
    T = {n: [A.alloc(n, [128, TB], F32) for _ in range(2)] for n in nm}
    CVb = [A.alloc("CVb", [128, TB], BF16) for _ in range(2)]
    for q in range(2):
        P.op("pool", lambda e, q=q: e.memset(RECp[q][:, 0:3], 0.0), writes=[("RECp", q, -1)])
    def chunk_pre(cc):
        q = cc % 2
        P.dma("pool", lambda e, cc=cc, q=q: e.dma_start(
            out=wrec[q][:], in_=dr["a_w_in"][idx][:, D + cc * 128:D + (cc + 1) * 128].rearrange("(k p) n -> p k n", p=128)),
            writes=[("wrec", q)])
        P.dma("pool", lambda e, cc=cc, q=q: e.dma_start(
            out=wgat[q][:], in_=dr["a_w_in"][idx][:, cc * 128:(cc + 1) * 128].rearrange("(k p) n -> p k n", p=128)),
            writes=[("wgat", q)])
        for tb in range(NTB):
            bk = next_bank(c)
            ps = c.ps[bk]
            for k in range(8):
                P.op("pe", lambda e, k=k, ps=ps, tb=tb, q=q: e.matmul(ps[:, :], lhsT=wrec[q][:, k, :], rhs=XT[:, k, tb * TB:(tb + 1) * TB],
                                                                   start=(k == 0), stop=(k == 7)),
                     reads=[("wrec", q)] + [("XT", i_) for i_ in range(tb * 4, tb * 4 + 4)], writes=[("ps", bk)])
            P.op("act", lambda e, ps=ps, tb=tb, q=q: e.activation(out=RECp[q][:, 3 + tb * TB:3 + (tb + 1) * TB], in_=ps[:, :], func=AF.Copy),
                 reads=[("ps", bk)], writes=[("RECp", q, tb)])

    def blk_G(cc, tb, b_, q):
        t_ = {n: T[n][b_] for n in nm}
        k_ = lambda n: (n, b_)
        bk = next_bank(c)
        ps = c.ps[bk]
        for k in range(8):
            P.op("pe", lambda e, k=k, ps=ps, tb=tb, q=q: e.matmul(ps[:, :], lhsT=wgat[q][:, k, :], rhs=XT[:, k, tb * TB:(tb + 1) * TB],
                                                               start=(k == 0), stop=(k == 7)),
                 reads=[("wgat", q)] + [("XT", i_) for i_ in range(tb * 4, tb * 4 + 4)], writes=[("ps", bk)])
        P.op("act", lambda e, ps=ps, t_=t_: e.activation(out=t_["GX"][:], in_=ps[:, :], func=AF.Copy), reads=[("ps", bk)], writes=[k_("GX")])
        P.op("act", lambda e, ps=ps, t_=t_: e.activation(out=t_["SQ"][:], in_=ps[:, :], func=AF.Square), reads=[("ps", bk)], writes=[k_("SQ")])
        P.op("dve", lambda e, t_=t_: e.tensor_scalar(out=t_["SQ"][:], in0=t_["SQ"][:], scalar1=0.044715, scalar2=1.0, op0=ALU.mult, op1=ALU.add),
             reads=[k_("SQ")], writes=[k_("SQ")])
        P.op("dve", lambda e, t_=t_: e.tensor_tensor(out=t_["SQ"][:], in0=t_["SQ"][:], in1=t_["GX"][:], op=ALU.mult),
             reads=[k_("SQ"), k_("GX")], writes=[k_("SQ")])
        P.op("act", lambda e, t_=t_: e.activation(out=t_["SG"][:], in_=t_["SQ"][:], func=AF.Sigmoid, scale=1.5957691216057308),
             reads=[k_("SQ")], writes=[k_("SG")])
        P.op("pool", lambda e, t_=t_: e.tensor_tensor(out=t_["SG"][:], in0=t_["GX"][:], in1=t_["SG"][:], op=ALU.mult),
             reads=[k_("GX"), k_("SG")], writes=[k_("SG")])

    def blk_R(cc, tb, b_, q):
        t_ = {n: T[n][b_] for n in nm}
        k_ = lambda n: (n, b_)
        rk = [("RECp", q, tb)] + ([("RECp", q, tb - 1)] if tb > 0 else [("RECp", q, -1)])
        P.op("dve", lambda e, t_=t_, tb=tb, q=q, cc=cc: e.tensor_scalar(
            out=t_["CV"][:], in0=RECp[q][:, tb * TB:tb * TB + TB], scalar1=pv[:, cc, 0:1], scalar2=pv[:, cc, 4:5], op0=ALU.mult, op1=ALU.add),
            reads=rk + [("pv",)], writes=[k_("CV")])
        for j in range(1, 4):
            P.op("dve", lambda e, t_=t_, tb=tb, q=q, cc=cc, j=j: e.scalar_tensor_tensor(
                out=t_["CV"][:], in0=RECp[q][:, tb * TB + j:tb * TB + j + TB], scalar=pv[:, cc, j:j + 1], in1=t_["CV"][:], op0=ALU.mult, op1=ALU.add),
                reads=rk + [("pv",), k_("CV")], writes=[k_("CV")])
        P.op("act", lambda e, t_=t_, b_=b_: e.activation(out=CVb[b_][:], in_=t_["CV"][:], func=AF.Copy), reads=[k_("CV")], writes=[("CVb", b_)])
        bkr = next_bank(c); bki = next_bank(c)
        P.op("pe", lambda e, b_=b_, cc=cc, bkr=bkr: e.matmul(c.ps[bkr][:, :], lhsT=wg[:, cc, :], rhs=CVb[b_][:], start=True, stop=True),
             reads=[("wgr",), ("CVb", b_)], writes=[("ps", bkr)])
        P.op("pe", lambda e, b_=b_, cc=cc, bki=bki: e.matmul(c.ps[bki][:, :], lhsT=wi[:, cc, :], rhs=CVb[b_][:], start=True, stop=True),
             reads=[("wgi",), ("CVb", b_)], writes=[("ps", bki)])
        P.op("act", lambda e, t_=t_, cc=cc, bkr=bkr: e.activation(out=t_["R"][:], in_=c.ps[bkr][:, :], func=AF.Sigmoid, bias=pv[:, cc, 5:6], scale=1.0),
             reads=[("ps", bkr), ("pv",)], writes=[k_("R")])
        P.op("act", lambda e, t_=t_, cc=cc, bki=bki: e.activation(out=t_["I"][:], in_=c.ps[bki][:, :], func=AF.Sigmoid, bias=pv[:, cc, 6:7], scale=1.0),
             reads=[("ps", bki), ("pv",)], writes=[k_("I")])
        P.op("act", lambda e, t_=t_, cc=cc: e.activation(out=t_["AA"][:], in_=t_["R"][:], func=AF.Exp, scale=cv1[:, cc:cc + 1]),
             reads=[k_("R"), ("cv1",)], writes=[k_("AA")])
        P.op("act", lambda e, t_=t_, cc=cc: e.activation(out=t_["OM"][:], in_=t_["R"][:], func=AF.Exp, scale=cv2[:, cc:cc + 1]),
             reads=[k_("R"), ("cv2",)], writes=[k_("OM")])
        P.op("dve", lambda e, t_=t_: e.tensor_scalar(out=t_["OM"][:], in0=t_["OM"][:], scalar1=-1.0, scalar2=1.0, op0=ALU.mult, op1=ALU.add),
             reads=[k_("OM")], writes=[k_("OM")])
        P.op("act", lambda e, t_=t_: e.activation(out=t_["OM"][:], in_=t_["OM"][:], func=AF.Sqrt), reads=[k_("OM")], writes=[k_("OM")])
        P.op("pool", lambda e, t_=t_: e.tensor_tensor(out=t_["I"][:], in0=t_["I"][:], in1=t_["CV"][:], op=ALU.mult),
             reads=[k_("I"), k_("CV")], writes=[k_("I")])
        P.op("dve", lambda e, t_=t_: e.tensor_tensor(out=t_["I"][:], in0=t_["I"][:], in1=t_["OM"][:], op=ALU.mult),
             reads=[k_("I"), k_("OM")], writes=[k_("I")])
        if tb == 0:
            P.op("dve", lambda e, t_=t_: e.tensor_tensor_scan(out=t_["H"][:], data0=t_["AA"][:], data1=t_["I"][:], initial=0.0,
                                                              op0=ALU.mult, op1=ALU.add),
                 reads=[k_("AA"), k_("I")], writes=[k_("H")])
        else:
            hp = T["H"][1 - b_]
            P.op("dve", lambda e, t_=t_, hp=hp: e.tensor_tensor_scan(out=t_["H"][:], data0=t_["AA"][:], data1=t_["I"][:],
                                                                     initial=hp[:, TB - 1:TB], op0=ALU.mult, op1=ALU.add),
                 reads=[k_("AA"), k_("I"), ("H", 1 - b_)], writes=[k_("H")])
        P.op("pool", lambda e, t_=t_, cc=cc, tb=tb: e.tensor_tensor(out=YT[:, cc, tb * TB:(tb + 1) * TB], in0=t_["SG"][:], in1=t_["H"][:], op=ALU.mult),
             reads=[k_("SG"), k_("H")], writes=[("YT", cc)])

    blocks = [(cc, tb, (cc * NTB + tb) % 2, cc % 2) for cc in range(8) for tb in range(NTB)]
    chunk_pre(0)
    blk_G(*blocks[0])
    for n_ in range(len(blocks)):
        if n_ + 1 < len(blocks):
            if blocks[n_ + 1][1] == 0:
                chunk_pre(blocks[n_ + 1][0])
            blk_G(*blocks[n_ + 1])
        blk_R(*blocks[n_])
    P.barrier()
    A.reset(mark_after_yt)
    emit_mixer_epilogue(c, layer, YT, 8, dr["a_w_out"][idx])


RET_H = 4
RET_EPS = 1e-6


def ret_consts():
    f32 = np.float32
    log_gamma = np.log1p(-np.exp2(-5.0 - np.arange(RET_H, dtype=f32))).astype(f32)
    pos = np.arange(128, dtype=f32)
    rel = pos[:, None] - pos[None, :]
    intra = np.where(rel >= 0, np.exp(log_gamma[:, None, None] * np.maximum(rel, 0.0)), 0.0).astype(f32)
    dt = np.ascontiguousarray(intra.transpose(0, 2, 1))
    qd = np.exp(log_gamma[:, None] * (pos + 1.0)).astype(f32)
    kd = np.exp(log_gamma[:, None] * (127.0 - pos)).astype(f32)
    cd = np.exp(log_gamma * 128.0).astype(f32)
    return {"c_ret_dt": np.ascontiguousarray(dt.transpose(1, 0, 2)),
            "c_ret_qd": np.ascontiguousarray(np.tile(qd[None], (128, 1, 1))),
            "c_ret_kd": np.ascontiguousarray((kd.T / 16.0).astype(f32)),
            }, [float(v) for v in cd]


def phase_ret(c, layer, idx):
    A, P, dr = c.A, c.P, c.dr
    X, XT = c.X, c.XT
    _, CDV = ret_consts()
    DT = A.alloc("rDT", [128, RET_H, 128], F32)
    QD = A.alloc("rQD", [128, RET_H, 128], F32)
    KD = A.alloc("rKD", [128, RET_H], F32)
    eps6 = A.alloc("eps6", [128, 1], F32)
    P.dma("sp", lambda e: e.dma_start(out=DT[:], in_=dr["c_ret_dt"]), writes=[("rDT",)])
    P.dma("sp", lambda e: e.dma_start(out=QD[:], in_=dr["c_ret_qd"]), writes=[("rQD",)])
    P.dma("sp", lambda e: e.dma_start(out=KD[:], in_=dr["c_ret_kd"]), writes=[("rKD",)])
    P.op("dve", lambda e: e.memset(eps6[:], RET_EPS), writes=[("eps6",)])
    Wq = [A.alloc("Wq", [128, 8, 256], BF16) for _ in range(2)]
    Wk = [A.alloc("Wk", [128, 8, 256], BF16) for _ in range(2)]
    Wv = [A.alloc("Wv", [128, 8, 512], BF16) for _ in range(2)]
    Wg = [A.alloc("Wg", [128, 8, 512], BF16) for _ in range(2)]
    Wo = [A.alloc("Wo", [128, 4, D], BF16) for _ in range(2)]
    Sf = A.alloc("Sf", [128, 2, 512], F32)
    Sb = [A.alloc("Sb", [128, 2, 512], BF16) for _ in range(2)]
    qT = [A.alloc("qT", [128, 2, 128], BF16) for _ in range(2)]
    qdT = [A.alloc("qdT", [128, 2, 128], BF16) for _ in range(2)]
    kT = [A.alloc("kT", [128, 2, 128], BF16) for _ in range(2)]
    kdec = [A.alloc("kdec", [128, 256], BF16) for _ in range(2)]
    vc = [A.alloc("vc", [128, 512], BF16) for _ in range(2)]
    sgc = [A.alloc("sgc", [128, 512], F32) for _ in range(2)]
    PT = [A.alloc("PT", [128, 128], BF16) for _ in range(2)]
    qf = [A.alloc("qf", [128, 256], F32) for _ in range(2)]
    kf = [A.alloc("kf", [128, 256], F32) for _ in range(2)]
    ktf = [A.alloc("ktf", [128, 256], F32) for _ in range(2)]
    scf = [A.alloc("scf", [128, 128], F32) for _ in range(2)]
    osq = [A.alloc("osq", [128, 512], F32) for _ in range(2)]
    ms = [A.alloc("ms", [128, 1], F32) for _ in range(2)]
    sd = [A.alloc("rsd", [128, 1], F32) for _ in range(2)]
    rs = [A.alloc("rrs", [128, 1], F32) for _ in range(2)]
    yc = [A.alloc("yc", [128, 512], BF16) for _ in range(2)]
    yT = [A.alloc("yT", [128, 4, 128], BF16) for _ in range(2)]
    W = dr["b_w_in"][idx]
    QW = 1024
    thr = A.alloc("thr", [128, 1], BF16)

    def load_head(h):
        q = h % 2
        for (dst, col0, n, nm) in ((Wq[q], h * 256, 256, "Wq"), (Wk[q], QW + h * 256, 256, "Wk"),
                                   (Wv[q], 2 * QW + h * 512, 512, "Wv"), (Wg[q], 2 * QW + 2048 + h * 512, 512, "Wg")):
            P.dma("pool", lambda e, dst=dst, col0=col0, n=n: e.dma_start(
                out=dst[:], in_=W[:, col0:col0 + n].rearrange("(k p) n -> p k n", p=128)), reads=[("throttle",)], writes=[(nm, q)])
        P.dma("pool", lambda e, q=q, h=h: e.dma_start(
            out=Wo[q][:], in_=dr["b_w_out"][idx][h * 512:(h + 1) * 512, :].rearrange("(k p) n -> p k n", p=128)), reads=[("throttle",)], writes=[("Wo", q)])

    import os
    DBG = int(os.environ.get("RET_DBG", "99"))
    NH_ = int(os.environ.get("RET_NH", "4"))
    NI_ = int(os.environ.get("RET_NI", "16"))

    def ret_P(h, i, b_, q):
        if h >= NH_ or i >= NI_ or DBG < 1:
            return
        xtk = [("XT", i)]
        tok = slice(i * 128, (i + 1) * 128)
        bkq = next_bank(c); bkk = next_bank(c)
        for dc in range(2):
            for k in range(8):
                P.op("pe", lambda e, dc=dc, k=k, bkq=bkq: e.matmul(c.ps[bkq][:, dc * 128:(dc + 1) * 128], lhsT=Wq[q][:, k, dc * 128:(dc + 1) * 128],
                                                             rhs=XT[:, k, tok], start=(k == 0), stop=(k == 7)),
                     reads=[("Wq", q)] + xtk, writes=[("ps", bkq)])
        for dc in range(2):
            for k in range(8):
                P.op("pe", lambda e, dc=dc, k=k, bkk=bkk: e.matmul(c.ps[bkk][:, dc * 128:(dc + 1) * 128], lhsT=Wk[q][:, k, dc * 128:(dc + 1) * 128],
                                                             rhs=XT[:, k, tok], start=(k == 0), stop=(k == 7)),
                     reads=[("Wk", q)] + xtk, writes=[("ps", bkk)])
        P.op("act", lambda e, bkq=bkq, b_=b_: e.activation(out=qf[b_][:], in_=c.ps[bkq][:, 0:256], func=AF.Copy),
             reads=[("ps", bkq)], writes=[("qf", b_)])
        P.op("act", lambda e, bkk=bkk, b_=b_: e.activation(out=kf[b_][:], in_=c.ps[bkk][:, 0:256], func=AF.Copy),
             reads=[("ps", bkk)], writes=[("kf", b_)])
        P.op("pool", lambda e, b_=b_: e.tensor_copy(out=qT[b_][:].rearrange("p a b -> p (a b)"), in_=qf[b_][:]),
             reads=[("qf", b_)], writes=[("qT", b_)])
        for dc in range(2):
            P.op("dve", lambda e, dc=dc, b_=b_, h=h: e.tensor_tensor(out=qdT[b_][:, dc, :], in0=qf[b_][:, dc * 128:(dc + 1) * 128],
                                                                   in1=QD[:, h, :], op=ALU.mult),
                 reads=[("qf", b_), ("rQD",)], writes=[("qdT", b_)])
        P.op("pool", lambda e, b_=b_: e.tensor_scalar(out=kT[b_][:].rearrange("p a b -> p (a b)"), in0=kf[b_][:], scalar1=0.0625, scalar2=None, op0=ALU.mult),
             reads=[("kf", b_)], writes=[("kT", b_)])
        if DBG < 2:
            return
        bkt = next_bank(c)
        for k in range(8):
            P.op("pe", lambda e, k=k, bkt=bkt: e.matmul(c.ps[bkt][:, 0:256], lhsT=XT[:, k, tok], rhs=Wk[q][:, k, :], start=(k == 0), stop=(k == 7)),
                 reads=[("Wk", q)] + xtk, writes=[("ps", bkt)])
        P.op("act", lambda e, bkt=bkt, b_=b_: e.activation(out=ktf[b_][:], in_=c.ps[bkt][:, 0:256], func=AF.Copy),
             reads=[("ps", bkt)], writes=[("ktf", b_)])
        P.op("dve", lambda e, b_=b_, h=h: e.tensor_scalar(out=kdec[b_][:], in0=ktf[b_][:], scalar1=KD[:, h:h + 1], scalar2=None, op0=ALU.mult),
             reads=[("ktf", b_), ("rKD",)], writes=[("kdec", b_)])
        bkv = next_bank(c)
        for k in range(8):
            P.op("pe", lambda e, k=k, bkv=bkv: e.matmul(c.ps[bkv][:, :], lhsT=XT[:, k, tok], rhs=Wv[q][:, k, :], start=(k == 0), stop=(k == 7)),
                 reads=[("Wv", q)] + xtk, writes=[("ps", bkv)])
        P.op("act", lambda e, bkv=bkv, b_=b_: e.activation(out=vc[b_][:], in_=c.ps[bkv][:, :], func=AF.Copy), reads=[("ps", bkv)], writes=[("vc", b_)])
        bkg = next_bank(c)
        for k in range(8):
            P.op("pe", lambda e, k=k, bkg=bkg: e.matmul(c.ps[bkg][:, :], lhsT=XT[:, k, tok], rhs=Wg[q][:, k, :], start=(k == 0), stop=(k == 7)),
                 reads=[("Wg", q)] + xtk, writes=[("ps", bkg)])
        P.op("act", lambda e, bkg=bkg, b_=b_: e.activation(out=sgc[b_][:], in_=c.ps[bkg][:, :], func=AF.Sigmoid), reads=[("ps", bkg)], writes=[("sgc", b_)])
        P.op("dve", lambda e, bkg=bkg, b_=b_: e.tensor_tensor(out=sgc[b_][:], in0=c.ps[bkg][:, :], in1=sgc[b_][:], op=ALU.mult),
             reads=[("ps", bkg), ("sgc", b_)], writes=[("sgc", b_)])

    def ret_S(h, i, b_, q):
        xtk = [("XT", i)]
        tok = slice(i * 128, (i + 1) * 128)
        bks = next_bank(c)
        for dc in range(2):
            P.op("pe", lambda e, dc=dc, bks=bks, b_=b_: e.matmul(c.ps[bks][:, 0:128], lhsT=kT[b_][:, dc, :], rhs=qT[b_][:, dc, :], start=(dc == 0), stop=(dc == 1)),
                 reads=[("kT", b_), ("qT", b_)], writes=[("ps", bks)])
        P.op("act", lambda e, bks=bks, b_=b_: e.activation(out=scf[b_][:], in_=c.ps[bks][:, 0:128], func=AF.Copy),
             reads=[("ps", bks)], writes=[("scf", b_)])
        P.op("dve", lambda e, b_=b_, h=h: e.tensor_tensor(out=PT[b_][:], in0=scf[b_][:], in1=DT[:, h, :], op=ALU.mult),
             reads=[("scf", b_), ("rDT",)], writes=[("PT", b_)])
        if i < NT - 1:
            sbn = Sb[i % 2]
            for dc in range(2):
                bkS = next_bank(c)
                P.op("pe", lambda e, dc=dc, bkS=bkS, b_=b_: e.matmul(c.ps[bkS][:, :], lhsT=kdec[b_][:, dc * 128:(dc + 1) * 128], rhs=vc[b_][:], start=True, stop=True),
                     reads=[("kdec", b_), ("vc", b_)], writes=[("ps", bkS)])
                if i == 0:
                    P.op("dve", lambda e, dc=dc, bkS=bkS: e.tensor_copy(out=Sf[:, dc, :], in_=c.ps[bkS][:, :]), reads=[("ps", bkS)], writes=[("Sf", dc)])
                else:
                    P.op("dve", lambda e, dc=dc, bkS=bkS, h=h: e.scalar_tensor_tensor(out=Sf[:, dc, :], in0=Sf[:, dc, :], scalar=CDV[h], in1=c.ps[bkS][:, :],
                                                                                  op0=ALU.mult, op1=ALU.add),
                         reads=[("ps", bkS), ("Sf", dc)], writes=[("Sf", dc)])
                P.op("pool", lambda e, dc=dc, sbn=sbn: e.tensor_copy(out=sbn[:, dc, :], in_=Sf[:, dc, :]), reads=[("Sf", dc)], writes=[("Sb", i % 2)])
        bko = next_bank(c)
        sbp = Sb[(i + 1) % 2]
        P.op("pe", lambda e, bko=bko, b_=b_: e.matmul(c.ps[bko][:, :], lhsT=PT[b_][:], rhs=vc[b_][:], start=True, stop=(i == 0)),
             reads=[("PT", b_), ("vc", b_)], writes=[("ps", bko)])
        if i > 0:
            for dc in range(2):
                P.op("pe", lambda e, dc=dc, bko=bko, b_=b_, sbp=sbp: e.matmul(c.ps[bko][:, :], lhsT=qdT[b_][:, dc, :], rhs=sbp[:, dc, :], start=False, stop=(dc == 1)),
                     reads=[("qdT", b_), ("Sb", (i + 1) % 2)], writes=[("ps", bko)])
        P.op("act", lambda e, bko=bko, b_=b_: e.activation(out=osq[b_][:], in_=c.ps[bko][:, :], func=AF.Square), reads=[("ps", bko)], writes=[("osq", b_)])
        P.op("dve", lambda e, b_=b_: e.reduce_sum(out=ms[b_][:], in_=osq[b_][:], axis=AX.X), reads=[("osq", b_)], writes=[("ms", b_)])
        P.op("act", lambda e, b_=b_: e.activation(out=sd[b_][:], in_=ms[b_][:], func=AF.Sqrt, bias=eps6[:], scale=1.0 / 512.0),
             reads=[("ms", b_), ("eps6",)], writes=[("rsd", b_)])
        P.op("dve", lambda e, b_=b_: e.reciprocal(out=rs[b_][:], in_=sd[b_][:]), reads=[("rsd", b_)], writes=[("rrs", b_)])
        P.op("dve", lambda e, bko=bko, b_=b_: e.scalar_tensor_tensor(out=osq[b_][:], in0=c.ps[bko][:, :], scalar=rs[b_][:, 0:1], in1=sgc[b_][:],
                                                                  op0=ALU.mult, op1=ALU.mult),
             reads=[("ps", bko), ("rrs", b_), ("sgc", b_), ("ms", b_)], writes=[("osq", b_)])
        P.op("pool", lambda e, b_=b_: e.tensor_copy(out=yc[b_][:], in_=osq[b_][:]), reads=[("osq", b_)], writes=[("yc", b_)])

    def ret_Y(h, i, b_, q):
        xtk = [("XT", i)]
        tok = slice(i * 128, (i + 1) * 128)
        bky = next_bank(c)
        psb = c.ps[bky][:].bitcast(BF16)
        for fc in range(4):
            P.op("pe", lambda e, fc=fc, psb=psb, b_=b_: e.transpose(out=psb[:, fc * 128:(fc + 1) * 128], in_=yc[b_][:, fc * 128:(fc + 1) * 128], identity=c.ident_b[:]),
                 reads=[("yc", b_), ("ident_b",)], writes=[("ps", bky)])
        P.op("act", lambda e, psb=psb, b_=b_: e.activation(out=yT[b_][:].rearrange("p a b -> p (a b)"), in_=psb[:, 0:512], func=AF.Copy),
             reads=[("ps", bky)], writes=[("yT", b_)])
        for nh in range(2):
            bkx = next_bank(c)
            for fc in range(4):
                P.op("pe", lambda e, fc=fc, nh=nh, bkx=bkx, b_=b_: e.matmul(c.ps[bkx][:, :], lhsT=yT[b_][:, fc, :], rhs=Wo[q][:, fc, nh * 512:(nh + 1) * 512],
                                                                     start=(fc == 0), stop=(fc == 3)),
                     reads=[("yT", b_), ("Wo", q)], writes=[("ps", bkx)])
            xs = X[:, i, nh * 512:(nh + 1) * 512]
            if h == 0:
                P.op("dve", lambda e, xs=xs, bkx=bkx: e.scalar_tensor_tensor(out=xs, in0=xs, scalar=ALPHA, in1=c.ps[bkx][:, :], op0=ALU.mult, op1=ALU.add),
                     reads=[("ps", bkx), ("X", i)], writes=[("X", i)])
            else:
                P.op("dve", lambda e, xs=xs, bkx=bkx: e.tensor_tensor(out=xs, in0=xs, in1=c.ps[bkx][:, :], op=ALU.add),
                     reads=[("ps", bkx), ("X", i)], writes=[("X", i)])


    load_head(0)
    it = 0
    for h in range(RET_H):
        q = h % 2
        for i in range(NT + 2):
            if i < NT:
                ret_P(h, i, i % 2, q)
            if 1 <= i <= NT:
                ret_S(h, i - 1, (i - 1) % 2, q)
            if i >= 2:
                ret_Y(h, i - 2, (i - 2) % 2, q)
            it += 1
            if i == 4 and h + 1 < RET_H:
                P.op("act", lambda e: e.activation(out=thr[:], in_=yT[0][:, 0, 0:1], func=AF.Copy),
                     reads=[("yT", 0)], writes=[("throttle",)])
                load_head(h + 1)
    P.barrier()
    A.reset(c.phase_base)
    emit_ln_route_epilogue(c, layer)


def emit_ln_route_epilogue(c, layer):
    r = alloc_route(c, layer)
    g1, b1 = load_ln_params(c, layer, 0, "a")
    lt = alloc_ln_tmp(c)
    t = alloc_prep(c)
    for i in range(NT):
        emit_ln(c, lt, c.X[:, i, :], ("X", i), i, g1, b1, "a")
        if i >= 1:
            emit_transpose_tile(c, t, i - 1, write_xt=False)
            emit_route_tile1(c, r, t, i - 1)
        if i >= 2:
            emit_route_tile2(c, r, t, i - 2)
    emit_transpose_tile(c, t, NT - 1, write_xt=False)
    emit_route_tile1(c, r, t, NT - 1)
    emit_route_tile2(c, r, t, NT - 2)
    emit_route_tile2(c, r, t, NT - 1)


ATT_PAT = ((128, 1), (512, 4), (2048, 16))
ATT_BIG = 1.0e9


def att_consts():
    u = np.arange(128)[:, None]
    j = np.arange(256)[None, :]
    steps = u + 128 - j
    valid = (steps >= 0) & (steps <= 128)
    neg = np.where(valid, -steps.astype(np.float32), -ATT_BIG).astype(np.float32)
    return {"c_att_neg": np.ascontiguousarray(neg)}


def phase_attn(c, layer, idx):
    A, P, dr = c.A, c.P, c.dr
    X, XT = c.X, c.XT
    nc = c.nc
    Wd = dr["c_w_in"][idx]
    NEG = A.alloc("aNEG", [128, 256], F32)
    P.dma("sp", lambda e: e.dma_start(out=NEG[:], in_=dr["c_att_neg"]), writes=[("aNEG",)])
    Wq = [A.alloc("aWq", [128, 8, 128], BF16) for _ in range(2)]
    Wk = [A.alloc("aWk", [128, 8, 128], BF16) for _ in range(2)]
    Wv = [A.alloc("aWv", [128, 8, 128], BF16) for _ in range(2)]
    qT = [A.alloc("aqT", [128, S], BF16) for _ in range(2)]
    kT = [A.alloc("akT", [128, S], BF16) for _ in range(2)]
    V = [A.alloc("aV", [128, NT, 128], BF16) for _ in range(2)]
    BI = [A.alloc("aBI", [128, 2, 256], F32) for _ in range(2)]
    Sb = [A.alloc("aSb", [128, 256], F32) for _ in range(4)]
    Pb = [A.alloc("aPb", [128, 256], BF16) for _ in range(4)]
    PT = [A.alloc("aPT", [128, 2, 128], BF16) for _ in range(2)]
    mx = [A.alloc("amx", [128, 1], F32) for _ in range(2)]
    nm = [A.alloc("anm", [128, 1], F32) for _ in range(4)]
    osb = [A.alloc("aosb", [128, 128], F32) for _ in range(2)]
    mst = [A.alloc("amst", [128, 2, 2], F32) for _ in range(4)]

    def load_w(g, hp, q):
        for (dst, s_, nm_) in ((Wq[q], 0, "aWq"), (Wk[q], 1, "aWk"), (Wv[q], 2, "aWv")):
            col0 = ((s_ * 3 + g) * 16 + 2 * hp) * 64
            P.dma("pool", lambda e, dst=dst, col0=col0: e.dma_start(
                out=dst[:], in_=Wd[:, col0:col0 + 128].rearrange("(k p) n -> p k n", p=128)), writes=[(nm_, q)])

    def tokslice(g, ut):
        d = ATT_PAT[g][1]
        nb = (S // d) // 128
        r, b = ut // nb, ut % nb
        st = 128 * b * d + r
        return slice(st, st + 127 * d + 1, d), b

    def proj_slices(g, hp, q):
        sl = []
        for (Wt, dst, nm_, wn_) in ((Wq[q], qT[q], "aqT", "aWq"), (Wk[q], kT[q], "akT", "aWk")):
            for tb in range(4):
                def f(Wt=Wt, dst=dst, nm_=nm_, wn_=wn_, tb=tb):
                    bk = next_bank(c, 6)
                    for k in range(8):
                        P.op("pe", lambda e, k=k: e.matmul(c.ps[bk][:, :], lhsT=Wt[:, k, :], rhs=XT[:, k, tb * 512:(tb + 1) * 512],
                                                         start=(k == 0), stop=(k == 7)),
                             reads=[(wn_, q)] + [("XT", i_) for i_ in range(tb * 4, tb * 4 + 4)], writes=[("ps", bk)])
                    P.op("act", lambda e: e.activation(out=dst[:, tb * 512:(tb + 1) * 512], in_=c.ps[bk][:, :], func=AF.Copy),
                         reads=[("ps", bk)], writes=[(nm_, q, tb)])
                sl.append(f)
        for ut in range(NT):
            def f(ut=ut):
                ts_, _ = tokslice(g, ut)
                bk = next_bank(c, 6)
                for k in range(8):
                    P.op("pe", lambda e, k=k: e.matmul(c.ps[bk][:, 0:128], lhsT=XT[:, k, ts_], rhs=Wv[q][:, k, :], start=(k == 0), stop=(k == 7)),
                         reads=[("aWv", q)] + [("XT", i_) for i_ in range(NT)], writes=[("ps", bk)])
                P.op("act", lambda e: e.activation(out=V[q][:, ut, :], in_=c.ps[bk][:, 0:128], func=AF.Copy),
                     reads=[("ps", bk)], writes=[("aV", q, ut)])
            sl.append(f)
        def f():
            d = ATT_PAT[g][1]
            for e_ in range(2):
                hh = 2 * hp + e_
                slope = float(2.0 ** (-8.0 * (hh + 1) / 16.0)) * d
                P.op("pool", lambda e, e_=e_, slope=slope: e.tensor_scalar(out=BI[q][:, e_, :], in0=NEG[:], scalar1=slope, scalar2=None, op0=ALU.mult),
                     reads=[("aNEG",)], writes=[("aBI", q, e_)])
        sl.append(f)
        return sl

    def QK_KEYS(q):
        return [("aqT", q, tb) for tb in range(4)] + [("akT", q, tb) for tb in range(4)]

    NSL = 4

    def stageA(g, hp, q, ut, e_, sl, bo):
        ts_, b = tokslice(g, ut)
        has_prev = b > 0
        pr = slice(e_ * 64, (e_ + 1) * 64)
        lo = 0 if has_prev else 128
        bk = next_bank(c, 6)
        if has_prev:
            tp_, _ = tokslice(g, ut - 1)
            P.op("pe", lambda e: e.matmul(c.ps[bk][:, 0:128], lhsT=qT[q][pr, ts_], rhs=kT[q][pr, tp_], start=True, stop=True),
                 reads=QK_KEYS(q), writes=[("ps", bk)])
        P.op("pe", lambda e: e.matmul(c.ps[bk][:, 128:256], lhsT=qT[q][pr, ts_], rhs=kT[q][pr, ts_], start=True, stop=True),
             reads=QK_KEYS(q), writes=[("ps", bk)])
        P.op("dve", lambda e: e.scalar_tensor_tensor(out=Sb[sl][:, lo:256], in0=c.ps[bk][:, lo:256], scalar=0.125, in1=BI[q][:, e_, lo:256],
                                                    op0=ALU.mult, op1=ALU.add),
             reads=[("ps", bk), ("aBI", q, e_)], writes=[("aSb", sl)])
        P.op("dve", lambda e: e.reduce_max(out=mst[bo][:, e_, 0:1], in_=Sb[sl][:, lo:256], axis=AX.X), reads=[("aSb", sl)], writes=[("amst", bo, e_, 0)])
        P.op("pool", lambda e: e.tensor_scalar(out=nm[sl][:], in0=mst[bo][:, e_, 0:1], scalar1=-1.0, scalar2=None, op0=ALU.mult),
             reads=[("amst", bo, e_, 0)], writes=[("anm", sl)])
        P.op("act", lambda e: e.activation(out=Pb[sl][:, lo:256], in_=Sb[sl][:, lo:256], func=AF.Exp, bias=nm[sl][:], scale=1.0),
             reads=[("aSb", sl), ("anm", sl)], writes=[("aPb", sl)])

    def stageA2(g, hp, q, ut, e_, sl, bo):
        ts_, b = tokslice(g, ut)
        lo = 0 if b > 0 else 128
        P.op("dve", lambda e: e.reduce_sum(out=mst[bo][:, e_, 1:2], in_=Pb[sl][:, lo:256], axis=AX.X),
             reads=[("aPb", sl)], writes=[("amst", bo, e_, 1)])

    def stageB1(g, hp, q, ut, e_, sl, bo):
        ts_, b = tokslice(g, ut)
        has_prev = b > 0
        lo = 0 if has_prev else 128
        halves = (0, 1) if has_prev else (1,)
        pt = PT[sl % 2]
        bkt = next_bank(c, 6)
        psb = c.ps[bkt][:].bitcast(BF16)
        for hf in halves:
            P.op("pe", lambda e, hf=hf: e.transpose(out=psb[:, hf * 128:(hf + 1) * 128], in_=Pb[sl][:, hf * 128:(hf + 1) * 128], identity=c.ident_b[:]),
                 reads=[("aPb", sl), ("ident_b",)], writes=[("ps", bkt)])
        P.op("act", lambda e: e.activation(out=pt[:].rearrange("p a b -> p (a b)")[:, lo:256], in_=psb[:, lo:256], func=AF.Copy),
             reads=[("ps", bkt)], writes=[("aPT", sl % 2)])

    def stageB(g, hp, q, ut, e_, sl, bo, obank):
        ts_, b = tokslice(g, ut)
        has_prev = b > 0
        pr = slice(e_ * 64, (e_ + 1) * 64)
        lo = 0 if has_prev else 128
        halves = (0, 1) if has_prev else (1,)
        pt = PT[sl % 2]
        for n_, hf in enumerate(halves):
            vt = ut - 1 if hf == 0 else ut
            P.op("pe", lambda e, hf=hf, vt=vt, n_=n_: e.matmul(c.ps[obank][:, pr], lhsT=pt[:, hf, :], rhs=V[q][:, vt, pr],
                                                            start=(n_ == 0), stop=(n_ == len(halves) - 1)),
                 reads=[("aPT", sl % 2), ("aV", q, vt)], writes=[("ps", obank)])
        if e_ == 1:
            ob = osb[bo % 2]
            P.op("dve", lambda e: e.tensor_copy(out=ob[:], in_=c.ps[obank][:, 0:128]), reads=[("ps", obank)], writes=[("aosb", bo % 2)])
            P.dma("sp", lambda e: e.dma_start(out=dr["att_o"][g, ts_, hp * 128:(hp + 1) * 128], in_=ob[:]), reads=[("aosb", bo % 2)], writes=[("att_o", g, hp, ut)])
            P.dma("sp", lambda e: e.dma_start(out=dr["att_ms"][g, ts_, 2 * hp:2 * hp + 2, :], in_=mst[bo][:]),
                  reads=[("amst", bo, e2, t2) for e2 in range(2) for t2 in range(2)], writes=[("att_ms", g, hp, ut)])

    import os
    NG_ = int(os.environ.get("ATT_NG", "3"))
    NHP_ = int(os.environ.get("ATT_NHP", "8"))
    LOOK = 3
    combos = [(g, hp) for g in range(NG_) for hp in range(NHP_)]
    load_w(combos[0][0], combos[0][1], 0)
    for f_ in proj_slices(combos[0][0], combos[0][1], 0):
        f_()
    gcount = 0
    for n, (g, hp) in enumerate(combos):
        q = n % 2
        nxt = []
        if n + 1 < len(combos):
            load_w(combos[n + 1][0], combos[n + 1][1], (n + 1) % 2)
            nxt = proj_slices(combos[n + 1][0], combos[n + 1][1], (n + 1) % 2)
        units = [(ut, e_) for ut in range(NT) for e_ in range(2)]
        def args(j):
            ut, e_ = units[j]
            gc = gcount + j
            return (g, hp, q, ut, e_, gc % NSL, (gc // 2) % NSL)
        nu = len(units)
        for j in range(-3, nu):
            if 0 <= j + 3 < nu:
                stageA(*args(j + 3))
            if 0 <= j + 1 < nu:
                stageA2(*args(j + 1))
                stageB1(*args(j + 1))
            if 0 <= j < nu:
                stageB(*args(j), 6 + ((gcount + j) // 2) % 2)
            if j >= 3 and nxt:
                nxt.pop(0)()
        while nxt:
            nxt.pop(0)()
        gcount += len(units)
    P.barrier()
    A.reset(c.phase_base)
    YT = A.alloc("YT", [128, 8, S], BF16)
    mark = A.off
    Og = [[A.alloc("aOg", [128, D], F32) for _ in range(3)] for _ in range(2)]
    MS = [A.alloc("aMS", [128, 3, 16, 2], F32) for _ in range(2)]
    Mx = [A.alloc("aMx", [128, 16], F32) for _ in range(2)]
    Wt_ = [A.alloc("aWt", [128, 3, 16], F32) for _ in range(2)]
    Ws = [A.alloc("aWs", [128, 3, 16], F32) for _ in range(2)]
    Dn = [A.alloc("aDn", [128, 16], F32) for _ in range(2)]
    Yt = [A.alloc("aYt", [128, D], F32) for _ in range(2)]
    Yb = [A.alloc("aYb", [128, D], BF16) for _ in range(2)]

    def merge_tile(i, p):
        for g in range(3):
            P.dma("sp", lambda e, g=g: e.dma_start(out=Og[p][g][:], in_=dr["att_o"][g, i * 128:(i + 1) * 128, :]), writes=[("aOg", p, g)])
        P.dma("sp", lambda e: e.dma_start(out=MS[p][:], in_=dr["att_ms"][:, i * 128:(i + 1) * 128, :, :].rearrange("g p h t -> p g h t")), writes=[("aMS", p)])
        P.op("dve", lambda e: e.tensor_tensor(out=Mx[p][:], in0=MS[p][:, 0, :, 0], in1=MS[p][:, 1, :, 0], op=ALU.max), reads=[("aMS", p)], writes=[("aMx", p)])
        P.op("dve", lambda e: e.tensor_tensor(out=Mx[p][:], in0=Mx[p][:], in1=MS[p][:, 2, :, 0], op=ALU.max), reads=[("aMS", p), ("aMx", p)], writes=[("aMx", p)])
        for g in range(3):
            P.op("dve", lambda e, g=g: e.tensor_tensor(out=Wt_[p][:, g, :], in0=MS[p][:, g, :, 0], in1=Mx[p][:], op=ALU.subtract),
                 reads=[("aMS", p), ("aMx", p)], writes=[("aWt", p, g)])
        P.op("act", lambda e: e.activation(out=Wt_[p][:].rearrange("p a b -> p (a b)"), in_=Wt_[p][:].rearrange("p a b -> p (a b)"), func=AF.Exp), reads=[("aWt", p, g) for g in range(3)], writes=[("aWt", p)])
        P.op("dve", lambda e: e.tensor_tensor(out=Ws[p][:], in0=Wt_[p][:], in1=MS[p][:, :, :, 1], op=ALU.mult), reads=[("aWt", p), ("aMS", p)], writes=[("aWs", p)])
        P.op("dve", lambda e: e.tensor_tensor(out=Dn[p][:], in0=Ws[p][:, 0, :], in1=Ws[p][:, 1, :], op=ALU.add), reads=[("aWs", p)], writes=[("aDn", p)])
        P.op("dve", lambda e: e.tensor_tensor(out=Dn[p][:], in0=Dn[p][:], in1=Ws[p][:, 2, :], op=ALU.add), reads=[("aWs", p), ("aDn", p)], writes=[("aDn", p)])
        P.op("dve", lambda e: e.reciprocal(out=Dn[p][:], in_=Dn[p][:]), reads=[("aDn", p)], writes=[("aDn", p)])
        for g in range(3):
            P.op("dve", lambda e, g=g: e.tensor_tensor(out=Ws[p][:, g, :], in0=Wt_[p][:, g, :], in1=Dn[p][:], op=ALU.mult),
                 reads=[("aWt", p), ("aDn", p), ("aWs", p)], writes=[("aWs", p)])
        for g in range(3):
            eng = "dve" if g != 1 else "pool"
            P.op(eng, lambda e, g=g: e.tensor_tensor(out=Og[p][g][:].rearrange("p (h d) -> p h d", h=16), in0=Og[p][g][:].rearrange("p (h d) -> p h d", h=16),
                                                     in1=Ws[p][:, g, :].unsqueeze(2).to_broadcast([128, 16, 64]), op=ALU.mult),
                 reads=[("aOg", p, g), ("aWs", p)], writes=[("aOg", p, g)])
        P.op("pool", lambda e: e.tensor_tensor(out=Yt[p][:], in0=Og[p][0][:], in1=Og[p][1][:], op=ALU.add), reads=[("aOg", p, 0), ("aOg", p, 1)], writes=[("aYt", p)])
        P.op("dve", lambda e: e.tensor_tensor(out=Yb[p][:], in0=Yt[p][:], in1=Og[p][2][:], op=ALU.add), reads=[("aYt", p), ("aOg", p, 2)], writes=[("aYb", p)])
        for half in range(2):
            bk = next_bank(c)
            psb = c.ps[bk][:].bitcast(BF16)
            for q4 in range(4):
                ch = half * 4 + q4
                P.op("pe", lambda e, ch=ch, q4=q4, psb=psb: e.transpose(out=psb[:, q4 * 128:(q4 + 1) * 128], in_=Yb[p][:, ch * 128:(ch + 1) * 128], identity=c.ident_b[:]),
                     reads=[("aYb", p), ("ident_b",)], writes=[("ps", bk)])
            P.op("act", lambda e, half=half, psb=psb: e.activation(out=YT[:, half * 4:(half + 1) * 4, i * 128:(i + 1) * 128],
                                                                 in_=psb[:, 0:512].rearrange("p (a b) -> p a b", a=4), func=AF.Copy),
                 reads=[("ps", bk)], writes=[("YT", half * 4 + j) for j in range(4)])

    for i in range(NT):
        merge_tile(i, i % 2)
    P.barrier()
    A.reset(mark)
    emit_mixer_epilogue(c, layer, YT, 8, dr["c_w_out"][idx])


WEIGHT_NAMES = ["a_w_in", "a_conv_w", "a_conv_b", "a_w_rgate", "a_b_rgate", "a_w_igate", "a_b_igate", "a_lambda",
                "a_w_out", "b_w_in", "b_w_out", "c_w_in", "c_w_out", "ln_gain", "ln_bias", "moe_w_router",
                "moe_b_router", "moe_w_gu", "moe_b_gu", "moe_w_down", "moe_b_down"]


def make_consts():
    ident = np.eye(128, dtype=np.float32)
    tri = np.triu(np.ones((128, 128), dtype=np.float32), k=1)
    ec = np.tile((np.arange(NE, dtype=np.float32) * CAP)[None, :], (128, 1))
    d = {"c_ident": ident, "c_tri": tri, "c_ec": ec}
    d.update(ret_consts()[0])
    d.update(att_consts())
    return d


def run_plan(plan, x, weights, used=None):
    used = used if used is not None else WEIGHT_NAMES
    ws = {k: weights[k].shape for k in used}
    nc = build(plan, ws)
    consts = make_consts()
    in_maps = []
    for b in range(8):
        m = {"x": np.ascontiguousarray(x[b])}
        for k in used:
            m[k] = weights[k]
        m.update(consts)
        in_maps.append(m)
    res = run_bass_kernel_spmd(nc, in_maps, core_ids=list(range(8)))
    return np.stack([r["out"] for r in res.results], axis=0)


FULL_PLAN = [("prep",),
             ("rglru", 0, 0), ("moe", 0, False),
             ("ret", 1, 0), ("moe", 1, False),
             ("attn", 2, 0), ("moe", 2, False),
             ("rglru", 3, 1), ("moe", 3, False)]


def kernel(**inputs):
    x = np.asarray(inputs["x"], dtype=np.float32)
    weights = {k: np.ascontiguousarray(np.asarray(inputs[k], dtype=np.float32)) for k in WEIGHT_NAMES}
    out = run_plan(FULL_PLAN, x, weights)
    return out.astype(np.float32)
```
